# Optimizing a Trainium2 kernel written in Bass

```python
import math
import jax
import jax.numpy as jnp
from jax import lax
import numpy as np

D_MODEL = 1024
BATCH = 2
SEQ = 8192
DEPTH = 2

N_META = 16
CHUNK = 128
PAD = CHUNK - N_META
EPS = 1e-6

S5_WIDTH = D_MODEL // 4
S5_GROUP = 16
S5_NGROUPS = S5_WIDTH // S5_GROUP
S5_STATE = 64
S5_STEP_MIN = 1e-3
S5_STEP_MAX = 1e-1

HG_HEADS = 4
HG_DK = 64
HG_DV = 64
HG_KEY_WIDTH = HG_HEADS * HG_DK
HG_WIDTH = HG_HEADS * HG_DV
CONV_K = 4

RET_HEADS = 8
RET_DK = 32
RET_DV = 64
RET_KEY_WIDTH = RET_HEADS * RET_DK
RET_WIDTH = RET_HEADS * RET_DV
ROPE_BASE = 10000.0

D_MIX = S5_WIDTH + HG_WIDTH + RET_WIDTH
IN_SPLITS = (S5_WIDTH, HG_KEY_WIDTH, HG_KEY_WIDTH, HG_WIDTH, HG_WIDTH,
             RET_KEY_WIDTH, RET_KEY_WIDTH, RET_WIDTH, RET_WIDTH)
IN_COLS = S5_WIDTH + 2 * HG_KEY_WIDTH + 2 * HG_WIDTH + 2 * RET_KEY_WIDTH + 2 * RET_WIDTH

D_FF = 2816
N_EXPERTS = 8
TOP_K = 2
D_FF_EXPERT = 3584
N_DENSE = (DEPTH + 1) // 2
N_MOE = DEPTH // 2

kernel_name = 'hybrid_s5_hgrn2_retention_moe'


def rmsnorm(x, g):
    xf = x.astype(jnp.float32)
    y = xf * lax.rsqrt(jnp.mean(xf * xf, axis=-1, keepdims=True) + EPS)
    return (y * g.astype(jnp.float32)).astype(x.dtype)


def head_rmsnorm(o):
    return o * lax.rsqrt(jnp.mean(o * o, axis=-1, keepdims=True) + EPS)


def head_groupnorm(o):
    c = o - jnp.mean(o, axis=-1, keepdims=True)
    return c * lax.rsqrt(jnp.mean(c * c, axis=-1, keepdims=True) + EPS)


def causal_dwconv(x, w):
    xp = jnp.pad(x, ((0, 0), (CONV_K - 1, 0), (0, 0)))
    return lax.conv_general_dilated(
        xp, w[:, None, :].astype(x.dtype), window_strides=(1,), padding='VALID',
        dimension_numbers=('NWC', 'WIO', 'NWC'), feature_group_count=x.shape[-1])


def rotary(t, pos):
    half = t.shape[-1] // 2
    inv_freq = ROPE_BASE ** (-jnp.arange(half, dtype=jnp.float32) / half)
    ang = pos[:, None] * inv_freq[None, :]
    cos = jnp.cos(ang)[None, :, None, :]
    sin = jnp.sin(ang)[None, :, None, :]
    t1, t2 = t[..., :half], t[..., half:]
    return jnp.concatenate([t1 * cos - t2 * sin, t1 * sin + t2 * cos], axis=-1)


def to_chunks(t):
    b, l, h, d = t.shape
    return t.reshape(b, l // CHUNK, CHUNK, h, d).transpose(1, 0, 3, 2, 4)


def from_chunks(t):
    n, b, h, c, d = t.shape
    return t.transpose(1, 0, 3, 2, 4).reshape(b, n * c, h, d)


def _ssm_combine(a, b):
    a_decay, a_state = a
    b_decay, b_state = b
    return b_decay * a_decay, b_decay * a_state + b_state


def s5_mixer(u, lam_re, lam_im, b_re, b_im, c_re, c_im, d_skip, log_step, w_glu):
    f32 = jnp.float32
    bsz, seq_len, _ = u.shape
    uf = u.astype(f32).reshape(bsz, seq_len, S5_NGROUPS, S5_GROUP)
    lam = lax.complex(lam_re.astype(f32), lam_im.astype(f32))
    step = jnp.exp(log_step.astype(f32))[:, None]
    lam_bar = jnp.exp(lam * step)
    b_mat = lax.complex(b_re.astype(f32), b_im.astype(f32))
    c_mat = lax.complex(c_re.astype(f32), c_im.astype(f32))
    b_bar = ((lam_bar - 1.0) / lam)[..., None] * b_mat
    bu = jnp.einsum('gph,blgh->blgp', b_bar, uf.astype(jnp.complex64))
    decay = jnp.broadcast_to(lam_bar, bu.shape)
    _, states = lax.associative_scan(_ssm_combine, (decay, bu), axis=1)
    y = jnp.einsum('ghp,blgp->blgh', c_mat, states).real + d_skip.astype(f32) * uf
    y = jax.nn.gelu(y.reshape(bsz, seq_len, S5_WIDTH))
    return y * jax.nn.sigmoid(y @ w_glu.astype(f32))


def hgrn2_chunked(q, k, log_f, v):
    bsz, _, h, dk = q.shape
    dv = v.shape[-1]
    causal = jnp.tril(jnp.ones((CHUNK, CHUNK), dtype=bool))

    def step(state, blk):
        qb, kb, gb, vb = blk
        g_cum = jnp.cumsum(gb, axis=2)
        o_inter = jnp.einsum('bhik,bhkv->bhiv', qb * jnp.exp(g_cum), state)
        rel = jnp.where(causal[None, None, :, :, None],
                        g_cum[:, :, :, None, :] - g_cum[:, :, None, :, :], -jnp.inf)
        scores = jnp.einsum('bhik,bhjk,bhijk->bhij', qb, kb, jnp.exp(rel))
        o_intra = jnp.einsum('bhij,bhjv->bhiv', scores, vb)
        g_last = g_cum[:, :, -1:, :]
        new_state = (jnp.exp(g_last[:, :, 0, :])[..., None] * state
                     + jnp.einsum('bhjk,bhjv->bhkv', kb * jnp.exp(g_last - g_cum), vb))
        return new_state, o_inter + o_intra

    s0 = jnp.zeros((bsz, h, dk, dv), jnp.float32)
    _, out = lax.scan(step, s0, (to_chunks(q), to_chunks(k), to_chunks(log_f), to_chunks(v)))
    return from_chunks(out)


def hgrn2_mixer(hq, hf, hi, gate, lb, out_g):
    f32 = jnp.float32
    bsz, seq_len, _ = hq.shape
    q = jax.nn.silu(hq.astype(f32)).reshape(bsz, seq_len, HG_HEADS, HG_DK)
    z = hf.astype(f32).reshape(bsz, seq_len, HG_HEADS, HG_DK)
    lbh = lb.reshape(HG_HEADS, HG_DK)
    log_f = jnp.logaddexp(jnp.log(lbh), jnp.log1p(-lbh) + jax.nn.log_sigmoid(z))
    k = -jnp.expm1(log_f)
    v = hi.astype(f32).reshape(bsz, seq_len, HG_HEADS, HG_DV)
    o = head_rmsnorm(hgrn2_chunked(q, k, log_f, v)).reshape(bsz, seq_len, HG_WIDTH)
    return o * out_g.astype(f32) * jax.nn.silu(gate.astype(f32))


def retention_chunked(q, k, v, log_gamma):
    bsz, _, h, dk = q.shape
    dv = v.shape[-1]
    n = jnp.arange(CHUNK, dtype=jnp.float32)
    lg = log_gamma[:, None]
    causal = jnp.tril(jnp.ones((CHUNK, CHUNK), dtype=bool))
    intra = jnp.exp(jnp.where(causal[None], (n[:, None] - n[None, :])[None] * lg[:, :, None], -jnp.inf))
    inter = jnp.exp((n[None, :] + 1.0) * lg)[None, :, :, None]
    to_state = jnp.exp((CHUNK - 1.0 - n[None, :]) * lg)[None, :, :, None]
    carry_decay = jnp.exp(CHUNK * lg)[None, :, :, None]

    def step(state, blk):
        qb, kb, vb = blk
        scores = jnp.einsum('bhik,bhjk->bhij', qb, kb) * intra[None]
        o = (jnp.einsum('bhij,bhjv->bhiv', scores, vb)
             + jnp.einsum('bhik,bhkv->bhiv', qb, state) * inter)
        new_state = carry_decay * state + jnp.einsum('bhjk,bhjv->bhkv', kb * to_state, vb)
        return new_state, o

    s0 = jnp.zeros((bsz, h, dk, dv), jnp.float32)
    _, out = lax.scan(step, s0, (to_chunks(q), to_chunks(k), to_chunks(v)))
    return from_chunks(out)


def retention_mixer(rq, rk, rv, gate, pos, log_gamma, out_g):
    f32 = jnp.float32
    bsz, seq_len, _ = rq.shape
    q = rotary(rq.astype(f32).reshape(bsz, seq_len, RET_HEADS, RET_DK), pos) * (RET_DK ** -0.5)
    k = rotary(rk.astype(f32).reshape(bsz, seq_len, RET_HEADS, RET_DK), pos)
    v = rv.astype(f32).reshape(bsz, seq_len, RET_HEADS, RET_DV)
    o = head_groupnorm(retention_chunked(q, k, v, log_gamma)).reshape(bsz, seq_len, RET_WIDTH)
    return o * out_g.astype(f32) * jax.nn.silu(gate.astype(f32))


def swiglu(h, w1, w3, w2):
    a = jnp.einsum('bld,df->blf', h, w1)
    b = jnp.einsum('bld,df->blf', h, w3)
    return jnp.einsum('blf,fd->bld', jax.nn.silu(a) * b, w2)


def moe_swiglu(h, router, w1, w3, w2):
    logits = jnp.einsum('bld,de->ble', h, router).astype(jnp.float32)
    top_vals, top_idx = lax.top_k(logits, TOP_K)
    gates = jax.nn.softmax(top_vals, axis=-1)
    dense_gate = jnp.sum(jax.nn.one_hot(top_idx, N_EXPERTS, dtype=jnp.float32) * gates[..., None], axis=-2)
    out = jnp.zeros_like(h)
    for e in range(N_EXPERTS):
        out = out + dense_gate[..., e:e + 1].astype(h.dtype) * swiglu(h, w1[e], w3[e], w2[e])
    return out


def setup_inputs(seed: int = 0) -> dict:
    key = jax.random.key(seed)
    keys = iter(jax.random.split(key, 40))

    def nrm(shape, scale):
        return scale * jax.random.normal(next(keys), shape, jnp.float32)

    def gain(shape):
        return 1.0 + nrm(shape, 0.02)

    n_idx = jnp.arange(S5_STATE, dtype=jnp.float32)
    return {
        'x': nrm((BATCH, SEQ, D_MODEL), 1.0),
        'meta_tokens': nrm((N_META, D_MODEL), 1.0),
        'norm_mix_g': gain((DEPTH, D_MODEL)),
        'w_in': nrm((DEPTH, D_MODEL, IN_COLS), D_MODEL ** -0.5),
        's5_lam_re': -0.5 + nrm((DEPTH, S5_NGROUPS, S5_STATE), 0.01),
        's5_lam_im': math.pi * n_idx + nrm((DEPTH, S5_NGROUPS, S5_STATE), 0.01),
        's5_b_re': nrm((DEPTH, S5_NGROUPS, S5_STATE, S5_GROUP), (2 * S5_GROUP) ** -0.5),
        's5_b_im': nrm((DEPTH, S5_NGROUPS, S5_STATE, S5_GROUP), (2 * S5_GROUP) ** -0.5),
        's5_c_re': nrm((DEPTH, S5_NGROUPS, S5_GROUP, S5_STATE), (2 * S5_STATE) ** -0.5),
        's5_c_im': nrm((DEPTH, S5_NGROUPS, S5_GROUP, S5_STATE), (2 * S5_STATE) ** -0.5),
        's5_d': nrm((DEPTH, S5_NGROUPS, S5_GROUP), 1.0),
        's5_log_step': jax.random.uniform(next(keys), (DEPTH, S5_NGROUPS), jnp.float32,
                                          math.log(S5_STEP_MIN), math.log(S5_STEP_MAX)),
        's5_w_glu': nrm((DEPTH, S5_WIDTH, S5_WIDTH), S5_WIDTH ** -0.5),
        's5_out_g': gain((DEPTH, S5_WIDTH)),
        'hg_conv_w': nrm((DEPTH, CONV_K, 2 * HG_KEY_WIDTH + HG_WIDTH), CONV_K ** -0.5),
        'hg_lb_param': nrm((DEPTH, HG_KEY_WIDTH), 1.0),
        'hg_out_g': gain((DEPTH, HG_WIDTH)),
        'ret_out_g': gain((DEPTH, RET_WIDTH)),
        'w_out': nrm((DEPTH, D_MIX, D_MODEL), D_MIX ** -0.5),
        'norm_ffn_g': gain((DEPTH, D_MODEL)),
        'ffn_w1': nrm((N_DENSE, D_MODEL, D_FF), D_MODEL ** -0.5),
        'ffn_w3': nrm((N_DENSE, D_MODEL, D_FF), D_MODEL ** -0.5),
        'ffn_w2': nrm((N_DENSE, D_FF, D_MODEL), D_FF ** -0.5),
        'moe_router': nrm((N_MOE, D_MODEL, N_EXPERTS), D_MODEL ** -0.5),
        'moe_w1': nrm((N_MOE, N_EXPERTS, D_MODEL, D_FF_EXPERT), D_MODEL ** -0.5),
        'moe_w3': nrm((N_MOE, N_EXPERTS, D_MODEL, D_FF_EXPERT), D_MODEL ** -0.5),
        'moe_w2': nrm((N_MOE, N_EXPERTS, D_FF_EXPERT, D_MODEL), D_FF_EXPERT ** -0.5),
        'final_norm_g': gain((D_MODEL,)),
    }


def reference(x, meta_tokens, norm_mix_g, w_in, s5_lam_re, s5_lam_im, s5_b_re, s5_b_im,
              s5_c_re, s5_c_im, s5_d, s5_log_step, s5_w_glu, s5_out_g, hg_conv_w,
              hg_lb_param, hg_out_g, ret_out_g, w_out, norm_ffn_g, ffn_w1, ffn_w3, ffn_w2,
              moe_router, moe_w1, moe_w3, moe_w2, final_norm_g):
    f32 = jnp.float32
    dt = x.dtype
    bsz, seq_len, d = x.shape
    total = seq_len + CHUNK
    meta = jnp.broadcast_to(meta_tokens.astype(dt)[None], (bsz, N_META, d))
    h = jnp.concatenate([jnp.zeros((bsz, PAD, d), dt), meta, x], axis=1)
    mask = (jnp.arange(total) >= PAD).astype(dt)[None, :, None]
    pos = (jnp.arange(total) - PAD).astype(f32)

    lb_all = jnp.cumsum(jax.nn.softmax(hg_lb_param.astype(f32), axis=0), axis=0)
    lb_all = lb_all - lb_all[0]
    log_gamma = jnp.log1p(-jnp.power(2.0, -5.0 - jnp.arange(RET_HEADS, dtype=f32)))

    split_idx = [int(i) for i in np.cumsum(IN_SPLITS)[:-1]]
    hg_idx = [HG_KEY_WIDTH, 2 * HG_KEY_WIDTH]

    for l in range(DEPTH):
        hn = rmsnorm(h, norm_mix_g[l])
        proj = jnp.einsum('bld,dc->blc', hn, w_in[l]) * mask
        u, hq, hf, hi, hgate, rq, rk, rv, rgate = jnp.split(proj, split_idx, axis=-1)

        y_a = rmsnorm(s5_mixer(u, s5_lam_re[l], s5_lam_im[l], s5_b_re[l], s5_b_im[l],
                               s5_c_re[l], s5_c_im[l], s5_d[l], s5_log_step[l], s5_w_glu[l]),
                      s5_out_g[l])
        hqfi = causal_dwconv(jnp.concatenate([hq, hf, hi], axis=-1), hg_conv_w[l])
        cq, cf, ci = jnp.split(hqfi, hg_idx, axis=-1)
        y_b = hgrn2_mixer(cq, cf, ci, hgate, lb_all[l], hg_out_g[l])
        y_c = retention_mixer(rq, rk, rv, rgate, pos, log_gamma, ret_out_g[l])

        mixed = jnp.concatenate([y_a, y_b, y_c], axis=-1).astype(dt)
        h = h + jnp.einsum('blc,cd->bld', mixed, w_out[l])

        hn = rmsnorm(h, norm_ffn_g[l])
        if l % 2 == 0:
            ffn = swiglu(hn, ffn_w1[l // 2], ffn_w3[l // 2], ffn_w2[l // 2])
        else:
            ffn = moe_swiglu(hn, moe_router[l // 2], moe_w1[l // 2], moe_w3[l // 2], moe_w2[l // 2])
        h = h + ffn

    h = rmsnorm(h, final_norm_g)
    return h[:, CHUNK:, :]
```

```python
import math
import contextlib
import numpy as np
import concourse.bass as bass
import concourse.mybir as mybir
from concourse.bass_utils import run_bass_kernel_spmd


F32 = mybir.dt.float32
BF16 = mybir.dt.bfloat16
I32 = mybir.dt.int32
AF = mybir.ActivationFunctionType
ALU = mybir.AluOpType
AX = mybir.AxisListType


class T:
    def __init__(self, ctx, name, handle, space):
        self.ctx = ctx
        self.name = name
        self.h = handle
        self.space = space
        self.st = {}
        self.dq = None

    def __getitem__(self, idx):
        return self.h[idx]

    def state(self, key):
        s = self.st.get(key)
        if s is None:
            s = {"w": None, "r": {}}
            self.st[key] = s
        return s


class Q:
    def __init__(self, ctx, name, step):
        self.ctx = ctx
        self.name = name
        self.sem = ctx.root.enter_context(ctx.nc.semaphore(name))
        self.count = 0
        self.step = step


class Ctx:
    def __init__(self, nc):
        self.nc = nc
        self.stack = contextlib.ExitStack()
        self.root = self.stack
        self.engs = {}
        for name, eng in (("pe", nc.tensor), ("dve", nc.vector), ("act", nc.scalar),
                          ("pool", nc.gpsimd), ("sp", nc.sync)):
            q = Q(self, "q_" + name, 1)
            self.engs[name] = (eng, q)
        self.seen = {name: {} for name in self.engs}
        self.dmaq = {}
        self.n_inst = 0
        self.uid = 0

    def sb(self, name, shape, dtype):
        self.uid += 1
        h = self.stack.enter_context(self.nc.sbuf_tensor(f"{name}_{self.uid}", list(shape), dtype))
        return T(self, name, h, "sb")

    def ps(self, name, shape, dtype=F32):
        self.uid += 1
        h = self.stack.enter_context(self.nc.psum_tensor(f"{name}_{self.uid}", list(shape), dtype))
        return T(self, name, h, "ps")

    def dram(self, name, shape, dtype, kind):
        h = self.nc.dram_tensor(name, list(shape), dtype, kind=kind)
        return T(self, name, h, "dram")

    def dma_q(self, name):
        q = self.dmaq.get(name)
        if q is None:
            q = Q(self, "dq_" + name, 16)
            self.dmaq[name] = q
        return q

    def _need(self, engname, q, value):
        if q is None:
            return
        seen = self.seen[engname]
        if q.step == 16:
            value = q.count
        if seen.get(q.name, 0) >= value:
            return
        eng = self.engs[engname][0]
        eng.wait_ge(q.sem, value)
        seen[q.name] = value

    def _deps(self, engname, reads, writes):
        for (t, key) in reads:
            s = t.state(key)
            if s["w"] is not None:
                self._need(engname, *s["w"])
        for (t, key) in writes:
            s = t.state(key)
            if s["w"] is not None:
                self._need(engname, *s["w"])
            for q, v in s["r"].values():
                self._need(engname, q, v)

    def _mark(self, q, value, reads, writes):
        for (t, key) in reads:
            s = t.state(key)
            s["r"][q.name] = (q, value)
        for (t, key) in writes:
            s = t.state(key)
            s["w"] = (q, value)
            s["r"] = {}

    @staticmethod
    def _norm(lst):
        out = []
        for x in lst:
            if isinstance(x, tuple):
                out.append(x)
            else:
                out.append((x, None))
        return out

    def op(self, engname, fn, reads=(), writes=()):
        reads = self._norm(reads)
        writes = self._norm(writes)
        eng, q = self.engs[engname]
        self._deps(engname, reads, writes)
        ins = fn(eng)
        q.count += 1
        ins.then_inc(q.sem, 1)
        self._mark(q, q.count, reads, writes)
        self.n_inst += 1
        return ins

    def dma(self, engname, out, in_, reads=(), writes=(), qt=None, **kw):
        reads = self._norm(reads)
        writes = self._norm(writes)
        eng, _ = self.engs[engname]
        self._deps(engname, reads, writes)
        if qt is None:
            cands = [t for (t, k) in writes if t.space == "sb"] + [t for (t, k) in reads if t.space == "sb"]
            qt = cands[0]
        if qt.dq is None:
            self.uid += 1
            qt.dq = Q(self, f"dq_{qt.name}_{self.uid}", 16)
            self.dmaq[qt.dq.name] = qt.dq
        dq = qt.dq
        ins = eng.dma_start(out=out, in_=in_, **kw)
        dq.count += 16
        ins.then_inc(dq.sem, 16)
        self._mark(dq, dq.count, reads, writes)
        self.n_inst += 1
        return ins

    def barrier(self):
        for name, (eng, q0) in self.engs.items():
            for dq in self.dmaq.values():
                if dq.count:
                    self._need(name, dq, dq.count)
            for other, (e2, q2) in self.engs.items():
                if other != name and q2.count:
                    self._need(name, q2, q2.count)

    @contextlib.contextmanager
    def scope(self):
        old = self.stack
        self.stack = contextlib.ExitStack()
        try:
            yield
        finally:
            self.barrier()
            self.stack.close()
            self.stack = old

    def finish(self, engname="sp"):
        eng = self.engs[engname][0]
        for dq in self.dmaq.values():
            if dq.count:
                eng.wait_ge(dq.sem, dq.count)
        for name, (e, q) in self.engs.items():
            if q.count and name != engname:
                eng.wait_ge(q.sem, q.count)

    def close(self):
        self.stack.close()


NT = 2080
D = 1024
KC = 8
EPS = 1e-6
NCOL_A = 3328


def tblocks(nt=NT, bs=512):
    out = []
    t = 0
    while t < nt:
        n = min(bs, nt - t)
        out.append((t, n))
        t += n
    return out


class Rot:
    def __init__(self, tiles):
        self.tiles = tiles
        self.i = 0

    def next(self):
        t = self.tiles[self.i % len(self.tiles)]
        self.i += 1
        return t


def emit_consts(c):
    k = {}
    k["ones_bf"] = c.sb("ones_bf", [128, 128], BF16)
    c.op("pool", lambda e: e.memset(k["ones_bf"][:], 1.0), writes=[k["ones_bf"]])
    return k


def emit_rmsnorm(c, k, hT, g_sb, hnT, sq_rot, rs_rot, ps_rot, d_chunks=KC, dim=D, src_key=True):
    for bi, (t0, n) in enumerate(tblocks()):
        sq = sq_rot.next()
        c.op("act", lambda e: e.activation(out=sq[:, :d_chunks, :n], in_=hT[:, :, t0:t0 + n], func=AF.Square),
             reads=[(hT, bi)], writes=[sq])
        ps = ps_rot.next()
        for kc in range(d_chunks):
            c.op("pe", lambda e: e.matmul(ps[:, :n], lhsT=k["ones_bf"][:], rhs=sq[:, kc, :n],
                                          start=(kc == 0), stop=(kc == d_chunks - 1)),
                 reads=[sq, k["ones_bf"]], writes=[ps])
        rs = rs_rot.next()
        c.op("act", lambda e: e.activation(out=rs[:, :n], in_=ps[:, :n], func=AF.Sqrt, scale=1.0 / dim, bias=k["eps"][:, 0:1]),
             reads=[ps, k["eps"]], writes=[rs])
        c.op("dve", lambda e: e.reciprocal(rs[:, :n], rs[:, :n]), reads=[rs], writes=[rs])
        for kc in range(d_chunks):
            c.op("dve", lambda e: e.scalar_tensor_tensor(out=hnT[:, kc, t0:t0 + n], in0=hT[:, kc, t0:t0 + n],
                                                         scalar=g_sb[:, kc:kc + 1], in1=rs[:, :n],
                                                         op0=ALU.mult, op1=ALU.mult),
                 reads=[(hT, bi), rs, g_sb], writes=[(hnT, bi)])


def build_A():
    nc = bass.Bass("TRN2", target_bir_lowering=False)
    c = Ctx(nc)
    hT_d = c.dram("hT", [D, NT], F32, "ExternalInput")
    g_d = c.dram("g", [128, KC], F32, "ExternalInput")
    w_d = c.dram("w", [D, NCOL_A], F32, "ExternalInput")
    out_d = c.dram("projT", [NCOL_A, NT], F32, "ExternalOutput")

    k = emit_consts(c)
    k["eps"] = c.sb("eps", [128, 1], F32)
    c.op("pool", lambda e: e.memset(k["eps"][:], EPS), writes=[k["eps"]])
    hT = c.sb("hT", [128, KC, NT], F32)
    hnT = c.sb("hnT", [128, KC, NT], BF16)
    g_sb = c.sb("g", [128, KC], F32)
    c.dma("sp", g_sb[:], g_d[:], writes=[g_sb])
    hT_v = hT_d.h.ap().rearrange("(kc kp) t -> kp kc t", kp=128)
    for bi, (t0, n) in enumerate(tblocks()):
        c.dma("sp", hT[:, :, t0:t0 + n], hT_v[:, :, t0:t0 + n], writes=[(hT, bi)])
    sq_rot = Rot([c.sb(f"sq{i}", [128, KC, 512], BF16) for i in range(2)])
    rs_rot = Rot([c.sb(f"rs{i}", [128, 512], F32) for i in range(2)])
    ps_rot = Rot([c.ps(f"ps{i}", [128, 512], F32) for i in range(6)])
    emit_rmsnorm(c, k, hT, g_sb, hnT, sq_rot, rs_rot, ps_rot)

    w_rot = Rot([c.sb(f"w{i}", [128, KC, 512], BF16) for i in range(2)])
    ost_rot = Rot([c.sb(f"ost{i}", [128, NT], F32) for i in range(3)])
    w_v = w_d.h.ap().rearrange("(kc kp) n -> kp kc n", kp=128)
    ev = 0
    for c0 in range(0, NCOL_A, 512):
        ncol = min(512, NCOL_A - c0)
        w_sb = w_rot.next()
        c.dma("pool", w_sb[:, :, :ncol], w_v[:, :, c0:c0 + ncol], writes=[w_sb])
        for cc in range(ncol // 128):
            ost = ost_rot.next()
            for bi, (t0, n) in enumerate(tblocks()):
                ps = ps_rot.next()
                for kc in range(KC):
                    c.op("pe", lambda e: e.matmul(ps[:, :n], lhsT=w_sb[:, kc, cc * 128:(cc + 1) * 128],
                                                  rhs=hnT[:, kc, t0:t0 + n], start=(kc == 0), stop=(kc == KC - 1)),
                         reads=[w_sb, (hnT, bi)], writes=[ps])
                if ev % 2 == 0:
                    c.op("act", lambda e: e.copy(out=ost[:, t0:t0 + n], in_=ps[:, :n]), reads=[ps], writes=[ost])
                else:
                    c.op("dve", lambda e: e.tensor_copy(ost[:, t0:t0 + n], ps[:, :n]), reads=[ps], writes=[ost])
                ev += 1
            r0 = c0 + cc * 128
            c.dma("sp", out_d[r0:r0 + 128, :], ost[:], reads=[ost])
    c.finish()
    c.close()
    print("phase A instructions:", c.n_inst)
    return nc


GF = 2


def emit_rmsnorm2(c, k, src, g_sb, dst_fn, sq_rot, rs_rot, ps_rot, d_chunks, dim, src_keyed=True):
    for bi, (t0, n) in enumerate(tblocks()):
        sk = (src, bi) if src_keyed else src
        sq = sq_rot.next()
        c.op("act", lambda e: e.activation(out=sq[:, :d_chunks, :n], in_=src[:, :, t0:t0 + n], func=AF.Square),
             reads=[sk], writes=[sq])
        ps = ps_rot.next()
        for kc in range(d_chunks):
            c.op("pe", lambda e: e.matmul(ps[:, :n], lhsT=k["ones_bf"][:], rhs=sq[:, kc, :n],
                                          start=(kc == 0), stop=(kc == d_chunks - 1)),
                 reads=[sq, k["ones_bf"]], writes=[ps])
        rs = rs_rot.next()
        c.op("act", lambda e: e.activation(out=rs[:, :n], in_=ps[:, :n], func=AF.Sqrt, scale=1.0 / dim, bias=k["eps"][:, 0:1]),
             reads=[ps, k["eps"]], writes=[rs])
        c.op("dve", lambda e: e.reciprocal(rs[:, :n], rs[:, :n]), reads=[rs], writes=[rs])
        for kc in range(d_chunks):
            ap, wk = dst_fn(bi, t0, n, kc)
            c.op("dve", lambda e: e.scalar_tensor_tensor(out=ap, in0=src[:, kc, t0:t0 + n],
                                                         scalar=g_sb[:, kc:kc + 1], in1=rs[:, :n],
                                                         op0=ALU.mult, op1=ALU.mult),
                 reads=[sk, rs, g_sb], writes=[wk])


def build_C(n_exp, F, final):
    moe = n_exp > 1
    nc = bass.Bass("TRN2", target_bir_lowering=False)
    c = Ctx(nc)
    hT_d = c.dram("hT", [D, NT], F32, "ExternalInput")
    ya_d = c.dram("yaT", [256, NT], F32, "ExternalInput")
    ybc_d = c.dram("ybcT", [768, NT], F32, "ExternalInput")
    wglu_d = c.dram("wglu", [256, 256], F32, "ExternalInput")
    s5g_d = c.dram("s5g", [128, 2], F32, "ExternalInput")
    wout_d = c.dram("wout", [D, D], F32, "ExternalInput")
    gffn_d = c.dram("gffn", [128, KC], F32, "ExternalInput")
    w1_d = c.dram("w1", [n_exp, D, F], F32, "ExternalInput")
    w3_d = c.dram("w3", [n_exp, D, F], F32, "ExternalInput")
    w2_d = c.dram("w2", [n_exp, F, D], F32, "ExternalInput")
    if moe:
        rt_d = c.dram("router", [D, 8], F32, "ExternalInput")
    if final:
        gfin_d = c.dram("gfin", [128, KC], F32, "ExternalInput")
    out_d = c.dram("hT_out", [D, NT], F32, "ExternalOutput")

    k = emit_consts(c)
    k["eps"] = c.sb("eps", [128, 1], F32)
    c.op("pool", lambda e: e.memset(k["eps"][:], EPS), writes=[k["eps"]])
    TB = tblocks()

    hT = c.sb("hT", [128, KC, NT], F32)
    hT_v = hT_d.h.ap().rearrange("(kc kp) t -> kp kc t", kp=128)
    for bi, (t0, n) in enumerate(TB):
        c.dma("sp", hT[:, :, t0:t0 + n], hT_v[:, :, t0:t0 + n], writes=[(hT, bi)], qt=hT)
    gffn = c.sb("gffn", [128, KC], F32)
    c.dma("sp", gffn[:], gffn_d[:], writes=[gffn])
    s5g = c.sb("s5g", [128, 2], F32)
    c.dma("sp", s5g[:], s5g_d[:], writes=[s5g])
    if final:
        gfin = c.sb("gfin", [128, KC], F32)
        c.dma("sp", gfin[:], gfin_d[:], writes=[gfin])

    ps_all = [c.ps(f"ps{i}", [128, 512], F32) for i in range(8)]
    ps_rot = Rot(ps_all[:7])
    sq_rot = Rot([c.sb(f"sq{i}", [128, KC, 512], BF16) for i in range(2)])
    rs_rot = Rot([c.sb(f"rs{i}", [128, 512], F32) for i in range(2)])

    with c.scope():
        yaT = c.sb("yaT", [128, 2, NT], F32)
        c.dma("sp", yaT[:], ya_d.h.ap().rearrange("(kc kp) t -> kp kc t", kp=128), writes=[yaT])
        mixedT = c.sb("mixedT", [128, KC, NT], BF16)
        ybc_v = ybc_d.h.ap().rearrange("(kc kp) t -> kp kc t", kp=128)
        for bi, (t0, n) in enumerate(TB):
            c.dma("pool", mixedT[:, 2:8, t0:t0 + n], ybc_v[:, :, t0:t0 + n], writes=[(mixedT, ("bc", bi))], qt=mixedT)
        wglu = c.sb("wglu", [128, 2, 256], BF16)
        c.dma("pool", wglu[:], wglu_d.h.ap().rearrange("(kc kp) n -> kp kc n", kp=128), writes=[wglu])
        wout = c.sb("wout", [128, KC, D], BF16)
        c.dma("pool", wout[:], wout_d.h.ap().rearrange("(kc kp) n -> kp kc n", kp=128), writes=[wout])

        t1_rot = Rot([c.sb(f"t1_{i}", [128, 2, 512], F32) for i in range(1)])
        t2_rot = Rot([c.sb(f"t2_{i}", [128, 2, 512], F32) for i in range(1)])
        ygf_rot = Rot([c.sb(f"ygf{i}", [128, 2, 512], F32) for i in range(1)])
        ygb_rot = Rot([c.sb(f"ygb{i}", [128, 2, 512], BF16) for i in range(2)])
        yaf_rot = Rot([c.sb(f"yaf{i}", [128, 2, 512], F32) for i in range(1)])
        sg_rot = Rot([c.sb(f"sg{i}", [128, 512], F32) for i in range(2)])
        for bi, (t0, n) in enumerate(TB):
            x = yaT[:, :, t0:t0 + n]
            t1 = t1_rot.next(); t2 = t2_rot.next(); ygf = ygf_rot.next(); ygb = ygb_rot.next(); yaf = yaf_rot.next()
            c.op("act", lambda e: e.activation(out=t1[:, :, :n], in_=x, func=AF.Square), reads=[yaT], writes=[t1])
            c.op("dve", lambda e: e.tensor_scalar(t1[:, :, :n], t1[:, :, :n], 0.044715, 1.0, op0=ALU.mult, op1=ALU.add),
                 reads=[t1], writes=[t1])
            c.op("dve", lambda e: e.tensor_tensor(out=t1[:, :, :n], in0=t1[:, :, :n], in1=x, op=ALU.mult),
                 reads=[t1, yaT], writes=[t1])
            c.op("act", lambda e: e.activation(out=t2[:, :, :n], in_=t1[:, :, :n], func=AF.Sigmoid, scale=1.5957691216057308),
                 reads=[t1], writes=[t2])
            c.op("dve", lambda e: e.tensor_tensor(out=ygf[:, :, :n], in0=t2[:, :, :n], in1=x, op=ALU.mult),
                 reads=[t2, yaT], writes=[ygf])
            c.op("act", lambda e: e.copy(out=ygb[:, :, :n], in_=ygf[:, :, :n]), reads=[ygf], writes=[ygb])
            for mo in range(2):
                ps = ps_rot.next()
                for ch in range(2):
                    c.op("pe", lambda e: e.matmul(ps[:, :n], lhsT=wglu[:, ch, mo * 128:(mo + 1) * 128], rhs=ygb[:, ch, :n],
                                                  start=(ch == 0), stop=(ch == 1)), reads=[wglu, ygb], writes=[ps])
                sg = sg_rot.next()
                c.op("act", lambda e: e.activation(out=sg[:, :n], in_=ps[:, :n], func=AF.Sigmoid), reads=[ps], writes=[sg])
                c.op("dve", lambda e: e.tensor_tensor(out=yaf[:, mo, :n], in0=ygf[:, mo, :n], in1=sg[:, :n], op=ALU.mult),
                     reads=[ygf, sg], writes=[yaf])
            sq = sq_rot.next()
            c.op("act", lambda e: e.activation(out=sq[:, :2, :n], in_=yaf[:, :, :n], func=AF.Square), reads=[yaf], writes=[sq])
            ps = ps_rot.next()
            for ch in range(2):
                c.op("pe", lambda e: e.matmul(ps[:, :n], lhsT=k["ones_bf"][:], rhs=sq[:, ch, :n], start=(ch == 0), stop=(ch == 1)),
                     reads=[sq, k["ones_bf"]], writes=[ps])
            rs = rs_rot.next()
            c.op("act", lambda e: e.activation(out=rs[:, :n], in_=ps[:, :n], func=AF.Sqrt, scale=1.0 / 256, bias=k["eps"][:, 0:1]),
                 reads=[ps, k["eps"]], writes=[rs])
            c.op("dve", lambda e: e.reciprocal(rs[:, :n], rs[:, :n]), reads=[rs], writes=[rs])
            for ch in range(2):
                c.op("dve", lambda e: e.scalar_tensor_tensor(out=mixedT[:, ch, t0:t0 + n], in0=yaf[:, ch, :n],
                                                             scalar=s5g[:, ch:ch + 1], in1=rs[:, :n], op0=ALU.mult, op1=ALU.mult),
                     reads=[yaf, rs, s5g], writes=[(mixedT, ("a", bi))])
            for dch in range(KC):
                ps = ps_rot.next()
                for cc in range(KC):
                    c.op("pe", lambda e: e.matmul(ps[:, :n], lhsT=wout[:, cc, dch * 128:(dch + 1) * 128], rhs=mixedT[:, cc, t0:t0 + n],
                                                  start=(cc == 0), stop=(cc == KC - 1)),
                         reads=[wout, (mixedT, ("a", bi)), (mixedT, ("bc", bi))], writes=[ps])
                c.op("dve", lambda e: e.tensor_tensor(out=hT[:, dch, t0:t0 + n], in0=hT[:, dch, t0:t0 + n], in1=ps[:, :n], op=ALU.add),
                     reads=[(hT, bi), ps], writes=[(hT, bi)])

    hnT = c.sb("hnT", [128, KC, NT], BF16)
    emit_rmsnorm2(c, k, hT, gffn, lambda bi, t0, n, kc: (hnT[:, kc, t0:t0 + n], (hnT, bi)), sq_rot, rs_rot, ps_rot, KC, D)

    if moe:
        gatesT = c.sb("gatesT", [8, NT], BF16)
        with c.scope():
            ident = c.sb("identf", [128, 128], F32)
            iof = c.sb("iof", [128, 128], F32)
            c.op("pool", lambda e: e.iota(iof[:], [[1, 128]], base=0, channel_multiplier=-1, allow_small_or_imprecise_dtypes=True), writes=[iof])
            c.op("dve", lambda e: e.tensor_single_scalar(ident[:], iof[:], 0.0, op=ALU.is_equal), reads=[iof], writes=[ident])
            rt = c.sb("rt", [128, KC, 8], F32)
            c.dma("sp", rt[:], rt_d.h.ap().rearrange("(kc kp) e -> kp kc e", kp=128), writes=[rt])
            gr = c.sb("gr", [128, KC, 16], F32)
            c.op("dve", lambda e: e.memset(gr[:], 0.0), writes=[gr])
            for kc in range(KC):
                c.op("dve", lambda e: e.tensor_scalar(gr[:, kc, 0:8], rt[:, kc, :], gffn[:, kc:kc + 1], None, op0=ALU.mult),
                     reads=[rt, gffn], writes=[gr])
            onesf = c.sb("onesf", [128, 1], F32)
            c.op("dve", lambda e: e.memset(onesf[:], 1.0), writes=[onesf])
            k["gatesT"] = gatesT
            sqf_rot = Rot([c.sb(f"sqf{i}", [128, KC, 128], F32) for i in range(2)])
            sm_rot = Rot([c.sb(f"sm{i}", [128, 64], F32) for i in range(3)])
            for ti, (t0, n) in enumerate(tblocks(NT, 128)):
                bi = t0 // 512
                sqf = sqf_rot.next()
                c.op("act", lambda e: e.activation(out=sqf[:, :, :n], in_=hT[:, :, t0:t0 + n], func=AF.Square), reads=[(hT, bi)], writes=[sqf])
                ps = ps_all[7]
                for kc in range(KC):
                    c.op("pe", lambda e: e.matmul(ps[:n, 0:8], lhsT=hT[:, kc, t0:t0 + n], rhs=gr[:, kc, 0:8], start=(kc == 0), stop=(kc == KC - 1)),
                         reads=[(hT, bi), gr], writes=[ps])
                for kc in range(KC):
                    c.op("pe", lambda e: e.matmul(ps[:n, 8:9], lhsT=sqf[:, kc, :n], rhs=onesf[:, 0:1], start=(kc == 0), stop=(kc == KC - 1)),
                         reads=[sqf, onesf], writes=[ps])
                sm = sm_rot.next()
                c.op("act", lambda e: e.activation(out=sm[:n, 0:1], in_=ps[:n, 8:9], func=AF.Sqrt, scale=1.0 / D, bias=k["eps"][:n, 0:1]),
                     reads=[ps, k["eps"]], writes=[sm])
                c.op("dve", lambda e: e.reciprocal(sm[:n, 0:1], sm[:n, 0:1]), reads=[sm], writes=[sm])
                c.op("dve", lambda e: e.tensor_scalar(sm[:n, 8:16], ps[:n, 0:8], sm[:n, 0:1], None, op0=ALU.mult), reads=[ps, sm], writes=[sm])
                c.op("dve", lambda e: e.max(out=sm[:n, 16:24], in_=sm[:n, 8:16]), reads=[sm], writes=[sm])
                c.op("dve", lambda e: e.tensor_tensor(out=sm[:n, 24:25], in0=sm[:n, 17:18], in1=sm[:n, 16:17], op=ALU.subtract), reads=[sm], writes=[sm])
                c.op("act", lambda e: e.activation(out=sm[:n, 24:25], in_=sm[:n, 24:25], func=AF.Exp), reads=[sm], writes=[sm])
                c.op("dve", lambda e: e.tensor_scalar(sm[:n, 25:26], sm[:n, 24:25], 1.0, None, op0=ALU.add), reads=[sm], writes=[sm])
                c.op("dve", lambda e: e.reciprocal(sm[:n, 25:26], sm[:n, 25:26]), reads=[sm], writes=[sm])
                c.op("dve", lambda e: e.tensor_tensor(out=sm[:n, 26:27], in0=sm[:n, 24:25], in1=sm[:n, 25:26], op=ALU.mult), reads=[sm], writes=[sm])
                c.op("dve", lambda e: e.tensor_scalar(sm[:n, 32:40], sm[:n, 8:16], sm[:n, 16:17], sm[:n, 25:26], op0=ALU.is_equal, op1=ALU.mult), reads=[sm], writes=[sm])
                c.op("dve", lambda e: e.tensor_scalar(sm[:n, 40:48], sm[:n, 8:16], sm[:n, 17:18], sm[:n, 26:27], op0=ALU.is_equal, op1=ALU.mult), reads=[sm], writes=[sm])
                c.op("dve", lambda e: e.tensor_tensor(out=sm[:n, 48:56], in0=sm[:n, 32:40], in1=sm[:n, 40:48], op=ALU.add), reads=[sm], writes=[sm])
                c.op("pe", lambda e: e.transpose(ps[0:8, 16:16 + n], sm[:n, 48:56], ident[:n, :n]), reads=[sm, ident], writes=[ps])
                c.op("act", lambda e: e.copy(out=gatesT[:, t0:t0 + n], in_=ps[0:8, 16:16 + n]), reads=[ps], writes=[gatesT])
        sel = c.sb("sel", [8, 8, 128], BF16)
        self_f = c.sb("sel_f", [8, 8, 128], F32)
        c.op("pool", lambda e: e.iota(self_f[:], [[-1, 8], [0, 128]], base=0, channel_multiplier=1, allow_small_or_imprecise_dtypes=True), writes=[self_f])
        c.op("dve", lambda e: e.tensor_single_scalar(sel[:], self_f[:], 0.0, op=ALU.is_equal), reads=[self_f], writes=[sel])

    with c.scope():
        ps_a = Rot(ps_all[0:2]); ps_b = Rot(ps_all[2:4]); ps_o = Rot(ps_all[4:6]); ps_g = Rot(ps_all[6:7])
        w1_rot = Rot([c.sb(f"w1g{i}", [128, KC, GF * 128], BF16) for i in range(2)])
        w3_rot = Rot([c.sb(f"w3g{i}", [128, KC, GF * 128], BF16) for i in range(2)])
        w2_rot = Rot([c.sb(f"w2g{i}", [128, GF, D], BF16) for i in range(2)])
        sa_rot = Rot([c.sb(f"sa{i}", [128, 512], F32) for i in range(2)])
        tt_rot = Rot([c.sb(f"tt{i}", [128, 512], F32) for i in range(2)])
        gT_rot = Rot([c.sb(f"gT{i}", [128, GF, 512], BF16) for i in range(2)])
        ngrp = F // (GF * 128)
        for ex in range(n_exp):
            w1_v = w1_d.h.ap()[ex].rearrange("(kc kp) f -> kp kc f", kp=128)
            w3_v = w3_d.h.ap()[ex].rearrange("(kc kp) f -> kp kc f", kp=128)
            w2_v = w2_d.h.ap()[ex].rearrange("(fc fp) d -> fp fc d", fp=128)
            for gi in range(ngrp):
                f0 = gi * GF * 128
                w1g = w1_rot.next(); w3g = w3_rot.next(); w2g = w2_rot.next()
                c.dma("pool", w1g[:], w1_v[:, :, f0:f0 + GF * 128], writes=[w1g])
                c.dma("pool", w3g[:], w3_v[:, :, f0:f0 + GF * 128], writes=[w3g])
                c.dma("pool", w2g[:], w2_v[:, gi * GF:(gi + 1) * GF, :], writes=[w2g])
                for bi, (t0, n) in enumerate(TB):
                    if moe:
                        pg = ps_g.next()
                        c.op("pe", lambda e: e.matmul(pg[:, :n], lhsT=sel[:, ex, :], rhs=k["gatesT"][:, t0:t0 + n], start=True, stop=True),
                             reads=[sel, k["gatesT"]], writes=[pg])
                    gT = gT_rot.next()
                    for fc in range(GF):
                        pa = ps_a.next(); pb = ps_b.next()
                        for kc in range(KC):
                            c.op("pe", lambda e: e.matmul(pa[:, :n], lhsT=w1g[:, kc, fc * 128:(fc + 1) * 128], rhs=hnT[:, kc, t0:t0 + n],
                                                          start=(kc == 0), stop=(kc == KC - 1)), reads=[w1g, (hnT, bi)], writes=[pa])
                        for kc in range(KC):
                            c.op("pe", lambda e: e.matmul(pb[:, :n], lhsT=w3g[:, kc, fc * 128:(fc + 1) * 128], rhs=hnT[:, kc, t0:t0 + n],
                                                          start=(kc == 0), stop=(kc == KC - 1)), reads=[w3g, (hnT, bi)], writes=[pb])
                        sa = sa_rot.next()
                        c.op("act", lambda e: e.activation(out=sa[:, :n], in_=pa[:, :n], func=AF.Silu), reads=[pa], writes=[sa])
                        if moe:
                            tt = tt_rot.next()
                            c.op("dve", lambda e: e.tensor_tensor(out=tt[:, :n], in0=sa[:, :n], in1=pb[:, :n], op=ALU.mult), reads=[sa, pb], writes=[tt])
                            c.op("dve", lambda e: e.tensor_tensor(out=gT[:, fc, :n], in0=tt[:, :n], in1=pg[:, :n], op=ALU.mult), reads=[tt, pg], writes=[gT])
                        else:
                            c.op("dve", lambda e: e.tensor_tensor(out=gT[:, fc, :n], in0=sa[:, :n], in1=pb[:, :n], op=ALU.mult), reads=[sa, pb], writes=[gT])
                    for dch in range(KC):
                        po = ps_o.next()
                        for fc in range(GF):
                            c.op("pe", lambda e: e.matmul(po[:, :n], lhsT=w2g[:, fc, dch * 128:(dch + 1) * 128], rhs=gT[:, fc, :n],
                                                          start=(fc == 0), stop=(fc == GF - 1)), reads=[w2g, gT], writes=[po])
                        c.op("dve", lambda e: e.tensor_tensor(out=hT[:, dch, t0:t0 + n], in0=hT[:, dch, t0:t0 + n], in1=po[:, :n], op=ALU.add),
                             reads=[(hT, bi), po], writes=[(hT, bi)])

    out_v = out_d.h.ap().rearrange("(kc kp) t -> kp kc t", kp=128)
    if final:
        fo_rot = Rot([c.sb(f"fo{i}", [128, KC, 512], F32) for i in range(2)])
        cur = {}

        def dst(bi, t0, n, kc):
            if kc == 0:
                cur["t"] = fo_rot.next()
            return cur["t"][:, kc, :n], cur["t"]
        for bi, (t0, n) in enumerate(TB):
            pass
        emit_final(c, k, hT, gfin, fo_rot, out_v, sq_rot, rs_rot, Rot(ps_all[0:6]))
    else:
        for bi, (t0, n) in enumerate(TB):
            c.dma("sp", out_v[:, :, t0:t0 + n], hT[:, :, t0:t0 + n], reads=[(hT, bi)], qt=hT)
    c.finish()
    c.close()
    print("phase C instructions:", c.n_inst)
    return nc


def emit_final(c, k, hT, gfin, fo_rot, out_v, sq_rot, rs_rot, ps_rot):
    for bi, (t0, n) in enumerate(tblocks()):
        sq = sq_rot.next()
        c.op("act", lambda e: e.activation(out=sq[:, :, :n], in_=hT[:, :, t0:t0 + n], func=AF.Square), reads=[(hT, bi)], writes=[sq])
        ps = ps_rot.next()
        for kc in range(KC):
            c.op("pe", lambda e: e.matmul(ps[:, :n], lhsT=k["ones_bf"][:], rhs=sq[:, kc, :n], start=(kc == 0), stop=(kc == KC - 1)),
                 reads=[sq, k["ones_bf"]], writes=[ps])
        rs = rs_rot.next()
        c.op("act", lambda e: e.activation(out=rs[:, :n], in_=ps[:, :n], func=AF.Sqrt, scale=1.0 / D, bias=k["eps"][:, 0:1]),
             reads=[ps, k["eps"]], writes=[rs])
        c.op("dve", lambda e: e.reciprocal(rs[:, :n], rs[:, :n]), reads=[rs], writes=[rs])
        fo = fo_rot.next()
        for kc in range(KC):
            c.op("dve", lambda e: e.scalar_tensor_tensor(out=fo[:, kc, :n], in0=hT[:, kc, t0:t0 + n], scalar=gfin[:, kc:kc + 1], in1=rs[:, :n],
                                                         op0=ALU.mult, op1=ALU.mult), reads=[(hT, bi), rs, gfin], writes=[fo])
        c.dma("sp", out_v[:, :, t0:t0 + n], fo[:, :, :n], reads=[fo])


NTOK = 8320
NCH = 65
EPS = 1e-6
CB = 5


def emit_pow_table(c, P, n, bT, br, bi, save_at=None):
    Gr = c.sb("Gr", [P, n], F32)
    Gi = c.sb("Gi", [P, n], F32)
    tmp = c.sb("Gtmp", [P, max(n // 2, 1)], F32)
    s = c.sb("Gs", [P, 6], F32)
    saved = c.sb("Gsaved", [P, 2], F32) if save_at else None
    V = "dve"
    c.op(V, lambda e: e.memset(Gr[:, 0:1], 1.0), writes=[Gr])
    c.op(V, lambda e: e.memset(Gi[:, 0:1], 0.0), writes=[Gi])
    c.op(V, lambda e: e.tensor_copy(s[:, 0:1], br), reads=[bT], writes=[s])
    c.op(V, lambda e: e.tensor_copy(s[:, 1:2], bi), reads=[bT], writes=[s])
    m = 1
    while m < n:
        if save_at == m:
            c.op(V, lambda e: e.tensor_copy(saved[:, 0:2], s[:, 0:2]), reads=[s], writes=[saved])
        c.op(V, lambda e: e.tensor_scalar(tmp[:, :m], Gi[:, :m], s[:, 1:2], None, op0=ALU.mult), reads=[Gi, s], writes=[tmp])
        c.op(V, lambda e: e.scalar_tensor_tensor(out=Gr[:, m:2 * m], in0=Gr[:, :m], scalar=s[:, 0:1], in1=tmp[:, :m],
                                                 op0=ALU.mult, op1=ALU.subtract), reads=[Gr, s, tmp], writes=[Gr])
        c.op(V, lambda e: e.tensor_scalar(tmp[:, :m], Gi[:, :m], s[:, 0:1], None, op0=ALU.mult), reads=[Gi, s], writes=[tmp])
        c.op(V, lambda e: e.scalar_tensor_tensor(out=Gi[:, m:2 * m], in0=Gr[:, :m], scalar=s[:, 1:2], in1=tmp[:, :m],
                                                 op0=ALU.mult, op1=ALU.add), reads=[Gr, s, tmp], writes=[Gi])
        c.op(V, lambda e: e.tensor_tensor(out=s[:, 2:3], in0=s[:, 0:1], in1=s[:, 0:1], op=ALU.mult), reads=[s], writes=[s])
        c.op(V, lambda e: e.tensor_tensor(out=s[:, 3:4], in0=s[:, 1:2], in1=s[:, 1:2], op=ALU.mult), reads=[s], writes=[s])
        c.op(V, lambda e: e.scalar_tensor_tensor(out=s[:, 1:2], in0=s[:, 0:1], scalar=2.0, in1=s[:, 1:2],
                                                 op0=ALU.mult, op1=ALU.mult), reads=[s], writes=[s])
        c.op(V, lambda e: e.tensor_tensor(out=s[:, 0:1], in0=s[:, 2:3], in1=s[:, 3:4], op=ALU.subtract), reads=[s], writes=[s])
        m *= 2
    if save_at == m:
        c.op(V, lambda e: e.tensor_copy(saved[:, 0:2], s[:, 0:2]), reads=[s], writes=[saved])
    return Gr, Gi, saved


def emit_sincos_small(c, P, wT, w, cs):
    V = "dve"
    x2 = cs[:, 2:3]
    acc = cs[:, 3:4]
    c.op(V, lambda e: e.tensor_tensor(out=x2, in0=w, in1=w, op=ALU.mult), reads=[wT], writes=[cs])
    c.op(V, lambda e: e.tensor_scalar(acc, x2, -1.0 / 110, 1.0, op0=ALU.mult, op1=ALU.add), reads=[cs], writes=[cs])
    for d in (72.0, 42.0, 20.0, 6.0):
        c.op(V, lambda e: e.tensor_tensor(out=acc, in0=acc, in1=x2, op=ALU.mult), reads=[cs], writes=[cs])
        c.op(V, lambda e: e.tensor_scalar(acc, acc, -1.0 / d, 1.0, op0=ALU.mult, op1=ALU.add), reads=[cs], writes=[cs])
    c.op(V, lambda e: e.tensor_tensor(out=cs[:, 1:2], in0=acc, in1=w, op=ALU.mult), reads=[cs, wT], writes=[cs])
    c.op(V, lambda e: e.tensor_scalar(acc, x2, -1.0 / 132, 1.0, op0=ALU.mult, op1=ALU.add), reads=[cs], writes=[cs])
    for d in (90.0, 56.0, 30.0, 12.0, 2.0):
        c.op(V, lambda e: e.tensor_tensor(out=acc, in0=acc, in1=x2, op=ALU.mult), reads=[cs], writes=[cs])
        c.op(V, lambda e: e.tensor_scalar(acc, acc, -1.0 / d, 1.0, op0=ALU.mult, op1=ALU.add), reads=[cs], writes=[cs])
    c.op(V, lambda e: e.tensor_copy(cs[:, 0:1], acc), reads=[cs], writes=[cs])


def emit_gamma(c, hT_, hidx_ap, out, P, ncol):
    LN2 = math.log(2.0)
    c.op("act", lambda e: e.activation(out=out[:, :ncol], in_=hidx_ap, func=AF.Exp, scale=-LN2, bias=c.k5[:P, 0:1]),
         reads=[c.k5, hT_], writes=[out])
    c.op("dve", lambda e: e.tensor_scalar(out[:, :ncol], out[:, :ncol], -1.0, 1.0, op0=ALU.mult, op1=ALU.add), reads=[out], writes=[out])
    c.op("act", lambda e: e.activation(out=out[:, :ncol], in_=out[:, :ncol], func=AF.Ln), reads=[out], writes=[out])


def build_ret(debug=False):
    nc = bass.Bass("TRN2", target_bir_lowering=False)
    c = Ctx(nc)
    q_d = c.dram("q", [64, NTOK], F32, "ExternalInput")
    qs_d = c.dram("qsw", [64, NTOK], F32, "ExternalInput")
    k_d = c.dram("k", [64, NTOK], F32, "ExternalInput")
    ks_d = c.dram("ksw", [64, NTOK], F32, "ExternalInput")
    v_d = c.dram("v", [NTOK, 128], F32, "ExternalInput")
    g_d = c.dram("gate", [NTOK, 128], F32, "ExternalInput")
    og_d = c.dram("outg", [128, 128], F32, "ExternalInput")
    hp_d = c.dram("hpart", [64, 1], F32, "ExternalInput")
    hs_d = c.dram("hsel", [128, 2], F32, "ExternalInput")
    y_d = c.dram("y", [NTOK, 128], F32, "ExternalOutput")

    V = "dve"
    c.k5 = c.sb("k5", [128, 1], F32)
    c.op("pool", lambda e: e.memset(c.k5[:], -5.0 * math.log(2.0)), writes=[c.k5])
    epsT = c.sb("eps", [128, 1], F32)
    c.op("pool", lambda e: e.memset(epsT[:], EPS), writes=[epsT])
    identb = c.sb("identb", [128, 128], BF16)
    iof = c.sb("iof", [128, 128], F32)
    c.op("pool", lambda e: e.iota(iof[:], [[1, 128]], base=0, channel_multiplier=-1, allow_small_or_imprecise_dtypes=True), writes=[iof])
    c.op(V, lambda e: e.tensor_single_scalar(identb[:], iof[:], 0.0, op=ALU.is_equal), reads=[iof], writes=[identb])
    hp = c.sb("hp", [64, 1], F32)
    c.dma("sp", hp[:], hp_d[:], writes=[hp])
    hs = c.sb("hs", [128, 2], F32)
    c.dma("sp", hs[:], hs_d[:], writes=[hs])
    og = c.sb("og", [128, 128], F32)
    c.dma("sp", og[:], og_d[:], writes=[og])
    lgP = c.sb("lgP", [64, 2], F32)
    emit_gamma(c, hp, hp[:, 0:1], lgP, 64, 1)
    lgB = c.sb("lgB", [128, 2], F32)
    emit_gamma(c, hs, hs[:, 0:2], lgB, 128, 2)
    maskT = c.sb("maskT", [128, 2, 128], F32)
    dpos = c.sb("dpos", [128, 128], F32)
    dge = c.sb("dge", [128, 128], F32)
    c.op(V, lambda e: e.tensor_single_scalar(dpos[:], iof[:], 0.0, op=ALU.max), reads=[iof], writes=[dpos])
    c.op(V, lambda e: e.tensor_scalar(dge[:], iof[:], 0.0, 32.0 ** -0.5, op0=ALU.is_ge, op1=ALU.mult), reads=[iof], writes=[dge])
    for hl in range(2):
        c.op("act", lambda e: e.activation(out=maskT[:, hl, :], in_=dpos[:], func=AF.Exp, scale=lgB[:, hl:hl + 1]),
             reads=[dpos, lgB], writes=[maskT])
        c.op(V, lambda e: e.tensor_tensor(out=maskT[:, hl, :], in0=maskT[:, hl, :], in1=dge[:], op=ALU.mult), reads=[maskT, dge], writes=[maskT])
    io1 = c.sb("io1", [64, 128], F32)
    c.op("pool", lambda e: e.iota(io1[:], [[1, 128]], base=1, channel_multiplier=0, allow_small_or_imprecise_dtypes=True), writes=[io1])
    io2 = c.sb("io2", [64, 128], F32)
    c.op("pool", lambda e: e.iota(io2[:], [[-1, 128]], base=127, channel_multiplier=0, allow_small_or_imprecise_dtypes=True), writes=[io2])
    qdf = c.sb("qdf", [64, 128], F32)
    kdf = c.sb("kdf", [64, 128], F32)
    c.op("act", lambda e: e.activation(out=qdf[:], in_=io1[:], func=AF.Exp, scale=lgP[:, 0:1]), reads=[io1, lgP], writes=[qdf])
    c.op(V, lambda e: e.tensor_scalar(qdf[:], qdf[:], 32.0 ** -0.5, None, op0=ALU.mult), reads=[qdf], writes=[qdf])
    c.op("act", lambda e: e.activation(out=kdf[:], in_=io2[:], func=AF.Exp, scale=lgP[:, 0:1]), reads=[io2, lgP], writes=[kdf])
    sdec = c.sb("sdec", [64, 1], F32)
    c.op("act", lambda e: e.activation(out=sdec[:], in_=lgP[:, 0:1], func=AF.Exp, scale=128.0), reads=[lgP], writes=[sdec])
    fr = c.sb("fr", [64, 4], F32)
    for hb in range(2):
        c.op("pool", lambda e: e.iota(fr[32 * hb:32 * hb + 32, 0:1], [[0, 1]], base=0, channel_multiplier=1,
                                      allow_small_or_imprecise_dtypes=True), writes=[fr])
    c.op(V, lambda e: e.tensor_single_scalar(fr[:, 3:4], fr[:, 0:1], 16.0, op=ALU.is_ge), reads=[fr], writes=[fr])
    c.op(V, lambda e: e.scalar_tensor_tensor(out=fr[:, 0:1], in0=fr[:, 3:4], scalar=-16.0, in1=fr[:, 0:1], op0=ALU.mult, op1=ALU.add),
         reads=[fr], writes=[fr])
    c.op(V, lambda e: e.tensor_scalar(fr[:, 2:3], fr[:, 3:4], 2.0, -1.0, op0=ALU.mult, op1=ALU.add), reads=[fr], writes=[fr])
    c.op("act", lambda e: e.activation(out=fr[:, 1:2], in_=fr[:, 0:1], func=AF.Exp, scale=-math.log(10000.0) / 16.0), reads=[fr], writes=[fr])
    cs = c.sb("cs", [64, 4], F32)
    emit_sincos_small(c, 64, fr, fr[:, 1:2], cs)
    Gr, Gi, s128 = emit_pow_table(c, 64, 256, cs, cs[:, 0:1], cs[:, 1:2], save_at=128)
    Fr, Fi, _ = emit_pow_table(c, 64, 64, s128, s128[:, 0:1], s128[:, 1:2])
    E1r = c.sb("E1r", [64, NCH], F32)
    E1i = c.sb("E1i", [64, NCH], F32)
    c.op(V, lambda e: e.tensor_copy(E1r[:, 1:NCH], Fr[:, 0:64]), reads=[Fr], writes=[E1r])
    c.op(V, lambda e: e.tensor_copy(E1i[:, 1:NCH], Fi[:, 0:64]), reads=[Fi], writes=[E1i])
    c.op(V, lambda e: e.tensor_copy(E1r[:, 0:1], Fr[:, 1:2]), reads=[Fr], writes=[E1r])
    c.op(V, lambda e: e.tensor_scalar(E1i[:, 0:1], Fi[:, 1:2], -1.0, None, op0=ALU.mult), reads=[Fi], writes=[E1i])
    E2r = Gr
    E2i = Gi

    QR = c.sb("QR", [64, NTOK], BF16)
    KR = c.sb("KR", [64, NTOK], BF16)
    QD = c.sb("QD", [64, NTOK], BF16)
    KD = c.sb("KD", [64, NTOK], BF16)
    v_sb = c.sb("v_sb", [128, NCH, 128], BF16)
    g_sb = c.sb("g_sb", [128, NCH, 128], BF16)
    y_sb = c.sb("y_sb", [128, NCH, 128], F32)
    c.dma("pool", v_sb[:], v_d.h.ap().rearrange("(c p) f -> p c f", p=128), writes=[v_sb])
    c.dma("pool", g_sb[:], g_d.h.ap().rearrange("(c p) f -> p c f", p=128), writes=[g_sb])

    nblk = NCH // CB
    BW = CB * 128
    x_rot = Rot([c.sb(f"x{i}", [64, BW], F32) for i in range(2)])
    xs_rot = Rot([c.sb(f"xs{i}", [64, BW], F32) for i in range(2)])
    COSb = c.sb("COSb", [64, CB, 128], F32)
    SINb = c.sb("SINb", [64, CB, 128], F32)
    tb1 = c.sb("tb1", [64, CB, 128], F32)
    tb2 = c.sb("tb2", [64, CB, 128], F32)
    r1 = c.sb("r1", [64, BW], F32)
    r2 = c.sb("r2", [64, BW], F32)
    for b in range(nblk):
        c0 = b * CB
        t0 = c0 * 128
        e1r = E1r[:, c0:c0 + CB].unsqueeze(2).to_broadcast([64, CB, 128])
        e1i = E1i[:, c0:c0 + CB].unsqueeze(2).to_broadcast([64, CB, 128])
        e2r = E2r[:, 16:144].unsqueeze(1).to_broadcast([64, CB, 128])
        e2i = E2i[:, 16:144].unsqueeze(1).to_broadcast([64, CB, 128])
        P_ = "pool"
        c.op(P_, lambda e: e.tensor_tensor(out=tb1[:], in0=e1r, in1=e2r, op=ALU.mult), reads=[E1r, E2r], writes=[tb1])
        c.op(P_, lambda e: e.tensor_tensor(out=tb2[:], in0=e1i, in1=e2i, op=ALU.mult), reads=[E1i, E2i], writes=[tb2])
        c.op(P_, lambda e: e.tensor_tensor(out=COSb[:], in0=tb1[:], in1=tb2[:], op=ALU.subtract), reads=[tb1, tb2], writes=[COSb])
        c.op(P_, lambda e: e.tensor_tensor(out=tb1[:], in0=e1r, in1=e2i, op=ALU.mult), reads=[E1r, E2i], writes=[tb1])
        c.op(P_, lambda e: e.tensor_tensor(out=tb2[:], in0=e1i, in1=e2r, op=ALU.mult), reads=[E1i, E2r], writes=[tb2])
        c.op(P_, lambda e: e.tensor_tensor(out=SINb[:], in0=tb1[:], in1=tb2[:], op=ALU.add), reads=[tb1, tb2], writes=[SINb])
        cosf = COSb[:].rearrange("p c j -> p (c j)")
        sinf = SINb[:].rearrange("p c j -> p (c j)")
        for (src_d, srcs_d, OUT, DEC, fac) in ((q_d, qs_d, QR, QD, qdf), (k_d, ks_d, KR, KD, kdf)):
            x = x_rot.next(); xs = xs_rot.next()
            c.dma("sp", x[:], src_d[:, t0:t0 + BW], writes=[x])
            c.dma("sp", xs[:], srcs_d[:, t0:t0 + BW], writes=[xs])
            c.op(V, lambda e: e.tensor_tensor(out=r1[:], in0=x[:], in1=cosf, op=ALU.mult), reads=[x, COSb], writes=[r1])
            c.op(V, lambda e: e.scalar_tensor_tensor(out=r2[:], in0=xs[:], scalar=fr[:, 2:3], in1=sinf, op0=ALU.mult, op1=ALU.mult),
                 reads=[xs, fr, SINb], writes=[r2])
            c.op(V, lambda e: e.tensor_tensor(out=OUT[:, t0:t0 + BW], in0=r1[:], in1=r2[:], op=ALU.add), reads=[r1, r2], writes=[(OUT, b)])
            facb = fac[:].unsqueeze(1).to_broadcast([64, CB, 128])
            c.op(V, lambda e: e.tensor_tensor(out=DEC[:, t0:t0 + BW].rearrange("p (c j) -> p c j", j=128),
                                              in0=OUT[:, t0:t0 + BW].rearrange("p (c j) -> p c j", j=128), in1=facb, op=ALU.mult),
                 reads=[(OUT, b), fac], writes=[(DEC, b)])

    ps_tr = Rot([c.ps(f"ps_tr{i}", [128, 64], BF16) for i in range(2)])
    ps_s = Rot([c.ps(f"ps_s{i}", [128, 2, 128], F32) for i in range(2)])
    ps_o = Rot([c.ps(f"ps_o{i}", [128, 128], F32) for i in range(2)])
    ps_d = Rot([c.ps(f"ps_d{i}", [64, 128], F32) for i in range(2)])
    kdt_rot = Rot([c.sb(f"kdt{i}", [128, 64], BF16) for i in range(2)])
    sT_rot = Rot([c.sb(f"sT{i}", [128, 2, 128], BF16) for i in range(2)])
    S32 = c.sb("S32", [64, 128], F32)
    c.op(V, lambda e: e.memset(S32[:], 0.0), writes=[S32])
    Sb_rot = Rot([c.sb(f"Sb{i}", [64, 128], BF16) for i in range(2)])
    Sb = Sb_rot.next()
    c.op(V, lambda e: e.memset(Sb[:], 0.0), writes=[Sb])
    o_rot = Rot([c.sb(f"o{i}", [128, 2, 64], F32) for i in range(2)])
    cen_rot = Rot([c.sb(f"cen{i}", [128, 2, 64], F32) for i in range(2)])
    sq_rot = Rot([c.sb(f"sqr{i}", [128, 2, 64], F32) for i in range(2)])
    st_rot = Rot([c.sb(f"st{i}", [128, 8], F32) for i in range(2)])
    gg_rot = Rot([c.sb(f"gg{i}", [128, 128], F32) for i in range(2)])
    for ch in range(NCH):
        b = ch // CB
        t0 = ch * 128
        ptr = ps_tr.next()
        c.op("pe", lambda e: e.transpose(ptr[:, :], KD[:, t0:t0 + 128], identb[0:64, 0:64]), reads=[(KD, b), identb], writes=[ptr])
        kdt = kdt_rot.next()
        c.op("act", lambda e: e.copy(out=kdt[:], in_=ptr[:]), reads=[ptr], writes=[kdt])
        pss = ps_s.next()
        for hl in range(2):
            c.op("pe", lambda e: e.matmul(pss[:, hl, :], lhsT=KR[32 * hl:32 * hl + 32, t0:t0 + 128], rhs=QR[32 * hl:32 * hl + 32, t0:t0 + 128],
                                          start=True, stop=True), reads=[(KR, b), (QR, b)], writes=[pss])
        sT = sT_rot.next()
        c.op(V, lambda e: e.tensor_tensor(out=sT[:], in0=pss[:], in1=maskT[:], op=ALU.mult), reads=[pss, maskT], writes=[sT])
        pso = ps_o.next()
        for hl in range(2):
            c.op("pe", lambda e: e.matmul(pso[:, hl * 64:(hl + 1) * 64], lhsT=sT[:, hl, :], rhs=v_sb[:, ch, hl * 64:(hl + 1) * 64],
                                          start=True, stop=False), reads=[sT, v_sb], writes=[pso])
            c.op("pe", lambda e: e.matmul(pso[:, hl * 64:(hl + 1) * 64], lhsT=QD[32 * hl:32 * hl + 32, t0:t0 + 128],
                                          rhs=Sb[32 * hl:32 * hl + 32, hl * 64:(hl + 1) * 64], start=False, stop=True),
                 reads=[(QD, b), Sb], writes=[pso])
        psd = ps_d.next()
        c.op("pe", lambda e: e.matmul(psd[:, :], lhsT=kdt[:], rhs=v_sb[:, ch, :], start=True, stop=True), reads=[kdt, v_sb], writes=[psd])
        c.op(V, lambda e: e.scalar_tensor_tensor(out=S32[:], in0=S32[:], scalar=sdec[:, 0:1], in1=psd[:], op0=ALU.mult, op1=ALU.add),
             reads=[S32, sdec, psd], writes=[S32])
        Sb = Sb_rot.next()
        c.op("act", lambda e: e.copy(out=Sb[:], in_=S32[:]), reads=[S32], writes=[Sb])
        o = o_rot.next(); cen = cen_rot.next(); sq = sq_rot.next(); st = st_rot.next(); gg = gg_rot.next()
        c.op("act", lambda e: e.copy(out=o[:].rearrange("p h v -> p (h v)"), in_=pso[:]), reads=[pso], writes=[o])
        c.op(V, lambda e: e.tensor_reduce(out=st[:, 0:2], in_=o[:], axis=AX.X, op=ALU.add), reads=[o], writes=[st])
        c.op(V, lambda e: e.tensor_scalar(st[:, 0:2], st[:, 0:2], 1.0 / 64, None, op0=ALU.mult), reads=[st], writes=[st])
        c.op(V, lambda e: e.tensor_tensor(out=cen[:], in0=o[:], in1=st[:, 0:2].unsqueeze(2).to_broadcast([128, 2, 64]), op=ALU.subtract),
             reads=[o, st], writes=[cen])
        c.op("pool", lambda e: e.tensor_tensor(out=sq[:], in0=cen[:], in1=cen[:], op=ALU.mult), reads=[cen], writes=[sq])
        c.op(V, lambda e: e.tensor_reduce(out=st[:, 2:4], in_=sq[:], axis=AX.X, op=ALU.add), reads=[sq, st], writes=[st])
        c.op("act", lambda e: e.activation(out=st[:, 2:4], in_=st[:, 2:4], func=AF.Sqrt, scale=1.0 / 64, bias=epsT[:, 0:1]), reads=[st, epsT], writes=[st])
        c.op(V, lambda e: e.reciprocal(st[:, 2:4], st[:, 2:4]), reads=[st], writes=[st])
        c.op("act", lambda e: e.activation(out=gg[:], in_=g_sb[:, ch, :], func=AF.Silu), reads=[g_sb], writes=[gg])
        c.op("pool", lambda e: e.tensor_tensor(out=gg[:], in0=gg[:], in1=og[:], op=ALU.mult), reads=[gg, og], writes=[gg])
        c.op(V, lambda e: e.tensor_tensor(out=cen[:], in0=cen[:], in1=st[:, 2:4].unsqueeze(2).to_broadcast([128, 2, 64]), op=ALU.mult),
             reads=[cen, st], writes=[cen])
        c.op(V, lambda e: e.tensor_tensor(out=y_sb[:, ch, :], in0=cen[:].rearrange("p h v -> p (h v)"), in1=gg[:], op=ALU.mult),
             reads=[cen, gg], writes=[(y_sb, ch)])
    c.barrier()
    if debug:
        dbg = {"lgP": lgP, "lgB": lgB, "fr": fr, "cs": cs, "E1r": E1r, "E1i": E1i, "Gr": Gr, "Gi": Gi, "maskT": maskT,
               "qdf": qdf, "kdf": kdf, "sdec": sdec, "S32": S32, "COSb": COSb, "SINb": SINb}
        for nm, t in dbg.items():
            shp = list(t.h.shape)
            dd = c.dram("dbg_" + nm, shp, F32, "ExternalOutput")
            c.dma("sp", dd[:], t[:], reads=[t])
        for nm, t in {"QR": QR, "KR": KR, "QD": QD, "KD": KD}.items():
            tmpf = c.sb("dbgf_" + nm, [64, 1024], F32)
            c.op("dve", lambda e: e.tensor_copy(tmpf[:], t[:, 0:1024]), writes=[tmpf])
            dd = c.dram("dbg_" + nm, [64, 1024], F32, "ExternalOutput")
            c.dma("sp", dd[:], tmpf[:], reads=[tmpf])
    c.dma("sp", y_d.h.ap().rearrange("(c p) f -> p c f", p=128), y_sb[:], reads=[y_sb], qt=y_sb)
    c.finish()
    c.close()
    print("ret instructions:", c.n_inst)
    return nc


NTOK = 8320
NG = 65
EPS = 1e-6
GB = 13
BW = GB * 128
NBLK = NG // GB
CS = 32


def build_hg(layer, debug=False):
    nc = bass.Bass("TRN2", target_bir_lowering=False)
    c = Ctx(nc)
    x_d = c.dram("x3", [3, 64, NTOK], F32, "ExternalInput")
    cw_d = c.dram("convw", [3, 64, 4], F32, "ExternalInput")
    g_d = c.dram("gate", [NTOK, 64], F32, "ExternalInput")
    og_d = c.dram("outg", [128, 64], F32, "ExternalInput")
    lb_d = c.dram("lbp", [64, 2], F32, "ExternalInput")
    y_d = c.dram("y", [NTOK, 64], F32, "ExternalOutput")
    V = "dve"

    epsT = c.sb("eps", [128, 1], F32)
    c.op("pool", lambda e: e.memset(epsT[:], EPS), writes=[epsT])
    iof = c.sb("iof", [128, 128], F32)
    c.op("pool", lambda e: e.iota(iof[:], [[1, 128]], base=0, channel_multiplier=-1, allow_small_or_imprecise_dtypes=True), writes=[iof])
    identf = c.sb("identf", [128, 128], F32)
    c.op(V, lambda e: e.tensor_single_scalar(identf[:], iof[:], 0.0, op=ALU.is_equal), reads=[iof], writes=[identf])
    identb = c.sb("identb", [128, 128], BF16)
    c.op(V, lambda e: e.tensor_copy(identb[:], identf[:]), reads=[identf], writes=[identb])
    dge = c.sb("dge", [128, 128], F32)
    c.op(V, lambda e: e.tensor_single_scalar(dge[:], iof[:], 0.0, op=ALU.is_ge), reads=[iof], writes=[dge])
    bmask = c.sb("bmask", [128, 128], F32)
    c.op(V, lambda e: e.memset(bmask[:], 0.0), writes=[bmask])
    for m in range(4):
        c.op(V, lambda e: e.tensor_copy(bmask[32 * m:32 * m + 32, 32 * m:32 * m + 32], dge[32 * m:32 * m + 32, 32 * m:32 * m + 32]),
             reads=[dge], writes=[bmask])
    cmask = c.sb("cmask", [128, 4, 64], F32)
    cm2 = c.sb("cm2", [128, 4, 64], F32)
    c.op("pool", lambda e: e.iota(cmask[:], [[-32, 4], [0, 64]], base=0, channel_multiplier=1, allow_small_or_imprecise_dtypes=True), writes=[cmask])
    c.op(V, lambda e: e.tensor_single_scalar(cm2[:], cmask[:], 32.0, op=ALU.is_lt), reads=[cmask], writes=[cm2])
    c.op(V, lambda e: e.tensor_single_scalar(cmask[:], cmask[:], 0.0, op=ALU.is_ge), reads=[cmask, cm2], writes=[cmask])
    c.op(V, lambda e: e.tensor_tensor(out=cmask[:], in0=cmask[:], in1=cm2[:], op=ALU.mult), reads=[cmask, cm2], writes=[cmask])
    colmask = c.sb("colmask", [64, 4, 128], F32)
    col2 = c.sb("col2", [64, 4, 128], F32)
    c.op("pool", lambda e: e.iota(colmask[:], [[-32, 4], [1, 128]], base=0, channel_multiplier=0, allow_small_or_imprecise_dtypes=True), writes=[colmask])
    c.op(V, lambda e: e.tensor_single_scalar(col2[:], colmask[:], 32.0, op=ALU.is_lt), reads=[colmask], writes=[col2])
    c.op(V, lambda e: e.tensor_single_scalar(colmask[:], colmask[:], 0.0, op=ALU.is_ge), reads=[colmask, col2], writes=[colmask])
    c.op(V, lambda e: e.tensor_tensor(out=colmask[:], in0=colmask[:], in1=col2[:], op=ALU.mult), reads=[colmask, col2], writes=[colmask])
    rmask = c.sb("rmask", [64, BW], F32)
    c.op("pool", lambda e: e.iota(rmask[:], [[0, BW // CS], [1, CS]], base=0, channel_multiplier=0, allow_small_or_imprecise_dtypes=True), writes=[rmask])
    c.op(V, lambda e: e.tensor_single_scalar(rmask[:], rmask[:], 0.0, op=ALU.is_gt), reads=[rmask], writes=[rmask])
    cw = c.sb("cw", [64, 3, 4], F32)
    c.dma("sp", cw[:], cw_d.h.ap().rearrange("s p k -> p s k"), writes=[cw])
    og = c.sb("og", [128, 64], F32)
    c.dma("sp", og[:], og_d[:], writes=[og])
    lbp = c.sb("lbp", [64, 4], F32)
    c.dma("sp", lbp[:, 0:2], lb_d[:], writes=[lbp])
    if layer == 0:
        c.op(V, lambda e: e.memset(lbp[:, 2:3], 0.0), reads=[lbp], writes=[lbp])
    else:
        c.op(V, lambda e: e.tensor_tensor(out=lbp[:, 2:3], in0=lbp[:, 1:2], in1=lbp[:, 0:1], op=ALU.subtract), reads=[lbp], writes=[lbp])
        c.op("act", lambda e: e.activation(out=lbp[:, 2:3], in_=lbp[:, 2:3], func=AF.Sigmoid), reads=[lbp], writes=[lbp])
    c.op(V, lambda e: e.tensor_scalar(lbp[:, 3:4], lbp[:, 2:3], -1.0, 1.0, op0=ALU.mult, op1=ALU.add), reads=[lbp], writes=[lbp])

    QT = c.sb("QT", [64, NTOK], BF16)
    KT = c.sb("KT", [64, NTOK], BF16)
    KDT = c.sb("KDT", [64, NTOK], BF16)
    Vtok = c.sb("Vtok", [128, NG, 64], BF16)
    KDtok = c.sb("KDtok", [128, NG, 64], BF16)
    g_sb = c.sb("g_sb", [128, NG, 64], BF16)
    y_sb = c.sb("y_sb", [128, NG, 64], F32)
    Dec = c.sb("Dec", [64, NTOK // CS], F32)
    c.dma("pool", g_sb[:], g_d.h.ap().rearrange("(c p) f -> p c f", p=128), writes=[g_sb])

    xq = c.sb("xq", [64, BW + 3], F32); xf = c.sb("xf", [64, BW + 3], F32); xi = c.sb("xi", [64, BW + 3], F32)
    cq = c.sb("cq", [64, BW], F32); cf = c.sb("cf", [64, BW], F32); ci = c.sb("ci", [64, BW], F32)
    gg_ = c.sb("g", [64, BW], F32); gcum = c.sb("gcum", [64, BW], F32); tmp = c.sb("tmp", [64, BW], F32)
    ps_t = Rot([c.ps(f"pst{i}", [128, 64], F32) for i in range(2)])
    ps_tb = Rot([c.ps(f"pstb{i}", [128, 64], BF16) for i in range(2)])
    NCB = BW // CS
    for b in range(NBLK):
        t0 = b * BW
        for si, (xt, ct) in enumerate(((xq, cq), (xf, cf), (xi, ci))):
            if b == 0:
                c.op(V, lambda e: e.memset(xt[:, 0:3], 0.0), writes=[xt])
                c.dma("sp", xt[:, 3:BW + 3], x_d.h.ap()[si, :, 0:BW], writes=[xt])
            else:
                c.dma("sp", xt[:, :], x_d.h.ap()[si, :, t0 - 3:t0 + BW], writes=[xt])
            c.op(V, lambda e: e.tensor_scalar(ct[:], xt[:, 0:BW], cw[:, si, 0:1], None, op0=ALU.mult), reads=[xt, cw], writes=[ct])
            for kk_ in range(1, 4):
                c.op(V, lambda e: e.scalar_tensor_tensor(out=ct[:], in0=xt[:, kk_:kk_ + BW], scalar=cw[:, si, kk_:kk_ + 1], in1=ct[:],
                                                         op0=ALU.mult, op1=ALU.add), reads=[xt, cw, ct], writes=[ct])
        c.op("act", lambda e: e.activation(out=cq[:], in_=cq[:], func=AF.Silu), reads=[cq], writes=[cq])
        c.op("act", lambda e: e.activation(out=cf[:], in_=cf[:], func=AF.Sigmoid), reads=[cf], writes=[cf])
        c.op(V, lambda e: e.tensor_scalar(cf[:], cf[:], lbp[:, 3:4], lbp[:, 2:3], op0=ALU.mult, op1=ALU.add), reads=[cf, lbp], writes=[cf])
        c.op("act", lambda e: e.activation(out=gg_[:], in_=cf[:], func=AF.Ln), reads=[cf], writes=[gg_])
        c.op(V, lambda e: e.tensor_scalar(cf[:], cf[:], -1.0, 1.0, op0=ALU.mult, op1=ALU.add), reads=[cf, gg_], writes=[cf])
        c.op(V, lambda e: e.tensor_tensor_scan(out=gcum[:], data0=rmask[:], data1=gg_[:], initial=0.0, op0=ALU.mult, op1=ALU.add),
             reads=[rmask, gg_], writes=[gcum])
        c.op("act", lambda e: e.activation(out=tmp[:], in_=gcum[:], func=AF.Exp), reads=[gcum], writes=[tmp])
        c.op(V, lambda e: e.tensor_tensor(out=QT[:, t0:t0 + BW], in0=cq[:], in1=tmp[:], op=ALU.mult), reads=[cq, tmp], writes=[(QT, b)])
        c.op(V, lambda e: e.tensor_single_scalar(tmp[:], gcum[:], -80.0, op=ALU.max), reads=[gcum, (QT, b)], writes=[tmp])
        c.op("act", lambda e: e.activation(out=tmp[:], in_=tmp[:], func=AF.Exp, scale=-1.0), reads=[tmp], writes=[tmp])
        c.op(V, lambda e: e.tensor_tensor(out=KT[:, t0:t0 + BW], in0=cf[:], in1=tmp[:], op=ALU.mult), reads=[cf, tmp], writes=[(KT, b)])
        gl = gcum[:].rearrange("p (c j) -> p c j", j=CS)[:, :, CS - 1:CS]
        c.op(V, lambda e: e.tensor_tensor(out=tmp[:].rearrange("p (c j) -> p c j", j=CS), in0=gl.to_broadcast([64, NCB, CS]),
                                          in1=gcum[:].rearrange("p (c j) -> p c j", j=CS), op=ALU.subtract), reads=[gcum, (KT, b)], writes=[tmp])
        c.op("act", lambda e: e.activation(out=tmp[:], in_=tmp[:], func=AF.Exp), reads=[tmp], writes=[tmp])
        c.op(V, lambda e: e.tensor_tensor(out=KDT[:, t0:t0 + BW], in0=cf[:], in1=tmp[:], op=ALU.mult), reads=[cf, tmp], writes=[(KDT, b)])
        c.op("act", lambda e: e.activation(out=Dec[:, b * NCB:(b + 1) * NCB].unsqueeze(2), in_=gl, func=AF.Exp), reads=[gcum], writes=[(Dec, b)])
        for gi in range(GB):
            G = b * GB + gi
            pt = ps_t.next()
            c.op("pe", lambda e: e.transpose(pt[:, :], ci[:, gi * 128:(gi + 1) * 128], identf[0:64, 0:64]), reads=[ci, identf], writes=[pt])
            c.op("act", lambda e: e.copy(out=Vtok[:, G, :], in_=pt[:]), reads=[pt], writes=[(Vtok, G)])
            ptb = ps_tb.next()
            c.op("pe", lambda e: e.transpose(ptb[:, :], KDT[:, t0 + gi * 128:t0 + (gi + 1) * 128], identb[0:64, 0:64]),
                 reads=[(KDT, b), identb], writes=[ptb])
            c.op("act", lambda e: e.copy(out=KDtok[:, G, :], in_=ptb[:]), reads=[ptb], writes=[(KDtok, G)])

    ps_s = Rot([c.ps(f"ps_s{i}", [128, 128], F32) for i in range(1)])
    ps_o = Rot([c.ps(f"ps_o{i}", [128, 64], F32) for i in range(2)])
    ps_d = Rot([c.ps(f"ps_d{i}", [64, 4, 64], F32) for i in range(1)])
    sT_rot = Rot([c.sb(f"sT{i}", [128, 128], BF16) for i in range(2)])
    S32 = c.sb("S32", [64, 64], F32)
    c.op(V, lambda e: e.memset(S32[:], 0.0), writes=[S32])
    Sb_rot = Rot([c.sb(f"Sb{i}", [64, 64], BF16) for i in range(6)])
    Sb = Sb_rot.next()
    c.op(V, lambda e: e.memset(Sb[:], 0.0), writes=[Sb])
    o_rot = Rot([c.sb(f"o{i}", [128, 64], F32) for i in range(2)])
    sq_rot = Rot([c.sb(f"sqr{i}", [128, 64], F32) for i in range(2)])
    st_rot = Rot([c.sb(f"st{i}", [128, 4], F32) for i in range(2)])
    gg_rot = Rot([c.sb(f"gg{i}", [128, 64], F32) for i in range(2)])
    vb_rot = Rot([c.sb(f"vb{i}", [128, 4, 64], BF16) for i in range(2)])
    qm_rot = Rot([c.sb(f"qm{i}", [64, 4, 128], BF16) for i in range(2)])
    for G in range(NG):
        b = G // GB
        t0 = G * 128
        pss = ps_s.next()
        c.op("pe", lambda e: e.matmul(pss[:, :], lhsT=KT[:, t0:t0 + 128], rhs=QT[:, t0:t0 + 128], start=True, stop=True),
             reads=[(KT, b), (QT, b)], writes=[pss])
        sT = sT_rot.next()
        c.op(V, lambda e: e.tensor_tensor(out=sT[:], in0=pss[:], in1=bmask[:], op=ALU.mult), reads=[pss, bmask], writes=[sT])
        psd = ps_d.next()
        vb = vb_rot.next()
        c.op("pool", lambda e: e.tensor_tensor(out=vb[:], in0=Vtok[:, G, :].unsqueeze(1).to_broadcast([128, 4, 64]), in1=cmask[:], op=ALU.mult),
             reads=[(Vtok, G), cmask], writes=[vb])
        c.op("pe", lambda e: e.matmul(psd[:].rearrange("p m v -> p (m v)"), lhsT=KDtok[:, G, :], rhs=vb[:].rearrange("p m v -> p (m v)"),
                                      start=True, stop=True), reads=[(KDtok, G), vb], writes=[psd])
        qm = qm_rot.next()
        c.op(V, lambda e: e.tensor_tensor(out=qm[:], in0=QT[:, t0:t0 + 128].unsqueeze(1).to_broadcast([64, 4, 128]), in1=colmask[:], op=ALU.mult),
             reads=[(QT, b), colmask], writes=[qm])
        pso = ps_o.next()
        c.op("pe", lambda e: e.matmul(pso[:, :], lhsT=sT[:], rhs=Vtok[:, G, :], start=True, stop=False), reads=[sT, (Vtok, G)], writes=[pso])
        for m in range(4):
            ch = G * 4 + m
            c.op("pe", lambda e: e.matmul(pso[:, :], lhsT=qm[:, m, :], rhs=Sb[:, :],
                                          start=False, stop=(m == 3)), reads=[qm, Sb], writes=[pso])
            c.op(V, lambda e: e.scalar_tensor_tensor(out=S32[:], in0=S32[:], scalar=Dec[:, ch:ch + 1], in1=psd[:, m, :],
                                                     op0=ALU.mult, op1=ALU.add), reads=[S32, (Dec, b), psd], writes=[S32])
            Sb = Sb_rot.next()
            c.op("act", lambda e: e.copy(out=Sb[:], in_=S32[:]), reads=[S32], writes=[Sb])
        o = o_rot.next(); sq = sq_rot.next(); st = st_rot.next(); gg = gg_rot.next()
        c.op("act", lambda e: e.copy(out=o[:], in_=pso[:]), reads=[pso], writes=[o])
        c.op("pool", lambda e: e.tensor_tensor(out=sq[:], in0=o[:], in1=o[:], op=ALU.mult), reads=[o], writes=[sq])
        c.op(V, lambda e: e.tensor_reduce(out=st[:, 0:1], in_=sq[:], axis=AX.X, op=ALU.add), reads=[sq], writes=[st])
        c.op("act", lambda e: e.activation(out=st[:, 0:1], in_=st[:, 0:1], func=AF.Sqrt, scale=1.0 / 64, bias=epsT[:, 0:1]), reads=[st, epsT], writes=[st])
        c.op(V, lambda e: e.reciprocal(st[:, 0:1], st[:, 0:1]), reads=[st], writes=[st])
        c.op("act", lambda e: e.activation(out=gg[:], in_=g_sb[:, G, :], func=AF.Silu), reads=[g_sb], writes=[gg])
        c.op("pool", lambda e: e.tensor_tensor(out=gg[:], in0=gg[:], in1=og[:], op=ALU.mult), reads=[gg, og], writes=[gg])
        c.op(V, lambda e: e.scalar_tensor_tensor(out=y_sb[:, G, :], in0=o[:], scalar=st[:, 0:1], in1=gg[:], op0=ALU.mult, op1=ALU.mult),
             reads=[o, st, gg], writes=[(y_sb, G)])
    c.barrier()
    c.dma("sp", y_d.h.ap().rearrange("(c p) f -> p c f", p=128), y_sb[:], reads=[y_sb], qt=y_sb)
    c.finish()
    c.close()
    print("hg instructions:", c.n_inst)
    return nc


NSB = 1040
NGL = 4
NLEV = 11


def emit_sincos_tile(c, x, xT, cs_c, cs_s, x2, acc, n):
    V = "dve"
    c.op(V, lambda e: e.tensor_tensor(out=x2[:], in0=x[:], in1=x[:], op=ALU.mult), reads=[x], writes=[x2])
    c.op(V, lambda e: e.tensor_scalar(acc[:], x2[:], -1.0 / 156, 1.0, op0=ALU.mult, op1=ALU.add), reads=[x2], writes=[acc])
    for d in (110.0, 72.0, 42.0, 20.0, 6.0):
        c.op(V, lambda e: e.tensor_tensor(out=acc[:], in0=acc[:], in1=x2[:], op=ALU.mult), reads=[acc, x2], writes=[acc])
        c.op(V, lambda e: e.tensor_scalar(acc[:], acc[:], -1.0 / d, 1.0, op0=ALU.mult, op1=ALU.add), reads=[acc], writes=[acc])
    c.op(V, lambda e: e.tensor_tensor(out=cs_s[:], in0=acc[:], in1=x[:], op=ALU.mult), reads=[acc, x], writes=[cs_s])
    c.op(V, lambda e: e.tensor_scalar(acc[:], x2[:], -1.0 / 182, 1.0, op0=ALU.mult, op1=ALU.add), reads=[x2, cs_s], writes=[acc])
    for d in (132.0, 90.0, 56.0, 30.0, 12.0, 2.0):
        c.op(V, lambda e: e.tensor_tensor(out=acc[:], in0=acc[:], in1=x2[:], op=ALU.mult), reads=[acc, x2], writes=[acc])
        c.op(V, lambda e: e.tensor_scalar(acc[:], acc[:], -1.0 / d, 1.0, op0=ALU.mult, op1=ALU.add), reads=[acc], writes=[acc])
    c.op(V, lambda e: e.tensor_copy(cs_c[:], acc[:]), reads=[acc], writes=[cs_c])


def emit_cdouble(c, cr, ci, t1, t2):
    V = "dve"
    c.op(V, lambda e: e.tensor_tensor(out=t1[:], in0=cr[:], in1=cr[:], op=ALU.mult), reads=[cr], writes=[t1])
    c.op(V, lambda e: e.tensor_tensor(out=t2[:], in0=ci[:], in1=ci[:], op=ALU.mult), reads=[ci], writes=[t2])
    c.op(V, lambda e: e.scalar_tensor_tensor(out=ci[:], in0=cr[:], scalar=2.0, in1=ci[:], op0=ALU.mult, op1=ALU.mult), reads=[cr, ci, t2], writes=[ci])
    c.op(V, lambda e: e.tensor_tensor(out=cr[:], in0=t1[:], in1=t2[:], op=ALU.subtract), reads=[t1, t2, ci], writes=[cr])


def build_s5(debug=False):
    nc = bass.Bass("TRN2", target_bir_lowering=False)
    c = Ctx(nc)
    U_d = c.dram("U", [NGL, 128, NSB], F32, "ExternalInput")
    lam_d = c.dram("lam", [128, NGL, 2], F32, "ExternalInput")
    ls_d = c.dram("ls", [128, NGL], F32, "ExternalInput")
    B_d = c.dram("Bm", [128, NGL, 2, 16], F32, "ExternalInput")
    C_d = c.dram("Cm", [128, NGL, 2, 16], F32, "ExternalInput")
    D_d = c.dram("Dm", [128, NGL], F32, "ExternalInput")
    y_d = c.dram("y", [NGL, 128, NSB], F32, "ExternalOutput")
    V = "dve"
    G4 = NGL

    def sb(name, shape, dt=F32):
        return c.sb(name, shape, dt)

    iof = sb("iof", [128, 128])
    c.op("pool", lambda e: e.iota(iof[:], [[1, 128]], base=0, channel_multiplier=-1, allow_small_or_imprecise_dtypes=True), writes=[iof])
    ident = sb("ident", [128, 128])
    c.op(V, lambda e: e.tensor_single_scalar(ident[:], iof[:], 0.0, op=ALU.is_equal), reads=[iof], writes=[ident])
    pswap = sb("pswap", [128, 128])
    ptmp = sb("ptmp", [128, 128])
    c.op(V, lambda e: e.tensor_single_scalar(pswap[:], iof[:], 64.0, op=ALU.is_equal), reads=[iof], writes=[pswap])
    c.op(V, lambda e: e.tensor_single_scalar(ptmp[:], iof[:], -64.0, op=ALU.is_equal), reads=[iof], writes=[ptmp])
    c.op(V, lambda e: e.tensor_tensor(out=pswap[:], in0=pswap[:], in1=ptmp[:], op=ALU.add), reads=[pswap, ptmp], writes=[pswap])
    tmask = sb("tmask", [128, 8, 16])
    c.op("pool", lambda e: e.iota(tmask[:], [[16, 8], [0, 16]], base=15, channel_multiplier=-1, allow_small_or_imprecise_dtypes=True), writes=[tmask])
    c.op(V, lambda e: e.tensor_single_scalar(tmask[:], tmask[:], 0.0, op=ALU.is_ge), reads=[tmask], writes=[tmask])
    sgnh = sb("sgnh", [128, 1])
    c.op(V, lambda e: e.memset(sgnh[0:64, :], 1.0), writes=[sgnh])
    c.op(V, lambda e: e.memset(sgnh[64:128, :], -1.0), writes=[sgnh])
    mv = sb("mv", [128, G4, 9])
    c.op("pool", lambda e: e.iota(mv[:], [[0, G4], [1, 9]], base=0, channel_multiplier=0, allow_small_or_imprecise_dtypes=True), writes=[mv])

    lam = sb("lam", [128, G4, 2]); ls = sb("ls", [128, G4]); Bm = sb("Bm", [128, G4, 2, 16]); Cm = sb("Cm", [128, G4, 2, 16]); Dm = sb("Dm", [128, G4])
    c.dma("sp", lam[:], lam_d[:], writes=[lam]); c.dma("sp", ls[:], ls_d[:], writes=[ls])
    c.dma("sp", Bm[:], B_d[:], writes=[Bm]); c.dma("sp", Cm[:], C_d[:], writes=[Cm]); c.dma("sp", Dm[:], D_d[:], writes=[Dm])

    st = sb("st", [128, G4]); a = sb("a", [128, G4]); th = sb("th", [128, G4])
    c.op("act", lambda e: e.activation(out=st[:], in_=ls[:], func=AF.Exp), reads=[ls], writes=[st])
    c.op(V, lambda e: e.tensor_tensor(out=a[:], in0=lam[:, :, 0], in1=st[:], op=ALU.mult), reads=[lam, st], writes=[a])
    c.op(V, lambda e: e.tensor_tensor(out=th[:], in0=lam[:, :, 1], in1=st[:], op=ALU.mult), reads=[lam, st], writes=[th])
    kq = sb("kq", [128, G4]); ki = sb("ki", [128, G4], I32); x = sb("x", [128, G4])
    c.op(V, lambda e: e.tensor_scalar(kq[:], th[:], 1.0 / (2 * math.pi), None, op0=ALU.mult), reads=[th], writes=[kq])
    c.op(V, lambda e: e.tensor_copy(ki[:], kq[:]), reads=[kq], writes=[ki])
    c.op(V, lambda e: e.tensor_copy(kq[:], ki[:]), reads=[ki], writes=[kq])
    c.op(V, lambda e: e.scalar_tensor_tensor(out=x[:], in0=kq[:], scalar=-2 * math.pi, in1=th[:], op0=ALU.mult, op1=ALU.add), reads=[kq, th], writes=[x])
    c.op(V, lambda e: e.tensor_scalar(x[:], x[:], 0.25, None, op0=ALU.mult), reads=[x], writes=[x])
    p1r = sb("p1r", [128, G4]); p1i = sb("p1i", [128, G4]); x2 = sb("x2", [128, G4]); acc = sb("acc", [128, G4])
    t1 = sb("t1", [128, G4]); t2 = sb("t2", [128, G4])
    emit_sincos_tile(c, x, x, p1r, p1i, x2, acc, G4)
    emit_cdouble(c, p1r, p1i, t1, t2)
    emit_cdouble(c, p1r, p1i, t1, t2)
    phr = sb("phr", [128, G4, 9]); phi = sb("phi", [128, G4, 9])
    c.op(V, lambda e: e.memset(phr[:, :, 0:1], 1.0), writes=[phr])
    c.op(V, lambda e: e.memset(phi[:, :, 0:1], 0.0), writes=[phi])
    for m in range(8):
        c.op(V, lambda e: e.tensor_tensor(out=t1[:], in0=phr[:, :, m], in1=p1r[:], op=ALU.mult), reads=[phr, p1r], writes=[t1])
        c.op(V, lambda e: e.tensor_tensor(out=t2[:], in0=phi[:, :, m], in1=p1i[:], op=ALU.mult), reads=[phi, p1i], writes=[t2])
        c.op(V, lambda e: e.tensor_tensor(out=phr[:, :, m + 1], in0=t1[:], in1=t2[:], op=ALU.subtract), reads=[t1, t2], writes=[phr])
        c.op(V, lambda e: e.tensor_tensor(out=t1[:], in0=phr[:, :, m], in1=p1i[:], op=ALU.mult), reads=[phr, p1i], writes=[t1])
        c.op(V, lambda e: e.tensor_tensor(out=t2[:], in0=phi[:, :, m], in1=p1r[:], op=ALU.mult), reads=[phi, p1r], writes=[t2])
        c.op(V, lambda e: e.tensor_tensor(out=phi[:, :, m + 1], in0=t1[:], in1=t2[:], op=ALU.add), reads=[t1, t2], writes=[phi])
    am = sb("am", [128, G4, 9]); magp = sb("magp", [128, G4, 9]); magn = sb("magn", [128, G4, 9])
    c.op(V, lambda e: e.tensor_tensor(out=am[:], in0=mv[:], in1=a[:].unsqueeze(2).to_broadcast([128, G4, 9]), op=ALU.mult), reads=[mv, a], writes=[am])
    c.op("act", lambda e: e.activation(out=magp[:], in_=am[:], func=AF.Exp), reads=[am], writes=[magp])
    c.op("act", lambda e: e.activation(out=magn[:], in_=am[:], func=AF.Exp, scale=-1.0), reads=[am], writes=[magn])
    LPr = sb("LPr", [128, G4, 9]); LPi = sb("LPi", [128, G4, 9]); LNr = sb("LNr", [128, G4, 9]); LNi = sb("LNi", [128, G4, 9])
    c.op(V, lambda e: e.tensor_tensor(out=LPr[:], in0=magp[:], in1=phr[:], op=ALU.mult), reads=[magp, phr], writes=[LPr])
    c.op(V, lambda e: e.tensor_tensor(out=LPi[:], in0=magp[:], in1=phi[:], op=ALU.mult), reads=[magp, phi], writes=[LPi])
    c.op(V, lambda e: e.tensor_tensor(out=LNr[:], in0=magn[:], in1=phr[:], op=ALU.mult), reads=[magn, phr], writes=[LNr])
    c.op(V, lambda e: e.scalar_tensor_tensor(out=LNi[:], in0=magn[:], scalar=-1.0, in1=phi[:], op0=ALU.mult, op1=ALU.mult), reads=[magn, phi], writes=[LNi])
    kr = sb("kr", [128, G4]); kim = sb("kim", [128, G4]); nr = sb("nr", [128, G4]); den = sb("den", [128, G4])
    c.op(V, lambda e: e.tensor_scalar(nr[:], LPr[:, :, 1], -1.0, None, op0=ALU.add), reads=[LPr], writes=[nr])
    c.op(V, lambda e: e.tensor_tensor(out=t1[:], in0=lam[:, :, 0], in1=lam[:, :, 0], op=ALU.mult), reads=[lam], writes=[t1])
    c.op(V, lambda e: e.tensor_tensor(out=t2[:], in0=lam[:, :, 1], in1=lam[:, :, 1], op=ALU.mult), reads=[lam], writes=[t2])
    c.op(V, lambda e: e.tensor_tensor(out=den[:], in0=t1[:], in1=t2[:], op=ALU.add), reads=[t1, t2], writes=[den])
    c.op(V, lambda e: e.reciprocal(den[:], den[:]), reads=[den], writes=[den])
    c.op(V, lambda e: e.tensor_tensor(out=t1[:], in0=nr[:], in1=lam[:, :, 0], op=ALU.mult), reads=[nr, lam], writes=[t1])
    c.op(V, lambda e: e.tensor_tensor(out=t2[:], in0=LPi[:, :, 1], in1=lam[:, :, 1], op=ALU.mult), reads=[LPi, lam], writes=[t2])
    c.op(V, lambda e: e.tensor_tensor(out=kr[:], in0=t1[:], in1=t2[:], op=ALU.add), reads=[t1, t2], writes=[kr])
    c.op(V, lambda e: e.tensor_tensor(out=kr[:], in0=kr[:], in1=den[:], op=ALU.mult), reads=[kr, den], writes=[kr])
    c.op(V, lambda e: e.tensor_tensor(out=t1[:], in0=LPi[:, :, 1], in1=lam[:, :, 0], op=ALU.mult), reads=[LPi, lam, kr], writes=[t1])
    c.op(V, lambda e: e.tensor_tensor(out=t2[:], in0=nr[:], in1=lam[:, :, 1], op=ALU.mult), reads=[nr, lam, kr], writes=[t2])
    c.op(V, lambda e: e.tensor_tensor(out=kim[:], in0=t1[:], in1=t2[:], op=ALU.subtract), reads=[t1, t2], writes=[kim])
    c.op(V, lambda e: e.tensor_tensor(out=kim[:], in0=kim[:], in1=den[:], op=ALU.mult), reads=[kim, den], writes=[kim])
    Bbr = sb("Bbr", [128, G4, 16]); Bbi = sb("Bbi", [128, G4, 16]); tb = sb("tb", [128, G4, 16])
    krb = kr[:].unsqueeze(2).to_broadcast([128, G4, 16]); kib = kim[:].unsqueeze(2).to_broadcast([128, G4, 16])
    c.op(V, lambda e: e.tensor_tensor(out=Bbr[:], in0=Bm[:, :, 0, :], in1=krb, op=ALU.mult), reads=[Bm, kr], writes=[Bbr])
    c.op(V, lambda e: e.tensor_tensor(out=tb[:], in0=Bm[:, :, 1, :], in1=kib, op=ALU.mult), reads=[Bm, kim], writes=[tb])
    c.op(V, lambda e: e.tensor_tensor(out=Bbr[:], in0=Bbr[:], in1=tb[:], op=ALU.subtract), reads=[Bbr, tb], writes=[Bbr])
    c.op(V, lambda e: e.tensor_tensor(out=Bbi[:], in0=Bm[:, :, 1, :], in1=krb, op=ALU.mult), reads=[Bm, kr], writes=[Bbi])
    c.op(V, lambda e: e.tensor_tensor(out=tb[:], in0=Bm[:, :, 0, :], in1=kib, op=ALU.mult), reads=[Bm, kim, Bbr], writes=[tb])
    c.op(V, lambda e: e.tensor_tensor(out=Bbi[:], in0=Bbi[:], in1=tb[:], op=ALU.add), reads=[Bbi, tb], writes=[Bbi])
    X1 = sb("X1", [128, G4, 16]); X2 = sb("X2", [128, G4, 16]); C1 = sb("C1", [128, G4, 16]); C2 = sb("C2", [128, G4, 16])
    c.op(V, lambda e: e.tensor_copy(X1[0:64], Bbr[0:64]), reads=[Bbr], writes=[X1])
    c.op(V, lambda e: e.tensor_copy(X1[64:128], Bbi[64:128]), reads=[Bbi], writes=[X1])
    c.op(V, lambda e: e.tensor_scalar(X2[0:64], Bbi[0:64], -1.0, None, op0=ALU.mult), reads=[Bbi], writes=[X2])
    c.op(V, lambda e: e.tensor_copy(X2[64:128], Bbr[64:128]), reads=[Bbr], writes=[X2])
    c.op(V, lambda e: e.tensor_copy(C1[0:64], Cm[0:64, :, 0, :]), reads=[Cm], writes=[C1])
    c.op(V, lambda e: e.tensor_scalar(C1[64:128], Cm[64:128, :, 1, :], -1.0, None, op0=ALU.mult), reads=[Cm], writes=[C1])
    c.op(V, lambda e: e.tensor_scalar(C2[0:64], Cm[0:64, :, 1, :], -1.0, None, op0=ALU.mult), reads=[Cm], writes=[C2])
    c.op(V, lambda e: e.tensor_scalar(C2[64:128], Cm[64:128, :, 0, :], -1.0, None, op0=ALU.mult), reads=[Cm], writes=[C2])
    Z = sb("Z", [128, G4, 8, 16]); Y = sb("Y", [128, G4, 8, 16]); Wc = sb("Wc", [128, G4, 8, 16]); tz = sb("tz", [128, 16])
    for g in range(G4):
        for j in range(8):
            for (OUT, Lr_, Li_, mi, A1, A2) in ((Z, LPr, LPi, 7 - j, X1, X2), (Y, LNr, LNi, j + 1, X1, X2), (Wc, LPr, LPi, j + 1, C1, C2)):
                c.op(V, lambda e: e.tensor_scalar(tz[:], A1[:, g, :], Lr_[:, g, mi:mi + 1], None, op0=ALU.mult), reads=[A1, Lr_], writes=[tz])
                c.op(V, lambda e: e.scalar_tensor_tensor(out=OUT[:, g, j, :], in0=A2[:, g, :], scalar=Li_[:, g, mi:mi + 1], in1=tz[:],
                                                         op0=ALU.mult, op1=ALU.add), reads=[A2, Li_, tz], writes=[OUT])
    ps_rot = Rot([c.ps(f"ps{i}", [128, 512], F32) for i in range(7)])
    Toep = sb("Toep", [128, G4, 128], BF16); W1 = sb("W1", [128, G4, 128], BF16); tf = sb("tf", [128, 128])
    for g in range(G4):
        ps = ps_rot.next()
        c.op("pe", lambda e: e.matmul(ps[:, 0:128], lhsT=Y[:, g].rearrange("p j h -> p (j h)"), rhs=Wc[:, g].rearrange("p j h -> p (j h)"),
                                      start=True, stop=True), reads=[Y, Wc], writes=[ps])
        c.op(V, lambda e: e.tensor_tensor(out=tf[:], in0=ps[:, 0:128], in1=tmask[:].rearrange("p j h -> p (j h)"), op=ALU.mult),
             reads=[ps, tmask], writes=[tf])
        c.op(V, lambda e: e.scalar_tensor_tensor(out=Toep[:, g, :], in0=ident[:], scalar=Dm[:, g:g + 1], in1=tf[:], op0=ALU.mult, op1=ALU.add),
             reads=[ident, Dm, tf], writes=[Toep])
        ps = ps_rot.next()
        c.op("pe", lambda e: e.transpose(ps[:, 0:128], Z[:, g].rearrange("p j h -> p (j h)"), ident[:]), reads=[Z, ident], writes=[ps])
        c.op("act", lambda e: e.copy(out=W1[:, g, :], in_=ps[:, 0:128]), reads=[ps], writes=[W1])
    R = sb("R", [128, G4, NLEV, 128])
    qr = sb("qr", [128, G4]); qi = sb("qi", [128, G4]); mg = sb("mg", [128, G4]); s1 = sb("s1", [128, G4]); s2 = sb("s2", [128, G4])
    c.op(V, lambda e: e.tensor_copy(qr[:], phr[:, :, 8]), reads=[phr], writes=[qr])
    c.op(V, lambda e: e.tensor_copy(qi[:], phi[:, :, 8]), reads=[phi], writes=[qi])
    for k in range(NLEV):
        c.op("act", lambda e: e.activation(out=mg[:], in_=a[:], func=AF.Exp, scale=float(8 * (2 ** k))), reads=[a], writes=[mg])
        c.op(V, lambda e: e.tensor_tensor(out=s1[:], in0=mg[:], in1=qr[:], op=ALU.mult), reads=[mg, qr], writes=[s1])
        c.op(V, lambda e: e.tensor_tensor(out=s2[:], in0=mg[:], in1=qi[:], op=ALU.mult), reads=[mg, qi], writes=[s2])
        c.op(V, lambda e: e.tensor_scalar(s2[:], s2[:], sgnh[:, 0:1], None, op0=ALU.mult), reads=[s2, sgnh], writes=[s2])
        for g in range(G4):
            c.op(V, lambda e: e.tensor_scalar(R[:, g, k, :], ident[:], s1[:, g:g + 1], None, op0=ALU.mult), reads=[ident, s1], writes=[R])
            c.op(V, lambda e: e.scalar_tensor_tensor(out=R[:, g, k, :], in0=pswap[:], scalar=s2[:, g:g + 1], in1=R[:, g, k, :],
                                                     op0=ALU.mult, op1=ALU.add), reads=[pswap, s2, R], writes=[R])
        if k < NLEV - 1:
            emit_cdouble(c, qr, qi, t1, t2)

    blocks = [(0, 512), (512, 512), (1024, NSB - 1024)]
    Ub = [sb(f"Ub{g}", [128, NSB], BF16) for g in range(G4)]
    Xp = [sb(f"Xp{g}", [128, NSB + 1]) for g in range(G4)]
    for g in range(G4):
        c.dma("pool", Ub[g][:], U_d.h.ap()[g], writes=[Ub[g]])
        c.op(V, lambda e: e.memset(Xp[g][:, 0:1], 0.0), writes=[Xp[g]])
        for (c0, n) in blocks:
            ps = ps_rot.next()
            c.op("pe", lambda e: e.matmul(ps[:, :n], lhsT=W1[:, g, :], rhs=Ub[g][:, c0:c0 + n], start=True, stop=True), reads=[W1, Ub[g]], writes=[ps])
            c.op("act", lambda e: e.copy(out=Xp[g][:, 1 + c0:1 + c0 + n], in_=ps[:, :n]), reads=[ps], writes=[Xp[g]])
    for k in range(NLEV):
        d = 2 ** k
        if d >= NSB:
            break
        L = NSB - d
        for g in range(G4):
            pl = []
            cc = 0
            while cc < L:
                n = min(512, L - cc)
                ps = ps_rot.next()
                c.op("pe", lambda e: e.matmul(ps[:, :n], lhsT=R[:, g, k, :], rhs=Xp[g][:, 1 + cc:1 + cc + n], start=True, stop=True),
                     reads=[R, Xp[g]], writes=[ps])
                pl.append((ps, cc, n))
                cc += n
            for (ps, cc, n) in pl:
                c.op(V, lambda e: e.tensor_tensor(out=Xp[g][:, 1 + d + cc:1 + d + cc + n], in0=Xp[g][:, 1 + d + cc:1 + d + cc + n], in1=ps[:, :n], op=ALU.add),
                     reads=[ps, Xp[g]], writes=[Xp[g]])
    y_rot = Rot([sb(f"ysb{i}", [128, NSB]) for i in range(2)])
    for g in range(G4):
        ysb = y_rot.next()
        for (c0, n) in blocks:
            ps = ps_rot.next()
            c.op("pe", lambda e: e.matmul(ps[:, :n], lhsT=Toep[:, g, :], rhs=Ub[g][:, c0:c0 + n], start=True, stop=False), reads=[Toep, Ub[g]], writes=[ps])
            c.op("pe", lambda e: e.matmul(ps[:, :n], lhsT=Wc[:, g].rearrange("p j h -> p (j h)"), rhs=Xp[g][:, c0:c0 + n], start=False, stop=True),
                 reads=[Wc, Xp[g]], writes=[ps])
            c.op("act", lambda e: e.copy(out=ysb[:, c0:c0 + n], in_=ps[:, :n]), reads=[ps], writes=[ysb])
        c.dma("sp", y_d.h.ap()[g], ysb[:], reads=[ysb])
    c.finish()
    c.close()
    print("s5 instructions:", c.n_inst)
    return nc


NT = 2080
def core_tok_idx(c):
    b, r = divmod(c, 4)
    return b, np.concatenate([np.arange(32 * r, 32 * r + 32), 128 + np.arange(2048 * r, 2048 * (r + 1))])

def build_H0(x, meta):
    B = x.shape[0]
    H = np.zeros((B, 8320, 1024), np.float32)
    H[:, 112:128, :] = meta[None]
    H[:, 128:, :] = x
    return H

def shard_T(H):
    out = []
    for c in range(8):
        b, idx = core_tok_idx(c)
        out.append(np.ascontiguousarray(H[b, idx, :].T))
    return out

def unshard_T(lst, C):
    H = np.zeros((2, 8320, C), lst[0].dtype)
    for c in range(8):
        b, idx = core_tok_idx(c)
        H[b, idx, :] = lst[c].T
    return H

def swap_cols():
    base = np.arange(256).reshape(8, 2, 16)[:, ::-1, :].reshape(256)
    return base

def w_in_ext(w_in_l):
    sw = swap_cols()
    q_sw = w_in_l[:, 1280:1536][:, sw]
    k_sw = w_in_l[:, 1536:1792][:, sw]
    return np.ascontiguousarray(np.concatenate([w_in_l, q_sw, k_sw], axis=1))

def gvec(g):
    return np.ascontiguousarray(g.reshape(-1, 128).T)

def s5_inputs(inp, L, u_b, r):
    gs = [4 * r + g for g in range(4)]
    U = np.stack([u_b[:, 16 * G:16 * G + 16].reshape(1040, 8, 16).transpose(1, 2, 0).reshape(128, 1040) for G in gs])
    def dup(a):
        return np.concatenate([a, a], axis=0)
    lam = np.stack([dup(np.stack([inp['s5_lam_re'][L, G], inp['s5_lam_im'][L, G]], -1)) for G in gs], 1)
    ls = np.tile(inp['s5_log_step'][L, gs][None, :], (128, 1))
    Bm = np.stack([dup(np.stack([inp['s5_b_re'][L, G], inp['s5_b_im'][L, G]], 1)) for G in gs], 1)
    Cm = np.stack([dup(np.stack([inp['s5_c_re'][L, G].T, inp['s5_c_im'][L, G].T], 1)) for G in gs], 1)
    Dm = np.stack([np.tile(inp['s5_d'][L, G], 8) for G in gs], 1)
    f = lambda a: np.ascontiguousarray(a.astype(np.float32))
    return {"U": f(U), "lam": f(lam), "ls": f(ls), "Bm": f(Bm), "Cm": f(Cm), "Dm": f(Dm)}

def s5_unpack(y, out_b, r):
    for g in range(4):
        G = 4 * r + g
        out_b[:, 16 * G:16 * G + 16] = y[g].reshape(8, 16, 1040).transpose(2, 0, 1).reshape(8320, 16)

def s5_raw_ref(inp, L, u):
    Bn, T, _ = u.shape
    out = np.zeros((Bn, T, 256))
    for G in range(16):
        lam = inp['s5_lam_re'][L, G].astype(np.float64) + 1j * inp['s5_lam_im'][L, G]
        step = np.exp(np.float64(inp['s5_log_step'][L, G]))
        lb = np.exp(lam * step)
        Bc = inp['s5_b_re'][L, G].astype(np.float64) + 1j * inp['s5_b_im'][L, G]
        Cc = inp['s5_c_re'][L, G].astype(np.float64) + 1j * inp['s5_c_im'][L, G]
        Bb = ((lb - 1) / lam)[:, None] * Bc
        d = inp['s5_d'][L, G].astype(np.float64)
        for b in range(Bn):
            ug = u[b, :, 16 * G:16 * G + 16].astype(np.float64)
            bu = ug @ Bb.T
            S = np.zeros(64, complex)
            y = np.zeros((T, 16))
            CH = 64
            pw = lb[None, :] ** np.arange(1, CH + 1)[:, None]
            ipw = lb[None, :] ** (-np.arange(1, CH + 1)[:, None])
            for t0 in range(0, T, CH):
                blk = bu[t0:t0 + CH]
                st = pw * (S[None, :] + np.cumsum(blk * ipw, axis=0))
                y[t0:t0 + CH] = (st @ Cc.T).real
                S = st[-1]
            out[b, :, 16 * G:16 * G + 16] = y + d * ug
    return out


_PROGS = {}


def _prog(key, fn):
    if key not in _PROGS:
        _PROGS[key] = fn()
    return _PROGS[key]


def _run(nc, maps):
    res = run_bass_kernel_spmd(nc, maps, core_ids=list(range(8)))
    return res.results


def kernel(**inp):
    inp = {k: np.asarray(v) for k, v in inp.items()}
    f32 = lambda a: np.ascontiguousarray(a, dtype=np.float32)
    H = build_H0(inp['x'], inp['meta_tokens'])
    sw = swap_cols()
    for L in range(2):
        hs = shard_T(H)
        W = w_in_ext(inp['w_in'][L])
        g = gvec(inp['norm_mix_g'][L])
        rA = _run(_prog("A", build_A), [{"hT": hs[c], "g": g, "w": W} for c in range(8)])
        proj = unshard_T([r["projT"] for r in rA], NCOL_A)
        rq = proj[:, :, 1280:1536]; rk = proj[:, :, 1536:1792]; rv = proj[:, :, 1792:2304]; rg = proj[:, :, 2304:2816]
        rqs = proj[:, :, 2816:3072]; rks = proj[:, :, 3072:3328]
        maps = []
        for c in range(8):
            b, r = divmod(c, 4)
            hds = [2 * r, 2 * r + 1]
            maps.append({"q": f32(rq[b, :, 64 * r:64 * r + 64].T), "qsw": f32(rqs[b, :, 64 * r:64 * r + 64].T),
                         "k": f32(rk[b, :, 64 * r:64 * r + 64].T), "ksw": f32(rks[b, :, 64 * r:64 * r + 64].T),
                         "v": f32(rv[b, :, 128 * r:128 * r + 128]), "gate": f32(rg[b, :, 128 * r:128 * r + 128]),
                         "outg": f32(np.tile(inp['ret_out_g'][L][128 * r:128 * r + 128][None, :], (128, 1))),
                         "hpart": f32(np.repeat(np.array(hds, np.float32), 32)[:, None]),
                         "hsel": f32(np.tile(np.array(hds, np.float32)[None, :], (128, 1)))})
        rR = _run(_prog("ret", build_ret), maps)
        yc = np.zeros((2, 8320, 512), np.float32)
        for c in range(8):
            b, r = divmod(c, 4)
            yc[b, :, 128 * r:128 * r + 128] = rR[c]["y"]
        cwL = inp['hg_conv_w'][L]
        maps = []
        for c in range(8):
            b, r = divmod(c, 4)
            x3 = np.stack([proj[b, :, 256 + 256 * s + 64 * r: 256 + 256 * s + 64 * r + 64].T for s in range(3)])
            convw = np.stack([cwL[:, 256 * s + 64 * r: 256 * s + 64 * r + 64].T for s in range(3)])
            maps.append({"x3": f32(x3), "convw": f32(convw), "gate": f32(proj[b, :, 1024 + 64 * r:1024 + 64 * r + 64]),
                         "outg": f32(np.tile(inp['hg_out_g'][L][64 * r:64 * r + 64][None, :], (128, 1))),
                         "lbp": f32(inp['hg_lb_param'][:, 64 * r:64 * r + 64].T)})
        rHg = _run(_prog(("hg", L), lambda: build_hg(L)), maps)
        yb = np.zeros((2, 8320, 256), np.float32)
        for c in range(8):
            b, r = divmod(c, 4)
            yb[b, :, 64 * r:64 * r + 64] = rHg[c]["y"]
        maps = []
        for c in range(8):
            b, r = divmod(c, 4)
            maps.append(s5_inputs(inp, L, proj[b, :, 0:256], r))
        rS = _run(_prog("s5", build_s5), maps)
        ya = np.zeros((2, 8320, 256), np.float32)
        for c in range(8):
            b, r = divmod(c, 4)
            s5_unpack(rS[c]["y"], ya[b], r)
        yas = shard_T(ya); ybcs = shard_T(np.concatenate([yb, yc], -1))
        moe = (L % 2 == 1)
        final = (L == 1)
        if not moe:
            w1 = f32(inp['ffn_w1'][L // 2][None]); w3 = f32(inp['ffn_w3'][L // 2][None]); w2 = f32(inp['ffn_w2'][L // 2][None])
            nc = _prog(("C", 1, final), lambda: build_C(1, 2816, final))
        else:
            w1 = f32(inp['moe_w1'][L // 2]); w3 = f32(inp['moe_w3'][L // 2]); w2 = f32(inp['moe_w2'][L // 2])
            nc = _prog(("C", 8, final), lambda: build_C(8, 3584, final))
        maps = []
        for c in range(8):
            m = {"hT": hs[c], "yaT": yas[c], "ybcT": ybcs[c], "wglu": f32(inp['s5_w_glu'][L]), "s5g": gvec(inp['s5_out_g'][L]),
                 "wout": f32(inp['w_out'][L]), "gffn": gvec(inp['norm_ffn_g'][L]), "w1": w1, "w3": w3, "w2": w2}
            if moe:
                m["router"] = f32(inp['moe_router'][L // 2])
            if final:
                m["gfin"] = gvec(inp['final_norm_g'])
            maps.append(m)
        rC = _run(nc, maps)
        H = unshard_T([r["hT_out"] for r in rC], 1024)
    return np.ascontiguousarray(H[:, 128:, :], dtype=np.float32)
```

```python
import math
import contextlib
import numpy as np
import concourse.bass as bass
import concourse.mybir as mybir
from concourse.bass_utils import run_bass_kernel_spmd


F32 = mybir.dt.float32
BF16 = mybir.dt.bfloat16
I32 = mybir.dt.int32
AF = mybir.ActivationFunctionType
ALU = mybir.AluOpType
AX = mybir.AxisListType


class T:
    def __init__(self, ctx, name, handle, space):
        self.ctx = ctx
        self.name = name
        self.h = handle
        self.space = space
        self.st = {}
        self.dq = None

    def __getitem__(self, idx):
        return self.h[idx]

    def state(self, key):
        s = self.st.get(key)
        if s is None:
            s = {"w": None, "r": {}}
            self.st[key] = s
        return s


class Q:
    def __init__(self, ctx, name, step):
        self.ctx = ctx
        self.name = name
        self.sem = ctx.root.enter_context(ctx.nc.semaphore(name))
        self.count = 0
        self.step = step


class Ctx:
    def __init__(self, nc):
        self.nc = nc
        self.stack = contextlib.ExitStack()
        self.root = self.stack
        self.engs = {}
        for name, eng in (("pe", nc.tensor), ("dve", nc.vector), ("act", nc.scalar),
                          ("pool", nc.gpsimd), ("sp", nc.sync)):
            q = Q(self, "q_" + name, 1)
            self.engs[name] = (eng, q)
        self.seen = {name: {} for name in self.engs}
        self.dmaq = {}
        self.n_inst = 0
        self.uid = 0
        self.pe_self_sync = False

    def sb(self, name, shape, dtype):
        self.uid += 1
        h = self.stack.enter_context(self.nc.sbuf_tensor(f"{name}_{self.uid}", list(shape), dtype))
        return T(self, name, h, "sb")

    def ps(self, name, shape, dtype=F32):
        self.uid += 1
        h = self.stack.enter_context(self.nc.psum_tensor(f"{name}_{self.uid}", list(shape), dtype))
        return T(self, name, h, "ps")

    def dram(self, name, shape, dtype, kind):
        h = self.nc.dram_tensor(name, list(shape), dtype, kind=kind)
        return T(self, name, h, "dram")

    def dma_q(self, name):
        q = self.dmaq.get(name)
        if q is None:
            q = Q(self, "dq_" + name, 16)
            self.dmaq[name] = q
        return q

    def _need(self, engname, q, value):
        if q is None:
            return
        if engname == "pe" and q is self.engs["pe"][1] and not self.pe_self_sync:
            return
        seen = self.seen[engname]
        if q.step == 16:
            value = q.count
        if seen.get(q.name, 0) >= value:
            return
        eng = self.engs[engname][0]
        eng.wait_ge(q.sem, value)
        seen[q.name] = value

    def _deps(self, engname, reads, writes):
        for (t, key) in reads:
            s = t.state(key)
            if s["w"] is not None:
                self._need(engname, *s["w"])
        for (t, key) in writes:
            s = t.state(key)
            if s["w"] is not None:
                self._need(engname, *s["w"])
            for q, v in s["r"].values():
                self._need(engname, q, v)

    def _mark(self, q, value, reads, writes):
        for (t, key) in reads:
            s = t.state(key)
            s["r"][q.name] = (q, value)
        for (t, key) in writes:
            s = t.state(key)
            s["w"] = (q, value)
            s["r"] = {}

    @staticmethod
    def _norm(lst):
        out = []
        for x in lst:
            if isinstance(x, tuple):
                out.append(x)
            else:
                out.append((x, None))
        return out

    def op(self, engname, fn, reads=(), writes=()):
        reads = self._norm(reads)
        writes = self._norm(writes)
        eng, q = self.engs[engname]
        self._deps(engname, reads, writes)
        ins = fn(eng)
        q.count += 1
        ins.then_inc(q.sem, 1)
        self._mark(q, q.count, reads, writes)
        self.n_inst += 1
        return ins

    def dma(self, engname, out, in_, reads=(), writes=(), qt=None, **kw):
        reads = self._norm(reads)
        writes = self._norm(writes)
        eng, _ = self.engs[engname]
        self._deps(engname, reads, writes)
        if qt is None:
            cands = [t for (t, k) in writes if t.space == "sb"] + [t for (t, k) in reads if t.space == "sb"]
            qt = cands[0]
        if qt.dq is None:
            self.uid += 1
            qt.dq = Q(self, f"dq_{qt.name}_{self.uid}", 16)
            self.dmaq[qt.dq.name] = qt.dq
        dq = qt.dq
        ins = eng.dma_start(out=out, in_=in_, **kw)
        dq.count += 16
        ins.then_inc(dq.sem, 16)
        self._mark(dq, dq.count, reads, writes)
        self.n_inst += 1
        return ins

    def barrier(self):
        for name, (eng, q0) in self.engs.items():
            for dq in self.dmaq.values():
                if dq.count:
                    self._need(name, dq, dq.count)
            for other, (e2, q2) in self.engs.items():
                if other != name and q2.count:
                    self._need(name, q2, q2.count)

    @contextlib.contextmanager
    def scope(self):
        old = self.stack
        self.stack = contextlib.ExitStack()
        try:
            yield
        finally:
            self.barrier()
            self.stack.close()
            self.stack = old

    def finish(self, engname="sp"):
        eng = self.engs[engname][0]
        for dq in self.dmaq.values():
            if dq.count:
                eng.wait_ge(dq.sem, dq.count)
        for name, (e, q) in self.engs.items():
            if q.count and name != engname:
                eng.wait_ge(q.sem, q.count)

    def close(self):
        self.stack.close()


NT = 2080
D = 1024
KC = 8
EPS = 1e-6
NCOL_A = 3328


def tblocks(nt=NT, bs=512):
    out = []
    t = 0
    while t < nt:
        n = min(bs, nt - t)
        out.append((t, n))
        t += n
    return out


class Rot:
    def __init__(self, tiles):
        self.tiles = tiles
        self.i = 0

    def next(self):
        t = self.tiles[self.i % len(self.tiles)]
        self.i += 1
        return t


def emit_consts(c):
    k = {}
    k["ones_bf"] = c.sb("ones_bf", [128, 128], BF16)
    c.op("pool", lambda e: e.memset(k["ones_bf"][:], 1.0), writes=[k["ones_bf"]])
    return k


def emit_rmsnorm(c, k, hT, g_sb, hnT, sq_rot, rs_rot, ps_rot, d_chunks=KC, dim=D, src_key=True):
    for bi, (t0, n) in enumerate(tblocks()):
        sq = sq_rot.next()
        c.op("act", lambda e: e.activation(out=sq[:, :d_chunks, :n], in_=hT[:, :, t0:t0 + n], func=AF.Square),
             reads=[(hT, bi)], writes=[sq])
        ps = ps_rot.next()
        for kc in range(d_chunks):
            c.op("pe", lambda e: e.matmul(ps[:, :n], lhsT=k["ones_bf"][:], rhs=sq[:, kc, :n],
                                          start=(kc == 0), stop=(kc == d_chunks - 1)),
                 reads=[sq, k["ones_bf"]], writes=[ps])
        rs = rs_rot.next()
        c.op("act", lambda e: e.activation(out=rs[:, :n], in_=ps[:, :n], func=AF.Sqrt, scale=1.0 / dim, bias=k["eps"][:, 0:1]),
             reads=[ps, k["eps"]], writes=[rs])
        c.op("dve", lambda e: e.reciprocal(rs[:, :n], rs[:, :n]), reads=[rs], writes=[rs])
        for kc in range(d_chunks):
            c.op("dve", lambda e: e.scalar_tensor_tensor(out=hnT[:, kc, t0:t0 + n], in0=hT[:, kc, t0:t0 + n],
                                                         scalar=g_sb[:, kc:kc + 1], in1=rs[:, :n],
                                                         op0=ALU.mult, op1=ALU.mult),
                 reads=[(hT, bi), rs, g_sb], writes=[(hnT, bi)])


def build_A():
    nc = bass.Bass("TRN2", target_bir_lowering=False)
    c = Ctx(nc)
    hT_d = c.dram("hT", [D, NT], F32, "ExternalInput")
    g_d = c.dram("g", [128, KC], F32, "ExternalInput")
    w_d = c.dram("w", [D, NCOL_A], F32, "ExternalInput")
    out_d = c.dram("projT", [NCOL_A, NT], F32, "ExternalOutput")

    k = emit_consts(c)
    k["eps"] = c.sb("eps", [128, 1], F32)
    c.op("pool", lambda e: e.memset(k["eps"][:], EPS), writes=[k["eps"]])
    hT = c.sb("hT", [128, KC, NT], F32)
    hnT = c.sb("hnT", [128, KC, NT], BF16)
    g_sb = c.sb("g", [128, KC], F32)
    c.dma("sp", g_sb[:], g_d[:], writes=[g_sb])
    hT_v = hT_d.h.ap().rearrange("(kc kp) t -> kp kc t", kp=128)
    for bi, (t0, n) in enumerate(tblocks()):
        c.dma("sp", hT[:, :, t0:t0 + n], hT_v[:, :, t0:t0 + n], writes=[(hT, bi)])
    sq_rot = Rot([c.sb(f"sq{i}", [128, KC, 512], BF16) for i in range(2)])
    rs_rot = Rot([c.sb(f"rs{i}", [128, 512], F32) for i in range(2)])
    ps_rot = Rot([c.ps(f"ps{i}", [128, 512], F32) for i in range(6)])
    emit_rmsnorm(c, k, hT, g_sb, hnT, sq_rot, rs_rot, ps_rot)

    w_rot = Rot([c.sb(f"w{i}", [128, KC, 512], BF16) for i in range(2)])
    w_st = c.sb("w_st", [128, KC, 512], F32)
    ost_rot = Rot([c.sb(f"ost{i}", [128, NT], F32) for i in range(3)])
    w_v = w_d.h.ap().rearrange("(kc kp) n -> kp kc n", kp=128)
    ev = 0
    for c0 in range(0, NCOL_A, 512):
        ncol = min(512, NCOL_A - c0)
        w_sb = w_rot.next()
        c.dma("sp", w_st[:, :, :ncol], w_v[:, :, c0:c0 + ncol], writes=[w_st])
        c.op("pool", lambda e: e.tensor_copy(w_sb[:, :, :ncol], w_st[:, :, :ncol]), reads=[w_st], writes=[w_sb])
        for cc in range(ncol // 128):
            ost = ost_rot.next()
            for bi, (t0, n) in enumerate(tblocks()):
                ps = ps_rot.next()
                for kc in range(KC):
                    c.op("pe", lambda e: e.matmul(ps[:, :n], lhsT=w_sb[:, kc, cc * 128:(cc + 1) * 128],
                                                  rhs=hnT[:, kc, t0:t0 + n], start=(kc == 0), stop=(kc == KC - 1)),
                         reads=[w_sb, (hnT, bi)], writes=[ps])
                if ev % 2 == 0:
                    c.op("act", lambda e: e.copy(out=ost[:, t0:t0 + n], in_=ps[:, :n]), reads=[ps], writes=[ost])
                else:
                    c.op("dve", lambda e: e.tensor_copy(ost[:, t0:t0 + n], ps[:, :n]), reads=[ps], writes=[ost])
                ev += 1
            r0 = c0 + cc * 128
            c.dma("sp", out_d[r0:r0 + 128, :], ost[:], reads=[ost])
    c.finish()
    c.close()
    print("phase A instructions:", c.n_inst)
    return nc


GF = 2
GH = 2


def emit_rmsnorm2(c, k, src, g_sb, dst_fn, sq_rot, rs_rot, ps_rot, d_chunks, dim, src_keyed=True):
    for bi, (t0, n) in enumerate(tblocks()):
        sk = (src, bi) if src_keyed else src
        sq = sq_rot.next()
        c.op("act", lambda e: e.activation(out=sq[:, :d_chunks, :n], in_=src[:, :, t0:t0 + n], func=AF.Square),
             reads=[sk], writes=[sq])
        ps = ps_rot.next()
        for kc in range(d_chunks):
            c.op("pe", lambda e: e.matmul(ps[:, :n], lhsT=k["ones_bf"][:], rhs=sq[:, kc, :n],
                                          start=(kc == 0), stop=(kc == d_chunks - 1)),
                 reads=[sq, k["ones_bf"]], writes=[ps])
        rs = rs_rot.next()
        c.op("act", lambda e: e.activation(out=rs[:, :n], in_=ps[:, :n], func=AF.Sqrt, scale=1.0 / dim, bias=k["eps"][:, 0:1]),
             reads=[ps, k["eps"]], writes=[rs])
        c.op("dve", lambda e: e.reciprocal(rs[:, :n], rs[:, :n]), reads=[rs], writes=[rs])
        for kc in range(d_chunks):
            ap, wk = dst_fn(bi, t0, n, kc)
            c.op("dve", lambda e: e.scalar_tensor_tensor(out=ap, in0=src[:, kc, t0:t0 + n],
                                                         scalar=g_sb[:, kc:kc + 1], in1=rs[:, :n],
                                                         op0=ALU.mult, op1=ALU.mult),
                 reads=[sk, rs, g_sb], writes=[wk])


def build_C(n_exp, F, final):
    moe = n_exp > 1
    nc = bass.Bass("TRN2", target_bir_lowering=False)
    c = Ctx(nc)
    hT_d = c.dram("hT", [D, NT], F32, "ExternalInput")
    ya_d = c.dram("yaT", [256, NT], F32, "ExternalInput")
    ybc_d = c.dram("ybcT", [768, NT], F32, "ExternalInput")
    wglu_d = c.dram("wglu", [256, 256], F32, "ExternalInput")
    s5g_d = c.dram("s5g", [128, 2], F32, "ExternalInput")
    wout_d = c.dram("wout", [D, D], F32, "ExternalInput")
    gffn_d = c.dram("gffn", [128, KC], F32, "ExternalInput")
    w1_d = c.dram("w1", [n_exp, D, F], F32, "ExternalInput")
    w3_d = c.dram("w3", [n_exp, D, F], F32, "ExternalInput")
    w2_d = c.dram("w2", [n_exp, F, D], F32, "ExternalInput")
    if moe:
        rt_d = c.dram("router", [D, 8], F32, "ExternalInput")
    if final:
        gfin_d = c.dram("gfin", [128, KC], F32, "ExternalInput")
    out_d = c.dram("hT_out", [D, NT], F32, "ExternalOutput")

    k = emit_consts(c)
    k["eps"] = c.sb("eps", [128, 1], F32)
    c.op("pool", lambda e: e.memset(k["eps"][:], EPS), writes=[k["eps"]])
    TB = tblocks()

    hT = c.sb("hT", [128, KC, NT], F32)
    hT_v = hT_d.h.ap().rearrange("(kc kp) t -> kp kc t", kp=128)
    for bi, (t0, n) in enumerate(TB):
        c.dma("sp", hT[:, :, t0:t0 + n], hT_v[:, :, t0:t0 + n], writes=[(hT, bi)], qt=hT)
    gffn = c.sb("gffn", [128, KC], F32)
    c.dma("sp", gffn[:], gffn_d[:], writes=[gffn])
    s5g = c.sb("s5g", [128, 2], F32)
    c.dma("sp", s5g[:], s5g_d[:], writes=[s5g])
    if final:
        gfin = c.sb("gfin", [128, KC], F32)
        c.dma("sp", gfin[:], gfin_d[:], writes=[gfin])

    ps_all = [c.ps(f"ps{i}", [128, 512], F32) for i in range(8)]
    ps_rot = Rot(ps_all[:7])

    with c.scope():
        sq_rot = Rot([c.sb(f"sq{i}", [128, KC, 512], BF16) for i in range(2)])
        rs_rot = Rot([c.sb(f"rs{i}", [128, 512], F32) for i in range(2)])
        yaT = c.sb("yaT", [128, 2, NT], F32)
        c.dma("sp", yaT[:], ya_d.h.ap().rearrange("(kc kp) t -> kp kc t", kp=128), writes=[yaT])
        mixedT = c.sb("mixedT", [128, KC, NT], BF16)
        ybc_v = ybc_d.h.ap().rearrange("(kc kp) t -> kp kc t", kp=128)
        for bi, (t0, n) in enumerate(TB):
            c.dma("pool", mixedT[:, 2:8, t0:t0 + n], ybc_v[:, :, t0:t0 + n], writes=[(mixedT, ("bc", bi))], qt=mixedT)
        wglu = c.sb("wglu", [128, 2, 256], BF16)
        c.dma("pool", wglu[:], wglu_d.h.ap().rearrange("(kc kp) n -> kp kc n", kp=128), writes=[wglu])
        wout = c.sb("wout", [128, KC, D], BF16)
        c.dma("pool", wout[:], wout_d.h.ap().rearrange("(kc kp) n -> kp kc n", kp=128), writes=[wout])

        t1_rot = Rot([c.sb(f"t1_{i}", [128, 2, 512], F32) for i in range(1)])
        t2_rot = Rot([c.sb(f"t2_{i}", [128, 2, 512], F32) for i in range(1)])
        ygf_rot = Rot([c.sb(f"ygf{i}", [128, 2, 512], F32) for i in range(1)])
        ygb_rot = Rot([c.sb(f"ygb{i}", [128, 2, 512], BF16) for i in range(2)])
        yaf_rot = Rot([c.sb(f"yaf{i}", [128, 2, 512], F32) for i in range(1)])
        sg_rot = Rot([c.sb(f"sg{i}", [128, 512], F32) for i in range(2)])
        for bi, (t0, n) in enumerate(TB):
            x = yaT[:, :, t0:t0 + n]
            t1 = t1_rot.next(); t2 = t2_rot.next(); ygf = ygf_rot.next(); ygb = ygb_rot.next(); yaf = yaf_rot.next()
            c.op("act", lambda e: e.activation(out=t1[:, :, :n], in_=x, func=AF.Square), reads=[yaT], writes=[t1])
            c.op("dve", lambda e: e.tensor_scalar(t1[:, :, :n], t1[:, :, :n], 0.044715, 1.0, op0=ALU.mult, op1=ALU.add),
                 reads=[t1], writes=[t1])
            c.op("dve", lambda e: e.tensor_tensor(out=t1[:, :, :n], in0=t1[:, :, :n], in1=x, op=ALU.mult),
                 reads=[t1, yaT], writes=[t1])
            c.op("act", lambda e: e.activation(out=t2[:, :, :n], in_=t1[:, :, :n], func=AF.Sigmoid, scale=1.5957691216057308),
                 reads=[t1], writes=[t2])
            c.op("dve", lambda e: e.tensor_tensor(out=ygf[:, :, :n], in0=t2[:, :, :n], in1=x, op=ALU.mult),
                 reads=[t2, yaT], writes=[ygf])
            c.op("act", lambda e: e.copy(out=ygb[:, :, :n], in_=ygf[:, :, :n]), reads=[ygf], writes=[ygb])
            for mo in range(2):
                ps = ps_rot.next()
                for ch in range(2):
                    c.op("pe", lambda e: e.matmul(ps[:, :n], lhsT=wglu[:, ch, mo * 128:(mo + 1) * 128], rhs=ygb[:, ch, :n],
                                                  start=(ch == 0), stop=(ch == 1)), reads=[wglu, ygb], writes=[ps])
                sg = sg_rot.next()
                c.op("act", lambda e: e.activation(out=sg[:, :n], in_=ps[:, :n], func=AF.Sigmoid), reads=[ps], writes=[sg])
                c.op("dve", lambda e: e.tensor_tensor(out=yaf[:, mo, :n], in0=ygf[:, mo, :n], in1=sg[:, :n], op=ALU.mult),
                     reads=[ygf, sg], writes=[yaf])
            sq = sq_rot.next()
            c.op("act", lambda e: e.activation(out=sq[:, :2, :n], in_=yaf[:, :, :n], func=AF.Square), reads=[yaf], writes=[sq])
            ps = ps_rot.next()
            for ch in range(2):
                c.op("pe", lambda e: e.matmul(ps[:, :n], lhsT=k["ones_bf"][:], rhs=sq[:, ch, :n], start=(ch == 0), stop=(ch == 1)),
                     reads=[sq, k["ones_bf"]], writes=[ps])
            rs = rs_rot.next()
            c.op("act", lambda e: e.activation(out=rs[:, :n], in_=ps[:, :n], func=AF.Sqrt, scale=1.0 / 256, bias=k["eps"][:, 0:1]),
                 reads=[ps, k["eps"]], writes=[rs])
            c.op("dve", lambda e: e.reciprocal(rs[:, :n], rs[:, :n]), reads=[rs], writes=[rs])
            for ch in range(2):
                c.op("dve", lambda e: e.scalar_tensor_tensor(out=mixedT[:, ch, t0:t0 + n], in0=yaf[:, ch, :n],
                                                             scalar=s5g[:, ch:ch + 1], in1=rs[:, :n], op0=ALU.mult, op1=ALU.mult),
                     reads=[yaf, rs, s5g], writes=[(mixedT, ("a", bi))])
            for dch in range(KC):
                ps = ps_rot.next()
                for cc in range(KC):
                    c.op("pe", lambda e: e.matmul(ps[:, :n], lhsT=wout[:, cc, dch * 128:(dch + 1) * 128], rhs=mixedT[:, cc, t0:t0 + n],
                                                  start=(cc == 0), stop=(cc == KC - 1)),
                         reads=[wout, (mixedT, ("a", bi)), (mixedT, ("bc", bi))], writes=[ps])
                c.op("dve", lambda e: e.tensor_tensor(out=hT[:, dch, t0:t0 + n], in0=hT[:, dch, t0:t0 + n], in1=ps[:, :n], op=ALU.add),
                     reads=[(hT, bi), ps], writes=[(hT, bi)])

    hnT = c.sb("hnT", [128, KC, NT], BF16)
    with c.scope():
        sq_rot = Rot([c.sb(f"sq{i}", [128, KC, 512], BF16) for i in range(2)])
        rs_rot = Rot([c.sb(f"rs{i}", [128, 512], F32) for i in range(2)])
        emit_rmsnorm2(c, k, hT, gffn, lambda bi, t0, n, kc: (hnT[:, kc, t0:t0 + n], (hnT, bi)), sq_rot, rs_rot, ps_rot, KC, D)

    if moe:
        gatesT = c.sb("gatesT", [8, NT], BF16)
        with c.scope():
            ident = c.sb("identf", [128, 128], F32)
            iof = c.sb("iof", [128, 128], F32)
            c.op("pool", lambda e: e.iota(iof[:], [[1, 128]], base=0, channel_multiplier=-1, allow_small_or_imprecise_dtypes=True), writes=[iof])
            c.op("dve", lambda e: e.tensor_single_scalar(ident[:], iof[:], 0.0, op=ALU.is_equal), reads=[iof], writes=[ident])
            rt = c.sb("rt", [128, KC, 8], F32)
            c.dma("sp", rt[:], rt_d.h.ap().rearrange("(kc kp) e -> kp kc e", kp=128), writes=[rt])
            gr = c.sb("gr", [128, KC, 16], F32)
            c.op("dve", lambda e: e.memset(gr[:], 0.0), writes=[gr])
            for kc in range(KC):
                c.op("dve", lambda e: e.tensor_scalar(gr[:, kc, 0:8], rt[:, kc, :], gffn[:, kc:kc + 1], None, op0=ALU.mult),
                     reads=[rt, gffn], writes=[gr])
            onesf = c.sb("onesf", [128, 1], F32)
            c.op("dve", lambda e: e.memset(onesf[:], 1.0), writes=[onesf])
            k["gatesT"] = gatesT
            sqf_rot = Rot([c.sb(f"sqf{i}", [128, KC, 128], F32) for i in range(2)])
            sm_rot = Rot([c.sb(f"sm{i}", [128, 64], F32) for i in range(3)])
            for ti, (t0, n) in enumerate(tblocks(NT, 128)):
                bi = t0 // 512
                sqf = sqf_rot.next()
                c.op("act", lambda e: e.activation(out=sqf[:, :, :n], in_=hT[:, :, t0:t0 + n], func=AF.Square), reads=[(hT, bi)], writes=[sqf])
                ps = ps_all[7]
                for kc in range(KC):
                    c.op("pe", lambda e: e.matmul(ps[:n, 0:8], lhsT=hT[:, kc, t0:t0 + n], rhs=gr[:, kc, 0:8], start=(kc == 0), stop=(kc == KC - 1)),
                         reads=[(hT, bi), gr], writes=[ps])
                for kc in range(KC):
                    c.op("pe", lambda e: e.matmul(ps[:n, 8:9], lhsT=sqf[:, kc, :n], rhs=onesf[:, 0:1], start=(kc == 0), stop=(kc == KC - 1)),
                         reads=[sqf, onesf], writes=[ps])
                sm = sm_rot.next()
                c.op("act", lambda e: e.activation(out=sm[:n, 0:1], in_=ps[:n, 8:9], func=AF.Sqrt, scale=1.0 / D, bias=k["eps"][:n, 0:1]),
                     reads=[ps, k["eps"]], writes=[sm])
                c.op("dve", lambda e: e.reciprocal(sm[:n, 0:1], sm[:n, 0:1]), reads=[sm], writes=[sm])
                c.op("dve", lambda e: e.tensor_scalar(sm[:n, 8:16], ps[:n, 0:8], sm[:n, 0:1], None, op0=ALU.mult), reads=[ps, sm], writes=[sm])
                c.op("dve", lambda e: e.max(out=sm[:n, 16:24], in_=sm[:n, 8:16]), reads=[sm], writes=[sm])
                c.op("dve", lambda e: e.tensor_tensor(out=sm[:n, 24:25], in0=sm[:n, 17:18], in1=sm[:n, 16:17], op=ALU.subtract), reads=[sm], writes=[sm])
                c.op("act", lambda e: e.activation(out=sm[:n, 24:25], in_=sm[:n, 24:25], func=AF.Exp), reads=[sm], writes=[sm])
                c.op("dve", lambda e: e.tensor_scalar(sm[:n, 25:26], sm[:n, 24:25], 1.0, None, op0=ALU.add), reads=[sm], writes=[sm])
                c.op("dve", lambda e: e.reciprocal(sm[:n, 25:26], sm[:n, 25:26]), reads=[sm], writes=[sm])
                c.op("dve", lambda e: e.tensor_tensor(out=sm[:n, 26:27], in0=sm[:n, 24:25], in1=sm[:n, 25:26], op=ALU.mult), reads=[sm], writes=[sm])
                c.op("dve", lambda e: e.tensor_scalar(sm[:n, 32:40], sm[:n, 8:16], sm[:n, 16:17], sm[:n, 25:26], op0=ALU.is_equal, op1=ALU.mult), reads=[sm], writes=[sm])
                c.op("dve", lambda e: e.tensor_scalar(sm[:n, 40:48], sm[:n, 8:16], sm[:n, 17:18], sm[:n, 26:27], op0=ALU.is_equal, op1=ALU.mult), reads=[sm], writes=[sm])
                c.op("dve", lambda e: e.tensor_tensor(out=sm[:n, 48:56], in0=sm[:n, 32:40], in1=sm[:n, 40:48], op=ALU.add), reads=[sm], writes=[sm])
                c.op("pe", lambda e: e.transpose(ps[0:8, 16:16 + n], sm[:n, 48:56], ident[:n, :n]), reads=[sm, ident], writes=[ps])
                c.op("act", lambda e: e.copy(out=gatesT[:, t0:t0 + n], in_=ps[0:8, 16:16 + n]), reads=[ps], writes=[gatesT])
        sel = c.sb("sel", [8, 8, 128], BF16)
        self_f = c.sb("sel_f", [8, 8, 128], F32)
        c.op("pool", lambda e: e.iota(self_f[:], [[-1, 8], [0, 128]], base=0, channel_multiplier=1, allow_small_or_imprecise_dtypes=True), writes=[self_f])
        c.op("dve", lambda e: e.tensor_single_scalar(sel[:], self_f[:], 0.0, op=ALU.is_equal), reads=[self_f], writes=[sel])

    with c.scope():
        ps_a = Rot(ps_all[0:2]); ps_b = Rot(ps_all[2:4]); ps_o = Rot(ps_all[4:7]); ps_g = Rot(ps_all[7:8])
        NWB = 2
        w1_rot = Rot([c.sb(f"w1g{i}", [128, KC, GF * 128], BF16) for i in range(NWB)])
        w3_rot = Rot([c.sb(f"w3g{i}", [128, KC, GF * 128], BF16) for i in range(NWB)])
        w2_rot = Rot([c.sb(f"w2g{i}", [128, GF, D], BF16) for i in range(NWB)])
        w1s = c.sb("w1s", [128, KC, GH * 128], F32)
        w3s = c.sb("w3s", [128, KC, GH * 128], F32)
        sa_rot = Rot([c.sb(f"sa{i}", [128, 512], F32) for i in range(2)])
        gT_rot = Rot([c.sb(f"gT{i}", [128, GF, 512], BF16) for i in range(3)])
        ngrp = F // (GF * 128)
        groups = [(ex, gi) for ex in range(n_exp) for gi in range(ngrp)]
        wbuf = {}

        def load_group(gidx):
            ex, gi = groups[gidx]
            w1_v = w1_d.h.ap()[ex].rearrange("(kc kp) f -> kp kc f", kp=128)
            w3_v = w3_d.h.ap()[ex].rearrange("(kc kp) f -> kp kc f", kp=128)
            w2_v = w2_d.h.ap()[ex].rearrange("(fc fp) d -> fp fc d", fp=128)
            f0 = gi * GF * 128
            w1g = w1_rot.next(); w3g = w3_rot.next(); w2g = w2_rot.next()
            for hh in range(GF // GH):
                fa = f0 + hh * GH * 128
                c.dma("sp", w1s[:], w1_v[:, :, fa:fa + GH * 128], writes=[w1s])
                c.op("pool", lambda e: e.tensor_copy(w1g[:, :, hh * GH * 128:(hh + 1) * GH * 128], w1s[:]), reads=[w1s], writes=[w1g])
                c.dma("sp", w3s[:], w3_v[:, :, fa:fa + GH * 128], writes=[w3s])
                c.op("pool", lambda e: e.tensor_copy(w3g[:, :, hh * GH * 128:(hh + 1) * GH * 128], w3s[:]), reads=[w3s], writes=[w3g])
            c.dma("pool", w2g[:], w2_v[:, gi * GF:(gi + 1) * GF, :], writes=[w2g])
            wbuf[gidx] = (w1g, w3g, w2g)

        gsb_rot = Rot([c.sb(f"gsb{i}", [128, 512], BF16) for i in range(2)]) if moe else None
        sa2_rot = Rot([c.sb(f"sa2_{i}", [128, 512], F32) for i in range(2)]) if moe else None

        def stage1_fc(gidx, bi, t0, n, fc, gT, gsb):
            ex, gi = groups[gidx]
            w1g, w3g, w2g = wbuf[gidx]
            pa = ps_a.next(); pb = ps_b.next()
            for kc in range(KC):
                c.op("pe", lambda e: e.matmul(pa[:, :n], lhsT=w1g[:, kc, fc * 128:(fc + 1) * 128], rhs=hnT[:, kc, t0:t0 + n],
                                              start=(kc == 0), stop=(kc == KC - 1)), reads=[w1g, (hnT, bi)], writes=[pa])
            for kc in range(KC):
                c.op("pe", lambda e: e.matmul(pb[:, :n], lhsT=w3g[:, kc, fc * 128:(fc + 1) * 128], rhs=hnT[:, kc, t0:t0 + n],
                                              start=(kc == 0), stop=(kc == KC - 1)), reads=[w3g, (hnT, bi)], writes=[pb])
            sa = sa_rot.next()
            c.op("act", lambda e: e.activation(out=sa[:, :n], in_=pa[:, :n], func=AF.Silu), reads=[pa], writes=[sa])
            if moe:
                sa2 = sa2_rot.next()
                c.op("pool", lambda e: e.tensor_tensor(out=sa2[:, :n], in0=sa[:, :n], in1=gsb[:, :n], op=ALU.mult), reads=[sa, gsb], writes=[sa2])
                c.op("dve", lambda e: e.tensor_tensor(out=gT[:, fc, :n], in0=sa2[:, :n], in1=pb[:, :n], op=ALU.mult), reads=[sa2, pb], writes=[gT])
            else:
                c.op("dve", lambda e: e.tensor_tensor(out=gT[:, fc, :n], in0=sa[:, :n], in1=pb[:, :n], op=ALU.mult), reads=[sa, pb], writes=[gT])

        def stage1_gate(gidx, bi, t0, n):
            ex, gi = groups[gidx]
            pg = ps_g.next()
            c.op("pe", lambda e: e.matmul(pg[:, :n], lhsT=sel[:, ex, :], rhs=k["gatesT"][:, t0:t0 + n], start=True, stop=True),
                 reads=[sel, k["gatesT"]], writes=[pg])
            gsb = gsb_rot.next()
            c.op("act", lambda e: e.copy(out=gsb[:, :n], in_=pg[:, :n]), reads=[pg], writes=[gsb])
            return gsb

        def stage2_part(gidx, bi, t0, n, gT, d0, d1):
            w1g, w3g, w2g = wbuf[gidx]
            for dch in range(d0, d1):
                po = ps_o.next()
                for fc in range(GF):
                    c.op("pe", lambda e: e.matmul(po[:, :n], lhsT=w2g[:, fc, dch * 128:(dch + 1) * 128], rhs=gT[:, fc, :n],
                                                  start=(fc == 0), stop=(fc == GF - 1)), reads=[w2g, gT], writes=[po])
                c.op("dve", lambda e: e.tensor_tensor(out=hT[:, dch, t0:t0 + n], in0=hT[:, dch, t0:t0 + n], in1=po[:, :n], op=ALU.add),
                     reads=[(hT, bi), po], writes=[(hT, bi)])

        load_group(0)
        pending = None
        DS = KC // GF
        for gidx in range(len(groups)):
            for bi, (t0, n) in enumerate(TB):
                gsb = stage1_gate(gidx, bi, t0, n) if moe else None
                gT = gT_rot.next()
                for fc in range(GF):
                    stage1_fc(gidx, bi, t0, n, fc, gT, gsb)
                    if pending is not None:
                        stage2_part(*pending, fc * DS, (fc + 1) * DS)
                pending = (gidx, bi, t0, n, gT)
                if bi == 0 and gidx + 1 < len(groups):
                    load_group(gidx + 1)
        stage2_part(*pending, 0, KC)

    out_v = out_d.h.ap().rearrange("(kc kp) t -> kp kc t", kp=128)
    if final:
        sq_rot = Rot([c.sb(f"sq{i}", [128, KC, 512], BF16) for i in range(2)])
        rs_rot = Rot([c.sb(f"rs{i}", [128, 512], F32) for i in range(2)])
        fo_rot = Rot([c.sb(f"fo{i}", [128, KC, 512], F32) for i in range(2)])
        cur = {}

        def dst(bi, t0, n, kc):
            if kc == 0:
                cur["t"] = fo_rot.next()
            return cur["t"][:, kc, :n], cur["t"]
        for bi, (t0, n) in enumerate(TB):
            pass
        emit_final(c, k, hT, gfin, fo_rot, out_v, sq_rot, rs_rot, Rot(ps_all[0:6]))
    else:
        for bi, (t0, n) in enumerate(TB):
            c.dma("sp", out_v[:, :, t0:t0 + n], hT[:, :, t0:t0 + n], reads=[(hT, bi)], qt=hT)
    c.finish()
    c.close()
    print("phase C instructions:", c.n_inst)
    return nc


def emit_final(c, k, hT, gfin, fo_rot, out_v, sq_rot, rs_rot, ps_rot):
    for bi, (t0, n) in enumerate(tblocks()):
        sq = sq_rot.next()
        c.op("act", lambda e: e.activation(out=sq[:, :, :n], in_=hT[:, :, t0:t0 + n], func=AF.Square), reads=[(hT, bi)], writes=[sq])
        ps = ps_rot.next()
        for kc in range(KC):
            c.op("pe", lambda e: e.matmul(ps[:, :n], lhsT=k["ones_bf"][:], rhs=sq[:, kc, :n], start=(kc == 0), stop=(kc == KC - 1)),
                 reads=[sq, k["ones_bf"]], writes=[ps])
        rs = rs_rot.next()
        c.op("act", lambda e: e.activation(out=rs[:, :n], in_=ps[:, :n], func=AF.Sqrt, scale=1.0 / D, bias=k["eps"][:, 0:1]),
             reads=[ps, k["eps"]], writes=[rs])
        c.op("dve", lambda e: e.reciprocal(rs[:, :n], rs[:, :n]), reads=[rs], writes=[rs])
        fo = fo_rot.next()
        for kc in range(KC):
            c.op("dve", lambda e: e.scalar_tensor_tensor(out=fo[:, kc, :n], in0=hT[:, kc, t0:t0 + n], scalar=gfin[:, kc:kc + 1], in1=rs[:, :n],
                                                         op0=ALU.mult, op1=ALU.mult), reads=[(hT, bi), rs, gfin], writes=[fo])
        c.dma("sp", out_v[:, :, t0:t0 + n], fo[:, :, :n], reads=[fo])


NTOK = 8320
NCH = 65
EPS = 1e-6
CB = 5


def emit_pow_table(c, P, n, bT, br, bi, save_at=None):
    Gr = c.sb("Gr", [P, n], F32)
    Gi = c.sb("Gi", [P, n], F32)
    tmp = c.sb("Gtmp", [P, max(n // 2, 1)], F32)
    s = c.sb("Gs", [P, 6], F32)
    saved = c.sb("Gsaved", [P, 2], F32) if save_at else None
    V = "dve"
    c.op(V, lambda e: e.memset(Gr[:, 0:1], 1.0), writes=[Gr])
    c.op(V, lambda e: e.memset(Gi[:, 0:1], 0.0), writes=[Gi])
    c.op(V, lambda e: e.tensor_copy(s[:, 0:1], br), reads=[bT], writes=[s])
    c.op(V, lambda e: e.tensor_copy(s[:, 1:2], bi), reads=[bT], writes=[s])
    m = 1
    while m < n:
        if save_at == m:
            c.op(V, lambda e: e.tensor_copy(saved[:, 0:2], s[:, 0:2]), reads=[s], writes=[saved])
        c.op(V, lambda e: e.tensor_scalar(tmp[:, :m], Gi[:, :m], s[:, 1:2], None, op0=ALU.mult), reads=[Gi, s], writes=[tmp])
        c.op(V, lambda e: e.scalar_tensor_tensor(out=Gr[:, m:2 * m], in0=Gr[:, :m], scalar=s[:, 0:1], in1=tmp[:, :m],
                                                 op0=ALU.mult, op1=ALU.subtract), reads=[Gr, s, tmp], writes=[Gr])
        c.op(V, lambda e: e.tensor_scalar(tmp[:, :m], Gi[:, :m], s[:, 0:1], None, op0=ALU.mult), reads=[Gi, s], writes=[tmp])
        c.op(V, lambda e: e.scalar_tensor_tensor(out=Gi[:, m:2 * m], in0=Gr[:, :m], scalar=s[:, 1:2], in1=tmp[:, :m],
                                                 op0=ALU.mult, op1=ALU.add), reads=[Gr, s, tmp], writes=[Gi])
        c.op(V, lambda e: e.tensor_tensor(out=s[:, 2:3], in0=s[:, 0:1], in1=s[:, 0:1], op=ALU.mult), reads=[s], writes=[s])
        c.op(V, lambda e: e.tensor_tensor(out=s[:, 3:4], in0=s[:, 1:2], in1=s[:, 1:2], op=ALU.mult), reads=[s], writes=[s])
        c.op(V, lambda e: e.scalar_tensor_tensor(out=s[:, 1:2], in0=s[:, 0:1], scalar=2.0, in1=s[:, 1:2],
                                                 op0=ALU.mult, op1=ALU.mult), reads=[s], writes=[s])
        c.op(V, lambda e: e.tensor_tensor(out=s[:, 0:1], in0=s[:, 2:3], in1=s[:, 3:4], op=ALU.subtract), reads=[s], writes=[s])
        m *= 2
    if save_at == m:
        c.op(V, lambda e: e.tensor_copy(saved[:, 0:2], s[:, 0:2]), reads=[s], writes=[saved])
    return Gr, Gi, saved


def emit_sincos_small(c, P, wT, w, cs):
    V = "dve"
    x2 = cs[:, 2:3]
    acc = cs[:, 3:4]
    c.op(V, lambda e: e.tensor_tensor(out=x2, in0=w, in1=w, op=ALU.mult), reads=[wT], writes=[cs])
    c.op(V, lambda e: e.tensor_scalar(acc, x2, -1.0 / 110, 1.0, op0=ALU.mult, op1=ALU.add), reads=[cs], writes=[cs])
    for d in (72.0, 42.0, 20.0, 6.0):
        c.op(V, lambda e: e.tensor_tensor(out=acc, in0=acc, in1=x2, op=ALU.mult), reads=[cs], writes=[cs])
        c.op(V, lambda e: e.tensor_scalar(acc, acc, -1.0 / d, 1.0, op0=ALU.mult, op1=ALU.add), reads=[cs], writes=[cs])
    c.op(V, lambda e: e.tensor_tensor(out=cs[:, 1:2], in0=acc, in1=w, op=ALU.mult), reads=[cs, wT], writes=[cs])
    c.op(V, lambda e: e.tensor_scalar(acc, x2, -1.0 / 132, 1.0, op0=ALU.mult, op1=ALU.add), reads=[cs], writes=[cs])
    for d in (90.0, 56.0, 30.0, 12.0, 2.0):
        c.op(V, lambda e: e.tensor_tensor(out=acc, in0=acc, in1=x2, op=ALU.mult), reads=[cs], writes=[cs])
        c.op(V, lambda e: e.tensor_scalar(acc, acc, -1.0 / d, 1.0, op0=ALU.mult, op1=ALU.add), reads=[cs], writes=[cs])
    c.op(V, lambda e: e.tensor_copy(cs[:, 0:1], acc), reads=[cs], writes=[cs])


def emit_gamma(c, hT_, hidx_ap, out, P, ncol):
    LN2 = math.log(2.0)
    c.op("act", lambda e: e.activation(out=out[:, :ncol], in_=hidx_ap, func=AF.Exp, scale=-LN2, bias=c.k5[:P, 0:1]),
         reads=[c.k5, hT_], writes=[out])
    c.op("dve", lambda e: e.tensor_scalar(out[:, :ncol], out[:, :ncol], -1.0, 1.0, op0=ALU.mult, op1=ALU.add), reads=[out], writes=[out])
    c.op("act", lambda e: e.activation(out=out[:, :ncol], in_=out[:, :ncol], func=AF.Ln), reads=[out], writes=[out])


def build_ret(debug=False):
    nc = bass.Bass("TRN2", target_bir_lowering=False)
    c = Ctx(nc)
    c.pe_self_sync = True
    q_d = c.dram("q", [64, NTOK], F32, "ExternalInput")
    qs_d = c.dram("qsw", [64, NTOK], F32, "ExternalInput")
    k_d = c.dram("k", [64, NTOK], F32, "ExternalInput")
    ks_d = c.dram("ksw", [64, NTOK], F32, "ExternalInput")
    v_d = c.dram("v", [NTOK, 128], F32, "ExternalInput")
    g_d = c.dram("gate", [NTOK, 128], F32, "ExternalInput")
    og_d = c.dram("outg", [128, 128], F32, "ExternalInput")
    hp_d = c.dram("hpart", [64, 1], F32, "ExternalInput")
    hs_d = c.dram("hsel", [128, 2], F32, "ExternalInput")
    y_d = c.dram("y", [NTOK, 128], F32, "ExternalOutput")

    V = "dve"
    c.k5 = c.sb("k5", [128, 1], F32)
    c.op("pool", lambda e: e.memset(c.k5[:], -5.0 * math.log(2.0)), writes=[c.k5])
    epsT = c.sb("eps", [128, 1], F32)
    c.op("pool", lambda e: e.memset(epsT[:], EPS), writes=[epsT])
    identb = c.sb("identb", [128, 128], BF16)
    iof = c.sb("iof", [128, 128], F32)
    c.op("pool", lambda e: e.iota(iof[:], [[1, 128]], base=0, channel_multiplier=-1, allow_small_or_imprecise_dtypes=True), writes=[iof])
    c.op(V, lambda e: e.tensor_single_scalar(identb[:], iof[:], 0.0, op=ALU.is_equal), reads=[iof], writes=[identb])
    hp = c.sb("hp", [64, 1], F32)
    c.dma("sp", hp[:], hp_d[:], writes=[hp])
    hs = c.sb("hs", [128, 2], F32)
    c.dma("sp", hs[:], hs_d[:], writes=[hs])
    og = c.sb("og", [128, 128], F32)
    c.dma("sp", og[:], og_d[:], writes=[og])
    lgP = c.sb("lgP", [64, 2], F32)
    emit_gamma(c, hp, hp[:, 0:1], lgP, 64, 1)
    lgB = c.sb("lgB", [128, 2], F32)
    emit_gamma(c, hs, hs[:, 0:2], lgB, 128, 2)
    maskT = c.sb("maskT", [128, 2, 128], F32)
    dpos = c.sb("dpos", [128, 128], F32)
    dge = c.sb("dge", [128, 128], F32)
    c.op(V, lambda e: e.tensor_single_scalar(dpos[:], iof[:], 0.0, op=ALU.max), reads=[iof], writes=[dpos])
    c.op(V, lambda e: e.tensor_scalar(dge[:], iof[:], 0.0, 32.0 ** -0.5, op0=ALU.is_ge, op1=ALU.mult), reads=[iof], writes=[dge])
    for hl in range(2):
        c.op("act", lambda e: e.activation(out=maskT[:, hl, :], in_=dpos[:], func=AF.Exp, scale=lgB[:, hl:hl + 1]),
             reads=[dpos, lgB], writes=[maskT])
        c.op(V, lambda e: e.tensor_tensor(out=maskT[:, hl, :], in0=maskT[:, hl, :], in1=dge[:], op=ALU.mult), reads=[maskT, dge], writes=[maskT])
    io1 = c.sb("io1", [64, 128], F32)
    c.op("pool", lambda e: e.iota(io1[:], [[1, 128]], base=1, channel_multiplier=0, allow_small_or_imprecise_dtypes=True), writes=[io1])
    io2 = c.sb("io2", [64, 128], F32)
    c.op("pool", lambda e: e.iota(io2[:], [[-1, 128]], base=127, channel_multiplier=0, allow_small_or_imprecise_dtypes=True), writes=[io2])
    qdf = c.sb("qdf", [64, 128], F32)
    kdf = c.sb("kdf", [64, 128], F32)
    c.op("act", lambda e: e.activation(out=qdf[:], in_=io1[:], func=AF.Exp, scale=lgP[:, 0:1]), reads=[io1, lgP], writes=[qdf])
    c.op(V, lambda e: e.tensor_scalar(qdf[:], qdf[:], 32.0 ** -0.5, None, op0=ALU.mult), reads=[qdf], writes=[qdf])
    c.op("act", lambda e: e.activation(out=kdf[:], in_=io2[:], func=AF.Exp, scale=lgP[:, 0:1]), reads=[io2, lgP], writes=[kdf])
    sdec = c.sb("sdec", [64, 1], F32)
    c.op("act", lambda e: e.activation(out=sdec[:], in_=lgP[:, 0:1], func=AF.Exp, scale=128.0), reads=[lgP], writes=[sdec])
    fr = c.sb("fr", [64, 4], F32)
    for hb in range(2):
        c.op("pool", lambda e: e.iota(fr[32 * hb:32 * hb + 32, 0:1], [[0, 1]], base=0, channel_multiplier=1,
                                      allow_small_or_imprecise_dtypes=True), writes=[fr])
    c.op(V, lambda e: e.tensor_single_scalar(fr[:, 3:4], fr[:, 0:1], 16.0, op=ALU.is_ge), reads=[fr], writes=[fr])
    c.op(V, lambda e: e.scalar_tensor_tensor(out=fr[:, 0:1], in0=fr[:, 3:4], scalar=-16.0, in1=fr[:, 0:1], op0=ALU.mult, op1=ALU.add),
         reads=[fr], writes=[fr])
    c.op(V, lambda e: e.tensor_scalar(fr[:, 2:3], fr[:, 3:4], 2.0, -1.0, op0=ALU.mult, op1=ALU.add), reads=[fr], writes=[fr])
    c.op("act", lambda e: e.activation(out=fr[:, 1:2], in_=fr[:, 0:1], func=AF.Exp, scale=-math.log(10000.0) / 16.0), reads=[fr], writes=[fr])
    cs = c.sb("cs", [64, 4], F32)
    emit_sincos_small(c, 64, fr, fr[:, 1:2], cs)
    Gr, Gi, s128 = emit_pow_table(c, 64, 256, cs, cs[:, 0:1], cs[:, 1:2], save_at=128)
    Fr, Fi, _ = emit_pow_table(c, 64, 64, s128, s128[:, 0:1], s128[:, 1:2])
    E1r = c.sb("E1r", [64, NCH], F32)
    E1i = c.sb("E1i", [64, NCH], F32)
    c.op(V, lambda e: e.tensor_copy(E1r[:, 1:NCH], Fr[:, 0:64]), reads=[Fr], writes=[E1r])
    c.op(V, lambda e: e.tensor_copy(E1i[:, 1:NCH], Fi[:, 0:64]), reads=[Fi], writes=[E1i])
    c.op(V, lambda e: e.tensor_copy(E1r[:, 0:1], Fr[:, 1:2]), reads=[Fr], writes=[E1r])
    c.op(V, lambda e: e.tensor_scalar(E1i[:, 0:1], Fi[:, 1:2], -1.0, None, op0=ALU.mult), reads=[Fi], writes=[E1i])
    E2r = Gr
    E2i = Gi

    QR = c.sb("QR", [64, NTOK], BF16)
    KR = c.sb("KR", [64, NTOK], BF16)
    QD = c.sb("QD", [64, NTOK], BF16)
    KD = c.sb("KD", [64, NTOK], BF16)
    v_sb = c.sb("v_sb", [128, NCH, 128], BF16)
    g_sb = c.sb("g_sb", [128, NCH, 128], BF16)
    y_sb = c.sb("y_sb", [128, NCH, 128], F32)
    c.dma("pool", v_sb[:], v_d.h.ap().rearrange("(c p) f -> p c f", p=128), writes=[v_sb])
    c.dma("pool", g_sb[:], g_d.h.ap().rearrange("(c p) f -> p c f", p=128), writes=[g_sb])

    nblk = NCH // CB
    BW = CB * 128
    x_rot = Rot([c.sb(f"x{i}", [64, BW], F32) for i in range(2)])
    xs_rot = Rot([c.sb(f"xs{i}", [64, BW], F32) for i in range(2)])
    COSb = c.sb("COSb", [64, CB, 128], F32)
    SINb = c.sb("SINb", [64, CB, 128], F32)
    tb1 = c.sb("tb1", [64, CB, 128], F32)
    tb2 = c.sb("tb2", [64, CB, 128], F32)
    r1 = c.sb("r1", [64, BW], F32)
    r2 = c.sb("r2", [64, BW], F32)
    for b in range(nblk):
        c0 = b * CB
        t0 = c0 * 128
        e1r = E1r[:, c0:c0 + CB].unsqueeze(2).to_broadcast([64, CB, 128])
        e1i = E1i[:, c0:c0 + CB].unsqueeze(2).to_broadcast([64, CB, 128])
        e2r = E2r[:, 16:144].unsqueeze(1).to_broadcast([64, CB, 128])
        e2i = E2i[:, 16:144].unsqueeze(1).to_broadcast([64, CB, 128])
        P_ = "pool"
        c.op(P_, lambda e: e.tensor_tensor(out=tb1[:], in0=e1r, in1=e2r, op=ALU.mult), reads=[E1r, E2r], writes=[tb1])
        c.op(P_, lambda e: e.tensor_tensor(out=tb2[:], in0=e1i, in1=e2i, op=ALU.mult), reads=[E1i, E2i], writes=[tb2])
        c.op(P_, lambda e: e.tensor_tensor(out=COSb[:], in0=tb1[:], in1=tb2[:], op=ALU.subtract), reads=[tb1, tb2], writes=[COSb])
        c.op(P_, lambda e: e.tensor_tensor(out=tb1[:], in0=e1r, in1=e2i, op=ALU.mult), reads=[E1r, E2i], writes=[tb1])
        c.op(P_, lambda e: e.tensor_tensor(out=tb2[:], in0=e1i, in1=e2r, op=ALU.mult), reads=[E1i, E2r], writes=[tb2])
        c.op(P_, lambda e: e.tensor_tensor(out=SINb[:], in0=tb1[:], in1=tb2[:], op=ALU.add), reads=[tb1, tb2], writes=[SINb])
        cosf = COSb[:].rearrange("p c j -> p (c j)")
        sinf = SINb[:].rearrange("p c j -> p (c j)")
        for (src_d, srcs_d, OUT, DEC, fac) in ((q_d, qs_d, QR, QD, qdf), (k_d, ks_d, KR, KD, kdf)):
            x = x_rot.next(); xs = xs_rot.next()
            c.dma("sp", x[:], src_d[:, t0:t0 + BW], writes=[x])
            c.dma("sp", xs[:], srcs_d[:, t0:t0 + BW], writes=[xs])
            c.op(V, lambda e: e.tensor_tensor(out=r1[:], in0=x[:], in1=cosf, op=ALU.mult), reads=[x, COSb], writes=[r1])
            c.op(V, lambda e: e.scalar_tensor_tensor(out=r2[:], in0=xs[:], scalar=fr[:, 2:3], in1=sinf, op0=ALU.mult, op1=ALU.mult),
                 reads=[xs, fr, SINb], writes=[r2])
            c.op(V, lambda e: e.tensor_tensor(out=OUT[:, t0:t0 + BW], in0=r1[:], in1=r2[:], op=ALU.add), reads=[r1, r2], writes=[(OUT, b)])
            facb = fac[:].unsqueeze(1).to_broadcast([64, CB, 128])
            c.op(V, lambda e: e.tensor_tensor(out=DEC[:, t0:t0 + BW].rearrange("p (c j) -> p c j", j=128),
                                              in0=OUT[:, t0:t0 + BW].rearrange("p (c j) -> p c j", j=128), in1=facb, op=ALU.mult),
                 reads=[(OUT, b), fac], writes=[(DEC, b)])

    ps_tr = Rot([c.ps(f"ps_tr{i}", [128, 64], BF16) for i in range(2)])
    ps_s = Rot([c.ps(f"ps_s{i}", [128, 2, 128], F32) for i in range(2)])
    ps_o = Rot([c.ps(f"ps_o{i}", [128, 128], F32) for i in range(2)])
    ps_d = Rot([c.ps(f"ps_d{i}", [64, 128], F32) for i in range(2)])
    kdt_rot = Rot([c.sb(f"kdt{i}", [128, 64], BF16) for i in range(2)])
    sT_rot = Rot([c.sb(f"sT{i}", [128, 2, 128], BF16) for i in range(2)])
    S32 = c.sb("S32", [64, 128], F32)
    c.op(V, lambda e: e.memset(S32[:], 0.0), writes=[S32])
    Sb_rot = Rot([c.sb(f"Sb{i}", [64, 128], BF16) for i in range(2)])
    Sb = Sb_rot.next()
    c.op(V, lambda e: e.memset(Sb[:], 0.0), writes=[Sb])
    o_rot = Rot([c.sb(f"o{i}", [128, 2, 64], F32) for i in range(2)])
    cen_rot = Rot([c.sb(f"cen{i}", [128, 2, 64], F32) for i in range(2)])
    sq_rot = Rot([c.sb(f"sqr{i}", [128, 2, 64], F32) for i in range(2)])
    st_rot = Rot([c.sb(f"st{i}", [128, 8], F32) for i in range(2)])
    gg_rot = Rot([c.sb(f"gg{i}", [128, 128], F32) for i in range(2)])
    for ch in range(NCH):
        b = ch // CB
        t0 = ch * 128
        ptr = ps_tr.next()
        c.op("pe", lambda e: e.transpose(ptr[:, :], KD[:, t0:t0 + 128], identb[0:64, 0:64]), reads=[(KD, b), identb], writes=[ptr])
        kdt = kdt_rot.next()
        c.op("act", lambda e: e.copy(out=kdt[:], in_=ptr[:]), reads=[ptr], writes=[kdt])
        pss = ps_s.next()
        for hl in range(2):
            c.op("pe", lambda e: e.matmul(pss[:, hl, :], lhsT=KR[32 * hl:32 * hl + 32, t0:t0 + 128], rhs=QR[32 * hl:32 * hl + 32, t0:t0 + 128],
                                          start=True, stop=True), reads=[(KR, b), (QR, b)], writes=[pss])
        sT = sT_rot.next()
        c.op(V, lambda e: e.tensor_tensor(out=sT[:], in0=pss[:], in1=maskT[:], op=ALU.mult), reads=[pss, maskT], writes=[sT])
        pso = ps_o.next()
        for hl in range(2):
            c.op("pe", lambda e: e.matmul(pso[:, hl * 64:(hl + 1) * 64], lhsT=sT[:, hl, :], rhs=v_sb[:, ch, hl * 64:(hl + 1) * 64],
                                          start=True, stop=False), reads=[sT, v_sb], writes=[pso])
            c.op("pe", lambda e: e.matmul(pso[:, hl * 64:(hl + 1) * 64], lhsT=QD[32 * hl:32 * hl + 32, t0:t0 + 128],
                                          rhs=Sb[32 * hl:32 * hl + 32, hl * 64:(hl + 1) * 64], start=False, stop=True),
                 reads=[(QD, b), Sb], writes=[pso])
        psd = ps_d.next()
        c.op("pe", lambda e: e.matmul(psd[:, :], lhsT=kdt[:], rhs=v_sb[:, ch, :], start=True, stop=True), reads=[kdt, v_sb], writes=[psd])
        c.op(V, lambda e: e.scalar_tensor_tensor(out=S32[:], in0=S32[:], scalar=sdec[:, 0:1], in1=psd[:], op0=ALU.mult, op1=ALU.add),
             reads=[S32, sdec, psd], writes=[S32])
        Sb = Sb_rot.next()
        c.op("act", lambda e: e.copy(out=Sb[:], in_=S32[:]), reads=[S32], writes=[Sb])
        o = o_rot.next(); cen = cen_rot.next(); sq = sq_rot.next(); st = st_rot.next(); gg = gg_rot.next()
        c.op("act", lambda e: e.copy(out=o[:].rearrange("p h v -> p (h v)"), in_=pso[:]), reads=[pso], writes=[o])
        c.op(V, lambda e: e.tensor_reduce(out=st[:, 0:2], in_=o[:], axis=AX.X, op=ALU.add), reads=[o], writes=[st])
        c.op(V, lambda e: e.tensor_scalar(st[:, 0:2], st[:, 0:2], 1.0 / 64, None, op0=ALU.mult), reads=[st], writes=[st])
        c.op(V, lambda e: e.tensor_tensor(out=cen[:], in0=o[:], in1=st[:, 0:2].unsqueeze(2).to_broadcast([128, 2, 64]), op=ALU.subtract),
             reads=[o, st], writes=[cen])
        c.op("pool", lambda e: e.tensor_tensor(out=sq[:], in0=cen[:], in1=cen[:], op=ALU.mult), reads=[cen], writes=[sq])
        c.op(V, lambda e: e.tensor_reduce(out=st[:, 2:4], in_=sq[:], axis=AX.X, op=ALU.add), reads=[sq, st], writes=[st])
        c.op("act", lambda e: e.activation(out=st[:, 2:4], in_=st[:, 2:4], func=AF.Sqrt, scale=1.0 / 64, bias=epsT[:, 0:1]), reads=[st, epsT], writes=[st])
        c.op(V, lambda e: e.reciprocal(st[:, 2:4], st[:, 2:4]), reads=[st], writes=[st])
        c.op("act", lambda e: e.activation(out=gg[:], in_=g_sb[:, ch, :], func=AF.Silu), reads=[g_sb], writes=[gg])
        c.op("pool", lambda e: e.tensor_tensor(out=gg[:], in0=gg[:], in1=og[:], op=ALU.mult), reads=[gg, og], writes=[gg])
        c.op(V, lambda e: e.tensor_tensor(out=cen[:], in0=cen[:], in1=st[:, 2:4].unsqueeze(2).to_broadcast([128, 2, 64]), op=ALU.mult),
             reads=[cen, st], writes=[cen])
        c.op(V, lambda e: e.tensor_tensor(out=y_sb[:, ch, :], in0=cen[:].rearrange("p h v -> p (h v)"), in1=gg[:], op=ALU.mult),
             reads=[cen, gg], writes=[(y_sb, ch)])
    c.barrier()
    if debug:
        dbg = {"lgP": lgP, "lgB": lgB, "fr": fr, "cs": cs, "E1r": E1r, "E1i": E1i, "Gr": Gr, "Gi": Gi, "maskT": maskT,
               "qdf": qdf, "kdf": kdf, "sdec": sdec, "S32": S32, "COSb": COSb, "SINb": SINb}
        for nm, t in dbg.items():
            shp = list(t.h.shape)
            dd = c.dram("dbg_" + nm, shp, F32, "ExternalOutput")
            c.dma("sp", dd[:], t[:], reads=[t])
        for nm, t in {"QR": QR, "KR": KR, "QD": QD, "KD": KD}.items():
            tmpf = c.sb("dbgf_" + nm, [64, 1024], F32)
            c.op("dve", lambda e: e.tensor_copy(tmpf[:], t[:, 0:1024]), writes=[tmpf])
            dd = c.dram("dbg_" + nm, [64, 1024], F32, "ExternalOutput")
            c.dma("sp", dd[:], tmpf[:], reads=[tmpf])
    c.dma("sp", y_d.h.ap().rearrange("(c p) f -> p c f", p=128), y_sb[:], reads=[y_sb], qt=y_sb)
    c.finish()
    c.close()
    print("ret instructions:", c.n_inst)
    return nc


NTOK = 8320
NG = 65
EPS = 1e-6
GB = 13
BW = GB * 128
NBLK = NG // GB
CS = 32


def build_hg(layer, debug=False):
    nc = bass.Bass("TRN2", target_bir_lowering=False)
    c = Ctx(nc)
    x_d = c.dram("x3", [3, 64, NTOK], F32, "ExternalInput")
    cw_d = c.dram("convw", [3, 64, 4], F32, "ExternalInput")
    g_d = c.dram("gate", [NTOK, 64], F32, "ExternalInput")
    og_d = c.dram("outg", [128, 64], F32, "ExternalInput")
    lb_d = c.dram("lbp", [64, 2], F32, "ExternalInput")
    y_d = c.dram("y", [NTOK, 64], F32, "ExternalOutput")
    V = "dve"

    epsT = c.sb("eps", [128, 1], F32)
    c.op("pool", lambda e: e.memset(epsT[:], EPS), writes=[epsT])
    iof = c.sb("iof", [128, 128], F32)
    c.op("pool", lambda e: e.iota(iof[:], [[1, 128]], base=0, channel_multiplier=-1, allow_small_or_imprecise_dtypes=True), writes=[iof])
    identf = c.sb("identf", [128, 128], F32)
    c.op(V, lambda e: e.tensor_single_scalar(identf[:], iof[:], 0.0, op=ALU.is_equal), reads=[iof], writes=[identf])
    identb = c.sb("identb", [128, 128], BF16)
    c.op(V, lambda e: e.tensor_copy(identb[:], identf[:]), reads=[identf], writes=[identb])
    dge = c.sb("dge", [128, 128], F32)
    c.op(V, lambda e: e.tensor_single_scalar(dge[:], iof[:], 0.0, op=ALU.is_ge), reads=[iof], writes=[dge])
    bmask = c.sb("bmask", [128, 128], F32)
    c.op(V, lambda e: e.memset(bmask[:], 0.0), writes=[bmask])
    for m in range(4):
        c.op(V, lambda e: e.tensor_copy(bmask[32 * m:32 * m + 32, 32 * m:32 * m + 32], dge[32 * m:32 * m + 32, 32 * m:32 * m + 32]),
             reads=[dge], writes=[bmask])
    cmask = c.sb("cmask", [128, 4, 64], F32)
    cm2 = c.sb("cm2", [128, 4, 64], F32)
    c.op("pool", lambda e: e.iota(cmask[:], [[-32, 4], [0, 64]], base=0, channel_multiplier=1, allow_small_or_imprecise_dtypes=True), writes=[cmask])
    c.op(V, lambda e: e.tensor_single_scalar(cm2[:], cmask[:], 32.0, op=ALU.is_lt), reads=[cmask], writes=[cm2])
    c.op(V, lambda e: e.tensor_single_scalar(cmask[:], cmask[:], 0.0, op=ALU.is_ge), reads=[cmask, cm2], writes=[cmask])
    c.op(V, lambda e: e.tensor_tensor(out=cmask[:], in0=cmask[:], in1=cm2[:], op=ALU.mult), reads=[cmask, cm2], writes=[cmask])
    colmask = c.sb("colmask", [64, 4, 128], F32)
    col2 = c.sb("col2", [64, 4, 128], F32)
    c.op("pool", lambda e: e.iota(colmask[:], [[-32, 4], [1, 128]], base=0, channel_multiplier=0, allow_small_or_imprecise_dtypes=True), writes=[colmask])
    c.op(V, lambda e: e.tensor_single_scalar(col2[:], colmask[:], 32.0, op=ALU.is_lt), reads=[colmask], writes=[col2])
    c.op(V, lambda e: e.tensor_single_scalar(colmask[:], colmask[:], 0.0, op=ALU.is_ge), reads=[colmask, col2], writes=[colmask])
    c.op(V, lambda e: e.tensor_tensor(out=colmask[:], in0=colmask[:], in1=col2[:], op=ALU.mult), reads=[colmask, col2], writes=[colmask])
    rmask = c.sb("rmask", [64, BW], F32)
    c.op("pool", lambda e: e.iota(rmask[:], [[0, BW // CS], [1, CS]], base=0, channel_multiplier=0, allow_small_or_imprecise_dtypes=True), writes=[rmask])
    c.op(V, lambda e: e.tensor_single_scalar(rmask[:], rmask[:], 0.0, op=ALU.is_gt), reads=[rmask], writes=[rmask])
    cw = c.sb("cw", [64, 3, 4], F32)
    c.dma("sp", cw[:], cw_d.h.ap().rearrange("s p k -> p s k"), writes=[cw])
    og = c.sb("og", [128, 64], F32)
    c.dma("sp", og[:], og_d[:], writes=[og])
    lbp = c.sb("lbp", [64, 4], F32)
    c.dma("sp", lbp[:, 0:2], lb_d[:], writes=[lbp])
    if layer == 0:
        c.op(V, lambda e: e.memset(lbp[:, 2:3], 0.0), reads=[lbp], writes=[lbp])
    else:
        c.op(V, lambda e: e.tensor_tensor(out=lbp[:, 2:3], in0=lbp[:, 1:2], in1=lbp[:, 0:1], op=ALU.subtract), reads=[lbp], writes=[lbp])
        c.op("act", lambda e: e.activation(out=lbp[:, 2:3], in_=lbp[:, 2:3], func=AF.Sigmoid), reads=[lbp], writes=[lbp])
    c.op(V, lambda e: e.tensor_scalar(lbp[:, 3:4], lbp[:, 2:3], -1.0, 1.0, op0=ALU.mult, op1=ALU.add), reads=[lbp], writes=[lbp])

    QT = c.sb("QT", [64, NTOK], BF16)
    KT = c.sb("KT", [64, NTOK], BF16)
    KDT = c.sb("KDT", [64, NTOK], BF16)
    Vtok = c.sb("Vtok", [128, NG, 64], BF16)
    KDtok = c.sb("KDtok", [128, NG, 64], BF16)
    g_sb = c.sb("g_sb", [128, NG, 64], BF16)
    y_sb = c.sb("y_sb", [128, NG, 64], F32)
    Dec = c.sb("Dec", [64, NTOK // CS], F32)
    c.dma("pool", g_sb[:], g_d.h.ap().rearrange("(c p) f -> p c f", p=128), writes=[g_sb])

    xq = c.sb("xq", [64, BW + 3], F32); xf = c.sb("xf", [64, BW + 3], F32); xi = c.sb("xi", [64, BW + 3], F32)
    cq = c.sb("cq", [64, BW], F32); cf = c.sb("cf", [64, BW], F32); ci = c.sb("ci", [64, BW], F32)
    gg_ = c.sb("g", [64, BW], F32); gcum = c.sb("gcum", [64, BW], F32); tmp = c.sb("tmp", [64, BW], F32)
    ps_t = Rot([c.ps(f"pst{i}", [128, 64], F32) for i in range(2)])
    ps_tb = Rot([c.ps(f"pstb{i}", [128, 64], BF16) for i in range(2)])
    NCB = BW // CS
    for b in range(NBLK):
        t0 = b * BW
        for si, (xt, ct) in enumerate(((xq, cq), (xf, cf), (xi, ci))):
            if b == 0:
                c.op(V, lambda e: e.memset(xt[:, 0:3], 0.0), writes=[xt])
                c.dma("sp", xt[:, 3:BW + 3], x_d.h.ap()[si, :, 0:BW], writes=[xt])
            else:
                c.dma("sp", xt[:, :], x_d.h.ap()[si, :, t0 - 3:t0 + BW], writes=[xt])
            c.op(V, lambda e: e.tensor_scalar(ct[:], xt[:, 0:BW], cw[:, si, 0:1], None, op0=ALU.mult), reads=[xt, cw], writes=[ct])
            for kk_ in range(1, 4):
                c.op(V, lambda e: e.scalar_tensor_tensor(out=ct[:], in0=xt[:, kk_:kk_ + BW], scalar=cw[:, si, kk_:kk_ + 1], in1=ct[:],
                                                         op0=ALU.mult, op1=ALU.add), reads=[xt, cw, ct], writes=[ct])
        c.op("act", lambda e: e.activation(out=cq[:], in_=cq[:], func=AF.Silu), reads=[cq], writes=[cq])
        c.op("act", lambda e: e.activation(out=cf[:], in_=cf[:], func=AF.Sigmoid), reads=[cf], writes=[cf])
        c.op(V, lambda e: e.tensor_scalar(cf[:], cf[:], lbp[:, 3:4], lbp[:, 2:3], op0=ALU.mult, op1=ALU.add), reads=[cf, lbp], writes=[cf])
        c.op("act", lambda e: e.activation(out=gg_[:], in_=cf[:], func=AF.Ln), reads=[cf], writes=[gg_])
        c.op(V, lambda e: e.tensor_scalar(cf[:], cf[:], -1.0, 1.0, op0=ALU.mult, op1=ALU.add), reads=[cf, gg_], writes=[cf])
        c.op(V, lambda e: e.tensor_tensor_scan(out=gcum[:], data0=rmask[:], data1=gg_[:], initial=0.0, op0=ALU.mult, op1=ALU.add),
             reads=[rmask, gg_], writes=[gcum])
        c.op("act", lambda e: e.activation(out=tmp[:], in_=gcum[:], func=AF.Exp), reads=[gcum], writes=[tmp])
        c.op(V, lambda e: e.tensor_tensor(out=QT[:, t0:t0 + BW], in0=cq[:], in1=tmp[:], op=ALU.mult), reads=[cq, tmp], writes=[(QT, b)])
        c.op(V, lambda e: e.tensor_single_scalar(tmp[:], gcum[:], -80.0, op=ALU.max), reads=[gcum, (QT, b)], writes=[tmp])
        c.op("act", lambda e: e.activation(out=tmp[:], in_=tmp[:], func=AF.Exp, scale=-1.0), reads=[tmp], writes=[tmp])
        c.op(V, lambda e: e.tensor_tensor(out=KT[:, t0:t0 + BW], in0=cf[:], in1=tmp[:], op=ALU.mult), reads=[cf, tmp], writes=[(KT, b)])
        gl = gcum[:].rearrange("p (c j) -> p c j", j=CS)[:, :, CS - 1:CS]
        c.op(V, lambda e: e.tensor_tensor(out=tmp[:].rearrange("p (c j) -> p c j", j=CS), in0=gl.to_broadcast([64, NCB, CS]),
                                          in1=gcum[:].rearrange("p (c j) -> p c j", j=CS), op=ALU.subtract), reads=[gcum, (KT, b)], writes=[tmp])
        c.op("act", lambda e: e.activation(out=tmp[:], in_=tmp[:], func=AF.Exp), reads=[tmp], writes=[tmp])
        c.op(V, lambda e: e.tensor_tensor(out=KDT[:, t0:t0 + BW], in0=cf[:], in1=tmp[:], op=ALU.mult), reads=[cf, tmp], writes=[(KDT, b)])
        c.op("act", lambda e: e.activation(out=Dec[:, b * NCB:(b + 1) * NCB].unsqueeze(2), in_=gl, func=AF.Exp), reads=[gcum], writes=[(Dec, b)])
        for gi in range(GB):
            G = b * GB + gi
            pt = ps_t.next()
            c.op("pe", lambda e: e.transpose(pt[:, :], ci[:, gi * 128:(gi + 1) * 128], identf[0:64, 0:64]), reads=[ci, identf], writes=[pt])
            c.op("act", lambda e: e.copy(out=Vtok[:, G, :], in_=pt[:]), reads=[pt], writes=[(Vtok, G)])
            ptb = ps_tb.next()
            c.op("pe", lambda e: e.transpose(ptb[:, :], KDT[:, t0 + gi * 128:t0 + (gi + 1) * 128], identb[0:64, 0:64]),
                 reads=[(KDT, b), identb], writes=[ptb])
            c.op("act", lambda e: e.copy(out=KDtok[:, G, :], in_=ptb[:]), reads=[ptb], writes=[(KDtok, G)])

    ps_s = Rot([c.ps(f"ps_s{i}", [128, 128], F32) for i in range(1)])
    ps_o = Rot([c.ps(f"ps_o{i}", [128, 64], F32) for i in range(2)])
    ps_d = Rot([c.ps(f"ps_d{i}", [64, 4, 64], F32) for i in range(1)])
    sT_rot = Rot([c.sb(f"sT{i}", [128, 128], BF16) for i in range(2)])
    S32 = c.sb("S32", [64, 64], F32)
    c.op(V, lambda e: e.memset(S32[:], 0.0), writes=[S32])
    Sb_rot = Rot([c.sb(f"Sb{i}", [64, 64], BF16) for i in range(6)])
    Sb = Sb_rot.next()
    c.op(V, lambda e: e.memset(Sb[:], 0.0), writes=[Sb])
    o_rot = Rot([c.sb(f"o{i}", [128, 64], F32) for i in range(2)])
    sq_rot = Rot([c.sb(f"sqr{i}", [128, 64], F32) for i in range(2)])
    st_rot = Rot([c.sb(f"st{i}", [128, 4], F32) for i in range(2)])
    gg_rot = Rot([c.sb(f"gg{i}", [128, 64], F32) for i in range(2)])
    vb_rot = Rot([c.sb(f"vb{i}", [128, 4, 64], BF16) for i in range(2)])
    qm_rot = Rot([c.sb(f"qm{i}", [64, 4, 128], BF16) for i in range(2)])
    for G in range(NG):
        b = G // GB
        t0 = G * 128
        pss = ps_s.next()
        c.op("pe", lambda e: e.matmul(pss[:, :], lhsT=KT[:, t0:t0 + 128], rhs=QT[:, t0:t0 + 128], start=True, stop=True),
             reads=[(KT, b), (QT, b)], writes=[pss])
        sT = sT_rot.next()
        c.op(V, lambda e: e.tensor_tensor(out=sT[:], in0=pss[:], in1=bmask[:], op=ALU.mult), reads=[pss, bmask], writes=[sT])
        psd = ps_d.next()
        vb = vb_rot.next()
        c.op("pool", lambda e: e.tensor_tensor(out=vb[:], in0=Vtok[:, G, :].unsqueeze(1).to_broadcast([128, 4, 64]), in1=cmask[:], op=ALU.mult),
             reads=[(Vtok, G), cmask], writes=[vb])
        c.op("pe", lambda e: e.matmul(psd[:].rearrange("p m v -> p (m v)"), lhsT=KDtok[:, G, :], rhs=vb[:].rearrange("p m v -> p (m v)"),
                                      start=True, stop=True), reads=[(KDtok, G), vb], writes=[psd])
        qm = qm_rot.next()
        c.op(V, lambda e: e.tensor_tensor(out=qm[:], in0=QT[:, t0:t0 + 128].unsqueeze(1).to_broadcast([64, 4, 128]), in1=colmask[:], op=ALU.mult),
             reads=[(QT, b), colmask], writes=[qm])
        pso = ps_o.next()
        c.op("pe", lambda e: e.matmul(pso[:, :], lhsT=sT[:], rhs=Vtok[:, G, :], start=True, stop=False), reads=[sT, (Vtok, G)], writes=[pso])
        for m in range(4):
            ch = G * 4 + m
            c.op("pe", lambda e: e.matmul(pso[:, :], lhsT=qm[:, m, :], rhs=Sb[:, :],
                                          start=False, stop=(m == 3)), reads=[qm, Sb], writes=[pso])
            c.op(V, lambda e: e.scalar_tensor_tensor(out=S32[:], in0=S32[:], scalar=Dec[:, ch:ch + 1], in1=psd[:, m, :],
                                                     op0=ALU.mult, op1=ALU.add), reads=[S32, (Dec, b), psd], writes=[S32])
            Sb = Sb_rot.next()
            c.op("act", lambda e: e.copy(out=Sb[:], in_=S32[:]), reads=[S32], writes=[Sb])
        o = o_rot.next(); sq = sq_rot.next(); st = st_rot.next(); gg = gg_rot.next()
        c.op("act", lambda e: e.copy(out=o[:], in_=pso[:]), reads=[pso], writes=[o])
        c.op("pool", lambda e: e.tensor_tensor(out=sq[:], in0=o[:], in1=o[:], op=ALU.mult), reads=[o], writes=[sq])
        c.op(V, lambda e: e.tensor_reduce(out=st[:, 0:1], in_=sq[:], axis=AX.X, op=ALU.add), reads=[sq], writes=[st])
        c.op("act", lambda e: e.activation(out=st[:, 0:1], in_=st[:, 0:1], func=AF.Sqrt, scale=1.0 / 64, bias=epsT[:, 0:1]), reads=[st, epsT], writes=[st])
        c.op(V, lambda e: e.reciprocal(st[:, 0:1], st[:, 0:1]), reads=[st], writes=[st])
        c.op("act", lambda e: e.activation(out=gg[:], in_=g_sb[:, G, :], func=AF.Silu), reads=[g_sb], writes=[gg])
        c.op("pool", lambda e: e.tensor_tensor(out=gg[:], in0=gg[:], in1=og[:], op=ALU.mult), reads=[gg, og], writes=[gg])
        c.op(V, lambda e: e.scalar_tensor_tensor(out=y_sb[:, G, :], in0=o[:], scalar=st[:, 0:1], in1=gg[:], op0=ALU.mult, op1=ALU.mult),
             reads=[o, st, gg], writes=[(y_sb, G)])
    c.barrier()
    c.dma("sp", y_d.h.ap().rearrange("(c p) f -> p c f", p=128), y_sb[:], reads=[y_sb], qt=y_sb)
    c.finish()
    c.close()
    print("hg instructions:", c.n_inst)
    return nc


NSB = 1040
NGL = 4
NLEV = 11


def emit_sincos_tile(c, x, xT, cs_c, cs_s, x2, acc, n):
    V = "dve"
    c.op(V, lambda e: e.tensor_tensor(out=x2[:], in0=x[:], in1=x[:], op=ALU.mult), reads=[x], writes=[x2])
    c.op(V, lambda e: e.tensor_scalar(acc[:], x2[:], -1.0 / 156, 1.0, op0=ALU.mult, op1=ALU.add), reads=[x2], writes=[acc])
    for d in (110.0, 72.0, 42.0, 20.0, 6.0):
        c.op(V, lambda e: e.tensor_tensor(out=acc[:], in0=acc[:], in1=x2[:], op=ALU.mult), reads=[acc, x2], writes=[acc])
        c.op(V, lambda e: e.tensor_scalar(acc[:], acc[:], -1.0 / d, 1.0, op0=ALU.mult, op1=ALU.add), reads=[acc], writes=[acc])
    c.op(V, lambda e: e.tensor_tensor(out=cs_s[:], in0=acc[:], in1=x[:], op=ALU.mult), reads=[acc, x], writes=[cs_s])
    c.op(V, lambda e: e.tensor_scalar(acc[:], x2[:], -1.0 / 182, 1.0, op0=ALU.mult, op1=ALU.add), reads=[x2, cs_s], writes=[acc])
    for d in (132.0, 90.0, 56.0, 30.0, 12.0, 2.0):
        c.op(V, lambda e: e.tensor_tensor(out=acc[:], in0=acc[:], in1=x2[:], op=ALU.mult), reads=[acc, x2], writes=[acc])
        c.op(V, lambda e: e.tensor_scalar(acc[:], acc[:], -1.0 / d, 1.0, op0=ALU.mult, op1=ALU.add), reads=[acc], writes=[acc])
    c.op(V, lambda e: e.tensor_copy(cs_c[:], acc[:]), reads=[acc], writes=[cs_c])


def emit_cdouble(c, cr, ci, t1, t2):
    V = "dve"
    c.op(V, lambda e: e.tensor_tensor(out=t1[:], in0=cr[:], in1=cr[:], op=ALU.mult), reads=[cr], writes=[t1])
    c.op(V, lambda e: e.tensor_tensor(out=t2[:], in0=ci[:], in1=ci[:], op=ALU.mult), reads=[ci], writes=[t2])
    c.op(V, lambda e: e.scalar_tensor_tensor(out=ci[:], in0=cr[:], scalar=2.0, in1=ci[:], op0=ALU.mult, op1=ALU.mult), reads=[cr, ci, t2], writes=[ci])
    c.op(V, lambda e: e.tensor_tensor(out=cr[:], in0=t1[:], in1=t2[:], op=ALU.subtract), reads=[t1, t2, ci], writes=[cr])


def build_s5(debug=False):
    nc = bass.Bass("TRN2", target_bir_lowering=False)
    c = Ctx(nc)
    U_d = c.dram("U", [NGL, 128, NSB], F32, "ExternalInput")
    lam_d = c.dram("lam", [128, NGL, 2], F32, "ExternalInput")
    ls_d = c.dram("ls", [128, NGL], F32, "ExternalInput")
    B_d = c.dram("Bm", [128, NGL, 2, 16], F32, "ExternalInput")
    C_d = c.dram("Cm", [128, NGL, 2, 16], F32, "ExternalInput")
    D_d = c.dram("Dm", [128, NGL], F32, "ExternalInput")
    y_d = c.dram("y", [NGL, 128, NSB], F32, "ExternalOutput")
    V = "dve"
    G4 = NGL

    def sb(name, shape, dt=F32):
        return c.sb(name, shape, dt)

    iof = sb("iof", [128, 128])
    c.op("pool", lambda e: e.iota(iof[:], [[1, 128]], base=0, channel_multiplier=-1, allow_small_or_imprecise_dtypes=True), writes=[iof])
    ident = sb("ident", [128, 128])
    c.op(V, lambda e: e.tensor_single_scalar(ident[:], iof[:], 0.0, op=ALU.is_equal), reads=[iof], writes=[ident])
    pswap = sb("pswap", [128, 128])
    ptmp = sb("ptmp", [128, 128])
    c.op(V, lambda e: e.tensor_single_scalar(pswap[:], iof[:], 64.0, op=ALU.is_equal), reads=[iof], writes=[pswap])
    c.op(V, lambda e: e.tensor_single_scalar(ptmp[:], iof[:], -64.0, op=ALU.is_equal), reads=[iof], writes=[ptmp])
    c.op(V, lambda e: e.tensor_tensor(out=pswap[:], in0=pswap[:], in1=ptmp[:], op=ALU.add), reads=[pswap, ptmp], writes=[pswap])
    tmask = sb("tmask", [128, 8, 16])
    c.op("pool", lambda e: e.iota(tmask[:], [[16, 8], [0, 16]], base=15, channel_multiplier=-1, allow_small_or_imprecise_dtypes=True), writes=[tmask])
    c.op(V, lambda e: e.tensor_single_scalar(tmask[:], tmask[:], 0.0, op=ALU.is_ge), reads=[tmask], writes=[tmask])
    sgnh = sb("sgnh", [128, 1])
    c.op(V, lambda e: e.memset(sgnh[0:64, :], 1.0), writes=[sgnh])
    c.op(V, lambda e: e.memset(sgnh[64:128, :], -1.0), writes=[sgnh])
    mv = sb("mv", [128, G4, 9])
    c.op("pool", lambda e: e.iota(mv[:], [[0, G4], [1, 9]], base=0, channel_multiplier=0, allow_small_or_imprecise_dtypes=True), writes=[mv])

    lam = sb("lam", [128, G4, 2]); ls = sb("ls", [128, G4]); Bm = sb("Bm", [128, G4, 2, 16]); Cm = sb("Cm", [128, G4, 2, 16]); Dm = sb("Dm", [128, G4])
    c.dma("sp", lam[:], lam_d[:], writes=[lam]); c.dma("sp", ls[:], ls_d[:], writes=[ls])
    c.dma("sp", Bm[:], B_d[:], writes=[Bm]); c.dma("sp", Cm[:], C_d[:], writes=[Cm]); c.dma("sp", Dm[:], D_d[:], writes=[Dm])

    st = sb("st", [128, G4]); a = sb("a", [128, G4]); th = sb("th", [128, G4])
    c.op("act", lambda e: e.activation(out=st[:], in_=ls[:], func=AF.Exp), reads=[ls], writes=[st])
    c.op(V, lambda e: e.tensor_tensor(out=a[:], in0=lam[:, :, 0], in1=st[:], op=ALU.mult), reads=[lam, st], writes=[a])
    c.op(V, lambda e: e.tensor_tensor(out=th[:], in0=lam[:, :, 1], in1=st[:], op=ALU.mult), reads=[lam, st], writes=[th])
    kq = sb("kq", [128, G4]); ki = sb("ki", [128, G4], I32); x = sb("x", [128, G4])
    c.op(V, lambda e: e.tensor_scalar(kq[:], th[:], 1.0 / (2 * math.pi), None, op0=ALU.mult), reads=[th], writes=[kq])
    c.op(V, lambda e: e.tensor_copy(ki[:], kq[:]), reads=[kq], writes=[ki])
    c.op(V, lambda e: e.tensor_copy(kq[:], ki[:]), reads=[ki], writes=[kq])
    c.op(V, lambda e: e.scalar_tensor_tensor(out=x[:], in0=kq[:], scalar=-2 * math.pi, in1=th[:], op0=ALU.mult, op1=ALU.add), reads=[kq, th], writes=[x])
    c.op(V, lambda e: e.tensor_scalar(x[:], x[:], 0.25, None, op0=ALU.mult), reads=[x], writes=[x])
    p1r = sb("p1r", [128, G4]); p1i = sb("p1i", [128, G4]); x2 = sb("x2", [128, G4]); acc = sb("acc", [128, G4])
    t1 = sb("t1", [128, G4]); t2 = sb("t2", [128, G4])
    emit_sincos_tile(c, x, x, p1r, p1i, x2, acc, G4)
    emit_cdouble(c, p1r, p1i, t1, t2)
    emit_cdouble(c, p1r, p1i, t1, t2)
    phr = sb("phr", [128, G4, 9]); phi = sb("phi", [128, G4, 9])
    c.op(V, lambda e: e.memset(phr[:, :, 0:1], 1.0), writes=[phr])
    c.op(V, lambda e: e.memset(phi[:, :, 0:1], 0.0), writes=[phi])
    for m in range(8):
        c.op(V, lambda e: e.tensor_tensor(out=t1[:], in0=phr[:, :, m], in1=p1r[:], op=ALU.mult), reads=[phr, p1r], writes=[t1])
        c.op(V, lambda e: e.tensor_tensor(out=t2[:], in0=phi[:, :, m], in1=p1i[:], op=ALU.mult), reads=[phi, p1i], writes=[t2])
        c.op(V, lambda e: e.tensor_tensor(out=phr[:, :, m + 1], in0=t1[:], in1=t2[:], op=ALU.subtract), reads=[t1, t2], writes=[phr])
        c.op(V, lambda e: e.tensor_tensor(out=t1[:], in0=phr[:, :, m], in1=p1i[:], op=ALU.mult), reads=[phr, p1i], writes=[t1])
        c.op(V, lambda e: e.tensor_tensor(out=t2[:], in0=phi[:, :, m], in1=p1r[:], op=ALU.mult), reads=[phi, p1r], writes=[t2])
        c.op(V, lambda e: e.tensor_tensor(out=phi[:, :, m + 1], in0=t1[:], in1=t2[:], op=ALU.add), reads=[t1, t2], writes=[phi])
    am = sb("am", [128, G4, 9]); magp = sb("magp", [128, G4, 9]); magn = sb("magn", [128, G4, 9])
    c.op(V, lambda e: e.tensor_tensor(out=am[:], in0=mv[:], in1=a[:].unsqueeze(2).to_broadcast([128, G4, 9]), op=ALU.mult), reads=[mv, a], writes=[am])
    c.op("act", lambda e: e.activation(out=magp[:], in_=am[:], func=AF.Exp), reads=[am], writes=[magp])
    c.op("act", lambda e: e.activation(out=magn[:], in_=am[:], func=AF.Exp, scale=-1.0), reads=[am], writes=[magn])
    LPr = sb("LPr", [128, G4, 9]); LPi = sb("LPi", [128, G4, 9]); LNr = sb("LNr", [128, G4, 9]); LNi = sb("LNi", [128, G4, 9])
    c.op(V, lambda e: e.tensor_tensor(out=LPr[:], in0=magp[:], in1=phr[:], op=ALU.mult), reads=[magp, phr], writes=[LPr])
    c.op(V, lambda e: e.tensor_tensor(out=LPi[:], in0=magp[:], in1=phi[:], op=ALU.mult), reads=[magp, phi], writes=[LPi])
    c.op(V, lambda e: e.tensor_tensor(out=LNr[:], in0=magn[:], in1=phr[:], op=ALU.mult), reads=[magn, phr], writes=[LNr])
    c.op(V, lambda e: e.scalar_tensor_tensor(out=LNi[:], in0=magn[:], scalar=-1.0, in1=phi[:], op0=ALU.mult, op1=ALU.mult), reads=[magn, phi], writes=[LNi])
    kr = sb("kr", [128, G4]); kim = sb("kim", [128, G4]); nr = sb("nr", [128, G4]); den = sb("den", [128, G4])
    c.op(V, lambda e: e.tensor_scalar(nr[:], LPr[:, :, 1], -1.0, None, op0=ALU.add), reads=[LPr], writes=[nr])
    c.op(V, lambda e: e.tensor_tensor(out=t1[:], in0=lam[:, :, 0], in1=lam[:, :, 0], op=ALU.mult), reads=[lam], writes=[t1])
    c.op(V, lambda e: e.tensor_tensor(out=t2[:], in0=lam[:, :, 1], in1=lam[:, :, 1], op=ALU.mult), reads=[lam], writes=[t2])
    c.op(V, lambda e: e.tensor_tensor(out=den[:], in0=t1[:], in1=t2[:], op=ALU.add), reads=[t1, t2], writes=[den])
    c.op(V, lambda e: e.reciprocal(den[:], den[:]), reads=[den], writes=[den])
    c.op(V, lambda e: e.tensor_tensor(out=t1[:], in0=nr[:], in1=lam[:, :, 0], op=ALU.mult), reads=[nr, lam], writes=[t1])
    c.op(V, lambda e: e.tensor_tensor(out=t2[:], in0=LPi[:, :, 1], in1=lam[:, :, 1], op=ALU.mult), reads=[LPi, lam], writes=[t2])
    c.op(V, lambda e: e.tensor_tensor(out=kr[:], in0=t1[:], in1=t2[:], op=ALU.add), reads=[t1, t2], writes=[kr])
    c.op(V, lambda e: e.tensor_tensor(out=kr[:], in0=kr[:], in1=den[:], op=ALU.mult), reads=[kr, den], writes=[kr])
    c.op(V, lambda e: e.tensor_tensor(out=t1[:], in0=LPi[:, :, 1], in1=lam[:, :, 0], op=ALU.mult), reads=[LPi, lam, kr], writes=[t1])
    c.op(V, lambda e: e.tensor_tensor(out=t2[:], in0=nr[:], in1=lam[:, :, 1], op=ALU.mult), reads=[nr, lam, kr], writes=[t2])
    c.op(V, lambda e: e.tensor_tensor(out=kim[:], in0=t1[:], in1=t2[:], op=ALU.subtract), reads=[t1, t2], writes=[kim])
    c.op(V, lambda e: e.tensor_tensor(out=kim[:], in0=kim[:], in1=den[:], op=ALU.mult), reads=[kim, den], writes=[kim])
    Bbr = sb("Bbr", [128, G4, 16]); Bbi = sb("Bbi", [128, G4, 16]); tb = sb("tb", [128, G4, 16])
    krb = kr[:].unsqueeze(2).to_broadcast([128, G4, 16]); kib = kim[:].unsqueeze(2).to_broadcast([128, G4, 16])
    c.op(V, lambda e: e.tensor_tensor(out=Bbr[:], in0=Bm[:, :, 0, :], in1=krb, op=ALU.mult), reads=[Bm, kr], writes=[Bbr])
    c.op(V, lambda e: e.tensor_tensor(out=tb[:], in0=Bm[:, :, 1, :], in1=kib, op=ALU.mult), reads=[Bm, kim], writes=[tb])
    c.op(V, lambda e: e.tensor_tensor(out=Bbr[:], in0=Bbr[:], in1=tb[:], op=ALU.subtract), reads=[Bbr, tb], writes=[Bbr])
    c.op(V, lambda e: e.tensor_tensor(out=Bbi[:], in0=Bm[:, :, 1, :], in1=krb, op=ALU.mult), reads=[Bm, kr], writes=[Bbi])
    c.op(V, lambda e: e.tensor_tensor(out=tb[:], in0=Bm[:, :, 0, :], in1=kib, op=ALU.mult), reads=[Bm, kim, Bbr], writes=[tb])
    c.op(V, lambda e: e.tensor_tensor(out=Bbi[:], in0=Bbi[:], in1=tb[:], op=ALU.add), reads=[Bbi, tb], writes=[Bbi])
    X1 = sb("X1", [128, G4, 16]); X2 = sb("X2", [128, G4, 16]); C1 = sb("C1", [128, G4, 16]); C2 = sb("C2", [128, G4, 16])
    c.op(V, lambda e: e.tensor_copy(X1[0:64], Bbr[0:64]), reads=[Bbr], writes=[X1])
    c.op(V, lambda e: e.tensor_copy(X1[64:128], Bbi[64:128]), reads=[Bbi], writes=[X1])
    c.op(V, lambda e: e.tensor_scalar(X2[0:64], Bbi[0:64], -1.0, None, op0=ALU.mult), reads=[Bbi], writes=[X2])
    c.op(V, lambda e: e.tensor_copy(X2[64:128], Bbr[64:128]), reads=[Bbr], writes=[X2])
    c.op(V, lambda e: e.tensor_copy(C1[0:64], Cm[0:64, :, 0, :]), reads=[Cm], writes=[C1])
    c.op(V, lambda e: e.tensor_scalar(C1[64:128], Cm[64:128, :, 1, :], -1.0, None, op0=ALU.mult), reads=[Cm], writes=[C1])
    c.op(V, lambda e: e.tensor_scalar(C2[0:64], Cm[0:64, :, 1, :], -1.0, None, op0=ALU.mult), reads=[Cm], writes=[C2])
    c.op(V, lambda e: e.tensor_scalar(C2[64:128], Cm[64:128, :, 0, :], -1.0, None, op0=ALU.mult), reads=[Cm], writes=[C2])
    Z = sb("Z", [128, G4, 8, 16]); Y = sb("Y", [128, G4, 8, 16]); Wc = sb("Wc", [128, G4, 8, 16]); tz = sb("tz", [128, 16])
    for g in range(G4):
        for j in range(8):
            for (OUT, Lr_, Li_, mi, A1, A2) in ((Z, LPr, LPi, 7 - j, X1, X2), (Y, LNr, LNi, j + 1, X1, X2), (Wc, LPr, LPi, j + 1, C1, C2)):
                c.op(V, lambda e: e.tensor_scalar(tz[:], A1[:, g, :], Lr_[:, g, mi:mi + 1], None, op0=ALU.mult), reads=[A1, Lr_], writes=[tz])
                c.op(V, lambda e: e.scalar_tensor_tensor(out=OUT[:, g, j, :], in0=A2[:, g, :], scalar=Li_[:, g, mi:mi + 1], in1=tz[:],
                                                         op0=ALU.mult, op1=ALU.add), reads=[A2, Li_, tz], writes=[OUT])
    ps_rot = Rot([c.ps(f"ps{i}", [128, 512], F32) for i in range(7)])
    Toep = sb("Toep", [128, G4, 128], BF16); W1 = sb("W1", [128, G4, 128], BF16); tf = sb("tf", [128, 128])
    for g in range(G4):
        ps = ps_rot.next()
        c.op("pe", lambda e: e.matmul(ps[:, 0:128], lhsT=Y[:, g].rearrange("p j h -> p (j h)"), rhs=Wc[:, g].rearrange("p j h -> p (j h)"),
                                      start=True, stop=True), reads=[Y, Wc], writes=[ps])
        c.op(V, lambda e: e.tensor_tensor(out=tf[:], in0=ps[:, 0:128], in1=tmask[:].rearrange("p j h -> p (j h)"), op=ALU.mult),
             reads=[ps, tmask], writes=[tf])
        c.op(V, lambda e: e.scalar_tensor_tensor(out=Toep[:, g, :], in0=ident[:], scalar=Dm[:, g:g + 1], in1=tf[:], op0=ALU.mult, op1=ALU.add),
             reads=[ident, Dm, tf], writes=[Toep])
        ps = ps_rot.next()
        c.op("pe", lambda e: e.transpose(ps[:, 0:128], Z[:, g].rearrange("p j h -> p (j h)"), ident[:]), reads=[Z, ident], writes=[ps])
        c.op("act", lambda e: e.copy(out=W1[:, g, :], in_=ps[:, 0:128]), reads=[ps], writes=[W1])
    R = sb("R", [128, G4, NLEV, 128])
    qr = sb("qr", [128, G4]); qi = sb("qi", [128, G4]); mg = sb("mg", [128, G4]); s1 = sb("s1", [128, G4]); s2 = sb("s2", [128, G4])
    c.op(V, lambda e: e.tensor_copy(qr[:], phr[:, :, 8]), reads=[phr], writes=[qr])
    c.op(V, lambda e: e.tensor_copy(qi[:], phi[:, :, 8]), reads=[phi], writes=[qi])
    for k in range(NLEV):
        c.op("act", lambda e: e.activation(out=mg[:], in_=a[:], func=AF.Exp, scale=float(8 * (2 ** k))), reads=[a], writes=[mg])
        c.op(V, lambda e: e.tensor_tensor(out=s1[:], in0=mg[:], in1=qr[:], op=ALU.mult), reads=[mg, qr], writes=[s1])
        c.op(V, lambda e: e.tensor_tensor(out=s2[:], in0=mg[:], in1=qi[:], op=ALU.mult), reads=[mg, qi], writes=[s2])
        c.op(V, lambda e: e.tensor_scalar(s2[:], s2[:], sgnh[:, 0:1], None, op0=ALU.mult), reads=[s2, sgnh], writes=[s2])
        for g in range(G4):
            c.op(V, lambda e: e.tensor_scalar(R[:, g, k, :], ident[:], s1[:, g:g + 1], None, op0=ALU.mult), reads=[ident, s1], writes=[R])
            c.op(V, lambda e: e.scalar_tensor_tensor(out=R[:, g, k, :], in0=pswap[:], scalar=s2[:, g:g + 1], in1=R[:, g, k, :],
                                                     op0=ALU.mult, op1=ALU.add), reads=[pswap, s2, R], writes=[R])
        if k < NLEV - 1:
            emit_cdouble(c, qr, qi, t1, t2)

    blocks = [(0, 512), (512, 512), (1024, NSB - 1024)]
    Ub = [sb(f"Ub{g}", [128, NSB], BF16) for g in range(G4)]
    Xp = [sb(f"Xp{g}", [128, NSB + 1]) for g in range(G4)]
    for g in range(G4):
        c.dma("pool", Ub[g][:], U_d.h.ap()[g], writes=[Ub[g]])
        c.op(V, lambda e: e.memset(Xp[g][:, 0:1], 0.0), writes=[Xp[g]])
        for (c0, n) in blocks:
            ps = ps_rot.next()
            c.op("pe", lambda e: e.matmul(ps[:, :n], lhsT=W1[:, g, :], rhs=Ub[g][:, c0:c0 + n], start=True, stop=True), reads=[W1, Ub[g]], writes=[ps])
            c.op("act", lambda e: e.copy(out=Xp[g][:, 1 + c0:1 + c0 + n], in_=ps[:, :n]), reads=[ps], writes=[Xp[g]])
    for k in range(NLEV):
        d = 2 ** k
        if d >= NSB:
            break
        L = NSB - d
        for g in range(G4):
            pl = []
            cc = 0
            while cc < L:
                n = min(512, L - cc)
                ps = ps_rot.next()
                c.op("pe", lambda e: e.matmul(ps[:, :n], lhsT=R[:, g, k, :], rhs=Xp[g][:, 1 + cc:1 + cc + n], start=True, stop=True),
                     reads=[R, Xp[g]], writes=[ps])
                pl.append((ps, cc, n))
                cc += n
            for (ps, cc, n) in pl:
                c.op(V, lambda e: e.tensor_tensor(out=Xp[g][:, 1 + d + cc:1 + d + cc + n], in0=Xp[g][:, 1 + d + cc:1 + d + cc + n], in1=ps[:, :n], op=ALU.add),
                     reads=[ps, Xp[g]], writes=[Xp[g]])
    y_rot = Rot([sb(f"ysb{i}", [128, NSB]) for i in range(2)])
    for g in range(G4):
        ysb = y_rot.next()
        for (c0, n) in blocks:
            ps = ps_rot.next()
            c.op("pe", lambda e: e.matmul(ps[:, :n], lhsT=Toep[:, g, :], rhs=Ub[g][:, c0:c0 + n], start=True, stop=False), reads=[Toep, Ub[g]], writes=[ps])
            c.op("pe", lambda e: e.matmul(ps[:, :n], lhsT=Wc[:, g].rearrange("p j h -> p (j h)"), rhs=Xp[g][:, c0:c0 + n], start=False, stop=True),
                 reads=[Wc, Xp[g]], writes=[ps])
            c.op("act", lambda e: e.copy(out=ysb[:, c0:c0 + n], in_=ps[:, :n]), reads=[ps], writes=[ysb])
        c.dma("sp", y_d.h.ap()[g], ysb[:], reads=[ysb])
    c.finish()
    c.close()
    print("s5 instructions:", c.n_inst)
    return nc


NT = 2080
def core_tok_idx(c):
    b, r = divmod(c, 4)
    return b, np.concatenate([np.arange(32 * r, 32 * r + 32), 128 + np.arange(2048 * r, 2048 * (r + 1))])

def build_H0(x, meta):
    B = x.shape[0]
    H = np.zeros((B, 8320, 1024), np.float32)
    H[:, 112:128, :] = meta[None]
    H[:, 128:, :] = x
    return H

def shard_T(H):
    out = []
    for c in range(8):
        b, idx = core_tok_idx(c)
        out.append(np.ascontiguousarray(H[b, idx, :].T))
    return out

def unshard_T(lst, C):
    H = np.zeros((2, 8320, C), lst[0].dtype)
    for c in range(8):
        b, idx = core_tok_idx(c)
        H[b, idx, :] = lst[c].T
    return H

def swap_cols():
    base = np.arange(256).reshape(8, 2, 16)[:, ::-1, :].reshape(256)
    return base

def w_in_ext(w_in_l):
    sw = swap_cols()
    q_sw = w_in_l[:, 1280:1536][:, sw]
    k_sw = w_in_l[:, 1536:1792][:, sw]
    return np.ascontiguousarray(np.concatenate([w_in_l, q_sw, k_sw], axis=1))

def gvec(g):
    return np.ascontiguousarray(g.reshape(-1, 128).T)

def s5_inputs(inp, L, u_b, r):
    gs = [4 * r + g for g in range(4)]
    U = np.stack([u_b[:, 16 * G:16 * G + 16].reshape(1040, 8, 16).transpose(1, 2, 0).reshape(128, 1040) for G in gs])
    def dup(a):
        return np.concatenate([a, a], axis=0)
    lam = np.stack([dup(np.stack([inp['s5_lam_re'][L, G], inp['s5_lam_im'][L, G]], -1)) for G in gs], 1)
    ls = np.tile(inp['s5_log_step'][L, gs][None, :], (128, 1))
    Bm = np.stack([dup(np.stack([inp['s5_b_re'][L, G], inp['s5_b_im'][L, G]], 1)) for G in gs], 1)
    Cm = np.stack([dup(np.stack([inp['s5_c_re'][L, G].T, inp['s5_c_im'][L, G].T], 1)) for G in gs], 1)
    Dm = np.stack([np.tile(inp['s5_d'][L, G], 8) for G in gs], 1)
    f = lambda a: np.ascontiguousarray(a.astype(np.float32))
    return {"U": f(U), "lam": f(lam), "ls": f(ls), "Bm": f(Bm), "Cm": f(Cm), "Dm": f(Dm)}

def s5_unpack(y, out_b, r):
    for g in range(4):
        G = 4 * r + g
        out_b[:, 16 * G:16 * G + 16] = y[g].reshape(8, 16, 1040).transpose(2, 0, 1).reshape(8320, 16)

def s5_raw_ref(inp, L, u):
    Bn, T, _ = u.shape
    out = np.zeros((Bn, T, 256))
    for G in range(16):
        lam = inp['s5_lam_re'][L, G].astype(np.float64) + 1j * inp['s5_lam_im'][L, G]
        step = np.exp(np.float64(inp['s5_log_step'][L, G]))
        lb = np.exp(lam * step)
        Bc = inp['s5_b_re'][L, G].astype(np.float64) + 1j * inp['s5_b_im'][L, G]
        Cc = inp['s5_c_re'][L, G].astype(np.float64) + 1j * inp['s5_c_im'][L, G]
        Bb = ((lb - 1) / lam)[:, None] * Bc
        d = inp['s5_d'][L, G].astype(np.float64)
        for b in range(Bn):
            ug = u[b, :, 16 * G:16 * G + 16].astype(np.float64)
            bu = ug @ Bb.T
            S = np.zeros(64, complex)
            y = np.zeros((T, 16))
            CH = 64
            pw = lb[None, :] ** np.arange(1, CH + 1)[:, None]
            ipw = lb[None, :] ** (-np.arange(1, CH + 1)[:, None])
            for t0 in range(0, T, CH):
                blk = bu[t0:t0 + CH]
                st = pw * (S[None, :] + np.cumsum(blk * ipw, axis=0))
                y[t0:t0 + CH] = (st @ Cc.T).real
                S = st[-1]
            out[b, :, 16 * G:16 * G + 16] = y + d * ug
    return out


_PROGS = {}


def _prog(key, fn):
    if key not in _PROGS:
        _PROGS[key] = fn()
    return _PROGS[key]


def _run(nc, maps):
    res = run_bass_kernel_spmd(nc, maps, core_ids=list(range(8)))
    return res.results


def kernel(**inp):
    inp = {k: np.asarray(v) for k, v in inp.items()}
    f32 = lambda a: np.ascontiguousarray(a, dtype=np.float32)
    H = build_H0(inp['x'], inp['meta_tokens'])
    sw = swap_cols()
    for L in range(2):
        hs = shard_T(H)
        W = w_in_ext(inp['w_in'][L])
        g = gvec(inp['norm_mix_g'][L])
        rA = _run(_prog("A", build_A), [{"hT": hs[c], "g": g, "w": W} for c in range(8)])
        proj = unshard_T([r["projT"] for r in rA], NCOL_A)
        rq = proj[:, :, 1280:1536]; rk = proj[:, :, 1536:1792]; rv = proj[:, :, 1792:2304]; rg = proj[:, :, 2304:2816]
        rqs = proj[:, :, 2816:3072]; rks = proj[:, :, 3072:3328]
        maps = []
        for c in range(8):
            b, r = divmod(c, 4)
            hds = [2 * r, 2 * r + 1]
            maps.append({"q": f32(rq[b, :, 64 * r:64 * r + 64].T), "qsw": f32(rqs[b, :, 64 * r:64 * r + 64].T),
                         "k": f32(rk[b, :, 64 * r:64 * r + 64].T), "ksw": f32(rks[b, :, 64 * r:64 * r + 64].T),
                         "v": f32(rv[b, :, 128 * r:128 * r + 128]), "gate": f32(rg[b, :, 128 * r:128 * r + 128]),
                         "outg": f32(np.tile(inp['ret_out_g'][L][128 * r:128 * r + 128][None, :], (128, 1))),
                         "hpart": f32(np.repeat(np.array(hds, np.float32), 32)[:, None]),
                         "hsel": f32(np.tile(np.array(hds, np.float32)[None, :], (128, 1)))})
        rR = _run(_prog("ret", build_ret), maps)
        yc = np.zeros((2, 8320, 512), np.float32)
        for c in range(8):
            b, r = divmod(c, 4)
            yc[b, :, 128 * r:128 * r + 128] = rR[c]["y"]
        cwL = inp['hg_conv_w'][L]
        maps = []
        for c in range(8):
            b, r = divmod(c, 4)
            x3 = np.stack([proj[b, :, 256 + 256 * s + 64 * r: 256 + 256 * s + 64 * r + 64].T for s in range(3)])
            convw = np.stack([cwL[:, 256 * s + 64 * r: 256 * s + 64 * r + 64].T for s in range(3)])
            maps.append({"x3": f32(x3), "convw": f32(convw), "gate": f32(proj[b, :, 1024 + 64 * r:1024 + 64 * r + 64]),
                         "outg": f32(np.tile(inp['hg_out_g'][L][64 * r:64 * r + 64][None, :], (128, 1))),
                         "lbp": f32(inp['hg_lb_param'][:, 64 * r:64 * r + 64].T)})
        rHg = _run(_prog(("hg", L), lambda: build_hg(L)), maps)
        yb = np.zeros((2, 8320, 256), np.float32)
        for c in range(8):
            b, r = divmod(c, 4)
            yb[b, :, 64 * r:64 * r + 64] = rHg[c]["y"]
        maps = []
        for c in range(8):
            b, r = divmod(c, 4)
            maps.append(s5_inputs(inp, L, proj[b, :, 0:256], r))
        rS = _run(_prog("s5", build_s5), maps)
        ya = np.zeros((2, 8320, 256), np.float32)
        for c in range(8):
            b, r = divmod(c, 4)
            s5_unpack(rS[c]["y"], ya[b], r)
        yas = shard_T(ya); ybcs = shard_T(np.concatenate([yb, yc], -1))
        moe = (L % 2 == 1)
        final = (L == 1)
        if not moe:
            w1 = f32(inp['ffn_w1'][L // 2][None]); w3 = f32(inp['ffn_w3'][L // 2][None]); w2 = f32(inp['ffn_w2'][L // 2][None])
            nc = _prog(("C", 1, final), lambda: build_C(1, 2816, final))
        else:
            w1 = f32(inp['moe_w1'][L // 2]); w3 = f32(inp['moe_w3'][L // 2]); w2 = f32(inp['moe_w2'][L // 2])
            nc = _prog(("C", 8, final), lambda: build_C(8, 3584, final))
        maps = []
        for c in range(8):
            m = {"hT": hs[c], "yaT": yas[c], "ybcT": ybcs[c], "wglu": f32(inp['s5_w_glu'][L]), "s5g": gvec(inp['s5_out_g'][L]),
                 "wout": f32(inp['w_out'][L]), "gffn": gvec(inp['norm_ffn_g'][L]), "w1": w1, "w3": w3, "w2": w2}
            if moe:
                m["router"] = f32(inp['moe_router'][L // 2])
            if final:
                m["gfin"] = gvec(inp['final_norm_g'])
            maps.append(m)
        rC = _run(nc, maps)
        H = unshard_T([r["hT_out"] for r in rC], 1024)
    return np.ascontiguousarray(H[:, 128:, :], dtype=np.float32)
```

```python
import math
import contextlib
import numpy as np
import concourse.bass as bass
import concourse.mybir as mybir
from concourse.bass_utils import run_bass_kernel_spmd


F32 = mybir.dt.float32
BF16 = mybir.dt.bfloat16
I32 = mybir.dt.int32
AF = mybir.ActivationFunctionType
ALU = mybir.AluOpType
AX = mybir.AxisListType


class T:
    def __init__(self, ctx, name, handle, space):
        self.ctx = ctx
        self.name = name
        self.h = handle
        self.space = space
        self.st = {}
        self.dq = None

    def __getitem__(self, idx):
        return self.h[idx]

    def state(self, key):
        s = self.st.get(key)
        if s is None:
            s = {"w": None, "r": {}}
            self.st[key] = s
        return s


class Q:
    def __init__(self, ctx, name, step):
        self.ctx = ctx
        self.name = name
        self.sem = ctx.root.enter_context(ctx.nc.semaphore(name))
        self.count = 0
        self.step = step


class Ctx:
    def __init__(self, nc):
        self.nc = nc
        self.stack = contextlib.ExitStack()
        self.root = self.stack
        self.engs = {}
        for name, eng in (("pe", nc.tensor), ("dve", nc.vector), ("act", nc.scalar),
                          ("pool", nc.gpsimd), ("sp", nc.sync)):
            q = Q(self, "q_" + name, 1)
            self.engs[name] = (eng, q)
        self.seen = {name: {} for name in self.engs}
        self.dmaq = {}
        self.n_inst = 0
        self.uid = 0
        self.pe_self_sync = False

    def sb(self, name, shape, dtype):
        self.uid += 1
        h = self.stack.enter_context(self.nc.sbuf_tensor(f"{name}_{self.uid}", list(shape), dtype))
        return T(self, name, h, "sb")

    def ps(self, name, shape, dtype=F32):
        self.uid += 1
        h = self.stack.enter_context(self.nc.psum_tensor(f"{name}_{self.uid}", list(shape), dtype))
        return T(self, name, h, "ps")

    def dram(self, name, shape, dtype, kind):
        h = self.nc.dram_tensor(name, list(shape), dtype, kind=kind)
        return T(self, name, h, "dram")

    def dma_q(self, name):
        q = self.dmaq.get(name)
        if q is None:
            q = Q(self, "dq_" + name, 16)
            self.dmaq[name] = q
        return q

    def _need(self, engname, q, value):
        if q is None:
            return
        if engname == "pe" and q is self.engs["pe"][1] and not self.pe_self_sync:
            return
        seen = self.seen[engname]
        if q.step == 16:
            value = q.count
        if seen.get(q.name, 0) >= value:
            return
        eng = self.engs[engname][0]
        eng.wait_ge(q.sem, value)
        seen[q.name] = value

    def _deps(self, engname, reads, writes):
        for (t, key) in reads:
            s = t.state(key)
            if s["w"] is not None:
                self._need(engname, *s["w"])
        for (t, key) in writes:
            s = t.state(key)
            if s["w"] is not None:
                self._need(engname, *s["w"])
            for q, v in s["r"].values():
                self._need(engname, q, v)

    def _mark(self, q, value, reads, writes):
        for (t, key) in reads:
            s = t.state(key)
            s["r"][q.name] = (q, value)
        for (t, key) in writes:
            s = t.state(key)
            s["w"] = (q, value)
            s["r"] = {}

    @staticmethod
    def _norm(lst):
        out = []
        for x in lst:
            if isinstance(x, tuple):
                out.append(x)
            else:
                out.append((x, None))
        return out

    def op(self, engname, fn, reads=(), writes=()):
        reads = self._norm(reads)
        writes = self._norm(writes)
        eng, q = self.engs[engname]
        self._deps(engname, reads, writes)
        ins = fn(eng)
        q.count += 1
        ins.then_inc(q.sem, 1)
        self._mark(q, q.count, reads, writes)
        self.n_inst += 1
        return ins

    def dma(self, engname, out, in_, reads=(), writes=(), qt=None, **kw):
        reads = self._norm(reads)
        writes = self._norm(writes)
        eng, _ = self.engs[engname]
        self._deps(engname, reads, writes)
        if qt is None:
            cands = [t for (t, k) in writes if t.space == "sb"] + [t for (t, k) in reads if t.space == "sb"]
            qt = cands[0]
        if qt.dq is None:
            self.uid += 1
            qt.dq = Q(self, f"dq_{qt.name}_{self.uid}", 16)
            self.dmaq[qt.dq.name] = qt.dq
        dq = qt.dq
        ins = eng.dma_start(out=out, in_=in_, **kw)
        dq.count += 16
        ins.then_inc(dq.sem, 16)
        self._mark(dq, dq.count, reads, writes)
        self.n_inst += 1
        return ins

    def barrier(self):
        for name, (eng, q0) in self.engs.items():
            for dq in self.dmaq.values():
                if dq.count:
                    self._need(name, dq, dq.count)
            for other, (e2, q2) in self.engs.items():
                if other != name and q2.count:
                    self._need(name, q2, q2.count)

    @contextlib.contextmanager
    def scope(self):
        old = self.stack
        self.stack = contextlib.ExitStack()
        try:
            yield
        finally:
            self.barrier()
            self.stack.close()
            self.stack = old

    def finish(self, engname="sp"):
        eng = self.engs[engname][0]
        for dq in self.dmaq.values():
            if dq.count:
                eng.wait_ge(dq.sem, dq.count)
        for name, (e, q) in self.engs.items():
            if q.count and name != engname:
                eng.wait_ge(q.sem, q.count)

    def close(self):
        self.stack.close()


NT = 2080
D = 1024
KC = 8
EPS = 1e-6
NCOL_A = 3328


def tblocks(nt=NT, bs=512):
    if nt == 2080 and bs == 512:
        return [(416 * i, 416) for i in range(5)]
    out = []
    t = 0
    while t < nt:
        n = min(bs, nt - t)
        out.append((t, n))
        t += n
    return out


class Rot:
    def __init__(self, tiles):
        self.tiles = tiles
        self.i = 0

    def next(self):
        t = self.tiles[self.i % len(self.tiles)]
        self.i += 1
        return t


def emit_consts(c):
    k = {}
    k["ones_bf"] = c.sb("ones_bf", [128, 128], BF16)
    c.op("pool", lambda e: e.memset(k["ones_bf"][:], 1.0), writes=[k["ones_bf"]])
    return k


def emit_rmsnorm(c, k, hT, g_sb, hnT, sq_rot, rs_rot, ps_rot, d_chunks=KC, dim=D, src_key=True):
    for bi, (t0, n) in enumerate(tblocks()):
        sq = sq_rot.next()
        c.op("act", lambda e: e.activation(out=sq[:, :d_chunks, :n], in_=hT[:, :, t0:t0 + n], func=AF.Square),
             reads=[(hT, bi)], writes=[sq])
        ps = ps_rot.next()
        for kc in range(d_chunks):
            c.op("pe", lambda e: e.matmul(ps[:, :n], lhsT=k["ones_bf"][:], rhs=sq[:, kc, :n],
                                          start=(kc == 0), stop=(kc == d_chunks - 1)),
                 reads=[sq, k["ones_bf"]], writes=[ps])
        rs = rs_rot.next()
        c.op("act", lambda e: e.activation(out=rs[:, :n], in_=ps[:, :n], func=AF.Sqrt, scale=1.0 / dim, bias=k["eps"][:, 0:1]),
             reads=[ps, k["eps"]], writes=[rs])
        c.op("dve", lambda e: e.reciprocal(rs[:, :n], rs[:, :n]), reads=[rs], writes=[rs])
        for kc in range(d_chunks):
            c.op("dve", lambda e: e.scalar_tensor_tensor(out=hnT[:, kc, t0:t0 + n], in0=hT[:, kc, t0:t0 + n],
                                                         scalar=g_sb[:, kc:kc + 1], in1=rs[:, :n],
                                                         op0=ALU.mult, op1=ALU.mult),
                 reads=[(hT, bi), rs, g_sb], writes=[(hnT, bi)])


def build_A():
    nc = bass.Bass("TRN2", target_bir_lowering=False)
    c = Ctx(nc)
    hT_d = c.dram("hT", [D, NT], F32, "ExternalInput")
    g_d = c.dram("g", [128, KC], F32, "ExternalInput")
    w_d = c.dram("w", [D, NCOL_A], F32, "ExternalInput")
    out_d = c.dram("projT", [NCOL_A, NT], F32, "ExternalOutput")

    k = emit_consts(c)
    k["eps"] = c.sb("eps", [128, 1], F32)
    c.op("pool", lambda e: e.memset(k["eps"][:], EPS), writes=[k["eps"]])
    hT = c.sb("hT", [128, KC, NT], F32)
    hnT = c.sb("hnT", [128, KC, NT], BF16)
    g_sb = c.sb("g", [128, KC], F32)
    c.dma("sp", g_sb[:], g_d[:], writes=[g_sb])
    hT_v = hT_d.h.ap().rearrange("(kc kp) t -> kp kc t", kp=128)
    for bi, (t0, n) in enumerate(tblocks()):
        c.dma("sp", hT[:, :, t0:t0 + n], hT_v[:, :, t0:t0 + n], writes=[(hT, bi)])
    sq_rot = Rot([c.sb(f"sq{i}", [128, KC, 512], BF16) for i in range(2)])
    rs_rot = Rot([c.sb(f"rs{i}", [128, 512], F32) for i in range(2)])
    ps_rot = Rot([c.ps(f"ps{i}", [128, 512], F32) for i in range(6)])
    emit_rmsnorm(c, k, hT, g_sb, hnT, sq_rot, rs_rot, ps_rot)

    w_rot = Rot([c.sb(f"w{i}", [128, KC, 512], BF16) for i in range(2)])
    w_st = c.sb("w_st", [128, KC, 512], F32)
    ost_rot = Rot([c.sb(f"ost{i}", [128, NT], F32) for i in range(3)])
    w_v = w_d.h.ap().rearrange("(kc kp) n -> kp kc n", kp=128)
    ev = 0
    for c0 in range(0, NCOL_A, 512):
        ncol = min(512, NCOL_A - c0)
        w_sb = w_rot.next()
        c.dma("sp", w_st[:, :, :ncol], w_v[:, :, c0:c0 + ncol], writes=[w_st])
        c.op("pool", lambda e: e.tensor_copy(w_sb[:, :, :ncol], w_st[:, :, :ncol]), reads=[w_st], writes=[w_sb])
        for cc in range(ncol // 128):
            ost = ost_rot.next()
            for bi, (t0, n) in enumerate(tblocks()):
                ps = ps_rot.next()
                for kc in range(KC):
                    c.op("pe", lambda e: e.matmul(ps[:, :n], lhsT=w_sb[:, kc, cc * 128:(cc + 1) * 128],
                                                  rhs=hnT[:, kc, t0:t0 + n], start=(kc == 0), stop=(kc == KC - 1)),
                         reads=[w_sb, (hnT, bi)], writes=[ps])
                if ev % 2 == 0:
                    c.op("act", lambda e: e.copy(out=ost[:, t0:t0 + n], in_=ps[:, :n]), reads=[ps], writes=[ost])
                else:
                    c.op("dve", lambda e: e.tensor_copy(ost[:, t0:t0 + n], ps[:, :n]), reads=[ps], writes=[ost])
                ev += 1
            r0 = c0 + cc * 128
            c.dma("sp", out_d[r0:r0 + 128, :], ost[:], reads=[ost])
    c.finish()
    c.close()
    print("phase A instructions:", c.n_inst)
    return nc


GF = 2
GH = 2


def emit_rmsnorm2(c, k, src, g_sb, dst_fn, sq_rot, rs_rot, ps_rot, d_chunks, dim, src_keyed=True):
    for bi, (t0, n) in enumerate(tblocks()):
        sk = (src, bi) if src_keyed else src
        sq = sq_rot.next()
        c.op("act", lambda e: e.activation(out=sq[:, :d_chunks, :n], in_=src[:, :, t0:t0 + n], func=AF.Square),
             reads=[sk], writes=[sq])
        ps = ps_rot.next()
        for kc in range(d_chunks):
            c.op("pe", lambda e: e.matmul(ps[:, :n], lhsT=k["ones_bf"][:], rhs=sq[:, kc, :n],
                                          start=(kc == 0), stop=(kc == d_chunks - 1)),
                 reads=[sq, k["ones_bf"]], writes=[ps])
        rs = rs_rot.next()
        c.op("act", lambda e: e.activation(out=rs[:, :n], in_=ps[:, :n], func=AF.Sqrt, scale=1.0 / dim, bias=k["eps"][:, 0:1]),
             reads=[ps, k["eps"]], writes=[rs])
        c.op("dve", lambda e: e.reciprocal(rs[:, :n], rs[:, :n]), reads=[rs], writes=[rs])
        for kc in range(d_chunks):
            ap, wk = dst_fn(bi, t0, n, kc)
            c.op("dve", lambda e: e.scalar_tensor_tensor(out=ap, in0=src[:, kc, t0:t0 + n],
                                                         scalar=g_sb[:, kc:kc + 1], in1=rs[:, :n],
                                                         op0=ALU.mult, op1=ALU.mult),
                 reads=[sk, rs, g_sb], writes=[wk])


def build_C(n_exp, F, final):
    moe = n_exp > 1
    nc = bass.Bass("TRN2", target_bir_lowering=False)
    c = Ctx(nc)
    hT_d = c.dram("hT", [D, NT], F32, "ExternalInput")
    ya_d = c.dram("yaT", [256, NT], F32, "ExternalInput")
    ybc_d = c.dram("ybcT", [768, NT], F32, "ExternalInput")
    wglu_d = c.dram("wglu", [256, 256], F32, "ExternalInput")
    s5g_d = c.dram("s5g", [128, 2], F32, "ExternalInput")
    wout_d = c.dram("wout", [D, D], F32, "ExternalInput")
    gffn_d = c.dram("gffn", [128, KC], F32, "ExternalInput")
    w1_d = c.dram("w1", [n_exp, D, F], F32, "ExternalInput")
    w3_d = c.dram("w3", [n_exp, D, F], F32, "ExternalInput")
    w2_d = c.dram("w2", [n_exp, F, D], F32, "ExternalInput")
    if moe:
        rt_d = c.dram("router", [D, 8], F32, "ExternalInput")
    if final:
        gfin_d = c.dram("gfin", [128, KC], F32, "ExternalInput")
    out_d = c.dram("hT_out", [D, NT], F32, "ExternalOutput")

    k = emit_consts(c)
    k["eps"] = c.sb("eps", [128, 1], F32)
    c.op("pool", lambda e: e.memset(k["eps"][:], EPS), writes=[k["eps"]])
    TB = tblocks()

    hT = c.sb("hT", [128, KC, NT], F32)
    hT_v = hT_d.h.ap().rearrange("(kc kp) t -> kp kc t", kp=128)
    for bi, (t0, n) in enumerate(TB):
        c.dma("sp", hT[:, :, t0:t0 + n], hT_v[:, :, t0:t0 + n], writes=[(hT, bi)], qt=hT)
    gffn = c.sb("gffn", [128, KC], F32)
    c.dma("sp", gffn[:], gffn_d[:], writes=[gffn])
    s5g = c.sb("s5g", [128, 2], F32)
    c.dma("sp", s5g[:], s5g_d[:], writes=[s5g])
    if final:
        gfin = c.sb("gfin", [128, KC], F32)
        c.dma("sp", gfin[:], gfin_d[:], writes=[gfin])

    ps_all = [c.ps(f"ps{i}", [128, 512], F32) for i in range(8)]
    ps_rot = Rot(ps_all[:7])

    with c.scope():
        sq_rot = Rot([c.sb(f"sq{i}", [128, KC, 512], BF16) for i in range(2)])
        rs_rot = Rot([c.sb(f"rs{i}", [128, 512], F32) for i in range(2)])
        yaT = c.sb("yaT", [128, 2, NT], F32)
        c.dma("sp", yaT[:], ya_d.h.ap().rearrange("(kc kp) t -> kp kc t", kp=128), writes=[yaT])
        mixedT = c.sb("mixedT", [128, KC, NT], BF16)
        ybc_v = ybc_d.h.ap().rearrange("(kc kp) t -> kp kc t", kp=128)
        for bi, (t0, n) in enumerate(TB):
            c.dma("pool", mixedT[:, 2:8, t0:t0 + n], ybc_v[:, :, t0:t0 + n], writes=[(mixedT, ("bc", bi))], qt=mixedT)
        wglu = c.sb("wglu", [128, 2, 256], BF16)
        c.dma("pool", wglu[:], wglu_d.h.ap().rearrange("(kc kp) n -> kp kc n", kp=128), writes=[wglu])
        wout = c.sb("wout", [128, KC, D], BF16)
        c.dma("pool", wout[:], wout_d.h.ap().rearrange("(kc kp) n -> kp kc n", kp=128), writes=[wout])

        t1_rot = Rot([c.sb(f"t1_{i}", [128, 2, 512], F32) for i in range(1)])
        t2_rot = Rot([c.sb(f"t2_{i}", [128, 2, 512], F32) for i in range(1)])
        ygf_rot = Rot([c.sb(f"ygf{i}", [128, 2, 512], F32) for i in range(1)])
        ygb_rot = Rot([c.sb(f"ygb{i}", [128, 2, 512], BF16) for i in range(2)])
        yaf_rot = Rot([c.sb(f"yaf{i}", [128, 2, 512], F32) for i in range(1)])
        sg_rot = Rot([c.sb(f"sg{i}", [128, 512], F32) for i in range(2)])
        for bi, (t0, n) in enumerate(TB):
            x = yaT[:, :, t0:t0 + n]
            t1 = t1_rot.next(); t2 = t2_rot.next(); ygf = ygf_rot.next(); ygb = ygb_rot.next(); yaf = yaf_rot.next()
            c.op("act", lambda e: e.activation(out=t1[:, :, :n], in_=x, func=AF.Square), reads=[yaT], writes=[t1])
            c.op("dve", lambda e: e.tensor_scalar(t1[:, :, :n], t1[:, :, :n], 0.044715, 1.0, op0=ALU.mult, op1=ALU.add),
                 reads=[t1], writes=[t1])
            c.op("dve", lambda e: e.tensor_tensor(out=t1[:, :, :n], in0=t1[:, :, :n], in1=x, op=ALU.mult),
                 reads=[t1, yaT], writes=[t1])
            c.op("act", lambda e: e.activation(out=t2[:, :, :n], in_=t1[:, :, :n], func=AF.Sigmoid, scale=1.5957691216057308),
                 reads=[t1], writes=[t2])
            c.op("dve", lambda e: e.tensor_tensor(out=ygf[:, :, :n], in0=t2[:, :, :n], in1=x, op=ALU.mult),
                 reads=[t2, yaT], writes=[ygf])
            c.op("act", lambda e: e.copy(out=ygb[:, :, :n], in_=ygf[:, :, :n]), reads=[ygf], writes=[ygb])
            for mo in range(2):
                ps = ps_rot.next()
                for ch in range(2):
                    c.op("pe", lambda e: e.matmul(ps[:, :n], lhsT=wglu[:, ch, mo * 128:(mo + 1) * 128], rhs=ygb[:, ch, :n],
                                                  start=(ch == 0), stop=(ch == 1)), reads=[wglu, ygb], writes=[ps])
                sg = sg_rot.next()
                c.op("act", lambda e: e.activation(out=sg[:, :n], in_=ps[:, :n], func=AF.Sigmoid), reads=[ps], writes=[sg])
                c.op("dve", lambda e: e.tensor_tensor(out=yaf[:, mo, :n], in0=ygf[:, mo, :n], in1=sg[:, :n], op=ALU.mult),
                     reads=[ygf, sg], writes=[yaf])
            sq = sq_rot.next()
            c.op("act", lambda e: e.activation(out=sq[:, :2, :n], in_=yaf[:, :, :n], func=AF.Square), reads=[yaf], writes=[sq])
            ps = ps_rot.next()
            for ch in range(2):
                c.op("pe", lambda e: e.matmul(ps[:, :n], lhsT=k["ones_bf"][:], rhs=sq[:, ch, :n], start=(ch == 0), stop=(ch == 1)),
                     reads=[sq, k["ones_bf"]], writes=[ps])
            rs = rs_rot.next()
            c.op("act", lambda e: e.activation(out=rs[:, :n], in_=ps[:, :n], func=AF.Sqrt, scale=1.0 / 256, bias=k["eps"][:, 0:1]),
                 reads=[ps, k["eps"]], writes=[rs])
            c.op("dve", lambda e: e.reciprocal(rs[:, :n], rs[:, :n]), reads=[rs], writes=[rs])
            for ch in range(2):
                c.op("dve", lambda e: e.scalar_tensor_tensor(out=mixedT[:, ch, t0:t0 + n], in0=yaf[:, ch, :n],
                                                             scalar=s5g[:, ch:ch + 1], in1=rs[:, :n], op0=ALU.mult, op1=ALU.mult),
                     reads=[yaf, rs, s5g], writes=[(mixedT, ("a", bi))])
            for dch in range(KC):
                ps = ps_rot.next()
                for cc in range(KC):
                    c.op("pe", lambda e: e.matmul(ps[:, :n], lhsT=wout[:, cc, dch * 128:(dch + 1) * 128], rhs=mixedT[:, cc, t0:t0 + n],
                                                  start=(cc == 0), stop=(cc == KC - 1)),
                         reads=[wout, (mixedT, ("a", bi)), (mixedT, ("bc", bi))], writes=[ps])
                c.op("dve", lambda e: e.tensor_tensor(out=hT[:, dch, t0:t0 + n], in0=hT[:, dch, t0:t0 + n], in1=ps[:, :n], op=ALU.add),
                     reads=[(hT, bi), ps], writes=[(hT, bi)])

    hnT = c.sb("hnT", [128, KC, NT], BF16)
    with c.scope():
        sq_rot = Rot([c.sb(f"sq{i}", [128, KC, 512], BF16) for i in range(2)])
        rs_rot = Rot([c.sb(f"rs{i}", [128, 512], F32) for i in range(2)])
        emit_rmsnorm2(c, k, hT, gffn, lambda bi, t0, n, kc: (hnT[:, kc, t0:t0 + n], (hnT, bi)), sq_rot, rs_rot, ps_rot, KC, D)

    if moe:
        gatesT = c.sb("gatesT", [8, NT], BF16)
        with c.scope():
            ident = c.sb("identf", [128, 128], F32)
            iof = c.sb("iof", [128, 128], F32)
            c.op("pool", lambda e: e.iota(iof[:], [[1, 128]], base=0, channel_multiplier=-1, allow_small_or_imprecise_dtypes=True), writes=[iof])
            c.op("dve", lambda e: e.tensor_single_scalar(ident[:], iof[:], 0.0, op=ALU.is_equal), reads=[iof], writes=[ident])
            rt = c.sb("rt", [128, KC, 8], F32)
            c.dma("sp", rt[:], rt_d.h.ap().rearrange("(kc kp) e -> kp kc e", kp=128), writes=[rt])
            gr = c.sb("gr", [128, KC, 16], F32)
            c.op("dve", lambda e: e.memset(gr[:], 0.0), writes=[gr])
            for kc in range(KC):
                c.op("dve", lambda e: e.tensor_scalar(gr[:, kc, 0:8], rt[:, kc, :], gffn[:, kc:kc + 1], None, op0=ALU.mult),
                     reads=[rt, gffn], writes=[gr])
            onesf = c.sb("onesf", [128, 1], F32)
            c.op("dve", lambda e: e.memset(onesf[:], 1.0), writes=[onesf])
            k["gatesT"] = gatesT
            sqf_rot = Rot([c.sb(f"sqf{i}", [128, KC, 128], F32) for i in range(2)])
            sm_rot = Rot([c.sb(f"sm{i}", [128, 64], F32) for i in range(3)])
            for ti, (t0, n) in enumerate(tblocks(NT, 128)):
                bi = t0 // 512
                sqf = sqf_rot.next()
                c.op("act", lambda e: e.activation(out=sqf[:, :, :n], in_=hT[:, :, t0:t0 + n], func=AF.Square), reads=[(hT, b_) for b_ in range(5)], writes=[sqf])
                ps = ps_all[7]
                for kc in range(KC):
                    c.op("pe", lambda e: e.matmul(ps[:n, 0:8], lhsT=hT[:, kc, t0:t0 + n], rhs=gr[:, kc, 0:8], start=(kc == 0), stop=(kc == KC - 1)),
                         reads=[(hT, b_) for b_ in range(5)] + [gr], writes=[ps])
                for kc in range(KC):
                    c.op("pe", lambda e: e.matmul(ps[:n, 8:9], lhsT=sqf[:, kc, :n], rhs=onesf[:, 0:1], start=(kc == 0), stop=(kc == KC - 1)),
                         reads=[sqf, onesf], writes=[ps])
                sm = sm_rot.next()
                c.op("act", lambda e: e.activation(out=sm[:n, 0:1], in_=ps[:n, 8:9], func=AF.Sqrt, scale=1.0 / D, bias=k["eps"][:n, 0:1]),
                     reads=[ps, k["eps"]], writes=[sm])
                c.op("dve", lambda e: e.reciprocal(sm[:n, 0:1], sm[:n, 0:1]), reads=[sm], writes=[sm])
                c.op("dve", lambda e: e.tensor_scalar(sm[:n, 8:16], ps[:n, 0:8], sm[:n, 0:1], None, op0=ALU.mult), reads=[ps, sm], writes=[sm])
                c.op("dve", lambda e: e.max(out=sm[:n, 16:24], in_=sm[:n, 8:16]), reads=[sm], writes=[sm])
                c.op("dve", lambda e: e.tensor_tensor(out=sm[:n, 24:25], in0=sm[:n, 17:18], in1=sm[:n, 16:17], op=ALU.subtract), reads=[sm], writes=[sm])
                c.op("act", lambda e: e.activation(out=sm[:n, 24:25], in_=sm[:n, 24:25], func=AF.Exp), reads=[sm], writes=[sm])
                c.op("dve", lambda e: e.tensor_scalar(sm[:n, 25:26], sm[:n, 24:25], 1.0, None, op0=ALU.add), reads=[sm], writes=[sm])
                c.op("dve", lambda e: e.reciprocal(sm[:n, 25:26], sm[:n, 25:26]), reads=[sm], writes=[sm])
                c.op("dve", lambda e: e.tensor_tensor(out=sm[:n, 26:27], in0=sm[:n, 24:25], in1=sm[:n, 25:26], op=ALU.mult), reads=[sm], writes=[sm])
                c.op("dve", lambda e: e.tensor_scalar(sm[:n, 32:40], sm[:n, 8:16], sm[:n, 16:17], sm[:n, 25:26], op0=ALU.is_equal, op1=ALU.mult), reads=[sm], writes=[sm])
                c.op("dve", lambda e: e.tensor_scalar(sm[:n, 40:48], sm[:n, 8:16], sm[:n, 17:18], sm[:n, 26:27], op0=ALU.is_equal, op1=ALU.mult), reads=[sm], writes=[sm])
                c.op("dve", lambda e: e.tensor_tensor(out=sm[:n, 48:56], in0=sm[:n, 32:40], in1=sm[:n, 40:48], op=ALU.add), reads=[sm], writes=[sm])
                c.op("pe", lambda e: e.transpose(ps[0:8, 16:16 + n], sm[:n, 48:56], ident[:n, :n]), reads=[sm, ident], writes=[ps])
                c.op("act", lambda e: e.copy(out=gatesT[:, t0:t0 + n], in_=ps[0:8, 16:16 + n]), reads=[ps], writes=[gatesT])
        sel = c.sb("sel", [8, 8, 128], BF16)
        self_f = c.sb("sel_f", [8, 8, 128], F32)
        c.op("pool", lambda e: e.iota(self_f[:], [[-1, 8], [0, 128]], base=0, channel_multiplier=1, allow_small_or_imprecise_dtypes=True), writes=[self_f])
        c.op("dve", lambda e: e.tensor_single_scalar(sel[:], self_f[:], 0.0, op=ALU.is_equal), reads=[self_f], writes=[sel])

    with c.scope():
        ps_a = Rot(ps_all[0:2]); ps_b = Rot(ps_all[2:4]); ps_o = Rot(ps_all[4:7]); ps_g = Rot(ps_all[7:8])
        NWB = 3
        w1_rot = Rot([c.sb(f"w1g{i}", [128, KC, GF * 128], BF16) for i in range(NWB)])
        w3_rot = Rot([c.sb(f"w3g{i}", [128, KC, GF * 128], BF16) for i in range(NWB)])
        w2_rot = Rot([c.sb(f"w2g{i}", [128, GF, D], BF16) for i in range(NWB)])
        w1s = c.sb("w1s", [128, KC, GH * 128], F32)
        w3s = c.sb("w3s", [128, KC, GH * 128], F32)
        w2s = c.sb("w2s", [128, GF, D], F32)
        sa_rot = Rot([c.sb(f"sa{i}", [128, 512], F32) for i in range(2)])
        gT_rot = Rot([c.sb(f"gT{i}", [128, GF, 512], BF16) for i in range(3)])
        ngrp = F // (GF * 128)
        groups = [(ex, gi) for ex in range(n_exp) for gi in range(ngrp)]
        wbuf = {}

        def load_dma(gidx):
            ex, gi = groups[gidx]
            w1_v = w1_d.h.ap()[ex].rearrange("(kc kp) f -> kp kc f", kp=128)
            w3_v = w3_d.h.ap()[ex].rearrange("(kc kp) f -> kp kc f", kp=128)
            w2_v = w2_d.h.ap()[ex].rearrange("(fc fp) d -> fp fc d", fp=128)
            f0 = gi * GF * 128
            c.dma("sp", w1s[:], w1_v[:, :, f0:f0 + GF * 128], writes=[w1s])
            c.dma("sp", w3s[:], w3_v[:, :, f0:f0 + GF * 128], writes=[w3s])
            c.dma("sp", w2s[:], w2_v[:, gi * GF:(gi + 1) * GF, :], writes=[w2s])

        def load_cast(gidx):
            w1g = w1_rot.next(); w3g = w3_rot.next(); w2g = w2_rot.next()
            c.op("act", lambda e: e.copy(out=w1g[:], in_=w1s[:]), reads=[w1s], writes=[w1g])
            c.op("act", lambda e: e.copy(out=w3g[:], in_=w3s[:]), reads=[w3s], writes=[w3g])
            c.op("act", lambda e: e.copy(out=w2g[:], in_=w2s[:]), reads=[w2s], writes=[w2g])
            wbuf[gidx] = (w1g, w3g, w2g)

        def load_group(gidx):
            load_dma(gidx)
            load_cast(gidx)

        gsb_rot = Rot([c.sb(f"gsb{i}", [128, 512], BF16) for i in range(2)]) if moe else None
        sa2_rot = Rot([c.sb(f"sa2_{i}", [128, 512], F32) for i in range(2)]) if moe else None

        def stage1_fc(gidx, bi, t0, n, fc, gT, gsb):
            ex, gi = groups[gidx]
            w1g, w3g, w2g = wbuf[gidx]
            pa = ps_a.next(); pb = ps_b.next()
            for kc in range(KC):
                c.op("pe", lambda e: e.matmul(pa[:, :n], lhsT=w1g[:, kc, fc * 128:(fc + 1) * 128], rhs=hnT[:, kc, t0:t0 + n],
                                              start=(kc == 0), stop=(kc == KC - 1)), reads=[w1g, (hnT, bi)], writes=[pa])
            for kc in range(KC):
                c.op("pe", lambda e: e.matmul(pb[:, :n], lhsT=w3g[:, kc, fc * 128:(fc + 1) * 128], rhs=hnT[:, kc, t0:t0 + n],
                                              start=(kc == 0), stop=(kc == KC - 1)), reads=[w3g, (hnT, bi)], writes=[pb])
            sa = sa_rot.next()
            c.op("act", lambda e: e.activation(out=sa[:, :n], in_=pa[:, :n], func=AF.Silu), reads=[pa], writes=[sa])
            if moe:
                sa2 = sa2_rot.next()
                c.op("pool", lambda e: e.tensor_tensor(out=sa2[:, :n], in0=sa[:, :n], in1=gsb[:, :n], op=ALU.mult), reads=[sa, gsb], writes=[sa2])
                return (sa2, pb)
            return (sa, pb)

        def stage1_fc_b(n, fc, gT, st):
            sx, pb = st
            c.op("dve", lambda e: e.tensor_tensor(out=gT[:, fc, :n], in0=sx[:, :n], in1=pb[:, :n], op=ALU.mult), reads=[sx, pb], writes=[gT])

        def stage1_gate(gidx, bi, t0, n):
            ex, gi = groups[gidx]
            pg = ps_g.next()
            c.op("pe", lambda e: e.matmul(pg[:, :n], lhsT=sel[:, ex, :], rhs=k["gatesT"][:, t0:t0 + n], start=True, stop=True),
                 reads=[sel, k["gatesT"]], writes=[pg])
            gsb = gsb_rot.next()
            c.op("act", lambda e: e.copy(out=gsb[:, :n], in_=pg[:, :n]), reads=[pg], writes=[gsb])
            return gsb

        def stage2_part(gidx, bi, t0, n, gT, d0, d1):
            w1g, w3g, w2g = wbuf[gidx]
            for dch in range(d0, d1):
                po = ps_o.next()
                for fc in range(GF):
                    c.op("pe", lambda e: e.matmul(po[:, :n], lhsT=w2g[:, fc, dch * 128:(dch + 1) * 128], rhs=gT[:, fc, :n],
                                                  start=(fc == 0), stop=(fc == GF - 1)), reads=[w2g, gT], writes=[po])
                c.op("dve", lambda e: e.tensor_tensor(out=hT[:, dch, t0:t0 + n], in0=hT[:, dch, t0:t0 + n], in1=po[:, :n], op=ALU.add),
                     reads=[(hT, bi), po], writes=[(hT, bi)])

        load_group(0)
        if len(groups) > 1:
            load_group(1)
        pending = None
        DS = KC // GF
        for gidx in range(len(groups)):
            for bi, (t0, n) in enumerate(TB):
                gsb = stage1_gate(gidx, bi, t0, n) if moe else None
                gT = gT_rot.next()
                for fc in range(GF):
                    st = stage1_fc(gidx, bi, t0, n, fc, gT, gsb)
                    if pending is not None:
                        stage2_part(*pending, fc * DS, (fc + 1) * DS)
                    stage1_fc_b(n, fc, gT, st)
                pending = (gidx, bi, t0, n, gT)
                if bi == 0 and gidx + 2 < len(groups):
                    load_dma(gidx + 2)
                if bi == 3 and gidx + 2 < len(groups):
                    load_cast(gidx + 2)
        stage2_part(*pending, 0, KC)

    out_v = out_d.h.ap().rearrange("(kc kp) t -> kp kc t", kp=128)
    if final:
        sq_rot = Rot([c.sb(f"sq{i}", [128, KC, 512], BF16) for i in range(2)])
        rs_rot = Rot([c.sb(f"rs{i}", [128, 512], F32) for i in range(2)])
        fo_rot = Rot([c.sb(f"fo{i}", [128, KC, 512], F32) for i in range(2)])
        cur = {}

        def dst(bi, t0, n, kc):
            if kc == 0:
                cur["t"] = fo_rot.next()
            return cur["t"][:, kc, :n], cur["t"]
        for bi, (t0, n) in enumerate(TB):
            pass
        emit_final(c, k, hT, gfin, fo_rot, out_v, sq_rot, rs_rot, Rot(ps_all[0:6]))
    else:
        for bi, (t0, n) in enumerate(TB):
            c.dma("sp", out_v[:, :, t0:t0 + n], hT[:, :, t0:t0 + n], reads=[(hT, bi)], qt=hT)
    c.finish()
    c.close()
    print("phase C instructions:", c.n_inst)
    return nc


def emit_final(c, k, hT, gfin, fo_rot, out_v, sq_rot, rs_rot, ps_rot):
    for bi, (t0, n) in enumerate(tblocks()):
        sq = sq_rot.next()
        c.op("act", lambda e: e.activation(out=sq[:, :, :n], in_=hT[:, :, t0:t0 + n], func=AF.Square), reads=[(hT, bi)], writes=[sq])
        ps = ps_rot.next()
        for kc in range(KC):
            c.op("pe", lambda e: e.matmul(ps[:, :n], lhsT=k["ones_bf"][:], rhs=sq[:, kc, :n], start=(kc == 0), stop=(kc == KC - 1)),
                 reads=[sq, k["ones_bf"]], writes=[ps])
        rs = rs_rot.next()
        c.op("act", lambda e: e.activation(out=rs[:, :n], in_=ps[:, :n], func=AF.Sqrt, scale=1.0 / D, bias=k["eps"][:, 0:1]),
             reads=[ps, k["eps"]], writes=[rs])
        c.op("dve", lambda e: e.reciprocal(rs[:, :n], rs[:, :n]), reads=[rs], writes=[rs])
        fo = fo_rot.next()
        for kc in range(KC):
            c.op("dve", lambda e: e.scalar_tensor_tensor(out=fo[:, kc, :n], in0=hT[:, kc, t0:t0 + n], scalar=gfin[:, kc:kc + 1], in1=rs[:, :n],
                                                         op0=ALU.mult, op1=ALU.mult), reads=[(hT, bi), rs, gfin], writes=[fo])
        c.dma("sp", out_v[:, :, t0:t0 + n], fo[:, :, :n], reads=[fo])


NTOK = 8320
NCH = 65
EPS = 1e-6
CB = 5


def emit_pow_table(c, P, n, bT, br, bi, save_at=None):
    Gr = c.sb("Gr", [P, n], F32)
    Gi = c.sb("Gi", [P, n], F32)
    tmp = c.sb("Gtmp", [P, max(n // 2, 1)], F32)
    s = c.sb("Gs", [P, 6], F32)
    saved = c.sb("Gsaved", [P, 2], F32) if save_at else None
    V = "dve"
    c.op(V, lambda e: e.memset(Gr[:, 0:1], 1.0), writes=[Gr])
    c.op(V, lambda e: e.memset(Gi[:, 0:1], 0.0), writes=[Gi])
    c.op(V, lambda e: e.tensor_copy(s[:, 0:1], br), reads=[bT], writes=[s])
    c.op(V, lambda e: e.tensor_copy(s[:, 1:2], bi), reads=[bT], writes=[s])
    m = 1
    while m < n:
        if save_at == m:
            c.op(V, lambda e: e.tensor_copy(saved[:, 0:2], s[:, 0:2]), reads=[s], writes=[saved])
        c.op(V, lambda e: e.tensor_scalar(tmp[:, :m], Gi[:, :m], s[:, 1:2], None, op0=ALU.mult), reads=[Gi, s], writes=[tmp])
        c.op(V, lambda e: e.scalar_tensor_tensor(out=Gr[:, m:2 * m], in0=Gr[:, :m], scalar=s[:, 0:1], in1=tmp[:, :m],
                                                 op0=ALU.mult, op1=ALU.subtract), reads=[Gr, s, tmp], writes=[Gr])
        c.op(V, lambda e: e.tensor_scalar(tmp[:, :m], Gi[:, :m], s[:, 0:1], None, op0=ALU.mult), reads=[Gi, s], writes=[tmp])
        c.op(V, lambda e: e.scalar_tensor_tensor(out=Gi[:, m:2 * m], in0=Gr[:, :m], scalar=s[:, 1:2], in1=tmp[:, :m],
                                                 op0=ALU.mult, op1=ALU.add), reads=[Gr, s, tmp], writes=[Gi])
        c.op(V, lambda e: e.tensor_tensor(out=s[:, 2:3], in0=s[:, 0:1], in1=s[:, 0:1], op=ALU.mult), reads=[s], writes=[s])
        c.op(V, lambda e: e.tensor_tensor(out=s[:, 3:4], in0=s[:, 1:2], in1=s[:, 1:2], op=ALU.mult), reads=[s], writes=[s])
        c.op(V, lambda e: e.scalar_tensor_tensor(out=s[:, 1:2], in0=s[:, 0:1], scalar=2.0, in1=s[:, 1:2],
                                                 op0=ALU.mult, op1=ALU.mult), reads=[s], writes=[s])
        c.op(V, lambda e: e.tensor_tensor(out=s[:, 0:1], in0=s[:, 2:3], in1=s[:, 3:4], op=ALU.subtract), reads=[s], writes=[s])
        m *= 2
    if save_at == m:
        c.op(V, lambda e: e.tensor_copy(saved[:, 0:2], s[:, 0:2]), reads=[s], writes=[saved])
    return Gr, Gi, saved


def emit_sincos_small(c, P, wT, w, cs):
    V = "dve"
    x2 = cs[:, 2:3]
    acc = cs[:, 3:4]
    c.op(V, lambda e: e.tensor_tensor(out=x2, in0=w, in1=w, op=ALU.mult), reads=[wT], writes=[cs])
    c.op(V, lambda e: e.tensor_scalar(acc, x2, -1.0 / 110, 1.0, op0=ALU.mult, op1=ALU.add), reads=[cs], writes=[cs])
    for d in (72.0, 42.0, 20.0, 6.0):
        c.op(V, lambda e: e.tensor_tensor(out=acc, in0=acc, in1=x2, op=ALU.mult), reads=[cs], writes=[cs])
        c.op(V, lambda e: e.tensor_scalar(acc, acc, -1.0 / d, 1.0, op0=ALU.mult, op1=ALU.add), reads=[cs], writes=[cs])
    c.op(V, lambda e: e.tensor_tensor(out=cs[:, 1:2], in0=acc, in1=w, op=ALU.mult), reads=[cs, wT], writes=[cs])
    c.op(V, lambda e: e.tensor_scalar(acc, x2, -1.0 / 132, 1.0, op0=ALU.mult, op1=ALU.add), reads=[cs], writes=[cs])
    for d in (90.0, 56.0, 30.0, 12.0, 2.0):
        c.op(V, lambda e: e.tensor_tensor(out=acc, in0=acc, in1=x2, op=ALU.mult), reads=[cs], writes=[cs])
        c.op(V, lambda e: e.tensor_scalar(acc, acc, -1.0 / d, 1.0, op0=ALU.mult, op1=ALU.add), reads=[cs], writes=[cs])
    c.op(V, lambda e: e.tensor_copy(cs[:, 0:1], acc), reads=[cs], writes=[cs])


def emit_gamma(c, hT_, hidx_ap, out, P, ncol):
    LN2 = math.log(2.0)
    c.op("act", lambda e: e.activation(out=out[:, :ncol], in_=hidx_ap, func=AF.Exp, scale=-LN2, bias=c.k5[:P, 0:1]),
         reads=[c.k5, hT_], writes=[out])
    c.op("dve", lambda e: e.tensor_scalar(out[:, :ncol], out[:, :ncol], -1.0, 1.0, op0=ALU.mult, op1=ALU.add), reads=[out], writes=[out])
    c.op("act", lambda e: e.activation(out=out[:, :ncol], in_=out[:, :ncol], func=AF.Ln), reads=[out], writes=[out])


def build_ret(debug=False):
    nc = bass.Bass("TRN2", target_bir_lowering=False)
    c = Ctx(nc)
    c.pe_self_sync = True
    q_d = c.dram("q", [64, NTOK], F32, "ExternalInput")
    qs_d = c.dram("qsw", [64, NTOK], F32, "ExternalInput")
    k_d = c.dram("k", [64, NTOK], F32, "ExternalInput")
    ks_d = c.dram("ksw", [64, NTOK], F32, "ExternalInput")
    v_d = c.dram("v", [NTOK, 128], F32, "ExternalInput")
    g_d = c.dram("gate", [NTOK, 128], F32, "ExternalInput")
    og_d = c.dram("outg", [128, 128], F32, "ExternalInput")
    hp_d = c.dram("hpart", [64, 1], F32, "ExternalInput")
    hs_d = c.dram("hsel", [128, 2], F32, "ExternalInput")
    y_d = c.dram("y", [NTOK, 128], F32, "ExternalOutput")

    V = "dve"
    c.k5 = c.sb("k5", [128, 1], F32)
    c.op("pool", lambda e: e.memset(c.k5[:], -5.0 * math.log(2.0)), writes=[c.k5])
    epsT = c.sb("eps", [128, 1], F32)
    c.op("pool", lambda e: e.memset(epsT[:], EPS), writes=[epsT])
    identb = c.sb("identb", [128, 128], BF16)
    iof = c.sb("iof", [128, 128], F32)
    c.op("pool", lambda e: e.iota(iof[:], [[1, 128]], base=0, channel_multiplier=-1, allow_small_or_imprecise_dtypes=True), writes=[iof])
    c.op(V, lambda e: e.tensor_single_scalar(identb[:], iof[:], 0.0, op=ALU.is_equal), reads=[iof], writes=[identb])
    hp = c.sb("hp", [64, 1], F32)
    c.dma("sp", hp[:], hp_d[:], writes=[hp])
    hs = c.sb("hs", [128, 2], F32)
    c.dma("sp", hs[:], hs_d[:], writes=[hs])
    og = c.sb("og", [128, 128], F32)
    c.dma("sp", og[:], og_d[:], writes=[og])
    lgP = c.sb("lgP", [64, 2], F32)
    emit_gamma(c, hp, hp[:, 0:1], lgP, 64, 1)
    lgB = c.sb("lgB", [128, 2], F32)
    emit_gamma(c, hs, hs[:, 0:2], lgB, 128, 2)
    maskT = c.sb("maskT", [128, 2, 128], F32)
    dpos = c.sb("dpos", [128, 128], F32)
    dge = c.sb("dge", [128, 128], F32)
    c.op(V, lambda e: e.tensor_single_scalar(dpos[:], iof[:], 0.0, op=ALU.max), reads=[iof], writes=[dpos])
    c.op(V, lambda e: e.tensor_scalar(dge[:], iof[:], 0.0, 32.0 ** -0.5, op0=ALU.is_ge, op1=ALU.mult), reads=[iof], writes=[dge])
    for hl in range(2):
        c.op("act", lambda e: e.activation(out=maskT[:, hl, :], in_=dpos[:], func=AF.Exp, scale=lgB[:, hl:hl + 1]),
             reads=[dpos, lgB], writes=[maskT])
        c.op(V, lambda e: e.tensor_tensor(out=maskT[:, hl, :], in0=maskT[:, hl, :], in1=dge[:], op=ALU.mult), reads=[maskT, dge], writes=[maskT])
    io1 = c.sb("io1", [64, 128], F32)
    c.op("pool", lambda e: e.iota(io1[:], [[1, 128]], base=1, channel_multiplier=0, allow_small_or_imprecise_dtypes=True), writes=[io1])
    io2 = c.sb("io2", [64, 128], F32)
    c.op("pool", lambda e: e.iota(io2[:], [[-1, 128]], base=127, channel_multiplier=0, allow_small_or_imprecise_dtypes=True), writes=[io2])
    qdf = c.sb("qdf", [64, 128], F32)
    kdf = c.sb("kdf", [64, 128], F32)
    c.op("act", lambda e: e.activation(out=qdf[:], in_=io1[:], func=AF.Exp, scale=lgP[:, 0:1]), reads=[io1, lgP], writes=[qdf])
    c.op(V, lambda e: e.tensor_scalar(qdf[:], qdf[:], 32.0 ** -0.5, None, op0=ALU.mult), reads=[qdf], writes=[qdf])
    c.op("act", lambda e: e.activation(out=kdf[:], in_=io2[:], func=AF.Exp, scale=lgP[:, 0:1]), reads=[io2, lgP], writes=[kdf])
    sdec = c.sb("sdec", [64, 1], F32)
    c.op("act", lambda e: e.activation(out=sdec[:], in_=lgP[:, 0:1], func=AF.Exp, scale=128.0), reads=[lgP], writes=[sdec])
    fr = c.sb("fr", [64, 4], F32)
    for hb in range(2):
        c.op("pool", lambda e: e.iota(fr[32 * hb:32 * hb + 32, 0:1], [[0, 1]], base=0, channel_multiplier=1,
                                      allow_small_or_imprecise_dtypes=True), writes=[fr])
    c.op(V, lambda e: e.tensor_single_scalar(fr[:, 3:4], fr[:, 0:1], 16.0, op=ALU.is_ge), reads=[fr], writes=[fr])
    c.op(V, lambda e: e.scalar_tensor_tensor(out=fr[:, 0:1], in0=fr[:, 3:4], scalar=-16.0, in1=fr[:, 0:1], op0=ALU.mult, op1=ALU.add),
         reads=[fr], writes=[fr])
    c.op(V, lambda e: e.tensor_scalar(fr[:, 2:3], fr[:, 3:4], 2.0, -1.0, op0=ALU.mult, op1=ALU.add), reads=[fr], writes=[fr])
    c.op("act", lambda e: e.activation(out=fr[:, 1:2], in_=fr[:, 0:1], func=AF.Exp, scale=-math.log(10000.0) / 16.0), reads=[fr], writes=[fr])
    cs = c.sb("cs", [64, 4], F32)
    emit_sincos_small(c, 64, fr, fr[:, 1:2], cs)
    Gr, Gi, s128 = emit_pow_table(c, 64, 256, cs, cs[:, 0:1], cs[:, 1:2], save_at=128)
    Fr, Fi, _ = emit_pow_table(c, 64, 64, s128, s128[:, 0:1], s128[:, 1:2])
    E1r = c.sb("E1r", [64, NCH], F32)
    E1i = c.sb("E1i", [64, NCH], F32)
    c.op(V, lambda e: e.tensor_copy(E1r[:, 1:NCH], Fr[:, 0:64]), reads=[Fr], writes=[E1r])
    c.op(V, lambda e: e.tensor_copy(E1i[:, 1:NCH], Fi[:, 0:64]), reads=[Fi], writes=[E1i])
    c.op(V, lambda e: e.tensor_copy(E1r[:, 0:1], Fr[:, 1:2]), reads=[Fr], writes=[E1r])
    c.op(V, lambda e: e.tensor_scalar(E1i[:, 0:1], Fi[:, 1:2], -1.0, None, op0=ALU.mult), reads=[Fi], writes=[E1i])
    E2r = Gr
    E2i = Gi

    QR = c.sb("QR", [64, NTOK], BF16)
    KR = c.sb("KR", [64, NTOK], BF16)
    QD = c.sb("QD", [64, NTOK], BF16)
    KD = c.sb("KD", [64, NTOK], BF16)
    v_sb = c.sb("v_sb", [128, NCH, 128], BF16)
    g_sb = c.sb("g_sb", [128, NCH, 128], BF16)
    y_sb = c.sb("y_sb", [128, NCH, 128], F32)
    c.dma("pool", v_sb[:], v_d.h.ap().rearrange("(c p) f -> p c f", p=128), writes=[v_sb])
    c.dma("pool", g_sb[:], g_d.h.ap().rearrange("(c p) f -> p c f", p=128), writes=[g_sb])

    nblk = NCH // CB
    BW = CB * 128
    x_rot = Rot([c.sb(f"x{i}", [64, BW], F32) for i in range(2)])
    xs_rot = Rot([c.sb(f"xs{i}", [64, BW], F32) for i in range(2)])
    COSb = c.sb("COSb", [64, CB, 128], F32)
    SINb = c.sb("SINb", [64, CB, 128], F32)
    tb1 = c.sb("tb1", [64, CB, 128], F32)
    tb2 = c.sb("tb2", [64, CB, 128], F32)
    r1 = c.sb("r1", [64, BW], F32)
    r2 = c.sb("r2", [64, BW], F32)
    for b in range(nblk):
        c0 = b * CB
        t0 = c0 * 128
        e1r = E1r[:, c0:c0 + CB].unsqueeze(2).to_broadcast([64, CB, 128])
        e1i = E1i[:, c0:c0 + CB].unsqueeze(2).to_broadcast([64, CB, 128])
        e2r = E2r[:, 16:144].unsqueeze(1).to_broadcast([64, CB, 128])
        e2i = E2i[:, 16:144].unsqueeze(1).to_broadcast([64, CB, 128])
        P_ = "pool"
        c.op(P_, lambda e: e.tensor_tensor(out=tb1[:], in0=e1r, in1=e2r, op=ALU.mult), reads=[E1r, E2r], writes=[tb1])
        c.op(P_, lambda e: e.tensor_tensor(out=tb2[:], in0=e1i, in1=e2i, op=ALU.mult), reads=[E1i, E2i], writes=[tb2])
        c.op(P_, lambda e: e.tensor_tensor(out=COSb[:], in0=tb1[:], in1=tb2[:], op=ALU.subtract), reads=[tb1, tb2], writes=[COSb])
        c.op(P_, lambda e: e.tensor_tensor(out=tb1[:], in0=e1r, in1=e2i, op=ALU.mult), reads=[E1r, E2i], writes=[tb1])
        c.op(P_, lambda e: e.tensor_tensor(out=tb2[:], in0=e1i, in1=e2r, op=ALU.mult), reads=[E1i, E2r], writes=[tb2])
        c.op(P_, lambda e: e.tensor_tensor(out=SINb[:], in0=tb1[:], in1=tb2[:], op=ALU.add), reads=[tb1, tb2], writes=[SINb])
        cosf = COSb[:].rearrange("p c j -> p (c j)")
        sinf = SINb[:].rearrange("p c j -> p (c j)")
        for (src_d, srcs_d, OUT, DEC, fac) in ((q_d, qs_d, QR, QD, qdf), (k_d, ks_d, KR, KD, kdf)):
            x = x_rot.next(); xs = xs_rot.next()
            c.dma("sp", x[:], src_d[:, t0:t0 + BW], writes=[x])
            c.dma("sp", xs[:], srcs_d[:, t0:t0 + BW], writes=[xs])
            c.op(V, lambda e: e.tensor_tensor(out=r1[:], in0=x[:], in1=cosf, op=ALU.mult), reads=[x, COSb], writes=[r1])
            c.op(V, lambda e: e.scalar_tensor_tensor(out=r2[:], in0=xs[:], scalar=fr[:, 2:3], in1=sinf, op0=ALU.mult, op1=ALU.mult),
                 reads=[xs, fr, SINb], writes=[r2])
            c.op(V, lambda e: e.tensor_tensor(out=OUT[:, t0:t0 + BW], in0=r1[:], in1=r2[:], op=ALU.add), reads=[r1, r2], writes=[(OUT, b)])
            facb = fac[:].unsqueeze(1).to_broadcast([64, CB, 128])
            c.op(V, lambda e: e.tensor_tensor(out=DEC[:, t0:t0 + BW].rearrange("p (c j) -> p c j", j=128),
                                              in0=OUT[:, t0:t0 + BW].rearrange("p (c j) -> p c j", j=128), in1=facb, op=ALU.mult),
                 reads=[(OUT, b), fac], writes=[(DEC, b)])

    ps_tr = Rot([c.ps(f"ps_tr{i}", [128, 64], BF16) for i in range(2)])
    ps_s = Rot([c.ps(f"ps_s{i}", [128, 2, 128], F32) for i in range(2)])
    ps_o = Rot([c.ps(f"ps_o{i}", [128, 128], F32) for i in range(2)])
    ps_d = Rot([c.ps(f"ps_d{i}", [64, 128], F32) for i in range(2)])
    kdt_rot = Rot([c.sb(f"kdt{i}", [128, 64], BF16) for i in range(2)])
    sT_rot = Rot([c.sb(f"sT{i}", [128, 2, 128], BF16) for i in range(2)])
    S32 = c.sb("S32", [64, 128], F32)
    c.op(V, lambda e: e.memset(S32[:], 0.0), writes=[S32])
    Sb_rot = Rot([c.sb(f"Sb{i}", [64, 128], BF16) for i in range(2)])
    Sb = Sb_rot.next()
    c.op(V, lambda e: e.memset(Sb[:], 0.0), writes=[Sb])
    o_rot = Rot([c.sb(f"o{i}", [128, 2, 64], F32) for i in range(2)])
    cen_rot = Rot([c.sb(f"cen{i}", [128, 2, 64], F32) for i in range(2)])
    sq_rot = Rot([c.sb(f"sqr{i}", [128, 2, 64], F32) for i in range(2)])
    st_rot = Rot([c.sb(f"st{i}", [128, 8], F32) for i in range(2)])
    gg_rot = Rot([c.sb(f"gg{i}", [128, 128], F32) for i in range(2)])
    for ch in range(NCH):
        b = ch // CB
        t0 = ch * 128
        ptr = ps_tr.next()
        c.op("pe", lambda e: e.transpose(ptr[:, :], KD[:, t0:t0 + 128], identb[0:64, 0:64]), reads=[(KD, b), identb], writes=[ptr])
        kdt = kdt_rot.next()
        c.op("act", lambda e: e.copy(out=kdt[:], in_=ptr[:]), reads=[ptr], writes=[kdt])
        pss = ps_s.next()
        for hl in range(2):
            c.op("pe", lambda e: e.matmul(pss[:, hl, :], lhsT=KR[32 * hl:32 * hl + 32, t0:t0 + 128], rhs=QR[32 * hl:32 * hl + 32, t0:t0 + 128],
                                          start=True, stop=True), reads=[(KR, b), (QR, b)], writes=[pss])
        sT = sT_rot.next()
        c.op(V, lambda e: e.tensor_tensor(out=sT[:], in0=pss[:], in1=maskT[:], op=ALU.mult), reads=[pss, maskT], writes=[sT])
        pso = ps_o.next()
        for hl in range(2):
            c.op("pe", lambda e: e.matmul(pso[:, hl * 64:(hl + 1) * 64], lhsT=sT[:, hl, :], rhs=v_sb[:, ch, hl * 64:(hl + 1) * 64],
                                          start=True, stop=False), reads=[sT, v_sb], writes=[pso])
            c.op("pe", lambda e: e.matmul(pso[:, hl * 64:(hl + 1) * 64], lhsT=QD[32 * hl:32 * hl + 32, t0:t0 + 128],
                                          rhs=Sb[32 * hl:32 * hl + 32, hl * 64:(hl + 1) * 64], start=False, stop=True),
                 reads=[(QD, b), Sb], writes=[pso])
        psd = ps_d.next()
        c.op("pe", lambda e: e.matmul(psd[:, :], lhsT=kdt[:], rhs=v_sb[:, ch, :], start=True, stop=True), reads=[kdt, v_sb], writes=[psd])
        c.op(V, lambda e: e.scalar_tensor_tensor(out=S32[:], in0=S32[:], scalar=sdec[:, 0:1], in1=psd[:], op0=ALU.mult, op1=ALU.add),
             reads=[S32, sdec, psd], writes=[S32])
        Sb = Sb_rot.next()
        c.op("act", lambda e: e.copy(out=Sb[:], in_=S32[:]), reads=[S32], writes=[Sb])
        o = o_rot.next(); cen = cen_rot.next(); sq = sq_rot.next(); st = st_rot.next(); gg = gg_rot.next()
        c.op("act", lambda e: e.copy(out=o[:].rearrange("p h v -> p (h v)"), in_=pso[:]), reads=[pso], writes=[o])
        c.op(V, lambda e: e.tensor_reduce(out=st[:, 0:2], in_=o[:], axis=AX.X, op=ALU.add), reads=[o], writes=[st])
        c.op(V, lambda e: e.tensor_scalar(st[:, 0:2], st[:, 0:2], 1.0 / 64, None, op0=ALU.mult), reads=[st], writes=[st])
        c.op(V, lambda e: e.tensor_tensor(out=cen[:], in0=o[:], in1=st[:, 0:2].unsqueeze(2).to_broadcast([128, 2, 64]), op=ALU.subtract),
             reads=[o, st], writes=[cen])
        c.op("pool", lambda e: e.tensor_tensor(out=sq[:], in0=cen[:], in1=cen[:], op=ALU.mult), reads=[cen], writes=[sq])
        c.op(V, lambda e: e.tensor_reduce(out=st[:, 2:4], in_=sq[:], axis=AX.X, op=ALU.add), reads=[sq, st], writes=[st])
        c.op("act", lambda e: e.activation(out=st[:, 2:4], in_=st[:, 2:4], func=AF.Sqrt, scale=1.0 / 64, bias=epsT[:, 0:1]), reads=[st, epsT], writes=[st])
        c.op(V, lambda e: e.reciprocal(st[:, 2:4], st[:, 2:4]), reads=[st], writes=[st])
        c.op("act", lambda e: e.activation(out=gg[:], in_=g_sb[:, ch, :], func=AF.Silu), reads=[g_sb], writes=[gg])
        c.op("pool", lambda e: e.tensor_tensor(out=gg[:], in0=gg[:], in1=og[:], op=ALU.mult), reads=[gg, og], writes=[gg])
        c.op(V, lambda e: e.tensor_tensor(out=cen[:], in0=cen[:], in1=st[:, 2:4].unsqueeze(2).to_broadcast([128, 2, 64]), op=ALU.mult),
             reads=[cen, st], writes=[cen])
        c.op(V, lambda e: e.tensor_tensor(out=y_sb[:, ch, :], in0=cen[:].rearrange("p h v -> p (h v)"), in1=gg[:], op=ALU.mult),
             reads=[cen, gg], writes=[(y_sb, ch)])
    c.barrier()
    if debug:
        dbg = {"lgP": lgP, "lgB": lgB, "fr": fr, "cs": cs, "E1r": E1r, "E1i": E1i, "Gr": Gr, "Gi": Gi, "maskT": maskT,
               "qdf": qdf, "kdf": kdf, "sdec": sdec, "S32": S32, "COSb": COSb, "SINb": SINb}
        for nm, t in dbg.items():
            shp = list(t.h.shape)
            dd = c.dram("dbg_" + nm, shp, F32, "ExternalOutput")
            c.dma("sp", dd[:], t[:], reads=[t])
        for nm, t in {"QR": QR, "KR": KR, "QD": QD, "KD": KD}.items():
            tmpf = c.sb("dbgf_" + nm, [64, 1024], F32)
            c.op("dve", lambda e: e.tensor_copy(tmpf[:], t[:, 0:1024]), writes=[tmpf])
            dd = c.dram("dbg_" + nm, [64, 1024], F32, "ExternalOutput")
            c.dma("sp", dd[:], tmpf[:], reads=[tmpf])
    c.dma("sp", y_d.h.ap().rearrange("(c p) f -> p c f", p=128), y_sb[:], reads=[y_sb], qt=y_sb)
    c.finish()
    c.close()
    print("ret instructions:", c.n_inst)
    return nc


NTOK = 8320
NG = 65
EPS = 1e-6
GB = 13
BW = GB * 128
NBLK = NG // GB
CS = 32


def build_hg(layer, debug=False):
    nc = bass.Bass("TRN2", target_bir_lowering=False)
    c = Ctx(nc)
    x_d = c.dram("x3", [3, 64, NTOK], F32, "ExternalInput")
    cw_d = c.dram("convw", [3, 64, 4], F32, "ExternalInput")
    g_d = c.dram("gate", [NTOK, 64], F32, "ExternalInput")
    og_d = c.dram("outg", [128, 64], F32, "ExternalInput")
    lb_d = c.dram("lbp", [64, 2], F32, "ExternalInput")
    y_d = c.dram("y", [NTOK, 64], F32, "ExternalOutput")
    V = "dve"

    epsT = c.sb("eps", [128, 1], F32)
    c.op("pool", lambda e: e.memset(epsT[:], EPS), writes=[epsT])
    iof = c.sb("iof", [128, 128], F32)
    c.op("pool", lambda e: e.iota(iof[:], [[1, 128]], base=0, channel_multiplier=-1, allow_small_or_imprecise_dtypes=True), writes=[iof])
    identf = c.sb("identf", [128, 128], F32)
    c.op(V, lambda e: e.tensor_single_scalar(identf[:], iof[:], 0.0, op=ALU.is_equal), reads=[iof], writes=[identf])
    identb = c.sb("identb", [128, 128], BF16)
    c.op(V, lambda e: e.tensor_copy(identb[:], identf[:]), reads=[identf], writes=[identb])
    dge = c.sb("dge", [128, 128], F32)
    c.op(V, lambda e: e.tensor_single_scalar(dge[:], iof[:], 0.0, op=ALU.is_ge), reads=[iof], writes=[dge])
    bmask = c.sb("bmask", [128, 128], F32)
    c.op(V, lambda e: e.memset(bmask[:], 0.0), writes=[bmask])
    for m in range(4):
        c.op(V, lambda e: e.tensor_copy(bmask[32 * m:32 * m + 32, 32 * m:32 * m + 32], dge[32 * m:32 * m + 32, 32 * m:32 * m + 32]),
             reads=[dge], writes=[bmask])
    cmask = c.sb("cmask", [128, 4, 64], F32)
    cm2 = c.sb("cm2", [128, 4, 64], F32)
    c.op("pool", lambda e: e.iota(cmask[:], [[-32, 4], [0, 64]], base=0, channel_multiplier=1, allow_small_or_imprecise_dtypes=True), writes=[cmask])
    c.op(V, lambda e: e.tensor_single_scalar(cm2[:], cmask[:], 32.0, op=ALU.is_lt), reads=[cmask], writes=[cm2])
    c.op(V, lambda e: e.tensor_single_scalar(cmask[:], cmask[:], 0.0, op=ALU.is_ge), reads=[cmask, cm2], writes=[cmask])
    c.op(V, lambda e: e.tensor_tensor(out=cmask[:], in0=cmask[:], in1=cm2[:], op=ALU.mult), reads=[cmask, cm2], writes=[cmask])
    colmask = c.sb("colmask", [64, 4, 128], F32)
    col2 = c.sb("col2", [64, 4, 128], F32)
    c.op("pool", lambda e: e.iota(colmask[:], [[-32, 4], [1, 128]], base=0, channel_multiplier=0, allow_small_or_imprecise_dtypes=True), writes=[colmask])
    c.op(V, lambda e: e.tensor_single_scalar(col2[:], colmask[:], 32.0, op=ALU.is_lt), reads=[colmask], writes=[col2])
    c.op(V, lambda e: e.tensor_single_scalar(colmask[:], colmask[:], 0.0, op=ALU.is_ge), reads=[colmask, col2], writes=[colmask])
    c.op(V, lambda e: e.tensor_tensor(out=colmask[:], in0=colmask[:], in1=col2[:], op=ALU.mult), reads=[colmask, col2], writes=[colmask])
    rmask = c.sb("rmask", [64, BW], F32)
    c.op("pool", lambda e: e.iota(rmask[:], [[0, BW // CS], [1, CS]], base=0, channel_multiplier=0, allow_small_or_imprecise_dtypes=True), writes=[rmask])
    c.op(V, lambda e: e.tensor_single_scalar(rmask[:], rmask[:], 0.0, op=ALU.is_gt), reads=[rmask], writes=[rmask])
    cw = c.sb("cw", [64, 3, 4], F32)
    c.dma("sp", cw[:], cw_d.h.ap().rearrange("s p k -> p s k"), writes=[cw])
    og = c.sb("og", [128, 64], F32)
    c.dma("sp", og[:], og_d[:], writes=[og])
    lbp = c.sb("lbp", [64, 4], F32)
    c.dma("sp", lbp[:, 0:2], lb_d[:], writes=[lbp])
    if layer == 0:
        c.op(V, lambda e: e.memset(lbp[:, 2:3], 0.0), reads=[lbp], writes=[lbp])
    else:
        c.op(V, lambda e: e.tensor_tensor(out=lbp[:, 2:3], in0=lbp[:, 1:2], in1=lbp[:, 0:1], op=ALU.subtract), reads=[lbp], writes=[lbp])
        c.op("act", lambda e: e.activation(out=lbp[:, 2:3], in_=lbp[:, 2:3], func=AF.Sigmoid), reads=[lbp], writes=[lbp])
    c.op(V, lambda e: e.tensor_scalar(lbp[:, 3:4], lbp[:, 2:3], -1.0, 1.0, op0=ALU.mult, op1=ALU.add), reads=[lbp], writes=[lbp])

    QT = c.sb("QT", [64, NTOK], BF16)
    KT = c.sb("KT", [64, NTOK], BF16)
    KDT = c.sb("KDT", [64, NTOK], BF16)
    Vtok = c.sb("Vtok", [128, NG, 64], BF16)
    KDtok = c.sb("KDtok", [128, NG, 64], BF16)
    g_sb = c.sb("g_sb", [128, NG, 64], BF16)
    y_sb = c.sb("y_sb", [128, NG, 64], F32)
    Dec = c.sb("Dec", [64, NTOK // CS], F32)
    c.dma("pool", g_sb[:], g_d.h.ap().rearrange("(c p) f -> p c f", p=128), writes=[g_sb])

    xq = c.sb("xq", [64, BW + 3], F32); xf = c.sb("xf", [64, BW + 3], F32); xi = c.sb("xi", [64, BW + 3], F32)
    cq = c.sb("cq", [64, BW], F32); cf = c.sb("cf", [64, BW], F32); ci = c.sb("ci", [64, BW], F32)
    gg_ = c.sb("g", [64, BW], F32); gcum = c.sb("gcum", [64, BW], F32); tmp = c.sb("tmp", [64, BW], F32)
    ps_t = Rot([c.ps(f"pst{i}", [128, 64], F32) for i in range(2)])
    ps_tb = Rot([c.ps(f"pstb{i}", [128, 64], BF16) for i in range(2)])
    NCB = BW // CS
    for b in range(NBLK):
        t0 = b * BW
        for si, (xt, ct) in enumerate(((xq, cq), (xf, cf), (xi, ci))):
            if b == 0:
                c.op(V, lambda e: e.memset(xt[:, 0:3], 0.0), writes=[xt])
                c.dma("sp", xt[:, 3:BW + 3], x_d.h.ap()[si, :, 0:BW], writes=[xt])
            else:
                c.dma("sp", xt[:, :], x_d.h.ap()[si, :, t0 - 3:t0 + BW], writes=[xt])
            c.op(V, lambda e: e.tensor_scalar(ct[:], xt[:, 0:BW], cw[:, si, 0:1], None, op0=ALU.mult), reads=[xt, cw], writes=[ct])
            for kk_ in range(1, 4):
                c.op(V, lambda e: e.scalar_tensor_tensor(out=ct[:], in0=xt[:, kk_:kk_ + BW], scalar=cw[:, si, kk_:kk_ + 1], in1=ct[:],
                                                         op0=ALU.mult, op1=ALU.add), reads=[xt, cw, ct], writes=[ct])
        c.op("act", lambda e: e.activation(out=cq[:], in_=cq[:], func=AF.Silu), reads=[cq], writes=[cq])
        c.op("act", lambda e: e.activation(out=cf[:], in_=cf[:], func=AF.Sigmoid), reads=[cf], writes=[cf])
        c.op(V, lambda e: e.tensor_scalar(cf[:], cf[:], lbp[:, 3:4], lbp[:, 2:3], op0=ALU.mult, op1=ALU.add), reads=[cf, lbp], writes=[cf])
        c.op("act", lambda e: e.activation(out=gg_[:], in_=cf[:], func=AF.Ln), reads=[cf], writes=[gg_])
        c.op(V, lambda e: e.tensor_scalar(cf[:], cf[:], -1.0, 1.0, op0=ALU.mult, op1=ALU.add), reads=[cf, gg_], writes=[cf])
        c.op(V, lambda e: e.tensor_tensor_scan(out=gcum[:], data0=rmask[:], data1=gg_[:], initial=0.0, op0=ALU.mult, op1=ALU.add),
             reads=[rmask, gg_], writes=[gcum])
        c.op("act", lambda e: e.activation(out=tmp[:], in_=gcum[:], func=AF.Exp), reads=[gcum], writes=[tmp])
        c.op(V, lambda e: e.tensor_tensor(out=QT[:, t0:t0 + BW], in0=cq[:], in1=tmp[:], op=ALU.mult), reads=[cq, tmp], writes=[(QT, b)])
        c.op(V, lambda e: e.tensor_single_scalar(tmp[:], gcum[:], -80.0, op=ALU.max), reads=[gcum, (QT, b)], writes=[tmp])
        c.op("act", lambda e: e.activation(out=tmp[:], in_=tmp[:], func=AF.Exp, scale=-1.0), reads=[tmp], writes=[tmp])
        c.op(V, lambda e: e.tensor_tensor(out=KT[:, t0:t0 + BW], in0=cf[:], in1=tmp[:], op=ALU.mult), reads=[cf, tmp], writes=[(KT, b)])
        gl = gcum[:].rearrange("p (c j) -> p c j", j=CS)[:, :, CS - 1:CS]
        c.op(V, lambda e: e.tensor_tensor(out=tmp[:].rearrange("p (c j) -> p c j", j=CS), in0=gl.to_broadcast([64, NCB, CS]),
                                          in1=gcum[:].rearrange("p (c j) -> p c j", j=CS), op=ALU.subtract), reads=[gcum, (KT, b)], writes=[tmp])
        c.op("act", lambda e: e.activation(out=tmp[:], in_=tmp[:], func=AF.Exp), reads=[tmp], writes=[tmp])
        c.op(V, lambda e: e.tensor_tensor(out=KDT[:, t0:t0 + BW], in0=cf[:], in1=tmp[:], op=ALU.mult), reads=[cf, tmp], writes=[(KDT, b)])
        c.op("act", lambda e: e.activation(out=Dec[:, b * NCB:(b + 1) * NCB].unsqueeze(2), in_=gl, func=AF.Exp), reads=[gcum], writes=[(Dec, b)])
        for gi in range(GB):
            G = b * GB + gi
            pt = ps_t.next()
            c.op("pe", lambda e: e.transpose(pt[:, :], ci[:, gi * 128:(gi + 1) * 128], identf[0:64, 0:64]), reads=[ci, identf], writes=[pt])
            c.op("act", lambda e: e.copy(out=Vtok[:, G, :], in_=pt[:]), reads=[pt], writes=[(Vtok, G)])
            ptb = ps_tb.next()
            c.op("pe", lambda e: e.transpose(ptb[:, :], KDT[:, t0 + gi * 128:t0 + (gi + 1) * 128], identb[0:64, 0:64]),
                 reads=[(KDT, b), identb], writes=[ptb])
            c.op("act", lambda e: e.copy(out=KDtok[:, G, :], in_=ptb[:]), reads=[ptb], writes=[(KDtok, G)])

    ps_s = Rot([c.ps(f"ps_s{i}", [128, 128], F32) for i in range(1)])
    ps_o = Rot([c.ps(f"ps_o{i}", [128, 64], F32) for i in range(2)])
    ps_d = Rot([c.ps(f"ps_d{i}", [64, 4, 64], F32) for i in range(1)])
    sT_rot = Rot([c.sb(f"sT{i}", [128, 128], BF16) for i in range(2)])
    S32 = c.sb("S32", [64, 64], F32)
    c.op(V, lambda e: e.memset(S32[:], 0.0), writes=[S32])
    Sb_rot = Rot([c.sb(f"Sb{i}", [64, 64], BF16) for i in range(6)])
    Sb = Sb_rot.next()
    c.op(V, lambda e: e.memset(Sb[:], 0.0), writes=[Sb])
    o_rot = Rot([c.sb(f"o{i}", [128, 64], F32) for i in range(2)])
    sq_rot = Rot([c.sb(f"sqr{i}", [128, 64], F32) for i in range(2)])
    st_rot = Rot([c.sb(f"st{i}", [128, 4], F32) for i in range(2)])
    gg_rot = Rot([c.sb(f"gg{i}", [128, 64], F32) for i in range(2)])
    vb_rot = Rot([c.sb(f"vb{i}", [128, 4, 64], BF16) for i in range(2)])
    qm_rot = Rot([c.sb(f"qm{i}", [64, 4, 128], BF16) for i in range(2)])
    for G in range(NG):
        b = G // GB
        t0 = G * 128
        pss = ps_s.next()
        c.op("pe", lambda e: e.matmul(pss[:, :], lhsT=KT[:, t0:t0 + 128], rhs=QT[:, t0:t0 + 128], start=True, stop=True),
             reads=[(KT, b), (QT, b)], writes=[pss])
        sT = sT_rot.next()
        c.op(V, lambda e: e.tensor_tensor(out=sT[:], in0=pss[:], in1=bmask[:], op=ALU.mult), reads=[pss, bmask], writes=[sT])
        psd = ps_d.next()
        vb = vb_rot.next()
        c.op("pool", lambda e: e.tensor_tensor(out=vb[:], in0=Vtok[:, G, :].unsqueeze(1).to_broadcast([128, 4, 64]), in1=cmask[:], op=ALU.mult),
             reads=[(Vtok, G), cmask], writes=[vb])
        c.op("pe", lambda e: e.matmul(psd[:].rearrange("p m v -> p (m v)"), lhsT=KDtok[:, G, :], rhs=vb[:].rearrange("p m v -> p (m v)"),
                                      start=True, stop=True), reads=[(KDtok, G), vb], writes=[psd])
        qm = qm_rot.next()
        c.op(V, lambda e: e.tensor_tensor(out=qm[:], in0=QT[:, t0:t0 + 128].unsqueeze(1).to_broadcast([64, 4, 128]), in1=colmask[:], op=ALU.mult),
             reads=[(QT, b), colmask], writes=[qm])
        pso = ps_o.next()
        c.op("pe", lambda e: e.matmul(pso[:, :], lhsT=sT[:], rhs=Vtok[:, G, :], start=True, stop=False), reads=[sT, (Vtok, G)], writes=[pso])
        for m in range(4):
            ch = G * 4 + m
            c.op("pe", lambda e: e.matmul(pso[:, :], lhsT=qm[:, m, :], rhs=Sb[:, :],
                                          start=False, stop=(m == 3)), reads=[qm, Sb], writes=[pso])
            c.op(V, lambda e: e.scalar_tensor_tensor(out=S32[:], in0=S32[:], scalar=Dec[:, ch:ch + 1], in1=psd[:, m, :],
                                                     op0=ALU.mult, op1=ALU.add), reads=[S32, (Dec, b), psd], writes=[S32])
            Sb = Sb_rot.next()
            c.op("act", lambda e: e.copy(out=Sb[:], in_=S32[:]), reads=[S32], writes=[Sb])
        o = o_rot.next(); sq = sq_rot.next(); st = st_rot.next(); gg = gg_rot.next()
        c.op("act", lambda e: e.copy(out=o[:], in_=pso[:]), reads=[pso], writes=[o])
        c.op("pool", lambda e: e.tensor_tensor(out=sq[:], in0=o[:], in1=o[:], op=ALU.mult), reads=[o], writes=[sq])
        c.op(V, lambda e: e.tensor_reduce(out=st[:, 0:1], in_=sq[:], axis=AX.X, op=ALU.add), reads=[sq], writes=[st])
        c.op("act", lambda e: e.activation(out=st[:, 0:1], in_=st[:, 0:1], func=AF.Sqrt, scale=1.0 / 64, bias=epsT[:, 0:1]), reads=[st, epsT], writes=[st])
        c.op(V, lambda e: e.reciprocal(st[:, 0:1], st[:, 0:1]), reads=[st], writes=[st])
        c.op("act", lambda e: e.activation(out=gg[:], in_=g_sb[:, G, :], func=AF.Silu), reads=[g_sb], writes=[gg])
        c.op("pool", lambda e: e.tensor_tensor(out=gg[:], in0=gg[:], in1=og[:], op=ALU.mult), reads=[gg, og], writes=[gg])
        c.op(V, lambda e: e.scalar_tensor_tensor(out=y_sb[:, G, :], in0=o[:], scalar=st[:, 0:1], in1=gg[:], op0=ALU.mult, op1=ALU.mult),
             reads=[o, st, gg], writes=[(y_sb, G)])
    c.barrier()
    c.dma("sp", y_d.h.ap().rearrange("(c p) f -> p c f", p=128), y_sb[:], reads=[y_sb], qt=y_sb)
    c.finish()
    c.close()
    print("hg instructions:", c.n_inst)
    return nc


NSB = 1040
NGL = 4
NLEV = 11


def emit_sincos_tile(c, x, xT, cs_c, cs_s, x2, acc, n):
    V = "dve"
    c.op(V, lambda e: e.tensor_tensor(out=x2[:], in0=x[:], in1=x[:], op=ALU.mult), reads=[x], writes=[x2])
    c.op(V, lambda e: e.tensor_scalar(acc[:], x2[:], -1.0 / 156, 1.0, op0=ALU.mult, op1=ALU.add), reads=[x2], writes=[acc])
    for d in (110.0, 72.0, 42.0, 20.0, 6.0):
        c.op(V, lambda e: e.tensor_tensor(out=acc[:], in0=acc[:], in1=x2[:], op=ALU.mult), reads=[acc, x2], writes=[acc])
        c.op(V, lambda e: e.tensor_scalar(acc[:], acc[:], -1.0 / d, 1.0, op0=ALU.mult, op1=ALU.add), reads=[acc], writes=[acc])
    c.op(V, lambda e: e.tensor_tensor(out=cs_s[:], in0=acc[:], in1=x[:], op=ALU.mult), reads=[acc, x], writes=[cs_s])
    c.op(V, lambda e: e.tensor_scalar(acc[:], x2[:], -1.0 / 182, 1.0, op0=ALU.mult, op1=ALU.add), reads=[x2, cs_s], writes=[acc])
    for d in (132.0, 90.0, 56.0, 30.0, 12.0, 2.0):
        c.op(V, lambda e: e.tensor_tensor(out=acc[:], in0=acc[:], in1=x2[:], op=ALU.mult), reads=[acc, x2], writes=[acc])
        c.op(V, lambda e: e.tensor_scalar(acc[:], acc[:], -1.0 / d, 1.0, op0=ALU.mult, op1=ALU.add), reads=[acc], writes=[acc])
    c.op(V, lambda e: e.tensor_copy(cs_c[:], acc[:]), reads=[acc], writes=[cs_c])


def emit_cdouble(c, cr, ci, t1, t2):
    V = "dve"
    c.op(V, lambda e: e.tensor_tensor(out=t1[:], in0=cr[:], in1=cr[:], op=ALU.mult), reads=[cr], writes=[t1])
    c.op(V, lambda e: e.tensor_tensor(out=t2[:], in0=ci[:], in1=ci[:], op=ALU.mult), reads=[ci], writes=[t2])
    c.op(V, lambda e: e.scalar_tensor_tensor(out=ci[:], in0=cr[:], scalar=2.0, in1=ci[:], op0=ALU.mult, op1=ALU.mult), reads=[cr, ci, t2], writes=[ci])
    c.op(V, lambda e: e.tensor_tensor(out=cr[:], in0=t1[:], in1=t2[:], op=ALU.subtract), reads=[t1, t2, ci], writes=[cr])


def build_s5(debug=False):
    nc = bass.Bass("TRN2", target_bir_lowering=False)
    c = Ctx(nc)
    U_d = c.dram("U", [NGL, 128, NSB], F32, "ExternalInput")
    lam_d = c.dram("lam", [128, NGL, 2], F32, "ExternalInput")
    ls_d = c.dram("ls", [128, NGL], F32, "ExternalInput")
    B_d = c.dram("Bm", [128, NGL, 2, 16], F32, "ExternalInput")
    C_d = c.dram("Cm", [128, NGL, 2, 16], F32, "ExternalInput")
    D_d = c.dram("Dm", [128, NGL], F32, "ExternalInput")
    y_d = c.dram("y", [NGL, 128, NSB], F32, "ExternalOutput")
    V = "dve"
    G4 = NGL

    def sb(name, shape, dt=F32):
        return c.sb(name, shape, dt)

    iof = sb("iof", [128, 128])
    c.op("pool", lambda e: e.iota(iof[:], [[1, 128]], base=0, channel_multiplier=-1, allow_small_or_imprecise_dtypes=True), writes=[iof])
    ident = sb("ident", [128, 128])
    c.op(V, lambda e: e.tensor_single_scalar(ident[:], iof[:], 0.0, op=ALU.is_equal), reads=[iof], writes=[ident])
    pswap = sb("pswap", [128, 128])
    ptmp = sb("ptmp", [128, 128])
    c.op(V, lambda e: e.tensor_single_scalar(pswap[:], iof[:], 64.0, op=ALU.is_equal), reads=[iof], writes=[pswap])
    c.op(V, lambda e: e.tensor_single_scalar(ptmp[:], iof[:], -64.0, op=ALU.is_equal), reads=[iof], writes=[ptmp])
    c.op(V, lambda e: e.tensor_tensor(out=pswap[:], in0=pswap[:], in1=ptmp[:], op=ALU.add), reads=[pswap, ptmp], writes=[pswap])
    tmask = sb("tmask", [128, 8, 16])
    c.op("pool", lambda e: e.iota(tmask[:], [[16, 8], [0, 16]], base=15, channel_multiplier=-1, allow_small_or_imprecise_dtypes=True), writes=[tmask])
    c.op(V, lambda e: e.tensor_single_scalar(tmask[:], tmask[:], 0.0, op=ALU.is_ge), reads=[tmask], writes=[tmask])
    sgnh = sb("sgnh", [128, 1])
    c.op(V, lambda e: e.memset(sgnh[0:64, :], 1.0), writes=[sgnh])
    c.op(V, lambda e: e.memset(sgnh[64:128, :], -1.0), writes=[sgnh])
    mv = sb("mv", [128, G4, 9])
    c.op("pool", lambda e: e.iota(mv[:], [[0, G4], [1, 9]], base=0, channel_multiplier=0, allow_small_or_imprecise_dtypes=True), writes=[mv])

    lam = sb("lam", [128, G4, 2]); ls = sb("ls", [128, G4]); Bm = sb("Bm", [128, G4, 2, 16]); Cm = sb("Cm", [128, G4, 2, 16]); Dm = sb("Dm", [128, G4])
    c.dma("sp", lam[:], lam_d[:], writes=[lam]); c.dma("sp", ls[:], ls_d[:], writes=[ls])
    c.dma("sp", Bm[:], B_d[:], writes=[Bm]); c.dma("sp", Cm[:], C_d[:], writes=[Cm]); c.dma("sp", Dm[:], D_d[:], writes=[Dm])

    st = sb("st", [128, G4]); a = sb("a", [128, G4]); th = sb("th", [128, G4])
    c.op("act", lambda e: e.activation(out=st[:], in_=ls[:], func=AF.Exp), reads=[ls], writes=[st])
    c.op(V, lambda e: e.tensor_tensor(out=a[:], in0=lam[:, :, 0], in1=st[:], op=ALU.mult), reads=[lam, st], writes=[a])
    c.op(V, lambda e: e.tensor_tensor(out=th[:], in0=lam[:, :, 1], in1=st[:], op=ALU.mult), reads=[lam, st], writes=[th])
    kq = sb("kq", [128, G4]); ki = sb("ki", [128, G4], I32); x = sb("x", [128, G4])
    c.op(V, lambda e: e.tensor_scalar(kq[:], th[:], 1.0 / (2 * math.pi), None, op0=ALU.mult), reads=[th], writes=[kq])
    c.op(V, lambda e: e.tensor_copy(ki[:], kq[:]), reads=[kq], writes=[ki])
    c.op(V, lambda e: e.tensor_copy(kq[:], ki[:]), reads=[ki], writes=[kq])
    c.op(V, lambda e: e.scalar_tensor_tensor(out=x[:], in0=kq[:], scalar=-2 * math.pi, in1=th[:], op0=ALU.mult, op1=ALU.add), reads=[kq, th], writes=[x])
    c.op(V, lambda e: e.tensor_scalar(x[:], x[:], 0.25, None, op0=ALU.mult), reads=[x], writes=[x])
    p1r = sb("p1r", [128, G4]); p1i = sb("p1i", [128, G4]); x2 = sb("x2", [128, G4]); acc = sb("acc", [128, G4])
    t1 = sb("t1", [128, G4]); t2 = sb("t2", [128, G4])
    emit_sincos_tile(c, x, x, p1r, p1i, x2, acc, G4)
    emit_cdouble(c, p1r, p1i, t1, t2)
    emit_cdouble(c, p1r, p1i, t1, t2)
    phr = sb("phr", [128, G4, 9]); phi = sb("phi", [128, G4, 9])
    c.op(V, lambda e: e.memset(phr[:, :, 0:1], 1.0), writes=[phr])
    c.op(V, lambda e: e.memset(phi[:, :, 0:1], 0.0), writes=[phi])
    for m in range(8):
        c.op(V, lambda e: e.tensor_tensor(out=t1[:], in0=phr[:, :, m], in1=p1r[:], op=ALU.mult), reads=[phr, p1r], writes=[t1])
        c.op(V, lambda e: e.tensor_tensor(out=t2[:], in0=phi[:, :, m], in1=p1i[:], op=ALU.mult), reads=[phi, p1i], writes=[t2])
        c.op(V, lambda e: e.tensor_tensor(out=phr[:, :, m + 1], in0=t1[:], in1=t2[:], op=ALU.subtract), reads=[t1, t2], writes=[phr])
        c.op(V, lambda e: e.tensor_tensor(out=t1[:], in0=phr[:, :, m], in1=p1i[:], op=ALU.mult), reads=[phr, p1i], writes=[t1])
        c.op(V, lambda e: e.tensor_tensor(out=t2[:], in0=phi[:, :, m], in1=p1r[:], op=ALU.mult), reads=[phi, p1r], writes=[t2])
        c.op(V, lambda e: e.tensor_tensor(out=phi[:, :, m + 1], in0=t1[:], in1=t2[:], op=ALU.add), reads=[t1, t2], writes=[phi])
    am = sb("am", [128, G4, 9]); magp = sb("magp", [128, G4, 9]); magn = sb("magn", [128, G4, 9])
    c.op(V, lambda e: e.tensor_tensor(out=am[:], in0=mv[:], in1=a[:].unsqueeze(2).to_broadcast([128, G4, 9]), op=ALU.mult), reads=[mv, a], writes=[am])
    c.op("act", lambda e: e.activation(out=magp[:], in_=am[:], func=AF.Exp), reads=[am], writes=[magp])
    c.op("act", lambda e: e.activation(out=magn[:], in_=am[:], func=AF.Exp, scale=-1.0), reads=[am], writes=[magn])
    LPr = sb("LPr", [128, G4, 9]); LPi = sb("LPi", [128, G4, 9]); LNr = sb("LNr", [128, G4, 9]); LNi = sb("LNi", [128, G4, 9])
    c.op(V, lambda e: e.tensor_tensor(out=LPr[:], in0=magp[:], in1=phr[:], op=ALU.mult), reads=[magp, phr], writes=[LPr])
    c.op(V, lambda e: e.tensor_tensor(out=LPi[:], in0=magp[:], in1=phi[:], op=ALU.mult), reads=[magp, phi], writes=[LPi])
    c.op(V, lambda e: e.tensor_tensor(out=LNr[:], in0=magn[:], in1=phr[:], op=ALU.mult), reads=[magn, phr], writes=[LNr])
    c.op(V, lambda e: e.scalar_tensor_tensor(out=LNi[:], in0=magn[:], scalar=-1.0, in1=phi[:], op0=ALU.mult, op1=ALU.mult), reads=[magn, phi], writes=[LNi])
    kr = sb("kr", [128, G4]); kim = sb("kim", [128, G4]); nr = sb("nr", [128, G4]); den = sb("den", [128, G4])
    c.op(V, lambda e: e.tensor_scalar(nr[:], LPr[:, :, 1], -1.0, None, op0=ALU.add), reads=[LPr], writes=[nr])
    c.op(V, lambda e: e.tensor_tensor(out=t1[:], in0=lam[:, :, 0], in1=lam[:, :, 0], op=ALU.mult), reads=[lam], writes=[t1])
    c.op(V, lambda e: e.tensor_tensor(out=t2[:], in0=lam[:, :, 1], in1=lam[:, :, 1], op=ALU.mult), reads=[lam], writes=[t2])
    c.op(V, lambda e: e.tensor_tensor(out=den[:], in0=t1[:], in1=t2[:], op=ALU.add), reads=[t1, t2], writes=[den])
    c.op(V, lambda e: e.reciprocal(den[:], den[:]), reads=[den], writes=[den])
    c.op(V, lambda e: e.tensor_tensor(out=t1[:], in0=nr[:], in1=lam[:, :, 0], op=ALU.mult), reads=[nr, lam], writes=[t1])
    c.op(V, lambda e: e.tensor_tensor(out=t2[:], in0=LPi[:, :, 1], in1=lam[:, :, 1], op=ALU.mult), reads=[LPi, lam], writes=[t2])
    c.op(V, lambda e: e.tensor_tensor(out=kr[:], in0=t1[:], in1=t2[:], op=ALU.add), reads=[t1, t2], writes=[kr])
    c.op(V, lambda e: e.tensor_tensor(out=kr[:], in0=kr[:], in1=den[:], op=ALU.mult), reads=[kr, den], writes=[kr])
    c.op(V, lambda e: e.tensor_tensor(out=t1[:], in0=LPi[:, :, 1], in1=lam[:, :, 0], op=ALU.mult), reads=[LPi, lam, kr], writes=[t1])
    c.op(V, lambda e: e.tensor_tensor(out=t2[:], in0=nr[:], in1=lam[:, :, 1], op=ALU.mult), reads=[nr, lam, kr], writes=[t2])
    c.op(V, lambda e: e.tensor_tensor(out=kim[:], in0=t1[:], in1=t2[:], op=ALU.subtract), reads=[t1, t2], writes=[kim])
    c.op(V, lambda e: e.tensor_tensor(out=kim[:], in0=kim[:], in1=den[:], op=ALU.mult), reads=[kim, den], writes=[kim])
    Bbr = sb("Bbr", [128, G4, 16]); Bbi = sb("Bbi", [128, G4, 16]); tb = sb("tb", [128, G4, 16])
    krb = kr[:].unsqueeze(2).to_broadcast([128, G4, 16]); kib = kim[:].unsqueeze(2).to_broadcast([128, G4, 16])
    c.op(V, lambda e: e.tensor_tensor(out=Bbr[:], in0=Bm[:, :, 0, :], in1=krb, op=ALU.mult), reads=[Bm, kr], writes=[Bbr])
    c.op(V, lambda e: e.tensor_tensor(out=tb[:], in0=Bm[:, :, 1, :], in1=kib, op=ALU.mult), reads=[Bm, kim], writes=[tb])
    c.op(V, lambda e: e.tensor_tensor(out=Bbr[:], in0=Bbr[:], in1=tb[:], op=ALU.subtract), reads=[Bbr, tb], writes=[Bbr])
    c.op(V, lambda e: e.tensor_tensor(out=Bbi[:], in0=Bm[:, :, 1, :], in1=krb, op=ALU.mult), reads=[Bm, kr], writes=[Bbi])
    c.op(V, lambda e: e.tensor_tensor(out=tb[:], in0=Bm[:, :, 0, :], in1=kib, op=ALU.mult), reads=[Bm, kim, Bbr], writes=[tb])
    c.op(V, lambda e: e.tensor_tensor(out=Bbi[:], in0=Bbi[:], in1=tb[:], op=ALU.add), reads=[Bbi, tb], writes=[Bbi])
    X1 = sb("X1", [128, G4, 16]); X2 = sb("X2", [128, G4, 16]); C1 = sb("C1", [128, G4, 16]); C2 = sb("C2", [128, G4, 16])
    c.op(V, lambda e: e.tensor_copy(X1[0:64], Bbr[0:64]), reads=[Bbr], writes=[X1])
    c.op(V, lambda e: e.tensor_copy(X1[64:128], Bbi[64:128]), reads=[Bbi], writes=[X1])
    c.op(V, lambda e: e.tensor_scalar(X2[0:64], Bbi[0:64], -1.0, None, op0=ALU.mult), reads=[Bbi], writes=[X2])
    c.op(V, lambda e: e.tensor_copy(X2[64:128], Bbr[64:128]), reads=[Bbr], writes=[X2])
    c.op(V, lambda e: e.tensor_copy(C1[0:64], Cm[0:64, :, 0, :]), reads=[Cm], writes=[C1])
    c.op(V, lambda e: e.tensor_scalar(C1[64:128], Cm[64:128, :, 1, :], -1.0, None, op0=ALU.mult), reads=[Cm], writes=[C1])
    c.op(V, lambda e: e.tensor_scalar(C2[0:64], Cm[0:64, :, 1, :], -1.0, None, op0=ALU.mult), reads=[Cm], writes=[C2])
    c.op(V, lambda e: e.tensor_scalar(C2[64:128], Cm[64:128, :, 0, :], -1.0, None, op0=ALU.mult), reads=[Cm], writes=[C2])
    Z = sb("Z", [128, G4, 8, 16]); Y = sb("Y", [128, G4, 8, 16]); Wc = sb("Wc", [128, G4, 8, 16]); tz = sb("tz", [128, 16])
    for g in range(G4):
        for j in range(8):
            for (OUT, Lr_, Li_, mi, A1, A2) in ((Z, LPr, LPi, 7 - j, X1, X2), (Y, LNr, LNi, j + 1, X1, X2), (Wc, LPr, LPi, j + 1, C1, C2)):
                c.op(V, lambda e: e.tensor_scalar(tz[:], A1[:, g, :], Lr_[:, g, mi:mi + 1], None, op0=ALU.mult), reads=[A1, Lr_], writes=[tz])
                c.op(V, lambda e: e.scalar_tensor_tensor(out=OUT[:, g, j, :], in0=A2[:, g, :], scalar=Li_[:, g, mi:mi + 1], in1=tz[:],
                                                         op0=ALU.mult, op1=ALU.add), reads=[A2, Li_, tz], writes=[OUT])
    ps_rot = Rot([c.ps(f"ps{i}", [128, 512], F32) for i in range(7)])
    Toep = sb("Toep", [128, G4, 128], BF16); W1 = sb("W1", [128, G4, 128], BF16); tf = sb("tf", [128, 128])
    for g in range(G4):
        ps = ps_rot.next()
        c.op("pe", lambda e: e.matmul(ps[:, 0:128], lhsT=Y[:, g].rearrange("p j h -> p (j h)"), rhs=Wc[:, g].rearrange("p j h -> p (j h)"),
                                      start=True, stop=True), reads=[Y, Wc], writes=[ps])
        c.op(V, lambda e: e.tensor_tensor(out=tf[:], in0=ps[:, 0:128], in1=tmask[:].rearrange("p j h -> p (j h)"), op=ALU.mult),
             reads=[ps, tmask], writes=[tf])
        c.op(V, lambda e: e.scalar_tensor_tensor(out=Toep[:, g, :], in0=ident[:], scalar=Dm[:, g:g + 1], in1=tf[:], op0=ALU.mult, op1=ALU.add),
             reads=[ident, Dm, tf], writes=[Toep])
        ps = ps_rot.next()
        c.op("pe", lambda e: e.transpose(ps[:, 0:128], Z[:, g].rearrange("p j h -> p (j h)"), ident[:]), reads=[Z, ident], writes=[ps])
        c.op("act", lambda e: e.copy(out=W1[:, g, :], in_=ps[:, 0:128]), reads=[ps], writes=[W1])
    R = sb("R", [128, G4, NLEV, 128])
    qr = sb("qr", [128, G4]); qi = sb("qi", [128, G4]); mg = sb("mg", [128, G4]); s1 = sb("s1", [128, G4]); s2 = sb("s2", [128, G4])
    c.op(V, lambda e: e.tensor_copy(qr[:], phr[:, :, 8]), reads=[phr], writes=[qr])
    c.op(V, lambda e: e.tensor_copy(qi[:], phi[:, :, 8]), reads=[phi], writes=[qi])
    for k in range(NLEV):
        c.op("act", lambda e: e.activation(out=mg[:], in_=a[:], func=AF.Exp, scale=float(8 * (2 ** k))), reads=[a], writes=[mg])
        c.op(V, lambda e: e.tensor_tensor(out=s1[:], in0=mg[:], in1=qr[:], op=ALU.mult), reads=[mg, qr], writes=[s1])
        c.op(V, lambda e: e.tensor_tensor(out=s2[:], in0=mg[:], in1=qi[:], op=ALU.mult), reads=[mg, qi], writes=[s2])
        c.op(V, lambda e: e.tensor_scalar(s2[:], s2[:], sgnh[:, 0:1], None, op0=ALU.mult), reads=[s2, sgnh], writes=[s2])
        for g in range(G4):
            c.op(V, lambda e: e.tensor_scalar(R[:, g, k, :], ident[:], s1[:, g:g + 1], None, op0=ALU.mult), reads=[ident, s1], writes=[R])
            c.op(V, lambda e: e.scalar_tensor_tensor(out=R[:, g, k, :], in0=pswap[:], scalar=s2[:, g:g + 1], in1=R[:, g, k, :],
                                                     op0=ALU.mult, op1=ALU.add), reads=[pswap, s2, R], writes=[R])
        if k < NLEV - 1:
            emit_cdouble(c, qr, qi, t1, t2)

    blocks = [(0, 512), (512, 512), (1024, NSB - 1024)]
    Ub = [sb(f"Ub{g}", [128, NSB], BF16) for g in range(G4)]
    Xp = [sb(f"Xp{g}", [128, NSB + 1]) for g in range(G4)]
    for g in range(G4):
        c.dma("pool", Ub[g][:], U_d.h.ap()[g], writes=[Ub[g]])
        c.op(V, lambda e: e.memset(Xp[g][:, 0:1], 0.0), writes=[Xp[g]])
        for (c0, n) in blocks:
            ps = ps_rot.next()
            c.op("pe", lambda e: e.matmul(ps[:, :n], lhsT=W1[:, g, :], rhs=Ub[g][:, c0:c0 + n], start=True, stop=True), reads=[W1, Ub[g]], writes=[ps])
            c.op("act", lambda e: e.copy(out=Xp[g][:, 1 + c0:1 + c0 + n], in_=ps[:, :n]), reads=[ps], writes=[Xp[g]])
    for k in range(NLEV):
        d = 2 ** k
        if d >= NSB:
            break
        L = NSB - d
        for g in range(G4):
            pl = []
            cc = 0
            while cc < L:
                n = min(512, L - cc)
                ps = ps_rot.next()
                c.op("pe", lambda e: e.matmul(ps[:, :n], lhsT=R[:, g, k, :], rhs=Xp[g][:, 1 + cc:1 + cc + n], start=True, stop=True),
                     reads=[R, Xp[g]], writes=[ps])
                pl.append((ps, cc, n))
                cc += n
            for (ps, cc, n) in pl:
                c.op(V, lambda e: e.tensor_tensor(out=Xp[g][:, 1 + d + cc:1 + d + cc + n], in0=Xp[g][:, 1 + d + cc:1 + d + cc + n], in1=ps[:, :n], op=ALU.add),
                     reads=[ps, Xp[g]], writes=[Xp[g]])
    y_rot = Rot([sb(f"ysb{i}", [128, NSB]) for i in range(2)])
    for g in range(G4):
        ysb = y_rot.next()
        for (c0, n) in blocks:
            ps = ps_rot.next()
            c.op("pe", lambda e: e.matmul(ps[:, :n], lhsT=Toep[:, g, :], rhs=Ub[g][:, c0:c0 + n], start=True, stop=False), reads=[Toep, Ub[g]], writes=[ps])
            c.op("pe", lambda e: e.matmul(ps[:, :n], lhsT=Wc[:, g].rearrange("p j h -> p (j h)"), rhs=Xp[g][:, c0:c0 + n], start=False, stop=True),
                 reads=[Wc, Xp[g]], writes=[ps])
            c.op("act", lambda e: e.copy(out=ysb[:, c0:c0 + n], in_=ps[:, :n]), reads=[ps], writes=[ysb])
        c.dma("sp", y_d.h.ap()[g], ysb[:], reads=[ysb])
    c.finish()
    c.close()
    print("s5 instructions:", c.n_inst)
    return nc


NT = 2080
def core_tok_idx(c):
    b, r = divmod(c, 4)
    return b, np.concatenate([np.arange(32 * r, 32 * r + 32), 128 + np.arange(2048 * r, 2048 * (r + 1))])

def build_H0(x, meta):
    B = x.shape[0]
    H = np.zeros((B, 8320, 1024), np.float32)
    H[:, 112:128, :] = meta[None]
    H[:, 128:, :] = x
    return H

def shard_T(H):
    out = []
    for c in range(8):
        b, idx = core_tok_idx(c)
        out.append(np.ascontiguousarray(H[b, idx, :].T))
    return out

def unshard_T(lst, C):
    H = np.zeros((2, 8320, C), lst[0].dtype)
    for c in range(8):
        b, idx = core_tok_idx(c)
        H[b, idx, :] = lst[c].T
    return H

def swap_cols():
    base = np.arange(256).reshape(8, 2, 16)[:, ::-1, :].reshape(256)
    return base

def w_in_ext(w_in_l):
    sw = swap_cols()
    q_sw = w_in_l[:, 1280:1536][:, sw]
    k_sw = w_in_l[:, 1536:1792][:, sw]
    return np.ascontiguousarray(np.concatenate([w_in_l, q_sw, k_sw], axis=1))

def gvec(g):
    return np.ascontiguousarray(g.reshape(-1, 128).T)

def s5_inputs(inp, L, u_b, r):
    gs = [4 * r + g for g in range(4)]
    U = np.stack([u_b[:, 16 * G:16 * G + 16].reshape(1040, 8, 16).transpose(1, 2, 0).reshape(128, 1040) for G in gs])
    def dup(a):
        return np.concatenate([a, a], axis=0)
    lam = np.stack([dup(np.stack([inp['s5_lam_re'][L, G], inp['s5_lam_im'][L, G]], -1)) for G in gs], 1)
    ls = np.tile(inp['s5_log_step'][L, gs][None, :], (128, 1))
    Bm = np.stack([dup(np.stack([inp['s5_b_re'][L, G], inp['s5_b_im'][L, G]], 1)) for G in gs], 1)
    Cm = np.stack([dup(np.stack([inp['s5_c_re'][L, G].T, inp['s5_c_im'][L, G].T], 1)) for G in gs], 1)
    Dm = np.stack([np.tile(inp['s5_d'][L, G], 8) for G in gs], 1)
    f = lambda a: np.ascontiguousarray(a.astype(np.float32))
    return {"U": f(U), "lam": f(lam), "ls": f(ls), "Bm": f(Bm), "Cm": f(Cm), "Dm": f(Dm)}

def s5_unpack(y, out_b, r):
    for g in range(4):
        G = 4 * r + g
        out_b[:, 16 * G:16 * G + 16] = y[g].reshape(8, 16, 1040).transpose(2, 0, 1).reshape(8320, 16)

def s5_raw_ref(inp, L, u):
    Bn, T, _ = u.shape
    out = np.zeros((Bn, T, 256))
    for G in range(16):
        lam = inp['s5_lam_re'][L, G].astype(np.float64) + 1j * inp['s5_lam_im'][L, G]
        step = np.exp(np.float64(inp['s5_log_step'][L, G]))
        lb = np.exp(lam * step)
        Bc = inp['s5_b_re'][L, G].astype(np.float64) + 1j * inp['s5_b_im'][L, G]
        Cc = inp['s5_c_re'][L, G].astype(np.float64) + 1j * inp['s5_c_im'][L, G]
        Bb = ((lb - 1) / lam)[:, None] * Bc
        d = inp['s5_d'][L, G].astype(np.float64)
        for b in range(Bn):
            ug = u[b, :, 16 * G:16 * G + 16].astype(np.float64)
            bu = ug @ Bb.T
            S = np.zeros(64, complex)
            y = np.zeros((T, 16))
            CH = 64
            pw = lb[None, :] ** np.arange(1, CH + 1)[:, None]
            ipw = lb[None, :] ** (-np.arange(1, CH + 1)[:, None])
            for t0 in range(0, T, CH):
                blk = bu[t0:t0 + CH]
                st = pw * (S[None, :] + np.cumsum(blk * ipw, axis=0))
                y[t0:t0 + CH] = (st @ Cc.T).real
                S = st[-1]
            out[b, :, 16 * G:16 * G + 16] = y + d * ug
    return out


_PROGS = {}


def _prog(key, fn):
    if key not in _PROGS:
        _PROGS[key] = fn()
    return _PROGS[key]


def _run(nc, maps):
    res = run_bass_kernel_spmd(nc, maps, core_ids=list(range(8)))
    return res.results


def kernel(**inp):
    inp = {k: np.asarray(v) for k, v in inp.items()}
    f32 = lambda a: np.ascontiguousarray(a, dtype=np.float32)
    H = build_H0(inp['x'], inp['meta_tokens'])
    sw = swap_cols()
    for L in range(2):
        hs = shard_T(H)
        W = w_in_ext(inp['w_in'][L])
        g = gvec(inp['norm_mix_g'][L])
        rA = _run(_prog("A", build_A), [{"hT": hs[c], "g": g, "w": W} for c in range(8)])
        proj = unshard_T([r["projT"] for r in rA], NCOL_A)
        rq = proj[:, :, 1280:1536]; rk = proj[:, :, 1536:1792]; rv = proj[:, :, 1792:2304]; rg = proj[:, :, 2304:2816]
        rqs = proj[:, :, 2816:3072]; rks = proj[:, :, 3072:3328]
        maps = []
        for c in range(8):
            b, r = divmod(c, 4)
            hds = [2 * r, 2 * r + 1]
            maps.append({"q": f32(rq[b, :, 64 * r:64 * r + 64].T), "qsw": f32(rqs[b, :, 64 * r:64 * r + 64].T),
                         "k": f32(rk[b, :, 64 * r:64 * r + 64].T), "ksw": f32(rks[b, :, 64 * r:64 * r + 64].T),
                         "v": f32(rv[b, :, 128 * r:128 * r + 128]), "gate": f32(rg[b, :, 128 * r:128 * r + 128]),
                         "outg": f32(np.tile(inp['ret_out_g'][L][128 * r:128 * r + 128][None, :], (128, 1))),
                         "hpart": f32(np.repeat(np.array(hds, np.float32), 32)[:, None]),
                         "hsel": f32(np.tile(np.array(hds, np.float32)[None, :], (128, 1)))})
        rR = _run(_prog("ret", build_ret), maps)
        yc = np.zeros((2, 8320, 512), np.float32)
        for c in range(8):
            b, r = divmod(c, 4)
            yc[b, :, 128 * r:128 * r + 128] = rR[c]["y"]
        cwL = inp['hg_conv_w'][L]
        maps = []
        for c in range(8):
            b, r = divmod(c, 4)
            x3 = np.stack([proj[b, :, 256 + 256 * s + 64 * r: 256 + 256 * s + 64 * r + 64].T for s in range(3)])
            convw = np.stack([cwL[:, 256 * s + 64 * r: 256 * s + 64 * r + 64].T for s in range(3)])
            maps.append({"x3": f32(x3), "convw": f32(convw), "gate": f32(proj[b, :, 1024 + 64 * r:1024 + 64 * r + 64]),
                         "outg": f32(np.tile(inp['hg_out_g'][L][64 * r:64 * r + 64][None, :], (128, 1))),
                         "lbp": f32(inp['hg_lb_param'][:, 64 * r:64 * r + 64].T)})
        rHg = _run(_prog(("hg", L), lambda: build_hg(L)), maps)
        yb = np.zeros((2, 8320, 256), np.float32)
        for c in range(8):
            b, r = divmod(c, 4)
            yb[b, :, 64 * r:64 * r + 64] = rHg[c]["y"]
        maps = []
        for c in range(8):
            b, r = divmod(c, 4)
            maps.append(s5_inputs(inp, L, proj[b, :, 0:256], r))
        rS = _run(_prog("s5", build_s5), maps)
        ya = np.zeros((2, 8320, 256), np.float32)
        for c in range(8):
            b, r = divmod(c, 4)
            s5_unpack(rS[c]["y"], ya[b], r)
        yas = shard_T(ya); ybcs = shard_T(np.concatenate([yb, yc], -1))
        moe = (L % 2 == 1)
        final = (L == 1)
        if not moe:
            w1 = f32(inp['ffn_w1'][L // 2][None]); w3 = f32(inp['ffn_w3'][L // 2][None]); w2 = f32(inp['ffn_w2'][L // 2][None])
            nc = _prog(("C", 1, final), lambda: build_C(1, 2816, final))
        else:
            w1 = f32(inp['moe_w1'][L // 2]); w3 = f32(inp['moe_w3'][L // 2]); w2 = f32(inp['moe_w2'][L // 2])
            nc = _prog(("C", 8, final), lambda: build_C(8, 3584, final))
        maps = []
        for c in range(8):
            m = {"hT": hs[c], "yaT": yas[c], "ybcT": ybcs[c], "wglu": f32(inp['s5_w_glu'][L]), "s5g": gvec(inp['s5_out_g'][L]),
                 "wout": f32(inp['w_out'][L]), "gffn": gvec(inp['norm_ffn_g'][L]), "w1": w1, "w3": w3, "w2": w2}
            if moe:
                m["router"] = f32(inp['moe_router'][L // 2])
            if final:
                m["gfin"] = gvec(inp['final_norm_g'])
            maps.append(m)
        rC = _run(nc, maps)
        H = unshard_T([r["hT_out"] for r in rC], 1024)
    return np.ascontiguousarray(H[:, 128:, :], dtype=np.float32)
```

```python
import math
import contextlib
import numpy as np
import concourse.bass as bass
import concourse.mybir as mybir
from concourse.bass_utils import run_bass_kernel_spmd


F32 = mybir.dt.float32
BF16 = mybir.dt.bfloat16
I32 = mybir.dt.int32
AF = mybir.ActivationFunctionType
ALU = mybir.AluOpType
AX = mybir.AxisListType


class T:
    def __init__(self, ctx, name, handle, space):
        self.ctx = ctx
        self.name = name
        self.h = handle
        self.space = space
        self.st = {}
        self.dq = None

    def __getitem__(self, idx):
        return self.h[idx]

    def state(self, key):
        s = self.st.get(key)
        if s is None:
            s = {"w": None, "r": {}}
            self.st[key] = s
        return s


class Q:
    def __init__(self, ctx, name, step):
        self.ctx = ctx
        self.name = name
        self.sem = ctx.root.enter_context(ctx.nc.semaphore(name))
        self.count = 0
        self.step = step


class Ctx:
    def __init__(self, nc):
        self.nc = nc
        self.stack = contextlib.ExitStack()
        self.root = self.stack
        self.engs = {}
        for name, eng in (("pe", nc.tensor), ("dve", nc.vector), ("act", nc.scalar),
                          ("pool", nc.gpsimd), ("sp", nc.sync)):
            q = Q(self, "q_" + name, 1)
            self.engs[name] = (eng, q)
        self.seen = {name: {} for name in self.engs}
        self.dmaq = {}
        self.n_inst = 0
        self.uid = 0
        self.pe_self_sync = False

    def sb(self, name, shape, dtype):
        self.uid += 1
        h = self.stack.enter_context(self.nc.sbuf_tensor(f"{name}_{self.uid}", list(shape), dtype))
        return T(self, name, h, "sb")

    def ps(self, name, shape, dtype=F32):
        self.uid += 1
        h = self.stack.enter_context(self.nc.psum_tensor(f"{name}_{self.uid}", list(shape), dtype))
        return T(self, name, h, "ps")

    def dram(self, name, shape, dtype, kind):
        h = self.nc.dram_tensor(name, list(shape), dtype, kind=kind)
        return T(self, name, h, "dram")

    def dma_q(self, name):
        q = self.dmaq.get(name)
        if q is None:
            q = Q(self, "dq_" + name, 16)
            self.dmaq[name] = q
        return q

    def _need(self, engname, q, value):
        if q is None:
            return
        if engname == "pe" and q is self.engs["pe"][1] and not self.pe_self_sync:
            return
        seen = self.seen[engname]
        if q.step == 16:
            value = q.count
        if seen.get(q.name, 0) >= value:
            return
        eng = self.engs[engname][0]
        eng.wait_ge(q.sem, value)
        seen[q.name] = value

    def _deps(self, engname, reads, writes):
        for (t, key) in reads:
            s = t.state(key)
            if s["w"] is not None:
                self._need(engname, *s["w"])
        for (t, key) in writes:
            s = t.state(key)
            if s["w"] is not None:
                self._need(engname, *s["w"])
            for q, v in s["r"].values():
                self._need(engname, q, v)

    def _mark(self, q, value, reads, writes):
        for (t, key) in reads:
            s = t.state(key)
            s["r"][q.name] = (q, value)
        for (t, key) in writes:
            s = t.state(key)
            s["w"] = (q, value)
            s["r"] = {}

    @staticmethod
    def _norm(lst):
        out = []
        for x in lst:
            if isinstance(x, tuple):
                out.append(x)
            else:
                out.append((x, None))
        return out

    def op(self, engname, fn, reads=(), writes=()):
        reads = self._norm(reads)
        writes = self._norm(writes)
        eng, q = self.engs[engname]
        self._deps(engname, reads, writes)
        ins = fn(eng)
        q.count += 1
        ins.then_inc(q.sem, 1)
        self._mark(q, q.count, reads, writes)
        self.n_inst += 1
        return ins

    def dma(self, engname, out, in_, reads=(), writes=(), qt=None, **kw):
        reads = self._norm(reads)
        writes = self._norm(writes)
        eng, _ = self.engs[engname]
        self._deps(engname, reads, writes)
        if qt is None:
            cands = [t for (t, k) in writes if t.space == "sb"] + [t for (t, k) in reads if t.space == "sb"]
            qt = cands[0]
        if qt.dq is None:
            self.uid += 1
            qt.dq = Q(self, f"dq_{qt.name}_{self.uid}", 16)
            self.dmaq[qt.dq.name] = qt.dq
        dq = qt.dq
        ins = eng.dma_start(out=out, in_=in_, **kw)
        dq.count += 16
        ins.then_inc(dq.sem, 16)
        self._mark(dq, dq.count, reads, writes)
        self.n_inst += 1
        return ins

    def barrier(self):
        for name, (eng, q0) in self.engs.items():
            for dq in self.dmaq.values():
                if dq.count:
                    self._need(name, dq, dq.count)
            for other, (e2, q2) in self.engs.items():
                if other != name and q2.count:
                    self._need(name, q2, q2.count)

    @contextlib.contextmanager
    def scope(self):
        old = self.stack
        self.stack = contextlib.ExitStack()
        try:
            yield
        finally:
            self.barrier()
            self.stack.close()
            self.stack = old

    def finish(self, engname="sp"):
        eng = self.engs[engname][0]
        for dq in self.dmaq.values():
            if dq.count:
                eng.wait_ge(dq.sem, dq.count)
        for name, (e, q) in self.engs.items():
            if q.count and name != engname:
                eng.wait_ge(q.sem, q.count)

    def close(self):
        self.stack.close()


NT = 2080
D = 1024
KC = 8
EPS = 1e-6
NCOL_A = 3328


def tblocks(nt=NT, bs=512):
    if nt == 2080 and bs == 512:
        return [(416 * i, 416) for i in range(5)]
    out = []
    t = 0
    while t < nt:
        n = min(bs, nt - t)
        out.append((t, n))
        t += n
    return out


class Rot:
    def __init__(self, tiles):
        self.tiles = tiles
        self.i = 0

    def next(self):
        t = self.tiles[self.i % len(self.tiles)]
        self.i += 1
        return t


def emit_consts(c):
    k = {}
    k["ones_bf"] = c.sb("ones_bf", [128, 128], BF16)
    c.op("pool", lambda e: e.memset(k["ones_bf"][:], 1.0), writes=[k["ones_bf"]])
    return k


def emit_rmsnorm(c, k, hT, g_sb, hnT, sq_rot, rs_rot, ps_rot, d_chunks=KC, dim=D, src_key=True):
    for bi, (t0, n) in enumerate(tblocks()):
        sq = sq_rot.next()
        c.op("act", lambda e: e.activation(out=sq[:, :d_chunks, :n], in_=hT[:, :, t0:t0 + n], func=AF.Square),
             reads=[(hT, bi)], writes=[sq])
        ps = ps_rot.next()
        for kc in range(d_chunks):
            c.op("pe", lambda e: e.matmul(ps[:, :n], lhsT=k["ones_bf"][:], rhs=sq[:, kc, :n],
                                          start=(kc == 0), stop=(kc == d_chunks - 1)),
                 reads=[sq, k["ones_bf"]], writes=[ps])
        rs = rs_rot.next()
        c.op("act", lambda e: e.activation(out=rs[:, :n], in_=ps[:, :n], func=AF.Sqrt, scale=1.0 / dim, bias=k["eps"][:, 0:1]),
             reads=[ps, k["eps"]], writes=[rs])
        c.op("dve", lambda e: e.reciprocal(rs[:, :n], rs[:, :n]), reads=[rs], writes=[rs])
        for kc in range(d_chunks):
            c.op("dve", lambda e: e.scalar_tensor_tensor(out=hnT[:, kc, t0:t0 + n], in0=hT[:, kc, t0:t0 + n],
                                                         scalar=g_sb[:, kc:kc + 1], in1=rs[:, :n],
                                                         op0=ALU.mult, op1=ALU.mult),
                 reads=[(hT, bi), rs, g_sb], writes=[(hnT, bi)])


def build_A():
    nc = bass.Bass("TRN2", target_bir_lowering=False)
    c = Ctx(nc)
    hT_d = c.dram("hT", [D, NT], F32, "ExternalInput")
    g_d = c.dram("g", [128, KC], F32, "ExternalInput")
    w_d = c.dram("w", [D, NCOL_A], F32, "ExternalInput")
    out_d = c.dram("projT", [NCOL_A, NT], F32, "ExternalOutput")

    k = emit_consts(c)
    k["eps"] = c.sb("eps", [128, 1], F32)
    c.op("pool", lambda e: e.memset(k["eps"][:], EPS), writes=[k["eps"]])
    hT = c.sb("hT", [128, KC, NT], F32)
    hnT = c.sb("hnT", [128, KC, NT], BF16)
    g_sb = c.sb("g", [128, KC], F32)
    c.dma("sp", g_sb[:], g_d[:], writes=[g_sb])
    hT_v = hT_d.h.ap().rearrange("(kc kp) t -> kp kc t", kp=128)
    for bi, (t0, n) in enumerate(tblocks()):
        c.dma("sp", hT[:, :, t0:t0 + n], hT_v[:, :, t0:t0 + n], writes=[(hT, bi)])
    sq_rot = Rot([c.sb(f"sq{i}", [128, KC, 512], BF16) for i in range(2)])
    rs_rot = Rot([c.sb(f"rs{i}", [128, 512], F32) for i in range(2)])
    ps_rot = Rot([c.ps(f"ps{i}", [128, 512], F32) for i in range(6)])
    emit_rmsnorm(c, k, hT, g_sb, hnT, sq_rot, rs_rot, ps_rot)

    w_rot = Rot([c.sb(f"w{i}", [128, KC, 512], BF16) for i in range(2)])
    w_st = c.sb("w_st", [128, KC, 512], F32)
    ost_rot = Rot([c.sb(f"ost{i}", [128, NT], F32) for i in range(3)])
    w_v = w_d.h.ap().rearrange("(kc kp) n -> kp kc n", kp=128)
    ev = 0
    for c0 in range(0, NCOL_A, 512):
        ncol = min(512, NCOL_A - c0)
        w_sb = w_rot.next()
        c.dma("sp", w_st[:, :, :ncol], w_v[:, :, c0:c0 + ncol], writes=[w_st])
        c.op("pool", lambda e: e.tensor_copy(w_sb[:, :, :ncol], w_st[:, :, :ncol]), reads=[w_st], writes=[w_sb])
        for cc in range(ncol // 128):
            ost = ost_rot.next()
            for bi, (t0, n) in enumerate(tblocks()):
                ps = ps_rot.next()
                for kc in range(KC):
                    c.op("pe", lambda e: e.matmul(ps[:, :n], lhsT=w_sb[:, kc, cc * 128:(cc + 1) * 128],
                                                  rhs=hnT[:, kc, t0:t0 + n], start=(kc == 0), stop=(kc == KC - 1)),
                         reads=[w_sb, (hnT, bi)], writes=[ps])
                if ev % 2 == 0:
                    c.op("act", lambda e: e.copy(out=ost[:, t0:t0 + n], in_=ps[:, :n]), reads=[ps], writes=[ost])
                else:
                    c.op("dve", lambda e: e.tensor_copy(ost[:, t0:t0 + n], ps[:, :n]), reads=[ps], writes=[ost])
                ev += 1
            r0 = c0 + cc * 128
            c.dma("sp", out_d[r0:r0 + 128, :], ost[:], reads=[ost])
    c.finish()
    c.close()
    print("phase A instructions:", c.n_inst)
    return nc


GF = 2
GH = 2


def emit_rmsnorm2(c, k, src, g_sb, dst_fn, sq_rot, rs_rot, ps_rot, d_chunks, dim, src_keyed=True):
    for bi, (t0, n) in enumerate(tblocks()):
        sk = (src, bi) if src_keyed else src
        sq = sq_rot.next()
        c.op("act", lambda e: e.activation(out=sq[:, :d_chunks, :n], in_=src[:, :, t0:t0 + n], func=AF.Square),
             reads=[sk], writes=[sq])
        ps = ps_rot.next()
        for kc in range(d_chunks):
            c.op("pe", lambda e: e.matmul(ps[:, :n], lhsT=k["ones_bf"][:], rhs=sq[:, kc, :n],
                                          start=(kc == 0), stop=(kc == d_chunks - 1)),
                 reads=[sq, k["ones_bf"]], writes=[ps])
        rs = rs_rot.next()
        c.op("act", lambda e: e.activation(out=rs[:, :n], in_=ps[:, :n], func=AF.Sqrt, scale=1.0 / dim, bias=k["eps"][:, 0:1]),
             reads=[ps, k["eps"]], writes=[rs])
        c.op("dve", lambda e: e.reciprocal(rs[:, :n], rs[:, :n]), reads=[rs], writes=[rs])
        for kc in range(d_chunks):
            ap, wk = dst_fn(bi, t0, n, kc)
            c.op("dve", lambda e: e.scalar_tensor_tensor(out=ap, in0=src[:, kc, t0:t0 + n],
                                                         scalar=g_sb[:, kc:kc + 1], in1=rs[:, :n],
                                                         op0=ALU.mult, op1=ALU.mult),
                 reads=[sk, rs, g_sb], writes=[wk])


def build_C(n_exp, F, final):
    moe = n_exp > 1
    nc = bass.Bass("TRN2", target_bir_lowering=False)
    c = Ctx(nc)
    hT_d = c.dram("hT", [D, NT], F32, "ExternalInput")
    ya_d = c.dram("yaT", [256, NT], F32, "ExternalInput")
    ybc_d = c.dram("ybcT", [768, NT], F32, "ExternalInput")
    wglu_d = c.dram("wglu", [256, 256], F32, "ExternalInput")
    s5g_d = c.dram("s5g", [128, 2], F32, "ExternalInput")
    wout_d = c.dram("wout", [D, D], F32, "ExternalInput")
    gffn_d = c.dram("gffn", [128, KC], F32, "ExternalInput")
    w1_d = c.dram("w1", [n_exp, D, F], F32, "ExternalInput")
    w3_d = c.dram("w3", [n_exp, D, F], F32, "ExternalInput")
    w2_d = c.dram("w2", [n_exp, F, D], F32, "ExternalInput")
    if moe:
        rt_d = c.dram("router", [D, 8], F32, "ExternalInput")
    if final:
        gfin_d = c.dram("gfin", [128, KC], F32, "ExternalInput")
    out_d = c.dram("hT_out", [D, NT], F32, "ExternalOutput")

    k = emit_consts(c)
    k["eps"] = c.sb("eps", [128, 1], F32)
    c.op("pool", lambda e: e.memset(k["eps"][:], EPS), writes=[k["eps"]])
    TB = tblocks()

    hT = c.sb("hT", [128, KC, NT], F32)
    hT_v = hT_d.h.ap().rearrange("(kc kp) t -> kp kc t", kp=128)
    for bi, (t0, n) in enumerate(TB):
        c.dma("sp", hT[:, :, t0:t0 + n], hT_v[:, :, t0:t0 + n], writes=[(hT, bi)], qt=hT)
    gffn = c.sb("gffn", [128, KC], F32)
    c.dma("sp", gffn[:], gffn_d[:], writes=[gffn])
    s5g = c.sb("s5g", [128, 2], F32)
    c.dma("sp", s5g[:], s5g_d[:], writes=[s5g])
    if final:
        gfin = c.sb("gfin", [128, KC], F32)
        c.dma("sp", gfin[:], gfin_d[:], writes=[gfin])

    ps_all = [c.ps(f"ps{i}", [128, 512], F32) for i in range(8)]
    ps_rot = Rot(ps_all[:7])

    with c.scope():
        sq_rot = Rot([c.sb(f"sq{i}", [128, KC, 512], BF16) for i in range(2)])
        rs_rot = Rot([c.sb(f"rs{i}", [128, 512], F32) for i in range(2)])
        yaT = c.sb("yaT", [128, 2, NT], F32)
        c.dma("sp", yaT[:], ya_d.h.ap().rearrange("(kc kp) t -> kp kc t", kp=128), writes=[yaT])
        mixedT = c.sb("mixedT", [128, KC, NT], BF16)
        ybc_v = ybc_d.h.ap().rearrange("(kc kp) t -> kp kc t", kp=128)
        for bi, (t0, n) in enumerate(TB):
            c.dma("pool", mixedT[:, 2:8, t0:t0 + n], ybc_v[:, :, t0:t0 + n], writes=[(mixedT, ("bc", bi))], qt=mixedT)
        wglu = c.sb("wglu", [128, 2, 256], BF16)
        c.dma("pool", wglu[:], wglu_d.h.ap().rearrange("(kc kp) n -> kp kc n", kp=128), writes=[wglu])
        wout = c.sb("wout", [128, KC, D], BF16)
        c.dma("pool", wout[:], wout_d.h.ap().rearrange("(kc kp) n -> kp kc n", kp=128), writes=[wout])

        t1_rot = Rot([c.sb(f"t1_{i}", [128, 2, 512], F32) for i in range(1)])
        t2_rot = Rot([c.sb(f"t2_{i}", [128, 2, 512], F32) for i in range(1)])
        ygf_rot = Rot([c.sb(f"ygf{i}", [128, 2, 512], F32) for i in range(1)])
        ygb_rot = Rot([c.sb(f"ygb{i}", [128, 2, 512], BF16) for i in range(2)])
        yaf_rot = Rot([c.sb(f"yaf{i}", [128, 2, 512], F32) for i in range(1)])
        sg_rot = Rot([c.sb(f"sg{i}", [128, 512], F32) for i in range(2)])
        for bi, (t0, n) in enumerate(TB):
            x = yaT[:, :, t0:t0 + n]
            t1 = t1_rot.next(); t2 = t2_rot.next(); ygf = ygf_rot.next(); ygb = ygb_rot.next(); yaf = yaf_rot.next()
            c.op("act", lambda e: e.activation(out=t1[:, :, :n], in_=x, func=AF.Square), reads=[yaT], writes=[t1])
            c.op("dve", lambda e: e.tensor_scalar(t1[:, :, :n], t1[:, :, :n], 0.044715, 1.0, op0=ALU.mult, op1=ALU.add),
                 reads=[t1], writes=[t1])
            c.op("dve", lambda e: e.tensor_tensor(out=t1[:, :, :n], in0=t1[:, :, :n], in1=x, op=ALU.mult),
                 reads=[t1, yaT], writes=[t1])
            c.op("act", lambda e: e.activation(out=t2[:, :, :n], in_=t1[:, :, :n], func=AF.Sigmoid, scale=1.5957691216057308),
                 reads=[t1], writes=[t2])
            c.op("dve", lambda e: e.tensor_tensor(out=ygf[:, :, :n], in0=t2[:, :, :n], in1=x, op=ALU.mult),
                 reads=[t2, yaT], writes=[ygf])
            c.op("act", lambda e: e.copy(out=ygb[:, :, :n], in_=ygf[:, :, :n]), reads=[ygf], writes=[ygb])
            for mo in range(2):
                ps = ps_rot.next()
                for ch in range(2):
                    c.op("pe", lambda e: e.matmul(ps[:, :n], lhsT=wglu[:, ch, mo * 128:(mo + 1) * 128], rhs=ygb[:, ch, :n],
                                                  start=(ch == 0), stop=(ch == 1)), reads=[wglu, ygb], writes=[ps])
                sg = sg_rot.next()
                c.op("act", lambda e: e.activation(out=sg[:, :n], in_=ps[:, :n], func=AF.Sigmoid), reads=[ps], writes=[sg])
                c.op("dve", lambda e: e.tensor_tensor(out=yaf[:, mo, :n], in0=ygf[:, mo, :n], in1=sg[:, :n], op=ALU.mult),
                     reads=[ygf, sg], writes=[yaf])
            sq = sq_rot.next()
            c.op("act", lambda e: e.activation(out=sq[:, :2, :n], in_=yaf[:, :, :n], func=AF.Square), reads=[yaf], writes=[sq])
            ps = ps_rot.next()
            for ch in range(2):
                c.op("pe", lambda e: e.matmul(ps[:, :n], lhsT=k["ones_bf"][:], rhs=sq[:, ch, :n], start=(ch == 0), stop=(ch == 1)),
                     reads=[sq, k["ones_bf"]], writes=[ps])
            rs = rs_rot.next()
            c.op("act", lambda e: e.activation(out=rs[:, :n], in_=ps[:, :n], func=AF.Sqrt, scale=1.0 / 256, bias=k["eps"][:, 0:1]),
                 reads=[ps, k["eps"]], writes=[rs])
            c.op("dve", lambda e: e.reciprocal(rs[:, :n], rs[:, :n]), reads=[rs], writes=[rs])
            for ch in range(2):
                c.op("dve", lambda e: e.scalar_tensor_tensor(out=mixedT[:, ch, t0:t0 + n], in0=yaf[:, ch, :n],
                                                             scalar=s5g[:, ch:ch + 1], in1=rs[:, :n], op0=ALU.mult, op1=ALU.mult),
                     reads=[yaf, rs, s5g], writes=[(mixedT, ("a", bi))])
            for dch in range(KC):
                ps = ps_rot.next()
                for cc in range(KC):
                    c.op("pe", lambda e: e.matmul(ps[:, :n], lhsT=wout[:, cc, dch * 128:(dch + 1) * 128], rhs=mixedT[:, cc, t0:t0 + n],
                                                  start=(cc == 0), stop=(cc == KC - 1)),
                         reads=[wout, (mixedT, ("a", bi)), (mixedT, ("bc", bi))], writes=[ps])
                c.op("dve", lambda e: e.tensor_tensor(out=hT[:, dch, t0:t0 + n], in0=hT[:, dch, t0:t0 + n], in1=ps[:, :n], op=ALU.add),
                     reads=[(hT, bi), ps], writes=[(hT, bi)])

    hnT = c.sb("hnT", [128, KC, NT], BF16)
    with c.scope():
        sq_rot = Rot([c.sb(f"sq{i}", [128, KC, 512], BF16) for i in range(2)])
        rs_rot = Rot([c.sb(f"rs{i}", [128, 512], F32) for i in range(2)])
        emit_rmsnorm2(c, k, hT, gffn, lambda bi, t0, n, kc: (hnT[:, kc, t0:t0 + n], (hnT, bi)), sq_rot, rs_rot, ps_rot, KC, D)

    if moe:
        gatesT = c.sb("gatesT", [8, NT], BF16)
        with c.scope():
            ident = c.sb("identf", [128, 128], F32)
            iof = c.sb("iof", [128, 128], F32)
            c.op("pool", lambda e: e.iota(iof[:], [[1, 128]], base=0, channel_multiplier=-1, allow_small_or_imprecise_dtypes=True), writes=[iof])
            c.op("dve", lambda e: e.tensor_single_scalar(ident[:], iof[:], 0.0, op=ALU.is_equal), reads=[iof], writes=[ident])
            rt = c.sb("rt", [128, KC, 8], F32)
            c.dma("sp", rt[:], rt_d.h.ap().rearrange("(kc kp) e -> kp kc e", kp=128), writes=[rt])
            gr = c.sb("gr", [128, KC, 16], F32)
            c.op("dve", lambda e: e.memset(gr[:], 0.0), writes=[gr])
            for kc in range(KC):
                c.op("dve", lambda e: e.tensor_scalar(gr[:, kc, 0:8], rt[:, kc, :], gffn[:, kc:kc + 1], None, op0=ALU.mult),
                     reads=[rt, gffn], writes=[gr])
            onesf = c.sb("onesf", [128, 1], F32)
            c.op("dve", lambda e: e.memset(onesf[:], 1.0), writes=[onesf])
            k["gatesT"] = gatesT
            sqf_rot = Rot([c.sb(f"sqf{i}", [128, KC, 128], F32) for i in range(2)])
            sm_rot = Rot([c.sb(f"sm{i}", [128, 64], F32) for i in range(3)])
            for ti, (t0, n) in enumerate(tblocks(NT, 128)):
                bi = t0 // 512
                sqf = sqf_rot.next()
                c.op("act", lambda e: e.activation(out=sqf[:, :, :n], in_=hT[:, :, t0:t0 + n], func=AF.Square), reads=[(hT, b_) for b_ in range(5)], writes=[sqf])
                ps = ps_all[7]
                for kc in range(KC):
                    c.op("pe", lambda e: e.matmul(ps[:n, 0:8], lhsT=hT[:, kc, t0:t0 + n], rhs=gr[:, kc, 0:8], start=(kc == 0), stop=(kc == KC - 1)),
                         reads=[(hT, b_) for b_ in range(5)] + [gr], writes=[ps])
                for kc in range(KC):
                    c.op("pe", lambda e: e.matmul(ps[:n, 8:9], lhsT=sqf[:, kc, :n], rhs=onesf[:, 0:1], start=(kc == 0), stop=(kc == KC - 1)),
                         reads=[sqf, onesf], writes=[ps])
                sm = sm_rot.next()
                c.op("act", lambda e: e.activation(out=sm[:n, 0:1], in_=ps[:n, 8:9], func=AF.Sqrt, scale=1.0 / D, bias=k["eps"][:n, 0:1]),
                     reads=[ps, k["eps"]], writes=[sm])
                c.op("dve", lambda e: e.reciprocal(sm[:n, 0:1], sm[:n, 0:1]), reads=[sm], writes=[sm])
                c.op("dve", lambda e: e.tensor_scalar(sm[:n, 8:16], ps[:n, 0:8], sm[:n, 0:1], None, op0=ALU.mult), reads=[ps, sm], writes=[sm])
                c.op("dve", lambda e: e.max(out=sm[:n, 16:24], in_=sm[:n, 8:16]), reads=[sm], writes=[sm])
                c.op("dve", lambda e: e.tensor_tensor(out=sm[:n, 24:25], in0=sm[:n, 17:18], in1=sm[:n, 16:17], op=ALU.subtract), reads=[sm], writes=[sm])
                c.op("act", lambda e: e.activation(out=sm[:n, 24:25], in_=sm[:n, 24:25], func=AF.Exp), reads=[sm], writes=[sm])
                c.op("dve", lambda e: e.tensor_scalar(sm[:n, 25:26], sm[:n, 24:25], 1.0, None, op0=ALU.add), reads=[sm], writes=[sm])
                c.op("dve", lambda e: e.reciprocal(sm[:n, 25:26], sm[:n, 25:26]), reads=[sm], writes=[sm])
                c.op("dve", lambda e: e.tensor_tensor(out=sm[:n, 26:27], in0=sm[:n, 24:25], in1=sm[:n, 25:26], op=ALU.mult), reads=[sm], writes=[sm])
                c.op("dve", lambda e: e.tensor_scalar(sm[:n, 32:40], sm[:n, 8:16], sm[:n, 16:17], sm[:n, 25:26], op0=ALU.is_equal, op1=ALU.mult), reads=[sm], writes=[sm])
                c.op("dve", lambda e: e.tensor_scalar(sm[:n, 40:48], sm[:n, 8:16], sm[:n, 17:18], sm[:n, 26:27], op0=ALU.is_equal, op1=ALU.mult), reads=[sm], writes=[sm])
                c.op("dve", lambda e: e.tensor_tensor(out=sm[:n, 48:56], in0=sm[:n, 32:40], in1=sm[:n, 40:48], op=ALU.add), reads=[sm], writes=[sm])
                c.op("pe", lambda e: e.transpose(ps[0:8, 16:16 + n], sm[:n, 48:56], ident[:n, :n]), reads=[sm, ident], writes=[ps])
                c.op("act", lambda e: e.copy(out=gatesT[:, t0:t0 + n], in_=ps[0:8, 16:16 + n]), reads=[ps], writes=[gatesT])
        sel = c.sb("sel", [8, 8, 128], BF16)
        self_f = c.sb("sel_f", [8, 8, 128], F32)
        c.op("pool", lambda e: e.iota(self_f[:], [[-1, 8], [0, 128]], base=0, channel_multiplier=1, allow_small_or_imprecise_dtypes=True), writes=[self_f])
        c.op("dve", lambda e: e.tensor_single_scalar(sel[:], self_f[:], 0.0, op=ALU.is_equal), reads=[self_f], writes=[sel])

    with c.scope():
        ps_a = Rot(ps_all[0:2]); ps_b = Rot(ps_all[2:4]); ps_o = Rot(ps_all[4:7]); ps_g = Rot(ps_all[7:8])
        NWB = 3
        w1_rot = Rot([c.sb(f"w1g{i}", [128, KC, GF * 128], BF16) for i in range(NWB)])
        w3_rot = Rot([c.sb(f"w3g{i}", [128, KC, GF * 128], BF16) for i in range(NWB)])
        w2_rot = Rot([c.sb(f"w2g{i}", [128, GF, D], BF16) for i in range(NWB)])
        w1s = c.sb("w1s", [128, KC, GH * 128], F32)
        w3s = c.sb("w3s", [128, KC, GH * 128], F32)
        w2s = c.sb("w2s", [128, GF, D], F32)
        sa_rot = Rot([c.sb(f"sa{i}", [128, 512], F32) for i in range(2)])
        gT_rot = Rot([c.sb(f"gT{i}", [128, GF, 512], BF16) for i in range(3)])
        ngrp = F // (GF * 128)
        groups = [(ex, gi) for ex in range(n_exp) for gi in range(ngrp)]
        wbuf = {}

        def load_dma(gidx):
            ex, gi = groups[gidx]
            w1_v = w1_d.h.ap()[ex].rearrange("(kc kp) f -> kp kc f", kp=128)
            w3_v = w3_d.h.ap()[ex].rearrange("(kc kp) f -> kp kc f", kp=128)
            w2_v = w2_d.h.ap()[ex].rearrange("(fc fp) d -> fp fc d", fp=128)
            f0 = gi * GF * 128
            c.dma("sp", w1s[:], w1_v[:, :, f0:f0 + GF * 128], writes=[w1s])
            c.dma("sp", w3s[:], w3_v[:, :, f0:f0 + GF * 128], writes=[w3s])
            c.dma("sp", w2s[:], w2_v[:, gi * GF:(gi + 1) * GF, :], writes=[w2s])

        def load_cast(gidx):
            w1g = w1_rot.next(); w3g = w3_rot.next(); w2g = w2_rot.next()
            c.op("act", lambda e: e.copy(out=w1g[:], in_=w1s[:]), reads=[w1s], writes=[w1g])
            c.op("act", lambda e: e.copy(out=w3g[:], in_=w3s[:]), reads=[w3s], writes=[w3g])
            c.op("act", lambda e: e.copy(out=w2g[:], in_=w2s[:]), reads=[w2s], writes=[w2g])
            wbuf[gidx] = (w1g, w3g, w2g)

        def load_group(gidx):
            load_dma(gidx)
            load_cast(gidx)

        gsb_rot = Rot([c.sb(f"gsb{i}", [128, 512], BF16) for i in range(2)]) if moe else None
        sa2_rot = Rot([c.sb(f"sa2_{i}", [128, 512], F32) for i in range(2)]) if moe else None

        def stage1_fc(gidx, bi, t0, n, fc, gT, gsb):
            ex, gi = groups[gidx]
            w1g, w3g, w2g = wbuf[gidx]
            pa = ps_a.next(); pb = ps_b.next()
            for kc in range(KC):
                c.op("pe", lambda e: e.matmul(pa[:, :n], lhsT=w1g[:, kc, fc * 128:(fc + 1) * 128], rhs=hnT[:, kc, t0:t0 + n],
                                              start=(kc == 0), stop=(kc == KC - 1)), reads=[w1g, (hnT, bi)], writes=[pa])
            for kc in range(KC):
                c.op("pe", lambda e: e.matmul(pb[:, :n], lhsT=w3g[:, kc, fc * 128:(fc + 1) * 128], rhs=hnT[:, kc, t0:t0 + n],
                                              start=(kc == 0), stop=(kc == KC - 1)), reads=[w3g, (hnT, bi)], writes=[pb])
            sa = sa_rot.next()
            c.op("act", lambda e: e.activation(out=sa[:, :n], in_=pa[:, :n], func=AF.Silu), reads=[pa], writes=[sa])
            if moe:
                sa2 = sa2_rot.next()
                c.op("pool", lambda e: e.tensor_tensor(out=sa2[:, :n], in0=sa[:, :n], in1=gsb[:, :n], op=ALU.mult), reads=[sa, gsb], writes=[sa2])
                return (sa2, pb)
            return (sa, pb)

        def stage1_fc_b(n, fc, gT, st):
            sx, pb = st
            c.op("dve", lambda e: e.tensor_tensor(out=gT[:, fc, :n], in0=sx[:, :n], in1=pb[:, :n], op=ALU.mult), reads=[sx, pb], writes=[gT])

        def stage1_gate(gidx, bi, t0, n):
            ex, gi = groups[gidx]
            pg = ps_g.next()
            c.op("pe", lambda e: e.matmul(pg[:, :n], lhsT=sel[:, ex, :], rhs=k["gatesT"][:, t0:t0 + n], start=True, stop=True),
                 reads=[sel, k["gatesT"]], writes=[pg])
            gsb = gsb_rot.next()
            c.op("act", lambda e: e.copy(out=gsb[:, :n], in_=pg[:, :n]), reads=[pg], writes=[gsb])
            return gsb

        def stage2_part(gidx, bi, t0, n, gT, d0, d1):
            w1g, w3g, w2g = wbuf[gidx]
            for dch in range(d0, d1):
                po = ps_o.next()
                for fc in range(GF):
                    c.op("pe", lambda e: e.matmul(po[:, :n], lhsT=w2g[:, fc, dch * 128:(dch + 1) * 128], rhs=gT[:, fc, :n],
                                                  start=(fc == 0), stop=(fc == GF - 1)), reads=[w2g, gT], writes=[po])
                c.op("dve", lambda e: e.tensor_tensor(out=hT[:, dch, t0:t0 + n], in0=hT[:, dch, t0:t0 + n], in1=po[:, :n], op=ALU.add),
                     reads=[(hT, bi), po], writes=[(hT, bi)])

        load_group(0)
        if len(groups) > 1:
            load_group(1)
        pending = None
        DS = KC // GF
        for gidx in range(len(groups)):
            for bi, (t0, n) in enumerate(TB):
                gsb = stage1_gate(gidx, bi, t0, n) if moe else None
                gT = gT_rot.next()
                for fc in range(GF):
                    st = stage1_fc(gidx, bi, t0, n, fc, gT, gsb)
                    if pending is not None:
                        stage2_part(*pending, fc * DS, (fc + 1) * DS)
                    stage1_fc_b(n, fc, gT, st)
                pending = (gidx, bi, t0, n, gT)
                if bi == 0 and gidx + 2 < len(groups):
                    load_dma(gidx + 2)
                if bi == 3 and gidx + 2 < len(groups):
                    load_cast(gidx + 2)
        stage2_part(*pending, 0, KC)

    out_v = out_d.h.ap().rearrange("(kc kp) t -> kp kc t", kp=128)
    if final:
        sq_rot = Rot([c.sb(f"sq{i}", [128, KC, 512], BF16) for i in range(2)])
        rs_rot = Rot([c.sb(f"rs{i}", [128, 512], F32) for i in range(2)])
        fo_rot = Rot([c.sb(f"fo{i}", [128, KC, 512], F32) for i in range(2)])
        cur = {}

        def dst(bi, t0, n, kc):
            if kc == 0:
                cur["t"] = fo_rot.next()
            return cur["t"][:, kc, :n], cur["t"]
        for bi, (t0, n) in enumerate(TB):
            pass
        emit_final(c, k, hT, gfin, fo_rot, out_v, sq_rot, rs_rot, Rot(ps_all[0:6]))
    else:
        for bi, (t0, n) in enumerate(TB):
            c.dma("sp", out_v[:, :, t0:t0 + n], hT[:, :, t0:t0 + n], reads=[(hT, bi)], qt=hT)
    c.finish()
    c.close()
    print("phase C instructions:", c.n_inst)
    return nc


def emit_final(c, k, hT, gfin, fo_rot, out_v, sq_rot, rs_rot, ps_rot):
    for bi, (t0, n) in enumerate(tblocks()):
        sq = sq_rot.next()
        c.op("act", lambda e: e.activation(out=sq[:, :, :n], in_=hT[:, :, t0:t0 + n], func=AF.Square), reads=[(hT, bi)], writes=[sq])
        ps = ps_rot.next()
        for kc in range(KC):
            c.op("pe", lambda e: e.matmul(ps[:, :n], lhsT=k["ones_bf"][:], rhs=sq[:, kc, :n], start=(kc == 0), stop=(kc == KC - 1)),
                 reads=[sq, k["ones_bf"]], writes=[ps])
        rs = rs_rot.next()
        c.op("act", lambda e: e.activation(out=rs[:, :n], in_=ps[:, :n], func=AF.Sqrt, scale=1.0 / D, bias=k["eps"][:, 0:1]),
             reads=[ps, k["eps"]], writes=[rs])
        c.op("dve", lambda e: e.reciprocal(rs[:, :n], rs[:, :n]), reads=[rs], writes=[rs])
        fo = fo_rot.next()
        for kc in range(KC):
            c.op("dve", lambda e: e.scalar_tensor_tensor(out=fo[:, kc, :n], in0=hT[:, kc, t0:t0 + n], scalar=gfin[:, kc:kc + 1], in1=rs[:, :n],
                                                         op0=ALU.mult, op1=ALU.mult), reads=[(hT, bi), rs, gfin], writes=[fo])
        c.dma("sp", out_v[:, :, t0:t0 + n], fo[:, :, :n], reads=[fo])


NTOK = 8320
NCH = 65
EPS = 1e-6
CB = 5


def emit_pow_table(c, P, n, bT, br, bi, save_at=None):
    Gr = c.sb("Gr", [P, n], F32)
    Gi = c.sb("Gi", [P, n], F32)
    tmp = c.sb("Gtmp", [P, max(n // 2, 1)], F32)
    s = c.sb("Gs", [P, 6], F32)
    saved = c.sb("Gsaved", [P, 2], F32) if save_at else None
    V = "dve"
    c.op(V, lambda e: e.memset(Gr[:, 0:1], 1.0), writes=[Gr])
    c.op(V, lambda e: e.memset(Gi[:, 0:1], 0.0), writes=[Gi])
    c.op(V, lambda e: e.tensor_copy(s[:, 0:1], br), reads=[bT], writes=[s])
    c.op(V, lambda e: e.tensor_copy(s[:, 1:2], bi), reads=[bT], writes=[s])
    m = 1
    while m < n:
        if save_at == m:
            c.op(V, lambda e: e.tensor_copy(saved[:, 0:2], s[:, 0:2]), reads=[s], writes=[saved])
        c.op(V, lambda e: e.tensor_scalar(tmp[:, :m], Gi[:, :m], s[:, 1:2], None, op0=ALU.mult), reads=[Gi, s], writes=[tmp])
        c.op(V, lambda e: e.scalar_tensor_tensor(out=Gr[:, m:2 * m], in0=Gr[:, :m], scalar=s[:, 0:1], in1=tmp[:, :m],
                                                 op0=ALU.mult, op1=ALU.subtract), reads=[Gr, s, tmp], writes=[Gr])
        c.op(V, lambda e: e.tensor_scalar(tmp[:, :m], Gi[:, :m], s[:, 0:1], None, op0=ALU.mult), reads=[Gi, s], writes=[tmp])
        c.op(V, lambda e: e.scalar_tensor_tensor(out=Gi[:, m:2 * m], in0=Gr[:, :m], scalar=s[:, 1:2], in1=tmp[:, :m],
                                                 op0=ALU.mult, op1=ALU.add), reads=[Gr, s, tmp], writes=[Gi])
        c.op(V, lambda e: e.tensor_tensor(out=s[:, 2:3], in0=s[:, 0:1], in1=s[:, 0:1], op=ALU.mult), reads=[s], writes=[s])
        c.op(V, lambda e: e.tensor_tensor(out=s[:, 3:4], in0=s[:, 1:2], in1=s[:, 1:2], op=ALU.mult), reads=[s], writes=[s])
        c.op(V, lambda e: e.scalar_tensor_tensor(out=s[:, 1:2], in0=s[:, 0:1], scalar=2.0, in1=s[:, 1:2],
                                                 op0=ALU.mult, op1=ALU.mult), reads=[s], writes=[s])
        c.op(V, lambda e: e.tensor_tensor(out=s[:, 0:1], in0=s[:, 2:3], in1=s[:, 3:4], op=ALU.subtract), reads=[s], writes=[s])
        m *= 2
    if save_at == m:
        c.op(V, lambda e: e.tensor_copy(saved[:, 0:2], s[:, 0:2]), reads=[s], writes=[saved])
    return Gr, Gi, saved


def emit_sincos_small(c, P, wT, w, cs):
    V = "dve"
    x2 = cs[:, 2:3]
    acc = cs[:, 3:4]
    c.op(V, lambda e: e.tensor_tensor(out=x2, in0=w, in1=w, op=ALU.mult), reads=[wT], writes=[cs])
    c.op(V, lambda e: e.tensor_scalar(acc, x2, -1.0 / 110, 1.0, op0=ALU.mult, op1=ALU.add), reads=[cs], writes=[cs])
    for d in (72.0, 42.0, 20.0, 6.0):
        c.op(V, lambda e: e.tensor_tensor(out=acc, in0=acc, in1=x2, op=ALU.mult), reads=[cs], writes=[cs])
        c.op(V, lambda e: e.tensor_scalar(acc, acc, -1.0 / d, 1.0, op0=ALU.mult, op1=ALU.add), reads=[cs], writes=[cs])
    c.op(V, lambda e: e.tensor_tensor(out=cs[:, 1:2], in0=acc, in1=w, op=ALU.mult), reads=[cs, wT], writes=[cs])
    c.op(V, lambda e: e.tensor_scalar(acc, x2, -1.0 / 132, 1.0, op0=ALU.mult, op1=ALU.add), reads=[cs], writes=[cs])
    for d in (90.0, 56.0, 30.0, 12.0, 2.0):
        c.op(V, lambda e: e.tensor_tensor(out=acc, in0=acc, in1=x2, op=ALU.mult), reads=[cs], writes=[cs])
        c.op(V, lambda e: e.tensor_scalar(acc, acc, -1.0 / d, 1.0, op0=ALU.mult, op1=ALU.add), reads=[cs], writes=[cs])
    c.op(V, lambda e: e.tensor_copy(cs[:, 0:1], acc), reads=[cs], writes=[cs])


def emit_gamma(c, hT_, hidx_ap, out, P, ncol):
    LN2 = math.log(2.0)
    c.op("act", lambda e: e.activation(out=out[:, :ncol], in_=hidx_ap, func=AF.Exp, scale=-LN2, bias=c.k5[:P, 0:1]),
         reads=[c.k5, hT_], writes=[out])
    c.op("dve", lambda e: e.tensor_scalar(out[:, :ncol], out[:, :ncol], -1.0, 1.0, op0=ALU.mult, op1=ALU.add), reads=[out], writes=[out])
    c.op("act", lambda e: e.activation(out=out[:, :ncol], in_=out[:, :ncol], func=AF.Ln), reads=[out], writes=[out])


def build_ret(debug=False):
    nc = bass.Bass("TRN2", target_bir_lowering=False)
    c = Ctx(nc)
    c.pe_self_sync = True
    q_d = c.dram("q", [64, NTOK], F32, "ExternalInput")
    qs_d = c.dram("qsw", [64, NTOK], F32, "ExternalInput")
    k_d = c.dram("k", [64, NTOK], F32, "ExternalInput")
    ks_d = c.dram("ksw", [64, NTOK], F32, "ExternalInput")
    v_d = c.dram("v", [NTOK, 128], F32, "ExternalInput")
    g_d = c.dram("gate", [NTOK, 128], F32, "ExternalInput")
    og_d = c.dram("outg", [128, 128], F32, "ExternalInput")
    hp_d = c.dram("hpart", [64, 1], F32, "ExternalInput")
    hs_d = c.dram("hsel", [128, 2], F32, "ExternalInput")
    y_d = c.dram("y", [NTOK, 128], F32, "ExternalOutput")

    V = "dve"
    c.k5 = c.sb("k5", [128, 1], F32)
    c.op("pool", lambda e: e.memset(c.k5[:], -5.0 * math.log(2.0)), writes=[c.k5])
    epsT = c.sb("eps", [128, 1], F32)
    c.op("pool", lambda e: e.memset(epsT[:], EPS), writes=[epsT])
    identb = c.sb("identb", [128, 128], BF16)
    iof = c.sb("iof", [128, 128], F32)
    c.op("pool", lambda e: e.iota(iof[:], [[1, 128]], base=0, channel_multiplier=-1, allow_small_or_imprecise_dtypes=True), writes=[iof])
    c.op(V, lambda e: e.tensor_single_scalar(identb[:], iof[:], 0.0, op=ALU.is_equal), reads=[iof], writes=[identb])
    hp = c.sb("hp", [64, 1], F32)
    c.dma("sp", hp[:], hp_d[:], writes=[hp])
    hs = c.sb("hs", [128, 2], F32)
    c.dma("sp", hs[:], hs_d[:], writes=[hs])
    og = c.sb("og", [128, 128], F32)
    c.dma("sp", og[:], og_d[:], writes=[og])
    lgP = c.sb("lgP", [64, 2], F32)
    emit_gamma(c, hp, hp[:, 0:1], lgP, 64, 1)
    lgB = c.sb("lgB", [128, 2], F32)
    emit_gamma(c, hs, hs[:, 0:2], lgB, 128, 2)
    maskT = c.sb("maskT", [128, 2, 128], F32)
    dpos = c.sb("dpos", [128, 128], F32)
    dge = c.sb("dge", [128, 128], F32)
    c.op(V, lambda e: e.tensor_single_scalar(dpos[:], iof[:], 0.0, op=ALU.max), reads=[iof], writes=[dpos])
    c.op(V, lambda e: e.tensor_scalar(dge[:], iof[:], 0.0, 32.0 ** -0.5, op0=ALU.is_ge, op1=ALU.mult), reads=[iof], writes=[dge])
    for hl in range(2):
        c.op("act", lambda e: e.activation(out=maskT[:, hl, :], in_=dpos[:], func=AF.Exp, scale=lgB[:, hl:hl + 1]),
             reads=[dpos, lgB], writes=[maskT])
        c.op(V, lambda e: e.tensor_tensor(out=maskT[:, hl, :], in0=maskT[:, hl, :], in1=dge[:], op=ALU.mult), reads=[maskT, dge], writes=[maskT])
    io1 = c.sb("io1", [64, 128], F32)
    c.op("pool", lambda e: e.iota(io1[:], [[1, 128]], base=1, channel_multiplier=0, allow_small_or_imprecise_dtypes=True), writes=[io1])
    io2 = c.sb("io2", [64, 128], F32)
    c.op("pool", lambda e: e.iota(io2[:], [[-1, 128]], base=127, channel_multiplier=0, allow_small_or_imprecise_dtypes=True), writes=[io2])
    qdf = c.sb("qdf", [64, 128], F32)
    kdf = c.sb("kdf", [64, 128], F32)
    c.op("act", lambda e: e.activation(out=qdf[:], in_=io1[:], func=AF.Exp, scale=lgP[:, 0:1]), reads=[io1, lgP], writes=[qdf])
    c.op(V, lambda e: e.tensor_scalar(qdf[:], qdf[:], 32.0 ** -0.5, None, op0=ALU.mult), reads=[qdf], writes=[qdf])
    c.op("act", lambda e: e.activation(out=kdf[:], in_=io2[:], func=AF.Exp, scale=lgP[:, 0:1]), reads=[io2, lgP], writes=[kdf])
    sdec = c.sb("sdec", [64, 1], F32)
    c.op("act", lambda e: e.activation(out=sdec[:], in_=lgP[:, 0:1], func=AF.Exp, scale=128.0), reads=[lgP], writes=[sdec])
    fr = c.sb("fr", [64, 4], F32)
    for hb in range(2):
        c.op("pool", lambda e: e.iota(fr[32 * hb:32 * hb + 32, 0:1], [[0, 1]], base=0, channel_multiplier=1,
                                      allow_small_or_imprecise_dtypes=True), writes=[fr])
    c.op(V, lambda e: e.tensor_single_scalar(fr[:, 3:4], fr[:, 0:1], 16.0, op=ALU.is_ge), reads=[fr], writes=[fr])
    c.op(V, lambda e: e.scalar_tensor_tensor(out=fr[:, 0:1], in0=fr[:, 3:4], scalar=-16.0, in1=fr[:, 0:1], op0=ALU.mult, op1=ALU.add),
         reads=[fr], writes=[fr])
    c.op(V, lambda e: e.tensor_scalar(fr[:, 2:3], fr[:, 3:4], 2.0, -1.0, op0=ALU.mult, op1=ALU.add), reads=[fr], writes=[fr])
    c.op("act", lambda e: e.activation(out=fr[:, 1:2], in_=fr[:, 0:1], func=AF.Exp, scale=-math.log(10000.0) / 16.0), reads=[fr], writes=[fr])
    cs = c.sb("cs", [64, 4], F32)
    emit_sincos_small(c, 64, fr, fr[:, 1:2], cs)
    Gr, Gi, s128 = emit_pow_table(c, 64, 256, cs, cs[:, 0:1], cs[:, 1:2], save_at=128)
    Fr, Fi, _ = emit_pow_table(c, 64, 64, s128, s128[:, 0:1], s128[:, 1:2])
    E1r = c.sb("E1r", [64, NCH], F32)
    E1i = c.sb("E1i", [64, NCH], F32)
    c.op(V, lambda e: e.tensor_copy(E1r[:, 1:NCH], Fr[:, 0:64]), reads=[Fr], writes=[E1r])
    c.op(V, lambda e: e.tensor_copy(E1i[:, 1:NCH], Fi[:, 0:64]), reads=[Fi], writes=[E1i])
    c.op(V, lambda e: e.tensor_copy(E1r[:, 0:1], Fr[:, 1:2]), reads=[Fr], writes=[E1r])
    c.op(V, lambda e: e.tensor_scalar(E1i[:, 0:1], Fi[:, 1:2], -1.0, None, op0=ALU.mult), reads=[Fi], writes=[E1i])
    E2r = Gr
    E2i = Gi

    QR = c.sb("QR", [64, NTOK], BF16)
    KR = c.sb("KR", [64, NTOK], BF16)
    QD = c.sb("QD", [64, NTOK], BF16)
    KD = c.sb("KD", [64, NTOK], BF16)
    v_sb = c.sb("v_sb", [128, NCH, 128], BF16)
    g_sb = c.sb("g_sb", [128, NCH, 128], BF16)
    y_sb = c.sb("y_sb", [128, NCH, 128], F32)
    c.dma("pool", v_sb[:], v_d.h.ap().rearrange("(c p) f -> p c f", p=128), writes=[v_sb])
    c.dma("pool", g_sb[:], g_d.h.ap().rearrange("(c p) f -> p c f", p=128), writes=[g_sb])

    nblk = NCH // CB
    BW = CB * 128
    x_rot = Rot([c.sb(f"x{i}", [64, BW], F32) for i in range(2)])
    xs_rot = Rot([c.sb(f"xs{i}", [64, BW], F32) for i in range(2)])
    COSb = c.sb("COSb", [64, CB, 128], F32)
    SINb = c.sb("SINb", [64, CB, 128], F32)
    tb1 = c.sb("tb1", [64, CB, 128], F32)
    tb2 = c.sb("tb2", [64, CB, 128], F32)
    r1 = c.sb("r1", [64, BW], F32)
    r2 = c.sb("r2", [64, BW], F32)
    for b in range(nblk):
        c0 = b * CB
        t0 = c0 * 128
        e1r = E1r[:, c0:c0 + CB].unsqueeze(2).to_broadcast([64, CB, 128])
        e1i = E1i[:, c0:c0 + CB].unsqueeze(2).to_broadcast([64, CB, 128])
        e2r = E2r[:, 16:144].unsqueeze(1).to_broadcast([64, CB, 128])
        e2i = E2i[:, 16:144].unsqueeze(1).to_broadcast([64, CB, 128])
        P_ = "pool"
        c.op(P_, lambda e: e.tensor_tensor(out=tb1[:], in0=e1r, in1=e2r, op=ALU.mult), reads=[E1r, E2r], writes=[tb1])
        c.op(P_, lambda e: e.tensor_tensor(out=tb2[:], in0=e1i, in1=e2i, op=ALU.mult), reads=[E1i, E2i], writes=[tb2])
        c.op(P_, lambda e: e.tensor_tensor(out=COSb[:], in0=tb1[:], in1=tb2[:], op=ALU.subtract), reads=[tb1, tb2], writes=[COSb])
        c.op(P_, lambda e: e.tensor_tensor(out=tb1[:], in0=e1r, in1=e2i, op=ALU.mult), reads=[E1r, E2i], writes=[tb1])
        c.op(P_, lambda e: e.tensor_tensor(out=tb2[:], in0=e1i, in1=e2r, op=ALU.mult), reads=[E1i, E2r], writes=[tb2])
        c.op(P_, lambda e: e.tensor_tensor(out=SINb[:], in0=tb1[:], in1=tb2[:], op=ALU.add), reads=[tb1, tb2], writes=[SINb])
        cosf = COSb[:].rearrange("p c j -> p (c j)")
        sinf = SINb[:].rearrange("p c j -> p (c j)")
        for (src_d, srcs_d, OUT, DEC, fac) in ((q_d, qs_d, QR, QD, qdf), (k_d, ks_d, KR, KD, kdf)):
            x = x_rot.next(); xs = xs_rot.next()
            c.dma("sp", x[:], src_d[:, t0:t0 + BW], writes=[x])
            c.dma("sp", xs[:], srcs_d[:, t0:t0 + BW], writes=[xs])
            c.op(V, lambda e: e.tensor_tensor(out=r1[:], in0=x[:], in1=cosf, op=ALU.mult), reads=[x, COSb], writes=[r1])
            c.op(V, lambda e: e.scalar_tensor_tensor(out=r2[:], in0=xs[:], scalar=fr[:, 2:3], in1=sinf, op0=ALU.mult, op1=ALU.mult),
                 reads=[xs, fr, SINb], writes=[r2])
            c.op(V, lambda e: e.tensor_tensor(out=OUT[:, t0:t0 + BW], in0=r1[:], in1=r2[:], op=ALU.add), reads=[r1, r2], writes=[(OUT, b)])
            facb = fac[:].unsqueeze(1).to_broadcast([64, CB, 128])
            c.op(V, lambda e: e.tensor_tensor(out=DEC[:, t0:t0 + BW].rearrange("p (c j) -> p c j", j=128),
                                              in0=OUT[:, t0:t0 + BW].rearrange("p (c j) -> p c j", j=128), in1=facb, op=ALU.mult),
                 reads=[(OUT, b), fac], writes=[(DEC, b)])

    gg_all = c.sb("gg_all", [128, NCH, 128], BF16)
    c.op("act", lambda e: e.activation(out=gg_all[:], in_=g_sb[:], func=AF.Silu), reads=[g_sb], writes=[gg_all])
    c.op("pool", lambda e: e.tensor_tensor(out=gg_all[:], in0=gg_all[:], in1=og[:].unsqueeze(1).to_broadcast([128, NCH, 128]), op=ALU.mult),
         reads=[gg_all, og], writes=[gg_all])
    ps_tr = Rot([c.ps(f"ps_tr{i}", [128, 64], BF16) for i in range(2)])
    ps_s = Rot([c.ps(f"ps_s{i}", [128, 2, 128], F32) for i in range(2)])
    ps_o = Rot([c.ps(f"ps_o{i}", [128, 128], F32) for i in range(2)])
    ps_d = Rot([c.ps(f"ps_d{i}", [64, 128], F32) for i in range(2)])
    kdt_rot = Rot([c.sb(f"kdt{i}", [128, 64], BF16) for i in range(2)])
    sT_rot = Rot([c.sb(f"sT{i}", [128, 2, 128], BF16) for i in range(2)])
    S32 = c.sb("S32", [64, 128], F32)
    c.op(V, lambda e: e.memset(S32[:], 0.0), writes=[S32])
    Sb_rot = Rot([c.sb(f"Sb{i}", [64, 128], BF16) for i in range(2)])
    Sb = Sb_rot.next()
    c.op(V, lambda e: e.memset(Sb[:], 0.0), writes=[Sb])
    o_rot = Rot([c.sb(f"o{i}", [128, 2, 64], F32) for i in range(2)])
    cen_rot = Rot([c.sb(f"cen{i}", [128, 2, 64], F32) for i in range(2)])
    sq_rot = Rot([c.sb(f"sqr{i}", [128, 2, 64], F32) for i in range(2)])
    st_rot = Rot([c.sb(f"st{i}", [128, 8], F32) for i in range(2)])
    gg_rot = Rot([c.sb(f"gg{i}", [128, 128], F32) for i in range(2)])
    for ch in range(NCH):
        b = ch // CB
        t0 = ch * 128
        ptr = ps_tr.next()
        c.op("pe", lambda e: e.transpose(ptr[:, :], KD[:, t0:t0 + 128], identb[0:64, 0:64]), reads=[(KD, b), identb], writes=[ptr])
        kdt = kdt_rot.next()
        c.op("act", lambda e: e.copy(out=kdt[:], in_=ptr[:]), reads=[ptr], writes=[kdt])
        pss = ps_s.next()
        for hl in range(2):
            c.op("pe", lambda e: e.matmul(pss[:, hl, :], lhsT=KR[32 * hl:32 * hl + 32, t0:t0 + 128], rhs=QR[32 * hl:32 * hl + 32, t0:t0 + 128],
                                          start=True, stop=True), reads=[(KR, b), (QR, b)], writes=[pss])
        sT = sT_rot.next()
        c.op(V, lambda e: e.tensor_tensor(out=sT[:], in0=pss[:], in1=maskT[:], op=ALU.mult), reads=[pss, maskT], writes=[sT])
        pso = ps_o.next()
        for hl in range(2):
            c.op("pe", lambda e: e.matmul(pso[:, hl * 64:(hl + 1) * 64], lhsT=sT[:, hl, :], rhs=v_sb[:, ch, hl * 64:(hl + 1) * 64],
                                          start=True, stop=False), reads=[sT, v_sb], writes=[pso])
            c.op("pe", lambda e: e.matmul(pso[:, hl * 64:(hl + 1) * 64], lhsT=QD[32 * hl:32 * hl + 32, t0:t0 + 128],
                                          rhs=Sb[32 * hl:32 * hl + 32, hl * 64:(hl + 1) * 64], start=False, stop=True),
                 reads=[(QD, b), Sb], writes=[pso])
        psd = ps_d.next()
        c.op("pe", lambda e: e.matmul(psd[:, :], lhsT=kdt[:], rhs=v_sb[:, ch, :], start=True, stop=True), reads=[kdt, v_sb], writes=[psd])
        c.op(V, lambda e: e.scalar_tensor_tensor(out=S32[:], in0=S32[:], scalar=sdec[:, 0:1], in1=psd[:], op0=ALU.mult, op1=ALU.add),
             reads=[S32, sdec, psd], writes=[S32])
        Sb = Sb_rot.next()
        c.op("act", lambda e: e.copy(out=Sb[:], in_=S32[:]), reads=[S32], writes=[Sb])
        o = o_rot.next(); cen = cen_rot.next(); sq = sq_rot.next(); st = st_rot.next(); gg = gg_rot.next()
        c.op("act", lambda e: e.copy(out=o[:].rearrange("p h v -> p (h v)"), in_=pso[:]), reads=[pso], writes=[o])
        c.op(V, lambda e: e.tensor_reduce(out=st[:, 0:2], in_=o[:], axis=AX.X, op=ALU.add), reads=[o], writes=[st])
        c.op(V, lambda e: e.tensor_scalar(st[:, 0:2], st[:, 0:2], 1.0 / 64, None, op0=ALU.mult), reads=[st], writes=[st])
        c.op(V, lambda e: e.tensor_tensor(out=cen[:], in0=o[:], in1=st[:, 0:2].unsqueeze(2).to_broadcast([128, 2, 64]), op=ALU.subtract),
             reads=[o, st], writes=[cen])
        c.op("pool", lambda e: e.tensor_tensor(out=sq[:], in0=cen[:], in1=cen[:], op=ALU.mult), reads=[cen], writes=[sq])
        c.op(V, lambda e: e.tensor_reduce(out=st[:, 2:4], in_=sq[:], axis=AX.X, op=ALU.add), reads=[sq, st], writes=[st])
        c.op("act", lambda e: e.activation(out=st[:, 2:4], in_=st[:, 2:4], func=AF.Sqrt, scale=1.0 / 64, bias=epsT[:, 0:1]), reads=[st, epsT], writes=[st])
        c.op(V, lambda e: e.reciprocal(st[:, 2:4], st[:, 2:4]), reads=[st], writes=[st])
        c.op(V, lambda e: e.tensor_tensor(out=cen[:], in0=cen[:], in1=st[:, 2:4].unsqueeze(2).to_broadcast([128, 2, 64]), op=ALU.mult),
             reads=[cen, st], writes=[cen])
        c.op(V, lambda e: e.tensor_tensor(out=y_sb[:, ch, :], in0=cen[:].rearrange("p h v -> p (h v)"), in1=gg_all[:, ch, :], op=ALU.mult),
             reads=[cen, gg_all], writes=[(y_sb, ch)])
    c.barrier()
    if debug:
        dbg = {"lgP": lgP, "lgB": lgB, "fr": fr, "cs": cs, "E1r": E1r, "E1i": E1i, "Gr": Gr, "Gi": Gi, "maskT": maskT,
               "qdf": qdf, "kdf": kdf, "sdec": sdec, "S32": S32, "COSb": COSb, "SINb": SINb}
        for nm, t in dbg.items():
            shp = list(t.h.shape)
            dd = c.dram("dbg_" + nm, shp, F32, "ExternalOutput")
            c.dma("sp", dd[:], t[:], reads=[t])
        for nm, t in {"QR": QR, "KR": KR, "QD": QD, "KD": KD}.items():
            tmpf = c.sb("dbgf_" + nm, [64, 1024], F32)
            c.op("dve", lambda e: e.tensor_copy(tmpf[:], t[:, 0:1024]), writes=[tmpf])
            dd = c.dram("dbg_" + nm, [64, 1024], F32, "ExternalOutput")
            c.dma("sp", dd[:], tmpf[:], reads=[tmpf])
    c.dma("sp", y_d.h.ap().rearrange("(c p) f -> p c f", p=128), y_sb[:], reads=[y_sb], qt=y_sb)
    c.finish()
    c.close()
    print("ret instructions:", c.n_inst)
    return nc


NTOK = 8320
NG = 65
EPS = 1e-6
GB = 13
BW = GB * 128
NBLK = NG // GB
CS = 32


def build_hg(layer, debug=False):
    nc = bass.Bass("TRN2", target_bir_lowering=False)
    c = Ctx(nc)
    x_d = c.dram("x3", [3, 64, NTOK], F32, "ExternalInput")
    cw_d = c.dram("convw", [3, 64, 4], F32, "ExternalInput")
    g_d = c.dram("gate", [NTOK, 64], F32, "ExternalInput")
    og_d = c.dram("outg", [128, 64], F32, "ExternalInput")
    lb_d = c.dram("lbp", [64, 2], F32, "ExternalInput")
    y_d = c.dram("y", [NTOK, 64], F32, "ExternalOutput")
    V = "dve"

    epsT = c.sb("eps", [128, 1], F32)
    c.op("pool", lambda e: e.memset(epsT[:], EPS), writes=[epsT])
    iof = c.sb("iof", [128, 128], F32)
    c.op("pool", lambda e: e.iota(iof[:], [[1, 128]], base=0, channel_multiplier=-1, allow_small_or_imprecise_dtypes=True), writes=[iof])
    identf = c.sb("identf", [128, 128], F32)
    c.op(V, lambda e: e.tensor_single_scalar(identf[:], iof[:], 0.0, op=ALU.is_equal), reads=[iof], writes=[identf])
    identb = c.sb("identb", [128, 128], BF16)
    c.op(V, lambda e: e.tensor_copy(identb[:], identf[:]), reads=[identf], writes=[identb])
    dge = c.sb("dge", [128, 128], F32)
    c.op(V, lambda e: e.tensor_single_scalar(dge[:], iof[:], 0.0, op=ALU.is_ge), reads=[iof], writes=[dge])
    bmask = c.sb("bmask", [128, 128], F32)
    c.op(V, lambda e: e.memset(bmask[:], 0.0), writes=[bmask])
    for m in range(4):
        c.op(V, lambda e: e.tensor_copy(bmask[32 * m:32 * m + 32, 32 * m:32 * m + 32], dge[32 * m:32 * m + 32, 32 * m:32 * m + 32]),
             reads=[dge], writes=[bmask])
    cmask = c.sb("cmask", [128, 4, 64], F32)
    cm2 = c.sb("cm2", [128, 4, 64], F32)
    c.op("pool", lambda e: e.iota(cmask[:], [[-32, 4], [0, 64]], base=0, channel_multiplier=1, allow_small_or_imprecise_dtypes=True), writes=[cmask])
    c.op(V, lambda e: e.tensor_single_scalar(cm2[:], cmask[:], 32.0, op=ALU.is_lt), reads=[cmask], writes=[cm2])
    c.op(V, lambda e: e.tensor_single_scalar(cmask[:], cmask[:], 0.0, op=ALU.is_ge), reads=[cmask, cm2], writes=[cmask])
    c.op(V, lambda e: e.tensor_tensor(out=cmask[:], in0=cmask[:], in1=cm2[:], op=ALU.mult), reads=[cmask, cm2], writes=[cmask])
    colmask = c.sb("colmask", [64, 4, 128], F32)
    col2 = c.sb("col2", [64, 4, 128], F32)
    c.op("pool", lambda e: e.iota(colmask[:], [[-32, 4], [1, 128]], base=0, channel_multiplier=0, allow_small_or_imprecise_dtypes=True), writes=[colmask])
    c.op(V, lambda e: e.tensor_single_scalar(col2[:], colmask[:], 32.0, op=ALU.is_lt), reads=[colmask], writes=[col2])
    c.op(V, lambda e: e.tensor_single_scalar(colmask[:], colmask[:], 0.0, op=ALU.is_ge), reads=[colmask, col2], writes=[colmask])
    c.op(V, lambda e: e.tensor_tensor(out=colmask[:], in0=colmask[:], in1=col2[:], op=ALU.mult), reads=[colmask, col2], writes=[colmask])
    rmask = c.sb("rmask", [64, BW], F32)
    c.op("pool", lambda e: e.iota(rmask[:], [[0, BW // CS], [1, CS]], base=0, channel_multiplier=0, allow_small_or_imprecise_dtypes=True), writes=[rmask])
    c.op(V, lambda e: e.tensor_single_scalar(rmask[:], rmask[:], 0.0, op=ALU.is_gt), reads=[rmask], writes=[rmask])
    cw = c.sb("cw", [64, 3, 4], F32)
    c.dma("sp", cw[:], cw_d.h.ap().rearrange("s p k -> p s k"), writes=[cw])
    og = c.sb("og", [128, 64], F32)
    c.dma("sp", og[:], og_d[:], writes=[og])
    lbp = c.sb("lbp", [64, 4], F32)
    c.dma("sp", lbp[:, 0:2], lb_d[:], writes=[lbp])
    if layer == 0:
        c.op(V, lambda e: e.memset(lbp[:, 2:3], 0.0), reads=[lbp], writes=[lbp])
    else:
        c.op(V, lambda e: e.tensor_tensor(out=lbp[:, 2:3], in0=lbp[:, 1:2], in1=lbp[:, 0:1], op=ALU.subtract), reads=[lbp], writes=[lbp])
        c.op("act", lambda e: e.activation(out=lbp[:, 2:3], in_=lbp[:, 2:3], func=AF.Sigmoid), reads=[lbp], writes=[lbp])
    c.op(V, lambda e: e.tensor_scalar(lbp[:, 3:4], lbp[:, 2:3], -1.0, 1.0, op0=ALU.mult, op1=ALU.add), reads=[lbp], writes=[lbp])

    QT = c.sb("QT", [64, NTOK], BF16)
    KT = c.sb("KT", [64, NTOK], BF16)
    KDT = c.sb("KDT", [64, NTOK], BF16)
    Vtok = c.sb("Vtok", [128, NG, 64], BF16)
    KDtok = c.sb("KDtok", [128, NG, 64], BF16)
    g_sb = c.sb("g_sb", [128, NG, 64], BF16)
    y_sb = c.sb("y_sb", [128, NG, 64], F32)
    Dec = c.sb("Dec", [64, NTOK // CS], F32)
    c.dma("pool", g_sb[:], g_d.h.ap().rearrange("(c p) f -> p c f", p=128), writes=[g_sb])

    xq = c.sb("xq", [64, BW + 3], F32); xf = c.sb("xf", [64, BW + 3], F32); xi = c.sb("xi", [64, BW + 3], F32)
    cq = c.sb("cq", [64, BW], F32); cf = c.sb("cf", [64, BW], F32); ci = c.sb("ci", [64, BW], F32)
    gg_ = c.sb("g", [64, BW], F32); gcum = c.sb("gcum", [64, BW], F32); tmp = c.sb("tmp", [64, BW], F32)
    ps_t = Rot([c.ps(f"pst{i}", [128, 64], F32) for i in range(2)])
    ps_tb = Rot([c.ps(f"pstb{i}", [128, 64], BF16) for i in range(2)])
    NCB = BW // CS
    for b in range(NBLK):
        t0 = b * BW
        for si, (xt, ct) in enumerate(((xq, cq), (xf, cf), (xi, ci))):
            if b == 0:
                c.op(V, lambda e: e.memset(xt[:, 0:3], 0.0), writes=[xt])
                c.dma("sp", xt[:, 3:BW + 3], x_d.h.ap()[si, :, 0:BW], writes=[xt])
            else:
                c.dma("sp", xt[:, :], x_d.h.ap()[si, :, t0 - 3:t0 + BW], writes=[xt])
            c.op(V, lambda e: e.tensor_scalar(ct[:], xt[:, 0:BW], cw[:, si, 0:1], None, op0=ALU.mult), reads=[xt, cw], writes=[ct])
            for kk_ in range(1, 4):
                c.op(V, lambda e: e.scalar_tensor_tensor(out=ct[:], in0=xt[:, kk_:kk_ + BW], scalar=cw[:, si, kk_:kk_ + 1], in1=ct[:],
                                                         op0=ALU.mult, op1=ALU.add), reads=[xt, cw, ct], writes=[ct])
        c.op("act", lambda e: e.activation(out=cq[:], in_=cq[:], func=AF.Silu), reads=[cq], writes=[cq])
        c.op("act", lambda e: e.activation(out=cf[:], in_=cf[:], func=AF.Sigmoid), reads=[cf], writes=[cf])
        c.op(V, lambda e: e.tensor_scalar(cf[:], cf[:], lbp[:, 3:4], lbp[:, 2:3], op0=ALU.mult, op1=ALU.add), reads=[cf, lbp], writes=[cf])
        c.op("act", lambda e: e.activation(out=gg_[:], in_=cf[:], func=AF.Ln), reads=[cf], writes=[gg_])
        c.op(V, lambda e: e.tensor_scalar(cf[:], cf[:], -1.0, 1.0, op0=ALU.mult, op1=ALU.add), reads=[cf, gg_], writes=[cf])
        c.op(V, lambda e: e.tensor_tensor_scan(out=gcum[:], data0=rmask[:], data1=gg_[:], initial=0.0, op0=ALU.mult, op1=ALU.add),
             reads=[rmask, gg_], writes=[gcum])
        c.op("act", lambda e: e.activation(out=tmp[:], in_=gcum[:], func=AF.Exp), reads=[gcum], writes=[tmp])
        c.op(V, lambda e: e.tensor_tensor(out=QT[:, t0:t0 + BW], in0=cq[:], in1=tmp[:], op=ALU.mult), reads=[cq, tmp], writes=[(QT, b)])
        c.op(V, lambda e: e.tensor_single_scalar(tmp[:], gcum[:], -80.0, op=ALU.max), reads=[gcum, (QT, b)], writes=[tmp])
        c.op("act", lambda e: e.activation(out=tmp[:], in_=tmp[:], func=AF.Exp, scale=-1.0), reads=[tmp], writes=[tmp])
        c.op(V, lambda e: e.tensor_tensor(out=KT[:, t0:t0 + BW], in0=cf[:], in1=tmp[:], op=ALU.mult), reads=[cf, tmp], writes=[(KT, b)])
        gl = gcum[:].rearrange("p (c j) -> p c j", j=CS)[:, :, CS - 1:CS]
        c.op(V, lambda e: e.tensor_tensor(out=tmp[:].rearrange("p (c j) -> p c j", j=CS), in0=gl.to_broadcast([64, NCB, CS]),
                                          in1=gcum[:].rearrange("p (c j) -> p c j", j=CS), op=ALU.subtract), reads=[gcum, (KT, b)], writes=[tmp])
        c.op("act", lambda e: e.activation(out=tmp[:], in_=tmp[:], func=AF.Exp), reads=[tmp], writes=[tmp])
        c.op(V, lambda e: e.tensor_tensor(out=KDT[:, t0:t0 + BW], in0=cf[:], in1=tmp[:], op=ALU.mult), reads=[cf, tmp], writes=[(KDT, b)])
        c.op("act", lambda e: e.activation(out=Dec[:, b * NCB:(b + 1) * NCB].unsqueeze(2), in_=gl, func=AF.Exp), reads=[gcum], writes=[(Dec, b)])
        for gi in range(GB):
            G = b * GB + gi
            pt = ps_t.next()
            c.op("pe", lambda e: e.transpose(pt[:, :], ci[:, gi * 128:(gi + 1) * 128], identf[0:64, 0:64]), reads=[ci, identf], writes=[pt])
            c.op("act", lambda e: e.copy(out=Vtok[:, G, :], in_=pt[:]), reads=[pt], writes=[(Vtok, G)])
            ptb = ps_tb.next()
            c.op("pe", lambda e: e.transpose(ptb[:, :], KDT[:, t0 + gi * 128:t0 + (gi + 1) * 128], identb[0:64, 0:64]),
                 reads=[(KDT, b), identb], writes=[ptb])
            c.op("act", lambda e: e.copy(out=KDtok[:, G, :], in_=ptb[:]), reads=[ptb], writes=[(KDtok, G)])

    gg_all = c.sb("gg_all", [128, NG, 64], F32)
    c.op("act", lambda e: e.activation(out=gg_all[:], in_=g_sb[:], func=AF.Silu), reads=[g_sb], writes=[gg_all])
    c.op("pool", lambda e: e.tensor_tensor(out=gg_all[:], in0=gg_all[:], in1=og[:].unsqueeze(1).to_broadcast([128, NG, 64]), op=ALU.mult),
         reads=[gg_all, og], writes=[gg_all])
    ps_s = Rot([c.ps(f"ps_s{i}", [128, 128], F32) for i in range(1)])
    ps_o = Rot([c.ps(f"ps_o{i}", [128, 64], F32) for i in range(2)])
    ps_d = Rot([c.ps(f"ps_d{i}", [64, 4, 64], F32) for i in range(1)])
    sT_rot = Rot([c.sb(f"sT{i}", [128, 128], BF16) for i in range(2)])
    S32 = c.sb("S32", [64, 64], F32)
    c.op(V, lambda e: e.memset(S32[:], 0.0), writes=[S32])
    Sb_rot = Rot([c.sb(f"Sb{i}", [64, 64], BF16) for i in range(6)])
    Sb = Sb_rot.next()
    c.op(V, lambda e: e.memset(Sb[:], 0.0), writes=[Sb])
    o_rot = Rot([c.sb(f"o{i}", [128, 64], F32) for i in range(2)])
    sq_rot = Rot([c.sb(f"sqr{i}", [128, 64], F32) for i in range(2)])
    st_rot = Rot([c.sb(f"st{i}", [128, 4], F32) for i in range(2)])
    gg_rot = Rot([c.sb(f"gg{i}", [128, 64], F32) for i in range(2)])
    vb_rot = Rot([c.sb(f"vb{i}", [128, 4, 64], BF16) for i in range(2)])
    qm_rot = Rot([c.sb(f"qm{i}", [64, 4, 128], BF16) for i in range(2)])
    for G in range(NG):
        b = G // GB
        t0 = G * 128
        pss = ps_s.next()
        c.op("pe", lambda e: e.matmul(pss[:, :], lhsT=KT[:, t0:t0 + 128], rhs=QT[:, t0:t0 + 128], start=True, stop=True),
             reads=[(KT, b), (QT, b)], writes=[pss])
        sT = sT_rot.next()
        c.op(V, lambda e: e.tensor_tensor(out=sT[:], in0=pss[:], in1=bmask[:], op=ALU.mult), reads=[pss, bmask], writes=[sT])
        psd = ps_d.next()
        vb = vb_rot.next()
        c.op("pool", lambda e: e.tensor_tensor(out=vb[:], in0=Vtok[:, G, :].unsqueeze(1).to_broadcast([128, 4, 64]), in1=cmask[:], op=ALU.mult),
             reads=[(Vtok, G), cmask], writes=[vb])
        c.op("pe", lambda e: e.matmul(psd[:].rearrange("p m v -> p (m v)"), lhsT=KDtok[:, G, :], rhs=vb[:].rearrange("p m v -> p (m v)"),
                                      start=True, stop=True), reads=[(KDtok, G), vb], writes=[psd])
        qm = qm_rot.next()
        c.op(V, lambda e: e.tensor_tensor(out=qm[:], in0=QT[:, t0:t0 + 128].unsqueeze(1).to_broadcast([64, 4, 128]), in1=colmask[:], op=ALU.mult),
             reads=[(QT, b), colmask], writes=[qm])
        pso = ps_o.next()
        c.op("pe", lambda e: e.matmul(pso[:, :], lhsT=sT[:], rhs=Vtok[:, G, :], start=True, stop=False), reads=[sT, (Vtok, G)], writes=[pso])
        for m in range(4):
            ch = G * 4 + m
            c.op("pe", lambda e: e.matmul(pso[:, :], lhsT=qm[:, m, :], rhs=Sb[:, :],
                                          start=False, stop=(m == 3)), reads=[qm, Sb], writes=[pso])
            c.op(V, lambda e: e.scalar_tensor_tensor(out=S32[:], in0=S32[:], scalar=Dec[:, ch:ch + 1], in1=psd[:, m, :],
                                                     op0=ALU.mult, op1=ALU.add), reads=[S32, (Dec, b), psd], writes=[S32])
            Sb = Sb_rot.next()
            c.op("act", lambda e: e.copy(out=Sb[:], in_=S32[:]), reads=[S32], writes=[Sb])
        o = o_rot.next(); sq = sq_rot.next(); st = st_rot.next(); gg = gg_rot.next()
        c.op("act", lambda e: e.copy(out=o[:], in_=pso[:]), reads=[pso], writes=[o])
        c.op("pool", lambda e: e.tensor_tensor(out=sq[:], in0=o[:], in1=o[:], op=ALU.mult), reads=[o], writes=[sq])
        c.op(V, lambda e: e.tensor_reduce(out=st[:, 0:1], in_=sq[:], axis=AX.X, op=ALU.add), reads=[sq], writes=[st])
        c.op("act", lambda e: e.activation(out=st[:, 0:1], in_=st[:, 0:1], func=AF.Sqrt, scale=1.0 / 64, bias=epsT[:, 0:1]), reads=[st, epsT], writes=[st])
        c.op(V, lambda e: e.reciprocal(st[:, 0:1], st[:, 0:1]), reads=[st], writes=[st])
        c.op(V, lambda e: e.scalar_tensor_tensor(out=y_sb[:, G, :], in0=o[:], scalar=st[:, 0:1], in1=gg_all[:, G, :], op0=ALU.mult, op1=ALU.mult),
             reads=[o, st, gg_all], writes=[(y_sb, G)])
    c.barrier()
    c.dma("sp", y_d.h.ap().rearrange("(c p) f -> p c f", p=128), y_sb[:], reads=[y_sb], qt=y_sb)
    c.finish()
    c.close()
    print("hg instructions:", c.n_inst)
    return nc


NSB = 1040
NGL = 4
NLEV = 11


def emit_sincos_tile(c, x, xT, cs_c, cs_s, x2, acc, n):
    V = "dve"
    c.op(V, lambda e: e.tensor_tensor(out=x2[:], in0=x[:], in1=x[:], op=ALU.mult), reads=[x], writes=[x2])
    c.op(V, lambda e: e.tensor_scalar(acc[:], x2[:], -1.0 / 156, 1.0, op0=ALU.mult, op1=ALU.add), reads=[x2], writes=[acc])
    for d in (110.0, 72.0, 42.0, 20.0, 6.0):
        c.op(V, lambda e: e.tensor_tensor(out=acc[:], in0=acc[:], in1=x2[:], op=ALU.mult), reads=[acc, x2], writes=[acc])
        c.op(V, lambda e: e.tensor_scalar(acc[:], acc[:], -1.0 / d, 1.0, op0=ALU.mult, op1=ALU.add), reads=[acc], writes=[acc])
    c.op(V, lambda e: e.tensor_tensor(out=cs_s[:], in0=acc[:], in1=x[:], op=ALU.mult), reads=[acc, x], writes=[cs_s])
    c.op(V, lambda e: e.tensor_scalar(acc[:], x2[:], -1.0 / 182, 1.0, op0=ALU.mult, op1=ALU.add), reads=[x2, cs_s], writes=[acc])
    for d in (132.0, 90.0, 56.0, 30.0, 12.0, 2.0):
        c.op(V, lambda e: e.tensor_tensor(out=acc[:], in0=acc[:], in1=x2[:], op=ALU.mult), reads=[acc, x2], writes=[acc])
        c.op(V, lambda e: e.tensor_scalar(acc[:], acc[:], -1.0 / d, 1.0, op0=ALU.mult, op1=ALU.add), reads=[acc], writes=[acc])
    c.op(V, lambda e: e.tensor_copy(cs_c[:], acc[:]), reads=[acc], writes=[cs_c])


def emit_cdouble(c, cr, ci, t1, t2):
    V = "dve"
    c.op(V, lambda e: e.tensor_tensor(out=t1[:], in0=cr[:], in1=cr[:], op=ALU.mult), reads=[cr], writes=[t1])
    c.op(V, lambda e: e.tensor_tensor(out=t2[:], in0=ci[:], in1=ci[:], op=ALU.mult), reads=[ci], writes=[t2])
    c.op(V, lambda e: e.scalar_tensor_tensor(out=ci[:], in0=cr[:], scalar=2.0, in1=ci[:], op0=ALU.mult, op1=ALU.mult), reads=[cr, ci, t2], writes=[ci])
    c.op(V, lambda e: e.tensor_tensor(out=cr[:], in0=t1[:], in1=t2[:], op=ALU.subtract), reads=[t1, t2, ci], writes=[cr])


def build_s5(debug=False):
    nc = bass.Bass("TRN2", target_bir_lowering=False)
    c = Ctx(nc)
    U_d = c.dram("U", [NGL, 128, NSB], F32, "ExternalInput")
    lam_d = c.dram("lam", [128, NGL, 2], F32, "ExternalInput")
    ls_d = c.dram("ls", [128, NGL], F32, "ExternalInput")
    B_d = c.dram("Bm", [128, NGL, 2, 16], F32, "ExternalInput")
    C_d = c.dram("Cm", [128, NGL, 2, 16], F32, "ExternalInput")
    D_d = c.dram("Dm", [128, NGL], F32, "ExternalInput")
    y_d = c.dram("y", [NGL, 128, NSB], F32, "ExternalOutput")
    V = "dve"
    G4 = NGL

    def sb(name, shape, dt=F32):
        return c.sb(name, shape, dt)

    iof = sb("iof", [128, 128])
    c.op("pool", lambda e: e.iota(iof[:], [[1, 128]], base=0, channel_multiplier=-1, allow_small_or_imprecise_dtypes=True), writes=[iof])
    ident = sb("ident", [128, 128])
    c.op(V, lambda e: e.tensor_single_scalar(ident[:], iof[:], 0.0, op=ALU.is_equal), reads=[iof], writes=[ident])
    pswap = sb("pswap", [128, 128])
    ptmp = sb("ptmp", [128, 128])
    c.op(V, lambda e: e.tensor_single_scalar(pswap[:], iof[:], 64.0, op=ALU.is_equal), reads=[iof], writes=[pswap])
    c.op(V, lambda e: e.tensor_single_scalar(ptmp[:], iof[:], -64.0, op=ALU.is_equal), reads=[iof], writes=[ptmp])
    c.op(V, lambda e: e.tensor_tensor(out=pswap[:], in0=pswap[:], in1=ptmp[:], op=ALU.add), reads=[pswap, ptmp], writes=[pswap])
    tmask = sb("tmask", [128, 8, 16])
    c.op("pool", lambda e: e.iota(tmask[:], [[16, 8], [0, 16]], base=15, channel_multiplier=-1, allow_small_or_imprecise_dtypes=True), writes=[tmask])
    c.op(V, lambda e: e.tensor_single_scalar(tmask[:], tmask[:], 0.0, op=ALU.is_ge), reads=[tmask], writes=[tmask])
    sgnh = sb("sgnh", [128, 1])
    c.op(V, lambda e: e.memset(sgnh[0:64, :], 1.0), writes=[sgnh])
    c.op(V, lambda e: e.memset(sgnh[64:128, :], -1.0), writes=[sgnh])
    mv = sb("mv", [128, G4, 9])
    c.op("pool", lambda e: e.iota(mv[:], [[0, G4], [1, 9]], base=0, channel_multiplier=0, allow_small_or_imprecise_dtypes=True), writes=[mv])

    lam = sb("lam", [128, G4, 2]); ls = sb("ls", [128, G4]); Bm = sb("Bm", [128, G4, 2, 16]); Cm = sb("Cm", [128, G4, 2, 16]); Dm = sb("Dm", [128, G4])
    c.dma("sp", lam[:], lam_d[:], writes=[lam]); c.dma("sp", ls[:], ls_d[:], writes=[ls])
    c.dma("sp", Bm[:], B_d[:], writes=[Bm]); c.dma("sp", Cm[:], C_d[:], writes=[Cm]); c.dma("sp", Dm[:], D_d[:], writes=[Dm])

    st = sb("st", [128, G4]); a = sb("a", [128, G4]); th = sb("th", [128, G4])
    c.op("act", lambda e: e.activation(out=st[:], in_=ls[:], func=AF.Exp), reads=[ls], writes=[st])
    c.op(V, lambda e: e.tensor_tensor(out=a[:], in0=lam[:, :, 0], in1=st[:], op=ALU.mult), reads=[lam, st], writes=[a])
    c.op(V, lambda e: e.tensor_tensor(out=th[:], in0=lam[:, :, 1], in1=st[:], op=ALU.mult), reads=[lam, st], writes=[th])
    kq = sb("kq", [128, G4]); ki = sb("ki", [128, G4], I32); x = sb("x", [128, G4])
    c.op(V, lambda e: e.tensor_scalar(kq[:], th[:], 1.0 / (2 * math.pi), None, op0=ALU.mult), reads=[th], writes=[kq])
    c.op(V, lambda e: e.tensor_copy(ki[:], kq[:]), reads=[kq], writes=[ki])
    c.op(V, lambda e: e.tensor_copy(kq[:], ki[:]), reads=[ki], writes=[kq])
    c.op(V, lambda e: e.scalar_tensor_tensor(out=x[:], in0=kq[:], scalar=-2 * math.pi, in1=th[:], op0=ALU.mult, op1=ALU.add), reads=[kq, th], writes=[x])
    c.op(V, lambda e: e.tensor_scalar(x[:], x[:], 0.25, None, op0=ALU.mult), reads=[x], writes=[x])
    p1r = sb("p1r", [128, G4]); p1i = sb("p1i", [128, G4]); x2 = sb("x2", [128, G4]); acc = sb("acc", [128, G4])
    t1 = sb("t1", [128, G4]); t2 = sb("t2", [128, G4])
    emit_sincos_tile(c, x, x, p1r, p1i, x2, acc, G4)
    emit_cdouble(c, p1r, p1i, t1, t2)
    emit_cdouble(c, p1r, p1i, t1, t2)
    phr = sb("phr", [128, G4, 9]); phi = sb("phi", [128, G4, 9])
    c.op(V, lambda e: e.memset(phr[:, :, 0:1], 1.0), writes=[phr])
    c.op(V, lambda e: e.memset(phi[:, :, 0:1], 0.0), writes=[phi])
    for m in range(8):
        c.op(V, lambda e: e.tensor_tensor(out=t1[:], in0=phr[:, :, m], in1=p1r[:], op=ALU.mult), reads=[phr, p1r], writes=[t1])
        c.op(V, lambda e: e.tensor_tensor(out=t2[:], in0=phi[:, :, m], in1=p1i[:], op=ALU.mult), reads=[phi, p1i], writes=[t2])
        c.op(V, lambda e: e.tensor_tensor(out=phr[:, :, m + 1], in0=t1[:], in1=t2[:], op=ALU.subtract), reads=[t1, t2], writes=[phr])
        c.op(V, lambda e: e.tensor_tensor(out=t1[:], in0=phr[:, :, m], in1=p1i[:], op=ALU.mult), reads=[phr, p1i], writes=[t1])
        c.op(V, lambda e: e.tensor_tensor(out=t2[:], in0=phi[:, :, m], in1=p1r[:], op=ALU.mult), reads=[phi, p1r], writes=[t2])
        c.op(V, lambda e: e.tensor_tensor(out=phi[:, :, m + 1], in0=t1[:], in1=t2[:], op=ALU.add), reads=[t1, t2], writes=[phi])
    am = sb("am", [128, G4, 9]); magp = sb("magp", [128, G4, 9]); magn = sb("magn", [128, G4, 9])
    c.op(V, lambda e: e.tensor_tensor(out=am[:], in0=mv[:], in1=a[:].unsqueeze(2).to_broadcast([128, G4, 9]), op=ALU.mult), reads=[mv, a], writes=[am])
    c.op("act", lambda e: e.activation(out=magp[:], in_=am[:], func=AF.Exp), reads=[am], writes=[magp])
    c.op("act", lambda e: e.activation(out=magn[:], in_=am[:], func=AF.Exp, scale=-1.0), reads=[am], writes=[magn])
    LPr = sb("LPr", [128, G4, 9]); LPi = sb("LPi", [128, G4, 9]); LNr = sb("LNr", [128, G4, 9]); LNi = sb("LNi", [128, G4, 9])
    c.op(V, lambda e: e.tensor_tensor(out=LPr[:], in0=magp[:], in1=phr[:], op=ALU.mult), reads=[magp, phr], writes=[LPr])
    c.op(V, lambda e: e.tensor_tensor(out=LPi[:], in0=magp[:], in1=phi[:], op=ALU.mult), reads=[magp, phi], writes=[LPi])
    c.op(V, lambda e: e.tensor_tensor(out=LNr[:], in0=magn[:], in1=phr[:], op=ALU.mult), reads=[magn, phr], writes=[LNr])
    c.op(V, lambda e: e.scalar_tensor_tensor(out=LNi[:], in0=magn[:], scalar=-1.0, in1=phi[:], op0=ALU.mult, op1=ALU.mult), reads=[magn, phi], writes=[LNi])
    kr = sb("kr", [128, G4]); kim = sb("kim", [128, G4]); nr = sb("nr", [128, G4]); den = sb("den", [128, G4])
    c.op(V, lambda e: e.tensor_scalar(nr[:], LPr[:, :, 1], -1.0, None, op0=ALU.add), reads=[LPr], writes=[nr])
    c.op(V, lambda e: e.tensor_tensor(out=t1[:], in0=lam[:, :, 0], in1=lam[:, :, 0], op=ALU.mult), reads=[lam], writes=[t1])
    c.op(V, lambda e: e.tensor_tensor(out=t2[:], in0=lam[:, :, 1], in1=lam[:, :, 1], op=ALU.mult), reads=[lam], writes=[t2])
    c.op(V, lambda e: e.tensor_tensor(out=den[:], in0=t1[:], in1=t2[:], op=ALU.add), reads=[t1, t2], writes=[den])
    c.op(V, lambda e: e.reciprocal(den[:], den[:]), reads=[den], writes=[den])
    c.op(V, lambda e: e.tensor_tensor(out=t1[:], in0=nr[:], in1=lam[:, :, 0], op=ALU.mult), reads=[nr, lam], writes=[t1])
    c.op(V, lambda e: e.tensor_tensor(out=t2[:], in0=LPi[:, :, 1], in1=lam[:, :, 1], op=ALU.mult), reads=[LPi, lam], writes=[t2])
    c.op(V, lambda e: e.tensor_tensor(out=kr[:], in0=t1[:], in1=t2[:], op=ALU.add), reads=[t1, t2], writes=[kr])
    c.op(V, lambda e: e.tensor_tensor(out=kr[:], in0=kr[:], in1=den[:], op=ALU.mult), reads=[kr, den], writes=[kr])
    c.op(V, lambda e: e.tensor_tensor(out=t1[:], in0=LPi[:, :, 1], in1=lam[:, :, 0], op=ALU.mult), reads=[LPi, lam, kr], writes=[t1])
    c.op(V, lambda e: e.tensor_tensor(out=t2[:], in0=nr[:], in1=lam[:, :, 1], op=ALU.mult), reads=[nr, lam, kr], writes=[t2])
    c.op(V, lambda e: e.tensor_tensor(out=kim[:], in0=t1[:], in1=t2[:], op=ALU.subtract), reads=[t1, t2], writes=[kim])
    c.op(V, lambda e: e.tensor_tensor(out=kim[:], in0=kim[:], in1=den[:], op=ALU.mult), reads=[kim, den], writes=[kim])
    Bbr = sb("Bbr", [128, G4, 16]); Bbi = sb("Bbi", [128, G4, 16]); tb = sb("tb", [128, G4, 16])
    krb = kr[:].unsqueeze(2).to_broadcast([128, G4, 16]); kib = kim[:].unsqueeze(2).to_broadcast([128, G4, 16])
    c.op(V, lambda e: e.tensor_tensor(out=Bbr[:], in0=Bm[:, :, 0, :], in1=krb, op=ALU.mult), reads=[Bm, kr], writes=[Bbr])
    c.op(V, lambda e: e.tensor_tensor(out=tb[:], in0=Bm[:, :, 1, :], in1=kib, op=ALU.mult), reads=[Bm, kim], writes=[tb])
    c.op(V, lambda e: e.tensor_tensor(out=Bbr[:], in0=Bbr[:], in1=tb[:], op=ALU.subtract), reads=[Bbr, tb], writes=[Bbr])
    c.op(V, lambda e: e.tensor_tensor(out=Bbi[:], in0=Bm[:, :, 1, :], in1=krb, op=ALU.mult), reads=[Bm, kr], writes=[Bbi])
    c.op(V, lambda e: e.tensor_tensor(out=tb[:], in0=Bm[:, :, 0, :], in1=kib, op=ALU.mult), reads=[Bm, kim, Bbr], writes=[tb])
    c.op(V, lambda e: e.tensor_tensor(out=Bbi[:], in0=Bbi[:], in1=tb[:], op=ALU.add), reads=[Bbi, tb], writes=[Bbi])
    X1 = sb("X1", [128, G4, 16]); X2 = sb("X2", [128, G4, 16]); C1 = sb("C1", [128, G4, 16]); C2 = sb("C2", [128, G4, 16])
    c.op(V, lambda e: e.tensor_copy(X1[0:64], Bbr[0:64]), reads=[Bbr], writes=[X1])
    c.op(V, lambda e: e.tensor_copy(X1[64:128], Bbi[64:128]), reads=[Bbi], writes=[X1])
    c.op(V, lambda e: e.tensor_scalar(X2[0:64], Bbi[0:64], -1.0, None, op0=ALU.mult), reads=[Bbi], writes=[X2])
    c.op(V, lambda e: e.tensor_copy(X2[64:128], Bbr[64:128]), reads=[Bbr], writes=[X2])
    c.op(V, lambda e: e.tensor_copy(C1[0:64], Cm[0:64, :, 0, :]), reads=[Cm], writes=[C1])
    c.op(V, lambda e: e.tensor_scalar(C1[64:128], Cm[64:128, :, 1, :], -1.0, None, op0=ALU.mult), reads=[Cm], writes=[C1])
    c.op(V, lambda e: e.tensor_scalar(C2[0:64], Cm[0:64, :, 1, :], -1.0, None, op0=ALU.mult), reads=[Cm], writes=[C2])
    c.op(V, lambda e: e.tensor_scalar(C2[64:128], Cm[64:128, :, 0, :], -1.0, None, op0=ALU.mult), reads=[Cm], writes=[C2])
    Z = sb("Z", [128, G4, 8, 16]); Y = sb("Y", [128, G4, 8, 16]); Wc = sb("Wc", [128, G4, 8, 16]); tz = sb("tz", [128, 16])
    for g in range(G4):
        for j in range(8):
            for (OUT, Lr_, Li_, mi, A1, A2) in ((Z, LPr, LPi, 7 - j, X1, X2), (Y, LNr, LNi, j + 1, X1, X2), (Wc, LPr, LPi, j + 1, C1, C2)):
                c.op(V, lambda e: e.tensor_scalar(tz[:], A1[:, g, :], Lr_[:, g, mi:mi + 1], None, op0=ALU.mult), reads=[A1, Lr_], writes=[tz])
                c.op(V, lambda e: e.scalar_tensor_tensor(out=OUT[:, g, j, :], in0=A2[:, g, :], scalar=Li_[:, g, mi:mi + 1], in1=tz[:],
                                                         op0=ALU.mult, op1=ALU.add), reads=[A2, Li_, tz], writes=[OUT])
    ps_rot = Rot([c.ps(f"ps{i}", [128, 512], F32) for i in range(7)])
    Toep = sb("Toep", [128, G4, 128], BF16); W1 = sb("W1", [128, G4, 128], BF16); tf = sb("tf", [128, 128])
    for g in range(G4):
        ps = ps_rot.next()
        c.op("pe", lambda e: e.matmul(ps[:, 0:128], lhsT=Y[:, g].rearrange("p j h -> p (j h)"), rhs=Wc[:, g].rearrange("p j h -> p (j h)"),
                                      start=True, stop=True), reads=[Y, Wc], writes=[ps])
        c.op(V, lambda e: e.tensor_tensor(out=tf[:], in0=ps[:, 0:128], in1=tmask[:].rearrange("p j h -> p (j h)"), op=ALU.mult),
             reads=[ps, tmask], writes=[tf])
        c.op(V, lambda e: e.scalar_tensor_tensor(out=Toep[:, g, :], in0=ident[:], scalar=Dm[:, g:g + 1], in1=tf[:], op0=ALU.mult, op1=ALU.add),
             reads=[ident, Dm, tf], writes=[Toep])
        ps = ps_rot.next()
        c.op("pe", lambda e: e.transpose(ps[:, 0:128], Z[:, g].rearrange("p j h -> p (j h)"), ident[:]), reads=[Z, ident], writes=[ps])
        c.op("act", lambda e: e.copy(out=W1[:, g, :], in_=ps[:, 0:128]), reads=[ps], writes=[W1])
    R = sb("R", [128, G4, NLEV, 128])
    qr = sb("qr", [128, G4]); qi = sb("qi", [128, G4]); mg = sb("mg", [128, G4]); s1 = sb("s1", [128, G4]); s2 = sb("s2", [128, G4])
    c.op(V, lambda e: e.tensor_copy(qr[:], phr[:, :, 8]), reads=[phr], writes=[qr])
    c.op(V, lambda e: e.tensor_copy(qi[:], phi[:, :, 8]), reads=[phi], writes=[qi])
    for k in range(NLEV):
        c.op("act", lambda e: e.activation(out=mg[:], in_=a[:], func=AF.Exp, scale=float(8 * (2 ** k))), reads=[a], writes=[mg])
        c.op(V, lambda e: e.tensor_tensor(out=s1[:], in0=mg[:], in1=qr[:], op=ALU.mult), reads=[mg, qr], writes=[s1])
        c.op(V, lambda e: e.tensor_tensor(out=s2[:], in0=mg[:], in1=qi[:], op=ALU.mult), reads=[mg, qi], writes=[s2])
        c.op(V, lambda e: e.tensor_scalar(s2[:], s2[:], sgnh[:, 0:1], None, op0=ALU.mult), reads=[s2, sgnh], writes=[s2])
        for g in range(G4):
            c.op(V, lambda e: e.tensor_scalar(R[:, g, k, :], ident[:], s1[:, g:g + 1], None, op0=ALU.mult), reads=[ident, s1], writes=[R])
            c.op(V, lambda e: e.scalar_tensor_tensor(out=R[:, g, k, :], in0=pswap[:], scalar=s2[:, g:g + 1], in1=R[:, g, k, :],
                                                     op0=ALU.mult, op1=ALU.add), reads=[pswap, s2, R], writes=[R])
        if k < NLEV - 1:
            emit_cdouble(c, qr, qi, t1, t2)

    blocks = [(0, 512), (512, 512), (1024, NSB - 1024)]
    Ub = [sb(f"Ub{g}", [128, NSB], BF16) for g in range(G4)]
    Xp = [sb(f"Xp{g}", [128, NSB + 1]) for g in range(G4)]
    for g in range(G4):
        c.dma("pool", Ub[g][:], U_d.h.ap()[g], writes=[Ub[g]])
        c.op(V, lambda e: e.memset(Xp[g][:, 0:1], 0.0), writes=[Xp[g]])
        for (c0, n) in blocks:
            ps = ps_rot.next()
            c.op("pe", lambda e: e.matmul(ps[:, :n], lhsT=W1[:, g, :], rhs=Ub[g][:, c0:c0 + n], start=True, stop=True), reads=[W1, Ub[g]], writes=[ps])
            c.op("act", lambda e: e.copy(out=Xp[g][:, 1 + c0:1 + c0 + n], in_=ps[:, :n]), reads=[ps], writes=[Xp[g]])
    for k in range(NLEV):
        d = 2 ** k
        if d >= NSB:
            break
        L = NSB - d
        for g in range(G4):
            pl = []
            cc = 0
            while cc < L:
                n = min(512, L - cc)
                ps = ps_rot.next()
                c.op("pe", lambda e: e.matmul(ps[:, :n], lhsT=R[:, g, k, :], rhs=Xp[g][:, 1 + cc:1 + cc + n], start=True, stop=True),
                     reads=[R, Xp[g]], writes=[ps])
                pl.append((ps, cc, n))
                cc += n
            for (ps, cc, n) in pl:
                c.op(V, lambda e: e.tensor_tensor(out=Xp[g][:, 1 + d + cc:1 + d + cc + n], in0=Xp[g][:, 1 + d + cc:1 + d + cc + n], in1=ps[:, :n], op=ALU.add),
                     reads=[ps, Xp[g]], writes=[Xp[g]])
    y_rot = Rot([sb(f"ysb{i}", [128, NSB]) for i in range(2)])
    for g in range(G4):
        ysb = y_rot.next()
        for (c0, n) in blocks:
            ps = ps_rot.next()
            c.op("pe", lambda e: e.matmul(ps[:, :n], lhsT=Toep[:, g, :], rhs=Ub[g][:, c0:c0 + n], start=True, stop=False), reads=[Toep, Ub[g]], writes=[ps])
            c.op("pe", lambda e: e.matmul(ps[:, :n], lhsT=Wc[:, g].rearrange("p j h -> p (j h)"), rhs=Xp[g][:, c0:c0 + n], start=False, stop=True),
                 reads=[Wc, Xp[g]], writes=[ps])
            c.op("act", lambda e: e.copy(out=ysb[:, c0:c0 + n], in_=ps[:, :n]), reads=[ps], writes=[ysb])
        c.dma("sp", y_d.h.ap()[g], ysb[:], reads=[ysb])
    c.finish()
    c.close()
    print("s5 instructions:", c.n_inst)
    return nc


NT = 2080
def core_tok_idx(c):
    b, r = divmod(c, 4)
    return b, np.concatenate([np.arange(32 * r, 32 * r + 32), 128 + np.arange(2048 * r, 2048 * (r + 1))])

def build_H0(x, meta):
    B = x.shape[0]
    H = np.zeros((B, 8320, 1024), np.float32)
    H[:, 112:128, :] = meta[None]
    H[:, 128:, :] = x
    return H

def shard_T(H):
    out = []
    for c in range(8):
        b, idx = core_tok_idx(c)
        out.append(np.ascontiguousarray(H[b, idx, :].T))
    return out

def unshard_T(lst, C):
    H = np.zeros((2, 8320, C), lst[0].dtype)
    for c in range(8):
        b, idx = core_tok_idx(c)
        H[b, idx, :] = lst[c].T
    return H

def swap_cols():
    base = np.arange(256).reshape(8, 2, 16)[:, ::-1, :].reshape(256)
    return base

def w_in_ext(w_in_l):
    sw = swap_cols()
    q_sw = w_in_l[:, 1280:1536][:, sw]
    k_sw = w_in_l[:, 1536:1792][:, sw]
    return np.ascontiguousarray(np.concatenate([w_in_l, q_sw, k_sw], axis=1))

def gvec(g):
    return np.ascontiguousarray(g.reshape(-1, 128).T)

def s5_inputs(inp, L, u_b, r):
    gs = [4 * r + g for g in range(4)]
    U = np.stack([u_b[:, 16 * G:16 * G + 16].reshape(1040, 8, 16).transpose(1, 2, 0).reshape(128, 1040) for G in gs])
    def dup(a):
        return np.concatenate([a, a], axis=0)
    lam = np.stack([dup(np.stack([inp['s5_lam_re'][L, G], inp['s5_lam_im'][L, G]], -1)) for G in gs], 1)
    ls = np.tile(inp['s5_log_step'][L, gs][None, :], (128, 1))
    Bm = np.stack([dup(np.stack([inp['s5_b_re'][L, G], inp['s5_b_im'][L, G]], 1)) for G in gs], 1)
    Cm = np.stack([dup(np.stack([inp['s5_c_re'][L, G].T, inp['s5_c_im'][L, G].T], 1)) for G in gs], 1)
    Dm = np.stack([np.tile(inp['s5_d'][L, G], 8) for G in gs], 1)
    f = lambda a: np.ascontiguousarray(a.astype(np.float32))
    return {"U": f(U), "lam": f(lam), "ls": f(ls), "Bm": f(Bm), "Cm": f(Cm), "Dm": f(Dm)}

def s5_unpack(y, out_b, r):
    for g in range(4):
        G = 4 * r + g
        out_b[:, 16 * G:16 * G + 16] = y[g].reshape(8, 16, 1040).transpose(2, 0, 1).reshape(8320, 16)

def s5_raw_ref(inp, L, u):
    Bn, T, _ = u.shape
    out = np.zeros((Bn, T, 256))
    for G in range(16):
        lam = inp['s5_lam_re'][L, G].astype(np.float64) + 1j * inp['s5_lam_im'][L, G]
        step = np.exp(np.float64(inp['s5_log_step'][L, G]))
        lb = np.exp(lam * step)
        Bc = inp['s5_b_re'][L, G].astype(np.float64) + 1j * inp['s5_b_im'][L, G]
        Cc = inp['s5_c_re'][L, G].astype(np.float64) + 1j * inp['s5_c_im'][L, G]
        Bb = ((lb - 1) / lam)[:, None] * Bc
        d = inp['s5_d'][L, G].astype(np.float64)
        for b in range(Bn):
            ug = u[b, :, 16 * G:16 * G + 16].astype(np.float64)
            bu = ug @ Bb.T
            S = np.zeros(64, complex)
            y = np.zeros((T, 16))
            CH = 64
            pw = lb[None, :] ** np.arange(1, CH + 1)[:, None]
            ipw = lb[None, :] ** (-np.arange(1, CH + 1)[:, None])
            for t0 in range(0, T, CH):
                blk = bu[t0:t0 + CH]
                st = pw * (S[None, :] + np.cumsum(blk * ipw, axis=0))
                y[t0:t0 + CH] = (st @ Cc.T).real
                S = st[-1]
            out[b, :, 16 * G:16 * G + 16] = y + d * ug
    return out


_PROGS = {}


def _prog(key, fn):
    if key not in _PROGS:
        _PROGS[key] = fn()
    return _PROGS[key]


def _run(nc, maps):
    res = run_bass_kernel_spmd(nc, maps, core_ids=list(range(8)))
    return res.results


def kernel(**inp):
    inp = {k: np.asarray(v) for k, v in inp.items()}
    f32 = lambda a: np.ascontiguousarray(a, dtype=np.float32)
    H = build_H0(inp['x'], inp['meta_tokens'])
    sw = swap_cols()
    for L in range(2):
        hs = shard_T(H)
        W = w_in_ext(inp['w_in'][L])
        g = gvec(inp['norm_mix_g'][L])
        rA = _run(_prog("A", build_A), [{"hT": hs[c], "g": g, "w": W} for c in range(8)])
        proj = unshard_T([r["projT"] for r in rA], NCOL_A)
        rq = proj[:, :, 1280:1536]; rk = proj[:, :, 1536:1792]; rv = proj[:, :, 1792:2304]; rg = proj[:, :, 2304:2816]
        rqs = proj[:, :, 2816:3072]; rks = proj[:, :, 3072:3328]
        maps = []
        for c in range(8):
            b, r = divmod(c, 4)
            hds = [2 * r, 2 * r + 1]
            maps.append({"q": f32(rq[b, :, 64 * r:64 * r + 64].T), "qsw": f32(rqs[b, :, 64 * r:64 * r + 64].T),
                         "k": f32(rk[b, :, 64 * r:64 * r + 64].T), "ksw": f32(rks[b, :, 64 * r:64 * r + 64].T),
                         "v": f32(rv[b, :, 128 * r:128 * r + 128]), "gate": f32(rg[b, :, 128 * r:128 * r + 128]),
                         "outg": f32(np.tile(inp['ret_out_g'][L][128 * r:128 * r + 128][None, :], (128, 1))),
                         "hpart": f32(np.repeat(np.array(hds, np.float32), 32)[:, None]),
                         "hsel": f32(np.tile(np.array(hds, np.float32)[None, :], (128, 1)))})
        rR = _run(_prog("ret", build_ret), maps)
        yc = np.zeros((2, 8320, 512), np.float32)
        for c in range(8):
            b, r = divmod(c, 4)
            yc[b, :, 128 * r:128 * r + 128] = rR[c]["y"]
        cwL = inp['hg_conv_w'][L]
        maps = []
        for c in range(8):
            b, r = divmod(c, 4)
            x3 = np.stack([proj[b, :, 256 + 256 * s + 64 * r: 256 + 256 * s + 64 * r + 64].T for s in range(3)])
            convw = np.stack([cwL[:, 256 * s + 64 * r: 256 * s + 64 * r + 64].T for s in range(3)])
            maps.append({"x3": f32(x3), "convw": f32(convw), "gate": f32(proj[b, :, 1024 + 64 * r:1024 + 64 * r + 64]),
                         "outg": f32(np.tile(inp['hg_out_g'][L][64 * r:64 * r + 64][None, :], (128, 1))),
                         "lbp": f32(inp['hg_lb_param'][:, 64 * r:64 * r + 64].T)})
        rHg = _run(_prog(("hg", L), lambda: build_hg(L)), maps)
        yb = np.zeros((2, 8320, 256), np.float32)
        for c in range(8):
            b, r = divmod(c, 4)
            yb[b, :, 64 * r:64 * r + 64] = rHg[c]["y"]
        maps = []
        for c in range(8):
            b, r = divmod(c, 4)
            maps.append(s5_inputs(inp, L, proj[b, :, 0:256], r))
        rS = _run(_prog("s5", build_s5), maps)
        ya = np.zeros((2, 8320, 256), np.float32)
        for c in range(8):
            b, r = divmod(c, 4)
            s5_unpack(rS[c]["y"], ya[b], r)
        yas = shard_T(ya); ybcs = shard_T(np.concatenate([yb, yc], -1))
        moe = (L % 2 == 1)
        final = (L == 1)
        if not moe:
            w1 = f32(inp['ffn_w1'][L // 2][None]); w3 = f32(inp['ffn_w3'][L // 2][None]); w2 = f32(inp['ffn_w2'][L // 2][None])
            nc = _prog(("C", 1, final), lambda: build_C(1, 2816, final))
        else:
            w1 = f32(inp['moe_w1'][L // 2]); w3 = f32(inp['moe_w3'][L // 2]); w2 = f32(inp['moe_w2'][L // 2])
            nc = _prog(("C", 8, final), lambda: build_C(8, 3584, final))
        maps = []
        for c in range(8):
            m = {"hT": hs[c], "yaT": yas[c], "ybcT": ybcs[c], "wglu": f32(inp['s5_w_glu'][L]), "s5g": gvec(inp['s5_out_g'][L]),
                 "wout": f32(inp['w_out'][L]), "gffn": gvec(inp['norm_ffn_g'][L]), "w1": w1, "w3": w3, "w2": w2}
            if moe:
                m["router"] = f32(inp['moe_router'][L // 2])
            if final:
                m["gfin"] = gvec(inp['final_norm_g'])
            maps.append(m)
        rC = _run(nc, maps)
        H = unshard_T([r["hT_out"] for r in rC], 1024)
    return np.ascontiguousarray(H[:, 128:, :], dtype=np.float32)
```

```python
import math
import contextlib
import numpy as np
import concourse.bass as bass
import concourse.mybir as mybir
from concourse.bass_utils import run_bass_kernel_spmd


F32 = mybir.dt.float32
BF16 = mybir.dt.bfloat16
I32 = mybir.dt.int32
AF = mybir.ActivationFunctionType
ALU = mybir.AluOpType
AX = mybir.AxisListType


class T:
    def __init__(self, ctx, name, handle, space):
        self.ctx = ctx
        self.name = name
        self.h = handle
        self.space = space
        self.st = {}
        self.dq = None

    def __getitem__(self, idx):
        return self.h[idx]

    def state(self, key):
        s = self.st.get(key)
        if s is None:
            s = {"w": None, "r": {}}
            self.st[key] = s
        return s


class Q:
    def __init__(self, ctx, name, step):
        self.ctx = ctx
        self.name = name
        self.sem = ctx.root.enter_context(ctx.nc.semaphore(name))
        self.count = 0
        self.step = step


class Ctx:
    def __init__(self, nc):
        self.nc = nc
        self.stack = contextlib.ExitStack()
        self.root = self.stack
        self.engs = {}
        for name, eng in (("pe", nc.tensor), ("dve", nc.vector), ("act", nc.scalar),
                          ("pool", nc.gpsimd), ("sp", nc.sync)):
            q = Q(self, "q_" + name, 1)
            self.engs[name] = (eng, q)
        self.seen = {name: {} for name in self.engs}
        self.dmaq = {}
        self.n_inst = 0
        self.uid = 0
        self.pe_self_sync = False

    def sb(self, name, shape, dtype):
        self.uid += 1
        h = self.stack.enter_context(self.nc.sbuf_tensor(f"{name}_{self.uid}", list(shape), dtype))
        return T(self, name, h, "sb")

    def ps(self, name, shape, dtype=F32):
        self.uid += 1
        h = self.stack.enter_context(self.nc.psum_tensor(f"{name}_{self.uid}", list(shape), dtype))
        return T(self, name, h, "ps")

    def dram(self, name, shape, dtype, kind):
        h = self.nc.dram_tensor(name, list(shape), dtype, kind=kind)
        return T(self, name, h, "dram")

    def dma_q(self, name):
        q = self.dmaq.get(name)
        if q is None:
            q = Q(self, "dq_" + name, 16)
            self.dmaq[name] = q
        return q

    def _need(self, engname, q, value):
        if q is None:
            return
        if engname == "pe" and q is self.engs["pe"][1] and not self.pe_self_sync:
            return
        seen = self.seen[engname]
        if q.step == 16:
            value = q.count
        if seen.get(q.name, 0) >= value:
            return
        eng = self.engs[engname][0]
        eng.wait_ge(q.sem, value)
        seen[q.name] = value

    def _deps(self, engname, reads, writes):
        for (t, key) in reads:
            s = t.state(key)
            if s["w"] is not None:
                self._need(engname, *s["w"])
        for (t, key) in writes:
            s = t.state(key)
            if s["w"] is not None:
                self._need(engname, *s["w"])
            for q, v in s["r"].values():
                self._need(engname, q, v)

    def _mark(self, q, value, reads, writes):
        for (t, key) in reads:
            s = t.state(key)
            s["r"][q.name] = (q, value)
        for (t, key) in writes:
            s = t.state(key)
            s["w"] = (q, value)
            s["r"] = {}

    @staticmethod
    def _norm(lst):
        out = []
        for x in lst:
            if isinstance(x, tuple):
                out.append(x)
            else:
                out.append((x, None))
        return out

    def op(self, engname, fn, reads=(), writes=()):
        reads = self._norm(reads)
        writes = self._norm(writes)
        eng, q = self.engs[engname]
        self._deps(engname, reads, writes)
        ins = fn(eng)
        q.count += 1
        ins.then_inc(q.sem, 1)
        self._mark(q, q.count, reads, writes)
        self.n_inst += 1
        return ins

    def dma(self, engname, out, in_, reads=(), writes=(), qt=None, **kw):
        reads = self._norm(reads)
        writes = self._norm(writes)
        eng, _ = self.engs[engname]
        self._deps(engname, reads, writes)
        if qt is None:
            cands = [t for (t, k) in writes if t.space == "sb"] + [t for (t, k) in reads if t.space == "sb"]
            qt = cands[0]
        if qt.dq is None:
            self.uid += 1
            qt.dq = Q(self, f"dq_{qt.name}_{self.uid}", 16)
            self.dmaq[qt.dq.name] = qt.dq
        dq = qt.dq
        ins = eng.dma_start(out=out, in_=in_, **kw)
        dq.count += 16
        ins.then_inc(dq.sem, 16)
        self._mark(dq, dq.count, reads, writes)
        self.n_inst += 1
        return ins

    def barrier(self):
        for name, (eng, q0) in self.engs.items():
            for dq in self.dmaq.values():
                if dq.count:
                    self._need(name, dq, dq.count)
            for other, (e2, q2) in self.engs.items():
                if other != name and q2.count:
                    self._need(name, q2, q2.count)

    @contextlib.contextmanager
    def scope(self):
        old = self.stack
        self.stack = contextlib.ExitStack()
        try:
            yield
        finally:
            self.barrier()
            self.stack.close()
            self.stack = old

    def finish(self, engname="sp"):
        eng = self.engs[engname][0]
        for dq in self.dmaq.values():
            if dq.count:
                eng.wait_ge(dq.sem, dq.count)
        for name, (e, q) in self.engs.items():
            if q.count and name != engname:
                eng.wait_ge(q.sem, q.count)

    def close(self):
        self.stack.close()


NT = 2080
D = 1024
KC = 8
EPS = 1e-6
NCOL_A = 3328


def tblocks(nt=NT, bs=512):
    if nt == 2080 and bs == 512:
        return [(416 * i, 416) for i in range(5)]
    out = []
    t = 0
    while t < nt:
        n = min(bs, nt - t)
        out.append((t, n))
        t += n
    return out


class Rot:
    def __init__(self, tiles):
        self.tiles = tiles
        self.i = 0

    def next(self):
        t = self.tiles[self.i % len(self.tiles)]
        self.i += 1
        return t


def emit_consts(c):
    k = {}
    k["ones_bf"] = c.sb("ones_bf", [128, 128], BF16)
    c.op("pool", lambda e: e.memset(k["ones_bf"][:], 1.0), writes=[k["ones_bf"]])
    return k


def emit_rmsnorm(c, k, hT, g_sb, hnT, sq_rot, rs_rot, ps_rot, d_chunks=KC, dim=D, src_key=True):
    for bi, (t0, n) in enumerate(tblocks()):
        sq = sq_rot.next()
        c.op("act", lambda e: e.activation(out=sq[:, :d_chunks, :n], in_=hT[:, :, t0:t0 + n], func=AF.Square),
             reads=[(hT, bi)], writes=[sq])
        ps = ps_rot.next()
        for kc in range(d_chunks):
            c.op("pe", lambda e: e.matmul(ps[:, :n], lhsT=k["ones_bf"][:], rhs=sq[:, kc, :n],
                                          start=(kc == 0), stop=(kc == d_chunks - 1)),
                 reads=[sq, k["ones_bf"]], writes=[ps])
        rs = rs_rot.next()
        c.op("act", lambda e: e.activation(out=rs[:, :n], in_=ps[:, :n], func=AF.Sqrt, scale=1.0 / dim, bias=k["eps"][:, 0:1]),
             reads=[ps, k["eps"]], writes=[rs])
        c.op("dve", lambda e: e.reciprocal(rs[:, :n], rs[:, :n]), reads=[rs], writes=[rs])
        for kc in range(d_chunks):
            c.op("dve", lambda e: e.scalar_tensor_tensor(out=hnT[:, kc, t0:t0 + n], in0=hT[:, kc, t0:t0 + n],
                                                         scalar=g_sb[:, kc:kc + 1], in1=rs[:, :n],
                                                         op0=ALU.mult, op1=ALU.mult),
                 reads=[(hT, bi), rs, g_sb], writes=[(hnT, bi)])


def build_A():
    nc = bass.Bass("TRN2", target_bir_lowering=False)
    c = Ctx(nc)
    hT_d = c.dram("hT", [D, NT], F32, "ExternalInput")
    g_d = c.dram("g", [128, KC], F32, "ExternalInput")
    w_d = c.dram("w", [D, NCOL_A], F32, "ExternalInput")
    out_d = c.dram("projT", [NCOL_A, NT], F32, "ExternalOutput")

    k = emit_consts(c)
    k["eps"] = c.sb("eps", [128, 1], F32)
    c.op("pool", lambda e: e.memset(k["eps"][:], EPS), writes=[k["eps"]])
    hT = c.sb("hT", [128, KC, NT], F32)
    hnT = c.sb("hnT", [128, KC, NT], BF16)
    g_sb = c.sb("g", [128, KC], F32)
    c.dma("sp", g_sb[:], g_d[:], writes=[g_sb])
    hT_v = hT_d.h.ap().rearrange("(kc kp) t -> kp kc t", kp=128)
    for bi, (t0, n) in enumerate(tblocks()):
        c.dma("sp", hT[:, :, t0:t0 + n], hT_v[:, :, t0:t0 + n], writes=[(hT, bi)])
    sq_rot = Rot([c.sb(f"sq{i}", [128, KC, 512], BF16) for i in range(2)])
    rs_rot = Rot([c.sb(f"rs{i}", [128, 512], F32) for i in range(2)])
    ps_rot = Rot([c.ps(f"ps{i}", [128, 512], F32) for i in range(6)])
    emit_rmsnorm(c, k, hT, g_sb, hnT, sq_rot, rs_rot, ps_rot)

    w_rot = Rot([c.sb(f"w{i}", [128, KC, 512], BF16) for i in range(2)])
    w_st = c.sb("w_st", [128, KC, 512], F32)
    ost_rot = Rot([c.sb(f"ost{i}", [128, NT], F32) for i in range(3)])
    w_v = w_d.h.ap().rearrange("(kc kp) n -> kp kc n", kp=128)
    ev = 0
    for c0 in range(0, NCOL_A, 512):
        ncol = min(512, NCOL_A - c0)
        w_sb = w_rot.next()
        c.dma("sp", w_st[:, :, :ncol], w_v[:, :, c0:c0 + ncol], writes=[w_st])
        c.op("pool", lambda e: e.tensor_copy(w_sb[:, :, :ncol], w_st[:, :, :ncol]), reads=[w_st], writes=[w_sb])
        for cc in range(ncol // 128):
            ost = ost_rot.next()
            for bi, (t0, n) in enumerate(tblocks()):
                ps = ps_rot.next()
                for kc in range(KC):
                    c.op("pe", lambda e: e.matmul(ps[:, :n], lhsT=w_sb[:, kc, cc * 128:(cc + 1) * 128],
                                                  rhs=hnT[:, kc, t0:t0 + n], start=(kc == 0), stop=(kc == KC - 1)),
                         reads=[w_sb, (hnT, bi)], writes=[ps])
                if ev % 2 == 0:
                    c.op("act", lambda e: e.copy(out=ost[:, t0:t0 + n], in_=ps[:, :n]), reads=[ps], writes=[ost])
                else:
                    c.op("dve", lambda e: e.tensor_copy(ost[:, t0:t0 + n], ps[:, :n]), reads=[ps], writes=[ost])
                ev += 1
            r0 = c0 + cc * 128
            c.dma("sp", out_d[r0:r0 + 128, :], ost[:], reads=[ost])
    c.finish()
    c.close()
    print("phase A instructions:", c.n_inst)
    return nc


GF = 2
GH = 2


def emit_rmsnorm2(c, k, src, g_sb, dst_fn, sq_rot, rs_rot, ps_rot, d_chunks, dim, src_keyed=True):
    for bi, (t0, n) in enumerate(tblocks()):
        sk = (src, bi) if src_keyed else src
        sq = sq_rot.next()
        c.op("act", lambda e: e.activation(out=sq[:, :d_chunks, :n], in_=src[:, :, t0:t0 + n], func=AF.Square),
             reads=[sk], writes=[sq])
        ps = ps_rot.next()
        for kc in range(d_chunks):
            c.op("pe", lambda e: e.matmul(ps[:, :n], lhsT=k["ones_bf"][:], rhs=sq[:, kc, :n],
                                          start=(kc == 0), stop=(kc == d_chunks - 1)),
                 reads=[sq, k["ones_bf"]], writes=[ps])
        rs = rs_rot.next()
        c.op("act", lambda e: e.activation(out=rs[:, :n], in_=ps[:, :n], func=AF.Sqrt, scale=1.0 / dim, bias=k["eps"][:, 0:1]),
             reads=[ps, k["eps"]], writes=[rs])
        c.op("dve", lambda e: e.reciprocal(rs[:, :n], rs[:, :n]), reads=[rs], writes=[rs])
        for kc in range(d_chunks):
            ap, wk = dst_fn(bi, t0, n, kc)
            c.op("dve", lambda e: e.scalar_tensor_tensor(out=ap, in0=src[:, kc, t0:t0 + n],
                                                         scalar=g_sb[:, kc:kc + 1], in1=rs[:, :n],
                                                         op0=ALU.mult, op1=ALU.mult),
                 reads=[sk, rs, g_sb], writes=[wk])


def build_C(n_exp, F, final):
    moe = n_exp > 1
    nc = bass.Bass("TRN2", target_bir_lowering=False)
    c = Ctx(nc)
    hT_d = c.dram("hT", [D, NT], F32, "ExternalInput")
    ya_d = c.dram("yaT", [256, NT], F32, "ExternalInput")
    ybc_d = c.dram("ybcT", [768, NT], F32, "ExternalInput")
    wglu_d = c.dram("wglu", [256, 256], F32, "ExternalInput")
    s5g_d = c.dram("s5g", [128, 2], F32, "ExternalInput")
    wout_d = c.dram("wout", [D, D], F32, "ExternalInput")
    gffn_d = c.dram("gffn", [128, KC], F32, "ExternalInput")
    w1_d = c.dram("w1", [n_exp, D, F], F32, "ExternalInput")
    w3_d = c.dram("w3", [n_exp, D, F], F32, "ExternalInput")
    w2_d = c.dram("w2", [n_exp, F, D], F32, "ExternalInput")
    if moe:
        rt_d = c.dram("router", [D, 8], F32, "ExternalInput")
    if final:
        gfin_d = c.dram("gfin", [128, KC], F32, "ExternalInput")
    out_d = c.dram("hT_out", [D, NT], F32, "ExternalOutput")

    k = emit_consts(c)
    k["eps"] = c.sb("eps", [128, 1], F32)
    c.op("pool", lambda e: e.memset(k["eps"][:], EPS), writes=[k["eps"]])
    TB = tblocks()

    hT = c.sb("hT", [128, KC, NT], F32)
    hT_v = hT_d.h.ap().rearrange("(kc kp) t -> kp kc t", kp=128)
    for bi, (t0, n) in enumerate(TB):
        c.dma("sp", hT[:, :, t0:t0 + n], hT_v[:, :, t0:t0 + n], writes=[(hT, bi)], qt=hT)
    gffn = c.sb("gffn", [128, KC], F32)
    c.dma("sp", gffn[:], gffn_d[:], writes=[gffn])
    s5g = c.sb("s5g", [128, 2], F32)
    c.dma("sp", s5g[:], s5g_d[:], writes=[s5g])
    if final:
        gfin = c.sb("gfin", [128, KC], F32)
        c.dma("sp", gfin[:], gfin_d[:], writes=[gfin])

    ps_all = [c.ps(f"ps{i}", [128, 512], F32) for i in range(8)]
    ps_rot = Rot(ps_all[:7])

    with c.scope():
        sq_rot = Rot([c.sb(f"sq{i}", [128, KC, 512], BF16) for i in range(2)])
        rs_rot = Rot([c.sb(f"rs{i}", [128, 512], F32) for i in range(2)])
        yaT = c.sb("yaT", [128, 2, NT], F32)
        c.dma("sp", yaT[:], ya_d.h.ap().rearrange("(kc kp) t -> kp kc t", kp=128), writes=[yaT])
        mixedT = c.sb("mixedT", [128, KC, NT], BF16)
        ybc_v = ybc_d.h.ap().rearrange("(kc kp) t -> kp kc t", kp=128)
        for bi, (t0, n) in enumerate(TB):
            c.dma("pool", mixedT[:, 2:8, t0:t0 + n], ybc_v[:, :, t0:t0 + n], writes=[(mixedT, ("bc", bi))], qt=mixedT)
        wglu = c.sb("wglu", [128, 2, 256], BF16)
        c.dma("pool", wglu[:], wglu_d.h.ap().rearrange("(kc kp) n -> kp kc n", kp=128), writes=[wglu])
        wout = c.sb("wout", [128, KC, D], BF16)
        c.dma("pool", wout[:], wout_d.h.ap().rearrange("(kc kp) n -> kp kc n", kp=128), writes=[wout])

        t1_rot = Rot([c.sb(f"t1_{i}", [128, 2, 512], F32) for i in range(1)])
        t2_rot = Rot([c.sb(f"t2_{i}", [128, 2, 512], F32) for i in range(1)])
        ygf_rot = Rot([c.sb(f"ygf{i}", [128, 2, 512], F32) for i in range(1)])
        ygb_rot = Rot([c.sb(f"ygb{i}", [128, 2, 512], BF16) for i in range(2)])
        yaf_rot = Rot([c.sb(f"yaf{i}", [128, 2, 512], F32) for i in range(1)])
        sg_rot = Rot([c.sb(f"sg{i}", [128, 512], F32) for i in range(2)])
        for bi, (t0, n) in enumerate(TB):
            x = yaT[:, :, t0:t0 + n]
            t1 = t1_rot.next(); t2 = t2_rot.next(); ygf = ygf_rot.next(); ygb = ygb_rot.next(); yaf = yaf_rot.next()
            c.op("act", lambda e: e.activation(out=t1[:, :, :n], in_=x, func=AF.Square), reads=[yaT], writes=[t1])
            c.op("dve", lambda e: e.tensor_scalar(t1[:, :, :n], t1[:, :, :n], 0.044715, 1.0, op0=ALU.mult, op1=ALU.add),
                 reads=[t1], writes=[t1])
            c.op("dve", lambda e: e.tensor_tensor(out=t1[:, :, :n], in0=t1[:, :, :n], in1=x, op=ALU.mult),
                 reads=[t1, yaT], writes=[t1])
            c.op("act", lambda e: e.activation(out=t2[:, :, :n], in_=t1[:, :, :n], func=AF.Sigmoid, scale=1.5957691216057308),
                 reads=[t1], writes=[t2])
            c.op("dve", lambda e: e.tensor_tensor(out=ygf[:, :, :n], in0=t2[:, :, :n], in1=x, op=ALU.mult),
                 reads=[t2, yaT], writes=[ygf])
            c.op("act", lambda e: e.copy(out=ygb[:, :, :n], in_=ygf[:, :, :n]), reads=[ygf], writes=[ygb])
            for mo in range(2):
                ps = ps_rot.next()
                for ch in range(2):
                    c.op("pe", lambda e: e.matmul(ps[:, :n], lhsT=wglu[:, ch, mo * 128:(mo + 1) * 128], rhs=ygb[:, ch, :n],
                                                  start=(ch == 0), stop=(ch == 1)), reads=[wglu, ygb], writes=[ps])
                sg = sg_rot.next()
                c.op("act", lambda e: e.activation(out=sg[:, :n], in_=ps[:, :n], func=AF.Sigmoid), reads=[ps], writes=[sg])
                c.op("dve", lambda e: e.tensor_tensor(out=yaf[:, mo, :n], in0=ygf[:, mo, :n], in1=sg[:, :n], op=ALU.mult),
                     reads=[ygf, sg], writes=[yaf])
            sq = sq_rot.next()
            c.op("act", lambda e: e.activation(out=sq[:, :2, :n], in_=yaf[:, :, :n], func=AF.Square), reads=[yaf], writes=[sq])
            ps = ps_rot.next()
            for ch in range(2):
                c.op("pe", lambda e: e.matmul(ps[:, :n], lhsT=k["ones_bf"][:], rhs=sq[:, ch, :n], start=(ch == 0), stop=(ch == 1)),
                     reads=[sq, k["ones_bf"]], writes=[ps])
            rs = rs_rot.next()
            c.op("act", lambda e: e.activation(out=rs[:, :n], in_=ps[:, :n], func=AF.Sqrt, scale=1.0 / 256, bias=k["eps"][:, 0:1]),
                 reads=[ps, k["eps"]], writes=[rs])
            c.op("dve", lambda e: e.reciprocal(rs[:, :n], rs[:, :n]), reads=[rs], writes=[rs])
            for ch in range(2):
                c.op("dve", lambda e: e.scalar_tensor_tensor(out=mixedT[:, ch, t0:t0 + n], in0=yaf[:, ch, :n],
                                                             scalar=s5g[:, ch:ch + 1], in1=rs[:, :n], op0=ALU.mult, op1=ALU.mult),
                     reads=[yaf, rs, s5g], writes=[(mixedT, ("a", bi))])
            for dch in range(KC):
                ps = ps_rot.next()
                for cc in range(KC):
                    c.op("pe", lambda e: e.matmul(ps[:, :n], lhsT=wout[:, cc, dch * 128:(dch + 1) * 128], rhs=mixedT[:, cc, t0:t0 + n],
                                                  start=(cc == 0), stop=(cc == KC - 1)),
                         reads=[wout, (mixedT, ("a", bi)), (mixedT, ("bc", bi))], writes=[ps])
                c.op("dve", lambda e: e.tensor_tensor(out=hT[:, dch, t0:t0 + n], in0=hT[:, dch, t0:t0 + n], in1=ps[:, :n], op=ALU.add),
                     reads=[(hT, bi), ps], writes=[(hT, bi)])

    hnT = c.sb("hnT", [128, KC, NT], BF16)
    with c.scope():
        sq_rot = Rot([c.sb(f"sq{i}", [128, KC, 512], BF16) for i in range(2)])
        rs_rot = Rot([c.sb(f"rs{i}", [128, 512], F32) for i in range(2)])
        emit_rmsnorm2(c, k, hT, gffn, lambda bi, t0, n, kc: (hnT[:, kc, t0:t0 + n], (hnT, bi)), sq_rot, rs_rot, ps_rot, KC, D)

    if moe:
        gatesT = c.sb("gatesT", [8, NT], BF16)
        with c.scope():
            ident = c.sb("identf", [128, 128], F32)
            iof = c.sb("iof", [128, 128], F32)
            c.op("pool", lambda e: e.iota(iof[:], [[1, 128]], base=0, channel_multiplier=-1, allow_small_or_imprecise_dtypes=True), writes=[iof])
            c.op("dve", lambda e: e.tensor_single_scalar(ident[:], iof[:], 0.0, op=ALU.is_equal), reads=[iof], writes=[ident])
            rt = c.sb("rt", [128, KC, 8], F32)
            c.dma("sp", rt[:], rt_d.h.ap().rearrange("(kc kp) e -> kp kc e", kp=128), writes=[rt])
            gr = c.sb("gr", [128, KC, 16], F32)
            c.op("dve", lambda e: e.memset(gr[:], 0.0), writes=[gr])
            for kc in range(KC):
                c.op("dve", lambda e: e.tensor_scalar(gr[:, kc, 0:8], rt[:, kc, :], gffn[:, kc:kc + 1], None, op0=ALU.mult),
                     reads=[rt, gffn], writes=[gr])
            onesf = c.sb("onesf", [128, 1], F32)
            c.op("dve", lambda e: e.memset(onesf[:], 1.0), writes=[onesf])
            k["gatesT"] = gatesT
            sqf_rot = Rot([c.sb(f"sqf{i}", [128, KC, 128], F32) for i in range(2)])
            sm_rot = Rot([c.sb(f"sm{i}", [128, 64], F32) for i in range(3)])
            for ti, (t0, n) in enumerate(tblocks(NT, 128)):
                bi = t0 // 512
                sqf = sqf_rot.next()
                c.op("act", lambda e: e.activation(out=sqf[:, :, :n], in_=hT[:, :, t0:t0 + n], func=AF.Square), reads=[(hT, b_) for b_ in range(5)], writes=[sqf])
                ps = ps_all[7]
                for kc in range(KC):
                    c.op("pe", lambda e: e.matmul(ps[:n, 0:8], lhsT=hT[:, kc, t0:t0 + n], rhs=gr[:, kc, 0:8], start=(kc == 0), stop=(kc == KC - 1)),
                         reads=[(hT, b_) for b_ in range(5)] + [gr], writes=[ps])
                for kc in range(KC):
                    c.op("pe", lambda e: e.matmul(ps[:n, 8:9], lhsT=sqf[:, kc, :n], rhs=onesf[:, 0:1], start=(kc == 0), stop=(kc == KC - 1)),
                         reads=[sqf, onesf], writes=[ps])
                sm = sm_rot.next()
                c.op("act", lambda e: e.activation(out=sm[:n, 0:1], in_=ps[:n, 8:9], func=AF.Sqrt, scale=1.0 / D, bias=k["eps"][:n, 0:1]),
                     reads=[ps, k["eps"]], writes=[sm])
                c.op("dve", lambda e: e.reciprocal(sm[:n, 0:1], sm[:n, 0:1]), reads=[sm], writes=[sm])
                c.op("dve", lambda e: e.tensor_scalar(sm[:n, 8:16], ps[:n, 0:8], sm[:n, 0:1], None, op0=ALU.mult), reads=[ps, sm], writes=[sm])
                c.op("dve", lambda e: e.max(out=sm[:n, 16:24], in_=sm[:n, 8:16]), reads=[sm], writes=[sm])
                c.op("dve", lambda e: e.tensor_tensor(out=sm[:n, 24:25], in0=sm[:n, 17:18], in1=sm[:n, 16:17], op=ALU.subtract), reads=[sm], writes=[sm])
                c.op("act", lambda e: e.activation(out=sm[:n, 24:25], in_=sm[:n, 24:25], func=AF.Exp), reads=[sm], writes=[sm])
                c.op("dve", lambda e: e.tensor_scalar(sm[:n, 25:26], sm[:n, 24:25], 1.0, None, op0=ALU.add), reads=[sm], writes=[sm])
                c.op("dve", lambda e: e.reciprocal(sm[:n, 25:26], sm[:n, 25:26]), reads=[sm], writes=[sm])
                c.op("dve", lambda e: e.tensor_tensor(out=sm[:n, 26:27], in0=sm[:n, 24:25], in1=sm[:n, 25:26], op=ALU.mult), reads=[sm], writes=[sm])
                c.op("dve", lambda e: e.tensor_scalar(sm[:n, 32:40], sm[:n, 8:16], sm[:n, 16:17], sm[:n, 25:26], op0=ALU.is_equal, op1=ALU.mult), reads=[sm], writes=[sm])
                c.op("dve", lambda e: e.tensor_scalar(sm[:n, 40:48], sm[:n, 8:16], sm[:n, 17:18], sm[:n, 26:27], op0=ALU.is_equal, op1=ALU.mult), reads=[sm], writes=[sm])
                c.op("dve", lambda e: e.tensor_tensor(out=sm[:n, 48:56], in0=sm[:n, 32:40], in1=sm[:n, 40:48], op=ALU.add), reads=[sm], writes=[sm])
                c.op("pe", lambda e: e.transpose(ps[0:8, 16:16 + n], sm[:n, 48:56], ident[:n, :n]), reads=[sm, ident], writes=[ps])
                c.op("act", lambda e: e.copy(out=gatesT[:, t0:t0 + n], in_=ps[0:8, 16:16 + n]), reads=[ps], writes=[gatesT])
        sel = c.sb("sel", [8, 8, 128], BF16)
        self_f = c.sb("sel_f", [8, 8, 128], F32)
        c.op("pool", lambda e: e.iota(self_f[:], [[-1, 8], [0, 128]], base=0, channel_multiplier=1, allow_small_or_imprecise_dtypes=True), writes=[self_f])
        c.op("dve", lambda e: e.tensor_single_scalar(sel[:], self_f[:], 0.0, op=ALU.is_equal), reads=[self_f], writes=[sel])

    with c.scope():
        ps_a = Rot(ps_all[0:2]); ps_b = Rot(ps_all[2:4]); ps_o = Rot(ps_all[4:7]); ps_g = Rot(ps_all[7:8])
        NWB = 3
        w1_rot = Rot([c.sb(f"w1g{i}", [128, KC, GF * 128], BF16) for i in range(NWB)])
        w3_rot = Rot([c.sb(f"w3g{i}", [128, KC, GF * 128], BF16) for i in range(NWB)])
        w2_rot = Rot([c.sb(f"w2g{i}", [128, GF, D], BF16) for i in range(NWB)])
        w1s = c.sb("w1s", [128, KC, GH * 128], F32)
        w3s = c.sb("w3s", [128, KC, GH * 128], F32)
        w2s = c.sb("w2s", [128, GF, D], F32)
        sa_rot = Rot([c.sb(f"sa{i}", [128, 512], F32) for i in range(2)])
        gT_rot = Rot([c.sb(f"gT{i}", [128, GF, 512], BF16) for i in range(3)])
        ngrp = F // (GF * 128)
        groups = [(ex, gi) for ex in range(n_exp) for gi in range(ngrp)]
        wbuf = {}

        def load_dma(gidx):
            ex, gi = groups[gidx]
            w1_v = w1_d.h.ap()[ex].rearrange("(kc kp) f -> kp kc f", kp=128)
            w3_v = w3_d.h.ap()[ex].rearrange("(kc kp) f -> kp kc f", kp=128)
            w2_v = w2_d.h.ap()[ex].rearrange("(fc fp) d -> fp fc d", fp=128)
            f0 = gi * GF * 128
            c.dma("sp", w1s[:], w1_v[:, :, f0:f0 + GF * 128], writes=[w1s])
            c.dma("sp", w3s[:], w3_v[:, :, f0:f0 + GF * 128], writes=[w3s])
            c.dma("sp", w2s[:], w2_v[:, gi * GF:(gi + 1) * GF, :], writes=[w2s])

        def load_cast(gidx):
            w1g = w1_rot.next(); w3g = w3_rot.next(); w2g = w2_rot.next()
            c.op("act", lambda e: e.copy(out=w1g[:], in_=w1s[:]), reads=[w1s], writes=[w1g])
            c.op("act", lambda e: e.copy(out=w3g[:], in_=w3s[:]), reads=[w3s], writes=[w3g])
            c.op("act", lambda e: e.copy(out=w2g[:], in_=w2s[:]), reads=[w2s], writes=[w2g])
            wbuf[gidx] = (w1g, w3g, w2g)

        def load_group(gidx):
            load_dma(gidx)
            load_cast(gidx)

        gsb_rot = Rot([c.sb(f"gsb{i}", [128, 512], BF16) for i in range(2)]) if moe else None
        sa2_rot = Rot([c.sb(f"sa2_{i}", [128, 512], F32) for i in range(2)]) if moe else None

        def stage1_fc(gidx, bi, t0, n, fc, gT, gsb):
            ex, gi = groups[gidx]
            w1g, w3g, w2g = wbuf[gidx]
            pa = ps_a.next(); pb = ps_b.next()
            for kc in range(KC):
                c.op("pe", lambda e: e.matmul(pa[:, :n], lhsT=w1g[:, kc, fc * 128:(fc + 1) * 128], rhs=hnT[:, kc, t0:t0 + n],
                                              start=(kc == 0), stop=(kc == KC - 1)), reads=[w1g, (hnT, bi)], writes=[pa])
            for kc in range(KC):
                c.op("pe", lambda e: e.matmul(pb[:, :n], lhsT=w3g[:, kc, fc * 128:(fc + 1) * 128], rhs=hnT[:, kc, t0:t0 + n],
                                              start=(kc == 0), stop=(kc == KC - 1)), reads=[w3g, (hnT, bi)], writes=[pb])
            sa = sa_rot.next()
            c.op("act", lambda e: e.activation(out=sa[:, :n], in_=pa[:, :n], func=AF.Silu), reads=[pa], writes=[sa])
            if moe:
                sa2 = sa2_rot.next()
                c.op("pool", lambda e: e.tensor_tensor(out=sa2[:, :n], in0=sa[:, :n], in1=gsb[:, :n], op=ALU.mult), reads=[sa, gsb], writes=[sa2])
                return (sa2, pb)
            return (sa, pb)

        def stage1_fc_b(n, fc, gT, st):
            sx, pb = st
            c.op("dve", lambda e: e.tensor_tensor(out=gT[:, fc, :n], in0=sx[:, :n], in1=pb[:, :n], op=ALU.mult), reads=[sx, pb], writes=[gT])

        def stage1_gate(gidx, bi, t0, n):
            ex, gi = groups[gidx]
            pg = ps_g.next()
            c.op("pe", lambda e: e.matmul(pg[:, :n], lhsT=sel[:, ex, :], rhs=k["gatesT"][:, t0:t0 + n], start=True, stop=True),
                 reads=[sel, k["gatesT"]], writes=[pg])
            gsb = gsb_rot.next()
            c.op("act", lambda e: e.copy(out=gsb[:, :n], in_=pg[:, :n]), reads=[pg], writes=[gsb])
            return gsb

        def stage2_part(gidx, bi, t0, n, gT, d0, d1):
            w1g, w3g, w2g = wbuf[gidx]
            for dch in range(d0, d1):
                po = ps_o.next()
                for fc in range(GF):
                    c.op("pe", lambda e: e.matmul(po[:, :n], lhsT=w2g[:, fc, dch * 128:(dch + 1) * 128], rhs=gT[:, fc, :n],
                                                  start=(fc == 0), stop=(fc == GF - 1)), reads=[w2g, gT], writes=[po])
                c.op("dve", lambda e: e.tensor_tensor(out=hT[:, dch, t0:t0 + n], in0=hT[:, dch, t0:t0 + n], in1=po[:, :n], op=ALU.add),
                     reads=[(hT, bi), po], writes=[(hT, bi)])

        load_group(0)
        if len(groups) > 1:
            load_group(1)
        pending = None
        DS = KC // GF
        for gidx in range(len(groups)):
            for bi, (t0, n) in enumerate(TB):
                gsb = stage1_gate(gidx, bi, t0, n) if moe else None
                gT = gT_rot.next()
                for fc in range(GF):
                    st = stage1_fc(gidx, bi, t0, n, fc, gT, gsb)
                    if pending is not None:
                        stage2_part(*pending, fc * DS, (fc + 1) * DS)
                    stage1_fc_b(n, fc, gT, st)
                pending = (gidx, bi, t0, n, gT)
                if bi == 0 and gidx + 2 < len(groups):
                    load_dma(gidx + 2)
                if bi == 3 and gidx + 2 < len(groups):
                    load_cast(gidx + 2)
        stage2_part(*pending, 0, KC)

    out_v = out_d.h.ap().rearrange("(kc kp) t -> kp kc t", kp=128)
    if final:
        sq_rot = Rot([c.sb(f"sq{i}", [128, KC, 512], BF16) for i in range(2)])
        rs_rot = Rot([c.sb(f"rs{i}", [128, 512], F32) for i in range(2)])
        fo_rot = Rot([c.sb(f"fo{i}", [128, KC, 512], F32) for i in range(2)])
        cur = {}

        def dst(bi, t0, n, kc):
            if kc == 0:
                cur["t"] = fo_rot.next()
            return cur["t"][:, kc, :n], cur["t"]
        for bi, (t0, n) in enumerate(TB):
            pass
        emit_final(c, k, hT, gfin, fo_rot, out_v, sq_rot, rs_rot, Rot(ps_all[0:6]))
    else:
        for bi, (t0, n) in enumerate(TB):
            c.dma("sp", out_v[:, :, t0:t0 + n], hT[:, :, t0:t0 + n], reads=[(hT, bi)], qt=hT)
    c.finish()
    c.close()
    print("phase C instructions:", c.n_inst)
    return nc


def emit_final(c, k, hT, gfin, fo_rot, out_v, sq_rot, rs_rot, ps_rot):
    for bi, (t0, n) in enumerate(tblocks()):
        sq = sq_rot.next()
        c.op("act", lambda e: e.activation(out=sq[:, :, :n], in_=hT[:, :, t0:t0 + n], func=AF.Square), reads=[(hT, bi)], writes=[sq])
        ps = ps_rot.next()
        for kc in range(KC):
            c.op("pe", lambda e: e.matmul(ps[:, :n], lhsT=k["ones_bf"][:], rhs=sq[:, kc, :n], start=(kc == 0), stop=(kc == KC - 1)),
                 reads=[sq, k["ones_bf"]], writes=[ps])
        rs = rs_rot.next()
        c.op("act", lambda e: e.activation(out=rs[:, :n], in_=ps[:, :n], func=AF.Sqrt, scale=1.0 / D, bias=k["eps"][:, 0:1]),
             reads=[ps, k["eps"]], writes=[rs])
        c.op("dve", lambda e: e.reciprocal(rs[:, :n], rs[:, :n]), reads=[rs], writes=[rs])
        fo = fo_rot.next()
        for kc in range(KC):
            c.op("dve", lambda e: e.scalar_tensor_tensor(out=fo[:, kc, :n], in0=hT[:, kc, t0:t0 + n], scalar=gfin[:, kc:kc + 1], in1=rs[:, :n],
                                                         op0=ALU.mult, op1=ALU.mult), reads=[(hT, bi), rs, gfin], writes=[fo])
        c.dma("sp", out_v[:, :, t0:t0 + n], fo[:, :, :n], reads=[fo])


NTOK = 8320
NCH = 65
EPS = 1e-6
CB = 5


def emit_pow_table(c, P, n, bT, br, bi, save_at=None):
    Gr = c.sb("Gr", [P, n], F32)
    Gi = c.sb("Gi", [P, n], F32)
    tmp = c.sb("Gtmp", [P, max(n // 2, 1)], F32)
    s = c.sb("Gs", [P, 6], F32)
    saved = c.sb("Gsaved", [P, 2], F32) if save_at else None
    V = "dve"
    c.op(V, lambda e: e.memset(Gr[:, 0:1], 1.0), writes=[Gr])
    c.op(V, lambda e: e.memset(Gi[:, 0:1], 0.0), writes=[Gi])
    c.op(V, lambda e: e.tensor_copy(s[:, 0:1], br), reads=[bT], writes=[s])
    c.op(V, lambda e: e.tensor_copy(s[:, 1:2], bi), reads=[bT], writes=[s])
    m = 1
    while m < n:
        if save_at == m:
            c.op(V, lambda e: e.tensor_copy(saved[:, 0:2], s[:, 0:2]), reads=[s], writes=[saved])
        c.op(V, lambda e: e.tensor_scalar(tmp[:, :m], Gi[:, :m], s[:, 1:2], None, op0=ALU.mult), reads=[Gi, s], writes=[tmp])
        c.op(V, lambda e: e.scalar_tensor_tensor(out=Gr[:, m:2 * m], in0=Gr[:, :m], scalar=s[:, 0:1], in1=tmp[:, :m],
                                                 op0=ALU.mult, op1=ALU.subtract), reads=[Gr, s, tmp], writes=[Gr])
        c.op(V, lambda e: e.tensor_scalar(tmp[:, :m], Gi[:, :m], s[:, 0:1], None, op0=ALU.mult), reads=[Gi, s], writes=[tmp])
        c.op(V, lambda e: e.scalar_tensor_tensor(out=Gi[:, m:2 * m], in0=Gr[:, :m], scalar=s[:, 1:2], in1=tmp[:, :m],
                                                 op0=ALU.mult, op1=ALU.add), reads=[Gr, s, tmp], writes=[Gi])
        c.op(V, lambda e: e.tensor_tensor(out=s[:, 2:3], in0=s[:, 0:1], in1=s[:, 0:1], op=ALU.mult), reads=[s], writes=[s])
        c.op(V, lambda e: e.tensor_tensor(out=s[:, 3:4], in0=s[:, 1:2], in1=s[:, 1:2], op=ALU.mult), reads=[s], writes=[s])
        c.op(V, lambda e: e.scalar_tensor_tensor(out=s[:, 1:2], in0=s[:, 0:1], scalar=2.0, in1=s[:, 1:2],
                                                 op0=ALU.mult, op1=ALU.mult), reads=[s], writes=[s])
        c.op(V, lambda e: e.tensor_tensor(out=s[:, 0:1], in0=s[:, 2:3], in1=s[:, 3:4], op=ALU.subtract), reads=[s], writes=[s])
        m *= 2
    if save_at == m:
        c.op(V, lambda e: e.tensor_copy(saved[:, 0:2], s[:, 0:2]), reads=[s], writes=[saved])
    return Gr, Gi, saved


def emit_sincos_small(c, P, wT, w, cs):
    V = "dve"
    x2 = cs[:, 2:3]
    acc = cs[:, 3:4]
    c.op(V, lambda e: e.tensor_tensor(out=x2, in0=w, in1=w, op=ALU.mult), reads=[wT], writes=[cs])
    c.op(V, lambda e: e.tensor_scalar(acc, x2, -1.0 / 110, 1.0, op0=ALU.mult, op1=ALU.add), reads=[cs], writes=[cs])
    for d in (72.0, 42.0, 20.0, 6.0):
        c.op(V, lambda e: e.tensor_tensor(out=acc, in0=acc, in1=x2, op=ALU.mult), reads=[cs], writes=[cs])
        c.op(V, lambda e: e.tensor_scalar(acc, acc, -1.0 / d, 1.0, op0=ALU.mult, op1=ALU.add), reads=[cs], writes=[cs])
    c.op(V, lambda e: e.tensor_tensor(out=cs[:, 1:2], in0=acc, in1=w, op=ALU.mult), reads=[cs, wT], writes=[cs])
    c.op(V, lambda e: e.tensor_scalar(acc, x2, -1.0 / 132, 1.0, op0=ALU.mult, op1=ALU.add), reads=[cs], writes=[cs])
    for d in (90.0, 56.0, 30.0, 12.0, 2.0):
        c.op(V, lambda e: e.tensor_tensor(out=acc, in0=acc, in1=x2, op=ALU.mult), reads=[cs], writes=[cs])
        c.op(V, lambda e: e.tensor_scalar(acc, acc, -1.0 / d, 1.0, op0=ALU.mult, op1=ALU.add), reads=[cs], writes=[cs])
    c.op(V, lambda e: e.tensor_copy(cs[:, 0:1], acc), reads=[cs], writes=[cs])


def emit_gamma(c, hT_, hidx_ap, out, P, ncol):
    LN2 = math.log(2.0)
    c.op("act", lambda e: e.activation(out=out[:, :ncol], in_=hidx_ap, func=AF.Exp, scale=-LN2, bias=c.k5[:P, 0:1]),
         reads=[c.k5, hT_], writes=[out])
    c.op("dve", lambda e: e.tensor_scalar(out[:, :ncol], out[:, :ncol], -1.0, 1.0, op0=ALU.mult, op1=ALU.add), reads=[out], writes=[out])
    c.op("act", lambda e: e.activation(out=out[:, :ncol], in_=out[:, :ncol], func=AF.Ln), reads=[out], writes=[out])


def build_ret(debug=False, c=None, pfx=""):
    own = c is None
    if own:
        nc = bass.Bass("TRN2", target_bir_lowering=False)
        c = Ctx(nc)
    sc_ = None if own else c.scope()
    if sc_ is not None:
        sc_.__enter__()
    c.pe_self_sync = True
    q_d = c.dram(pfx + "q", [64, NTOK], F32, "ExternalInput")
    qs_d = c.dram(pfx + "qsw", [64, NTOK], F32, "ExternalInput")
    k_d = c.dram(pfx + "k", [64, NTOK], F32, "ExternalInput")
    ks_d = c.dram(pfx + "ksw", [64, NTOK], F32, "ExternalInput")
    v_d = c.dram(pfx + "v", [NTOK, 128], F32, "ExternalInput")
    g_d = c.dram(pfx + "gate", [NTOK, 128], F32, "ExternalInput")
    og_d = c.dram(pfx + "outg", [128, 128], F32, "ExternalInput")
    hp_d = c.dram(pfx + "hpart", [64, 1], F32, "ExternalInput")
    hs_d = c.dram(pfx + "hsel", [128, 2], F32, "ExternalInput")
    y_d = c.dram(pfx + "y", [NTOK, 128], F32, "ExternalOutput")

    V = "dve"
    c.k5 = c.sb("k5", [128, 1], F32)
    c.op("pool", lambda e: e.memset(c.k5[:], -5.0 * math.log(2.0)), writes=[c.k5])
    epsT = c.sb("eps", [128, 1], F32)
    c.op("pool", lambda e: e.memset(epsT[:], EPS), writes=[epsT])
    identb = c.sb("identb", [128, 128], BF16)
    iof = c.sb("iof", [128, 128], F32)
    c.op("pool", lambda e: e.iota(iof[:], [[1, 128]], base=0, channel_multiplier=-1, allow_small_or_imprecise_dtypes=True), writes=[iof])
    c.op(V, lambda e: e.tensor_single_scalar(identb[:], iof[:], 0.0, op=ALU.is_equal), reads=[iof], writes=[identb])
    hp = c.sb("hp", [64, 1], F32)
    c.dma("sp", hp[:], hp_d[:], writes=[hp])
    hs = c.sb("hs", [128, 2], F32)
    c.dma("sp", hs[:], hs_d[:], writes=[hs])
    og = c.sb("og", [128, 128], F32)
    c.dma("sp", og[:], og_d[:], writes=[og])
    lgP = c.sb("lgP", [64, 2], F32)
    emit_gamma(c, hp, hp[:, 0:1], lgP, 64, 1)
    lgB = c.sb("lgB", [128, 2], F32)
    emit_gamma(c, hs, hs[:, 0:2], lgB, 128, 2)
    maskT = c.sb("maskT", [128, 2, 128], F32)
    dpos = c.sb("dpos", [128, 128], F32)
    dge = c.sb("dge", [128, 128], F32)
    c.op(V, lambda e: e.tensor_single_scalar(dpos[:], iof[:], 0.0, op=ALU.max), reads=[iof], writes=[dpos])
    c.op(V, lambda e: e.tensor_scalar(dge[:], iof[:], 0.0, 32.0 ** -0.5, op0=ALU.is_ge, op1=ALU.mult), reads=[iof], writes=[dge])
    for hl in range(2):
        c.op("act", lambda e: e.activation(out=maskT[:, hl, :], in_=dpos[:], func=AF.Exp, scale=lgB[:, hl:hl + 1]),
             reads=[dpos, lgB], writes=[maskT])
        c.op(V, lambda e: e.tensor_tensor(out=maskT[:, hl, :], in0=maskT[:, hl, :], in1=dge[:], op=ALU.mult), reads=[maskT, dge], writes=[maskT])
    io1 = c.sb("io1", [64, 128], F32)
    c.op("pool", lambda e: e.iota(io1[:], [[1, 128]], base=1, channel_multiplier=0, allow_small_or_imprecise_dtypes=True), writes=[io1])
    io2 = c.sb("io2", [64, 128], F32)
    c.op("pool", lambda e: e.iota(io2[:], [[-1, 128]], base=127, channel_multiplier=0, allow_small_or_imprecise_dtypes=True), writes=[io2])
    qdf = c.sb("qdf", [64, 128], F32)
    kdf = c.sb("kdf", [64, 128], F32)
    c.op("act", lambda e: e.activation(out=qdf[:], in_=io1[:], func=AF.Exp, scale=lgP[:, 0:1]), reads=[io1, lgP], writes=[qdf])
    c.op(V, lambda e: e.tensor_scalar(qdf[:], qdf[:], 32.0 ** -0.5, None, op0=ALU.mult), reads=[qdf], writes=[qdf])
    c.op("act", lambda e: e.activation(out=kdf[:], in_=io2[:], func=AF.Exp, scale=lgP[:, 0:1]), reads=[io2, lgP], writes=[kdf])
    sdec = c.sb("sdec", [64, 1], F32)
    c.op("act", lambda e: e.activation(out=sdec[:], in_=lgP[:, 0:1], func=AF.Exp, scale=128.0), reads=[lgP], writes=[sdec])
    fr = c.sb("fr", [64, 4], F32)
    for hb in range(2):
        c.op("pool", lambda e: e.iota(fr[32 * hb:32 * hb + 32, 0:1], [[0, 1]], base=0, channel_multiplier=1,
                                      allow_small_or_imprecise_dtypes=True), writes=[fr])
    c.op(V, lambda e: e.tensor_single_scalar(fr[:, 3:4], fr[:, 0:1], 16.0, op=ALU.is_ge), reads=[fr], writes=[fr])
    c.op(V, lambda e: e.scalar_tensor_tensor(out=fr[:, 0:1], in0=fr[:, 3:4], scalar=-16.0, in1=fr[:, 0:1], op0=ALU.mult, op1=ALU.add),
         reads=[fr], writes=[fr])
    c.op(V, lambda e: e.tensor_scalar(fr[:, 2:3], fr[:, 3:4], 2.0, -1.0, op0=ALU.mult, op1=ALU.add), reads=[fr], writes=[fr])
    c.op("act", lambda e: e.activation(out=fr[:, 1:2], in_=fr[:, 0:1], func=AF.Exp, scale=-math.log(10000.0) / 16.0), reads=[fr], writes=[fr])
    cs = c.sb("cs", [64, 4], F32)
    emit_sincos_small(c, 64, fr, fr[:, 1:2], cs)
    Gr, Gi, s128 = emit_pow_table(c, 64, 256, cs, cs[:, 0:1], cs[:, 1:2], save_at=128)
    Fr, Fi, _ = emit_pow_table(c, 64, 64, s128, s128[:, 0:1], s128[:, 1:2])
    E1r = c.sb("E1r", [64, NCH], F32)
    E1i = c.sb("E1i", [64, NCH], F32)
    c.op(V, lambda e: e.tensor_copy(E1r[:, 1:NCH], Fr[:, 0:64]), reads=[Fr], writes=[E1r])
    c.op(V, lambda e: e.tensor_copy(E1i[:, 1:NCH], Fi[:, 0:64]), reads=[Fi], writes=[E1i])
    c.op(V, lambda e: e.tensor_copy(E1r[:, 0:1], Fr[:, 1:2]), reads=[Fr], writes=[E1r])
    c.op(V, lambda e: e.tensor_scalar(E1i[:, 0:1], Fi[:, 1:2], -1.0, None, op0=ALU.mult), reads=[Fi], writes=[E1i])
    E2r = Gr
    E2i = Gi

    QR = c.sb("QR", [64, NTOK], BF16)
    KR = c.sb("KR", [64, NTOK], BF16)
    QD = c.sb("QD", [64, NTOK], BF16)
    KD = c.sb("KD", [64, NTOK], BF16)
    v_sb = c.sb("v_sb", [128, NCH, 128], BF16)
    g_sb = c.sb("g_sb", [128, NCH, 128], BF16)
    y_sb = c.sb("y_sb", [128, NCH, 128], F32)
    c.dma("pool", v_sb[:], v_d.h.ap().rearrange("(c p) f -> p c f", p=128), writes=[v_sb])
    c.dma("pool", g_sb[:], g_d.h.ap().rearrange("(c p) f -> p c f", p=128), writes=[g_sb])

    nblk = NCH // CB
    BW = CB * 128
    x_rot = Rot([c.sb(f"x{i}", [64, BW], F32) for i in range(2)])
    xs_rot = Rot([c.sb(f"xs{i}", [64, BW], F32) for i in range(2)])
    COSb = c.sb("COSb", [64, CB, 128], F32)
    SINb = c.sb("SINb", [64, CB, 128], F32)
    tb1 = c.sb("tb1", [64, CB, 128], F32)
    tb2 = c.sb("tb2", [64, CB, 128], F32)
    r1 = c.sb("r1", [64, BW], F32)
    r2 = c.sb("r2", [64, BW], F32)
    for b in range(nblk):
        c0 = b * CB
        t0 = c0 * 128
        e1r = E1r[:, c0:c0 + CB].unsqueeze(2).to_broadcast([64, CB, 128])
        e1i = E1i[:, c0:c0 + CB].unsqueeze(2).to_broadcast([64, CB, 128])
        e2r = E2r[:, 16:144].unsqueeze(1).to_broadcast([64, CB, 128])
        e2i = E2i[:, 16:144].unsqueeze(1).to_broadcast([64, CB, 128])
        P_ = "pool"
        c.op(P_, lambda e: e.tensor_tensor(out=tb1[:], in0=e1r, in1=e2r, op=ALU.mult), reads=[E1r, E2r], writes=[tb1])
        c.op(P_, lambda e: e.tensor_tensor(out=tb2[:], in0=e1i, in1=e2i, op=ALU.mult), reads=[E1i, E2i], writes=[tb2])
        c.op(P_, lambda e: e.tensor_tensor(out=COSb[:], in0=tb1[:], in1=tb2[:], op=ALU.subtract), reads=[tb1, tb2], writes=[COSb])
        c.op(P_, lambda e: e.tensor_tensor(out=tb1[:], in0=e1r, in1=e2i, op=ALU.mult), reads=[E1r, E2i], writes=[tb1])
        c.op(P_, lambda e: e.tensor_tensor(out=tb2[:], in0=e1i, in1=e2r, op=ALU.mult), reads=[E1i, E2r], writes=[tb2])
        c.op(P_, lambda e: e.tensor_tensor(out=SINb[:], in0=tb1[:], in1=tb2[:], op=ALU.add), reads=[tb1, tb2], writes=[SINb])
        cosf = COSb[:].rearrange("p c j -> p (c j)")
        sinf = SINb[:].rearrange("p c j -> p (c j)")
        for (src_d, srcs_d, OUT, DEC, fac) in ((q_d, qs_d, QR, QD, qdf), (k_d, ks_d, KR, KD, kdf)):
            x = x_rot.next(); xs = xs_rot.next()
            c.dma("sp", x[:], src_d[:, t0:t0 + BW], writes=[x])
            c.dma("sp", xs[:], srcs_d[:, t0:t0 + BW], writes=[xs])
            c.op(V, lambda e: e.tensor_tensor(out=r1[:], in0=x[:], in1=cosf, op=ALU.mult), reads=[x, COSb], writes=[r1])
            c.op(V, lambda e: e.scalar_tensor_tensor(out=r2[:], in0=xs[:], scalar=fr[:, 2:3], in1=sinf, op0=ALU.mult, op1=ALU.mult),
                 reads=[xs, fr, SINb], writes=[r2])
            c.op(V, lambda e: e.tensor_tensor(out=OUT[:, t0:t0 + BW], in0=r1[:], in1=r2[:], op=ALU.add), reads=[r1, r2], writes=[(OUT, b)])
            facb = fac[:].unsqueeze(1).to_broadcast([64, CB, 128])
            c.op(V, lambda e: e.tensor_tensor(out=DEC[:, t0:t0 + BW].rearrange("p (c j) -> p c j", j=128),
                                              in0=OUT[:, t0:t0 + BW].rearrange("p (c j) -> p c j", j=128), in1=facb, op=ALU.mult),
                 reads=[(OUT, b), fac], writes=[(DEC, b)])

    gg_all = c.sb("gg_all", [128, NCH, 128], BF16)
    c.op("act", lambda e: e.activation(out=gg_all[:], in_=g_sb[:], func=AF.Silu), reads=[g_sb], writes=[gg_all])
    c.op("pool", lambda e: e.tensor_tensor(out=gg_all[:], in0=gg_all[:], in1=og[:].unsqueeze(1).to_broadcast([128, NCH, 128]), op=ALU.mult),
         reads=[gg_all, og], writes=[gg_all])
    ps_tr = Rot([c.ps(f"ps_tr{i}", [128, 64], BF16) for i in range(2)])
    ps_s = Rot([c.ps(f"ps_s{i}", [128, 2, 128], F32) for i in range(2)])
    ps_o = Rot([c.ps(f"ps_o{i}", [128, 128], F32) for i in range(2)])
    ps_d = Rot([c.ps(f"ps_d{i}", [64, 128], F32) for i in range(2)])
    kdt_rot = Rot([c.sb(f"kdt{i}", [128, 64], BF16) for i in range(2)])
    sT_rot = Rot([c.sb(f"sT{i}", [128, 2, 128], BF16) for i in range(2)])
    S32 = c.sb("S32", [64, 128], F32)
    c.op(V, lambda e: e.memset(S32[:], 0.0), writes=[S32])
    Sb_rot = Rot([c.sb(f"Sb{i}", [64, 128], BF16) for i in range(2)])
    Sb = Sb_rot.next()
    c.op(V, lambda e: e.memset(Sb[:], 0.0), writes=[Sb])
    o_rot = Rot([c.sb(f"o{i}", [128, 2, 64], F32) for i in range(2)])
    cen_rot = Rot([c.sb(f"cen{i}", [128, 2, 64], F32) for i in range(2)])
    sq_rot = Rot([c.sb(f"sqr{i}", [128, 2, 64], F32) for i in range(2)])
    st_rot = Rot([c.sb(f"st{i}", [128, 8], F32) for i in range(2)])
    gg_rot = Rot([c.sb(f"gg{i}", [128, 128], F32) for i in range(2)])
    for ch in range(NCH):
        b = ch // CB
        t0 = ch * 128
        ptr = ps_tr.next()
        c.op("pe", lambda e: e.transpose(ptr[:, :], KD[:, t0:t0 + 128], identb[0:64, 0:64]), reads=[(KD, b), identb], writes=[ptr])
        kdt = kdt_rot.next()
        c.op("act", lambda e: e.copy(out=kdt[:], in_=ptr[:]), reads=[ptr], writes=[kdt])
        pss = ps_s.next()
        for hl in range(2):
            c.op("pe", lambda e: e.matmul(pss[:, hl, :], lhsT=KR[32 * hl:32 * hl + 32, t0:t0 + 128], rhs=QR[32 * hl:32 * hl + 32, t0:t0 + 128],
                                          start=True, stop=True), reads=[(KR, b), (QR, b)], writes=[pss])
        sT = sT_rot.next()
        c.op(V, lambda e: e.tensor_tensor(out=sT[:], in0=pss[:], in1=maskT[:], op=ALU.mult), reads=[pss, maskT], writes=[sT])
        pso = ps_o.next()
        for hl in range(2):
            c.op("pe", lambda e: e.matmul(pso[:, hl * 64:(hl + 1) * 64], lhsT=sT[:, hl, :], rhs=v_sb[:, ch, hl * 64:(hl + 1) * 64],
                                          start=True, stop=False), reads=[sT, v_sb], writes=[pso])
            c.op("pe", lambda e: e.matmul(pso[:, hl * 64:(hl + 1) * 64], lhsT=QD[32 * hl:32 * hl + 32, t0:t0 + 128],
                                          rhs=Sb[32 * hl:32 * hl + 32, hl * 64:(hl + 1) * 64], start=False, stop=True),
                 reads=[(QD, b), Sb], writes=[pso])
        psd = ps_d.next()
        c.op("pe", lambda e: e.matmul(psd[:, :], lhsT=kdt[:], rhs=v_sb[:, ch, :], start=True, stop=True), reads=[kdt, v_sb], writes=[psd])
        c.op(V, lambda e: e.scalar_tensor_tensor(out=S32[:], in0=S32[:], scalar=sdec[:, 0:1], in1=psd[:], op0=ALU.mult, op1=ALU.add),
             reads=[S32, sdec, psd], writes=[S32])
        Sb = Sb_rot.next()
        c.op("act", lambda e: e.copy(out=Sb[:], in_=S32[:]), reads=[S32], writes=[Sb])
        o = o_rot.next(); cen = cen_rot.next(); sq = sq_rot.next(); st = st_rot.next(); gg = gg_rot.next()
        c.op("act", lambda e: e.copy(out=o[:].rearrange("p h v -> p (h v)"), in_=pso[:]), reads=[pso], writes=[o])
        c.op(V, lambda e: e.tensor_reduce(out=st[:, 0:2], in_=o[:], axis=AX.X, op=ALU.add), reads=[o], writes=[st])
        c.op(V, lambda e: e.tensor_scalar(st[:, 0:2], st[:, 0:2], 1.0 / 64, None, op0=ALU.mult), reads=[st], writes=[st])
        c.op(V, lambda e: e.tensor_tensor(out=cen[:], in0=o[:], in1=st[:, 0:2].unsqueeze(2).to_broadcast([128, 2, 64]), op=ALU.subtract),
             reads=[o, st], writes=[cen])
        c.op("pool", lambda e: e.tensor_tensor(out=sq[:], in0=cen[:], in1=cen[:], op=ALU.mult), reads=[cen], writes=[sq])
        c.op(V, lambda e: e.tensor_reduce(out=st[:, 2:4], in_=sq[:], axis=AX.X, op=ALU.add), reads=[sq, st], writes=[st])
        c.op("act", lambda e: e.activation(out=st[:, 2:4], in_=st[:, 2:4], func=AF.Sqrt, scale=1.0 / 64, bias=epsT[:, 0:1]), reads=[st, epsT], writes=[st])
        c.op(V, lambda e: e.reciprocal(st[:, 2:4], st[:, 2:4]), reads=[st], writes=[st])
        c.op(V, lambda e: e.tensor_tensor(out=cen[:], in0=cen[:], in1=st[:, 2:4].unsqueeze(2).to_broadcast([128, 2, 64]), op=ALU.mult),
             reads=[cen, st], writes=[cen])
        c.op(V, lambda e: e.tensor_tensor(out=y_sb[:, ch, :], in0=cen[:].rearrange("p h v -> p (h v)"), in1=gg_all[:, ch, :], op=ALU.mult),
             reads=[cen, gg_all], writes=[(y_sb, ch)])
    c.barrier()
    if debug:
        dbg = {"lgP": lgP, "lgB": lgB, "fr": fr, "cs": cs, "E1r": E1r, "E1i": E1i, "Gr": Gr, "Gi": Gi, "maskT": maskT,
               "qdf": qdf, "kdf": kdf, "sdec": sdec, "S32": S32, "COSb": COSb, "SINb": SINb}
        for nm, t in dbg.items():
            shp = list(t.h.shape)
            dd = c.dram(pfx + "dbg_" + nm, shp, F32, "ExternalOutput")
            c.dma("sp", dd[:], t[:], reads=[t])
        for nm, t in {"QR": QR, "KR": KR, "QD": QD, "KD": KD}.items():
            tmpf = c.sb("dbgf_" + nm, [64, 1024], F32)
            c.op("dve", lambda e: e.tensor_copy(tmpf[:], t[:, 0:1024]), writes=[tmpf])
            dd = c.dram(pfx + "dbg_" + nm, [64, 1024], F32, "ExternalOutput")
            c.dma("sp", dd[:], tmpf[:], reads=[tmpf])
    c.dma("sp", y_d.h.ap().rearrange("(c p) f -> p c f", p=128), y_sb[:], reads=[y_sb], qt=y_sb)
    c.pe_self_sync = False
    if sc_ is not None:
        sc_.__exit__(None, None, None)
        return None
    c.finish()
    c.close()
    print("ret instructions:", c.n_inst)
    return nc


NTOK = 8320
NG = 65
EPS = 1e-6
GB = 13
BW = GB * 128
NBLK = NG // GB
CS = 32


def build_hg(layer, debug=False, c=None, pfx=""):
    own = c is None
    if own:
        nc = bass.Bass("TRN2", target_bir_lowering=False)
        c = Ctx(nc)
    sc_ = None if own else c.scope()
    if sc_ is not None:
        sc_.__enter__()
    x_d = c.dram(pfx + "x3", [3, 64, NTOK], F32, "ExternalInput")
    cw_d = c.dram(pfx + "convw", [3, 64, 4], F32, "ExternalInput")
    g_d = c.dram(pfx + "gate", [NTOK, 64], F32, "ExternalInput")
    og_d = c.dram(pfx + "outg", [128, 64], F32, "ExternalInput")
    lb_d = c.dram(pfx + "lbp", [64, 2], F32, "ExternalInput")
    y_d = c.dram(pfx + "y", [NTOK, 64], F32, "ExternalOutput")
    V = "dve"

    epsT = c.sb("eps", [128, 1], F32)
    c.op("pool", lambda e: e.memset(epsT[:], EPS), writes=[epsT])
    iof = c.sb("iof", [128, 128], F32)
    c.op("pool", lambda e: e.iota(iof[:], [[1, 128]], base=0, channel_multiplier=-1, allow_small_or_imprecise_dtypes=True), writes=[iof])
    identf = c.sb("identf", [128, 128], F32)
    c.op(V, lambda e: e.tensor_single_scalar(identf[:], iof[:], 0.0, op=ALU.is_equal), reads=[iof], writes=[identf])
    identb = c.sb("identb", [128, 128], BF16)
    c.op(V, lambda e: e.tensor_copy(identb[:], identf[:]), reads=[identf], writes=[identb])
    dge = c.sb("dge", [128, 128], F32)
    c.op(V, lambda e: e.tensor_single_scalar(dge[:], iof[:], 0.0, op=ALU.is_ge), reads=[iof], writes=[dge])
    bmask = c.sb("bmask", [128, 128], F32)
    c.op(V, lambda e: e.memset(bmask[:], 0.0), writes=[bmask])
    for m in range(4):
        c.op(V, lambda e: e.tensor_copy(bmask[32 * m:32 * m + 32, 32 * m:32 * m + 32], dge[32 * m:32 * m + 32, 32 * m:32 * m + 32]),
             reads=[dge], writes=[bmask])
    cmask = c.sb("cmask", [128, 4, 64], F32)
    cm2 = c.sb("cm2", [128, 4, 64], F32)
    c.op("pool", lambda e: e.iota(cmask[:], [[-32, 4], [0, 64]], base=0, channel_multiplier=1, allow_small_or_imprecise_dtypes=True), writes=[cmask])
    c.op(V, lambda e: e.tensor_single_scalar(cm2[:], cmask[:], 32.0, op=ALU.is_lt), reads=[cmask], writes=[cm2])
    c.op(V, lambda e: e.tensor_single_scalar(cmask[:], cmask[:], 0.0, op=ALU.is_ge), reads=[cmask, cm2], writes=[cmask])
    c.op(V, lambda e: e.tensor_tensor(out=cmask[:], in0=cmask[:], in1=cm2[:], op=ALU.mult), reads=[cmask, cm2], writes=[cmask])
    colmask = c.sb("colmask", [64, 4, 128], F32)
    col2 = c.sb("col2", [64, 4, 128], F32)
    c.op("pool", lambda e: e.iota(colmask[:], [[-32, 4], [1, 128]], base=0, channel_multiplier=0, allow_small_or_imprecise_dtypes=True), writes=[colmask])
    c.op(V, lambda e: e.tensor_single_scalar(col2[:], colmask[:], 32.0, op=ALU.is_lt), reads=[colmask], writes=[col2])
    c.op(V, lambda e: e.tensor_single_scalar(colmask[:], colmask[:], 0.0, op=ALU.is_ge), reads=[colmask, col2], writes=[colmask])
    c.op(V, lambda e: e.tensor_tensor(out=colmask[:], in0=colmask[:], in1=col2[:], op=ALU.mult), reads=[colmask, col2], writes=[colmask])
    rmask = c.sb("rmask", [64, BW], F32)
    c.op("pool", lambda e: e.iota(rmask[:], [[0, BW // CS], [1, CS]], base=0, channel_multiplier=0, allow_small_or_imprecise_dtypes=True), writes=[rmask])
    c.op(V, lambda e: e.tensor_single_scalar(rmask[:], rmask[:], 0.0, op=ALU.is_gt), reads=[rmask], writes=[rmask])
    cw = c.sb("cw", [64, 3, 4], F32)
    c.dma("sp", cw[:], cw_d.h.ap().rearrange("s p k -> p s k"), writes=[cw])
    og = c.sb("og", [128, 64], F32)
    c.dma("sp", og[:], og_d[:], writes=[og])
    lbp = c.sb("lbp", [64, 4], F32)
    c.dma("sp", lbp[:, 0:2], lb_d[:], writes=[lbp])
    if layer == 0:
        c.op(V, lambda e: e.memset(lbp[:, 2:3], 0.0), reads=[lbp], writes=[lbp])
    else:
        c.op(V, lambda e: e.tensor_tensor(out=lbp[:, 2:3], in0=lbp[:, 1:2], in1=lbp[:, 0:1], op=ALU.subtract), reads=[lbp], writes=[lbp])
        c.op("act", lambda e: e.activation(out=lbp[:, 2:3], in_=lbp[:, 2:3], func=AF.Sigmoid), reads=[lbp], writes=[lbp])
    c.op(V, lambda e: e.tensor_scalar(lbp[:, 3:4], lbp[:, 2:3], -1.0, 1.0, op0=ALU.mult, op1=ALU.add), reads=[lbp], writes=[lbp])

    QT = c.sb("QT", [64, NTOK], BF16)
    KT = c.sb("KT", [64, NTOK], BF16)
    KDT = c.sb("KDT", [64, NTOK], BF16)
    Vtok = c.sb("Vtok", [128, NG, 64], BF16)
    KDtok = c.sb("KDtok", [128, NG, 64], BF16)
    g_sb = c.sb("g_sb", [128, NG, 64], BF16)
    y_sb = c.sb("y_sb", [128, NG, 64], F32)
    Dec = c.sb("Dec", [64, NTOK // CS], F32)
    c.dma("pool", g_sb[:], g_d.h.ap().rearrange("(c p) f -> p c f", p=128), writes=[g_sb])

    xq = c.sb("xq", [64, BW + 3], F32); xf = c.sb("xf", [64, BW + 3], F32); xi = c.sb("xi", [64, BW + 3], F32)
    cq = c.sb("cq", [64, BW], F32); cf = c.sb("cf", [64, BW], F32); ci = c.sb("ci", [64, BW], F32)
    gg_ = c.sb("g", [64, BW], F32); gcum = c.sb("gcum", [64, BW], F32); tmp = c.sb("tmp", [64, BW], F32)
    ps_t = Rot([c.ps(f"pst{i}", [128, 64], F32) for i in range(2)])
    ps_tb = Rot([c.ps(f"pstb{i}", [128, 64], BF16) for i in range(2)])
    NCB = BW // CS
    for b in range(NBLK):
        t0 = b * BW
        for si, (xt, ct) in enumerate(((xq, cq), (xf, cf), (xi, ci))):
            if b == 0:
                c.op(V, lambda e: e.memset(xt[:, 0:3], 0.0), writes=[xt])
                c.dma("sp", xt[:, 3:BW + 3], x_d.h.ap()[si, :, 0:BW], writes=[xt])
            else:
                c.dma("sp", xt[:, :], x_d.h.ap()[si, :, t0 - 3:t0 + BW], writes=[xt])
            c.op(V, lambda e: e.tensor_scalar(ct[:], xt[:, 0:BW], cw[:, si, 0:1], None, op0=ALU.mult), reads=[xt, cw], writes=[ct])
            for kk_ in range(1, 4):
                c.op(V, lambda e: e.scalar_tensor_tensor(out=ct[:], in0=xt[:, kk_:kk_ + BW], scalar=cw[:, si, kk_:kk_ + 1], in1=ct[:],
                                                         op0=ALU.mult, op1=ALU.add), reads=[xt, cw, ct], writes=[ct])
        c.op("act", lambda e: e.activation(out=cq[:], in_=cq[:], func=AF.Silu), reads=[cq], writes=[cq])
        c.op("act", lambda e: e.activation(out=cf[:], in_=cf[:], func=AF.Sigmoid), reads=[cf], writes=[cf])
        c.op(V, lambda e: e.tensor_scalar(cf[:], cf[:], lbp[:, 3:4], lbp[:, 2:3], op0=ALU.mult, op1=ALU.add), reads=[cf, lbp], writes=[cf])
        c.op("act", lambda e: e.activation(out=gg_[:], in_=cf[:], func=AF.Ln), reads=[cf], writes=[gg_])
        c.op(V, lambda e: e.tensor_scalar(cf[:], cf[:], -1.0, 1.0, op0=ALU.mult, op1=ALU.add), reads=[cf, gg_], writes=[cf])
        c.op(V, lambda e: e.tensor_tensor_scan(out=gcum[:], data0=rmask[:], data1=gg_[:], initial=0.0, op0=ALU.mult, op1=ALU.add),
             reads=[rmask, gg_], writes=[gcum])
        c.op("act", lambda e: e.activation(out=tmp[:], in_=gcum[:], func=AF.Exp), reads=[gcum], writes=[tmp])
        c.op(V, lambda e: e.tensor_tensor(out=QT[:, t0:t0 + BW], in0=cq[:], in1=tmp[:], op=ALU.mult), reads=[cq, tmp], writes=[(QT, b)])
        c.op(V, lambda e: e.tensor_single_scalar(tmp[:], gcum[:], -80.0, op=ALU.max), reads=[gcum, (QT, b)], writes=[tmp])
        c.op("act", lambda e: e.activation(out=tmp[:], in_=tmp[:], func=AF.Exp, scale=-1.0), reads=[tmp], writes=[tmp])
        c.op(V, lambda e: e.tensor_tensor(out=KT[:, t0:t0 + BW], in0=cf[:], in1=tmp[:], op=ALU.mult), reads=[cf, tmp], writes=[(KT, b)])
        gl = gcum[:].rearrange("p (c j) -> p c j", j=CS)[:, :, CS - 1:CS]
        c.op(V, lambda e: e.tensor_tensor(out=tmp[:].rearrange("p (c j) -> p c j", j=CS), in0=gl.to_broadcast([64, NCB, CS]),
                                          in1=gcum[:].rearrange("p (c j) -> p c j", j=CS), op=ALU.subtract), reads=[gcum, (KT, b)], writes=[tmp])
        c.op("act", lambda e: e.activation(out=tmp[:], in_=tmp[:], func=AF.Exp), reads=[tmp], writes=[tmp])
        c.op(V, lambda e: e.tensor_tensor(out=KDT[:, t0:t0 + BW], in0=cf[:], in1=tmp[:], op=ALU.mult), reads=[cf, tmp], writes=[(KDT, b)])
        c.op("act", lambda e: e.activation(out=Dec[:, b * NCB:(b + 1) * NCB].unsqueeze(2), in_=gl, func=AF.Exp), reads=[gcum], writes=[(Dec, b)])
        for gi in range(GB):
            G = b * GB + gi
            pt = ps_t.next()
            c.op("pe", lambda e: e.transpose(pt[:, :], ci[:, gi * 128:(gi + 1) * 128], identf[0:64, 0:64]), reads=[ci, identf], writes=[pt])
            c.op("act", lambda e: e.copy(out=Vtok[:, G, :], in_=pt[:]), reads=[pt], writes=[(Vtok, G)])
            ptb = ps_tb.next()
            c.op("pe", lambda e: e.transpose(ptb[:, :], KDT[:, t0 + gi * 128:t0 + (gi + 1) * 128], identb[0:64, 0:64]),
                 reads=[(KDT, b), identb], writes=[ptb])
            c.op("act", lambda e: e.copy(out=KDtok[:, G, :], in_=ptb[:]), reads=[ptb], writes=[(KDtok, G)])

    gg_all = c.sb("gg_all", [128, NG, 64], F32)
    c.op("act", lambda e: e.activation(out=gg_all[:], in_=g_sb[:], func=AF.Silu), reads=[g_sb], writes=[gg_all])
    c.op("pool", lambda e: e.tensor_tensor(out=gg_all[:], in0=gg_all[:], in1=og[:].unsqueeze(1).to_broadcast([128, NG, 64]), op=ALU.mult),
         reads=[gg_all, og], writes=[gg_all])
    ps_s = Rot([c.ps(f"ps_s{i}", [128, 128], F32) for i in range(1)])
    ps_o = Rot([c.ps(f"ps_o{i}", [128, 64], F32) for i in range(2)])
    ps_d = Rot([c.ps(f"ps_d{i}", [64, 4, 64], F32) for i in range(1)])
    sT_rot = Rot([c.sb(f"sT{i}", [128, 128], BF16) for i in range(2)])
    S32 = c.sb("S32", [64, 64], F32)
    c.op(V, lambda e: e.memset(S32[:], 0.0), writes=[S32])
    Sb_rot = Rot([c.sb(f"Sb{i}", [64, 64], BF16) for i in range(6)])
    Sb = Sb_rot.next()
    c.op(V, lambda e: e.memset(Sb[:], 0.0), writes=[Sb])
    o_rot = Rot([c.sb(f"o{i}", [128, 64], F32) for i in range(2)])
    sq_rot = Rot([c.sb(f"sqr{i}", [128, 64], F32) for i in range(2)])
    st_rot = Rot([c.sb(f"st{i}", [128, 4], F32) for i in range(2)])
    gg_rot = Rot([c.sb(f"gg{i}", [128, 64], F32) for i in range(2)])
    vb_rot = Rot([c.sb(f"vb{i}", [128, 4, 64], BF16) for i in range(2)])
    qm_rot = Rot([c.sb(f"qm{i}", [64, 4, 128], BF16) for i in range(2)])
    for G in range(NG):
        b = G // GB
        t0 = G * 128
        pss = ps_s.next()
        c.op("pe", lambda e: e.matmul(pss[:, :], lhsT=KT[:, t0:t0 + 128], rhs=QT[:, t0:t0 + 128], start=True, stop=True),
             reads=[(KT, b), (QT, b)], writes=[pss])
        sT = sT_rot.next()
        c.op(V, lambda e: e.tensor_tensor(out=sT[:], in0=pss[:], in1=bmask[:], op=ALU.mult), reads=[pss, bmask], writes=[sT])
        psd = ps_d.next()
        vb = vb_rot.next()
        c.op("pool", lambda e: e.tensor_tensor(out=vb[:], in0=Vtok[:, G, :].unsqueeze(1).to_broadcast([128, 4, 64]), in1=cmask[:], op=ALU.mult),
             reads=[(Vtok, G), cmask], writes=[vb])
        c.op("pe", lambda e: e.matmul(psd[:].rearrange("p m v -> p (m v)"), lhsT=KDtok[:, G, :], rhs=vb[:].rearrange("p m v -> p (m v)"),
                                      start=True, stop=True), reads=[(KDtok, G), vb], writes=[psd])
        qm = qm_rot.next()
        c.op(V, lambda e: e.tensor_tensor(out=qm[:], in0=QT[:, t0:t0 + 128].unsqueeze(1).to_broadcast([64, 4, 128]), in1=colmask[:], op=ALU.mult),
             reads=[(QT, b), colmask], writes=[qm])
        pso = ps_o.next()
        c.op("pe", lambda e: e.matmul(pso[:, :], lhsT=sT[:], rhs=Vtok[:, G, :], start=True, stop=False), reads=[sT, (Vtok, G)], writes=[pso])
        for m in range(4):
            ch = G * 4 + m
            c.op("pe", lambda e: e.matmul(pso[:, :], lhsT=qm[:, m, :], rhs=Sb[:, :],
                                          start=False, stop=(m == 3)), reads=[qm, Sb], writes=[pso])
            c.op(V, lambda e: e.scalar_tensor_tensor(out=S32[:], in0=S32[:], scalar=Dec[:, ch:ch + 1], in1=psd[:, m, :],
                                                     op0=ALU.mult, op1=ALU.add), reads=[S32, (Dec, b), psd], writes=[S32])
            Sb = Sb_rot.next()
            c.op("act", lambda e: e.copy(out=Sb[:], in_=S32[:]), reads=[S32], writes=[Sb])
        o = o_rot.next(); sq = sq_rot.next(); st = st_rot.next(); gg = gg_rot.next()
        c.op("act", lambda e: e.copy(out=o[:], in_=pso[:]), reads=[pso], writes=[o])
        c.op("pool", lambda e: e.tensor_tensor(out=sq[:], in0=o[:], in1=o[:], op=ALU.mult), reads=[o], writes=[sq])
        c.op(V, lambda e: e.tensor_reduce(out=st[:, 0:1], in_=sq[:], axis=AX.X, op=ALU.add), reads=[sq], writes=[st])
        c.op("act", lambda e: e.activation(out=st[:, 0:1], in_=st[:, 0:1], func=AF.Sqrt, scale=1.0 / 64, bias=epsT[:, 0:1]), reads=[st, epsT], writes=[st])
        c.op(V, lambda e: e.reciprocal(st[:, 0:1], st[:, 0:1]), reads=[st], writes=[st])
        c.op(V, lambda e: e.scalar_tensor_tensor(out=y_sb[:, G, :], in0=o[:], scalar=st[:, 0:1], in1=gg_all[:, G, :], op0=ALU.mult, op1=ALU.mult),
             reads=[o, st, gg_all], writes=[(y_sb, G)])
    c.barrier()
    c.dma("sp", y_d.h.ap().rearrange("(c p) f -> p c f", p=128), y_sb[:], reads=[y_sb], qt=y_sb)
    c.pe_self_sync = False
    if sc_ is not None:
        sc_.__exit__(None, None, None)
        return None
    c.finish()
    c.close()
    print("hg instructions:", c.n_inst)
    return nc


NSB = 1040
NGL = 4
NLEV = 11


def emit_sincos_tile(c, x, xT, cs_c, cs_s, x2, acc, n):
    V = "dve"
    c.op(V, lambda e: e.tensor_tensor(out=x2[:], in0=x[:], in1=x[:], op=ALU.mult), reads=[x], writes=[x2])
    c.op(V, lambda e: e.tensor_scalar(acc[:], x2[:], -1.0 / 156, 1.0, op0=ALU.mult, op1=ALU.add), reads=[x2], writes=[acc])
    for d in (110.0, 72.0, 42.0, 20.0, 6.0):
        c.op(V, lambda e: e.tensor_tensor(out=acc[:], in0=acc[:], in1=x2[:], op=ALU.mult), reads=[acc, x2], writes=[acc])
        c.op(V, lambda e: e.tensor_scalar(acc[:], acc[:], -1.0 / d, 1.0, op0=ALU.mult, op1=ALU.add), reads=[acc], writes=[acc])
    c.op(V, lambda e: e.tensor_tensor(out=cs_s[:], in0=acc[:], in1=x[:], op=ALU.mult), reads=[acc, x], writes=[cs_s])
    c.op(V, lambda e: e.tensor_scalar(acc[:], x2[:], -1.0 / 182, 1.0, op0=ALU.mult, op1=ALU.add), reads=[x2, cs_s], writes=[acc])
    for d in (132.0, 90.0, 56.0, 30.0, 12.0, 2.0):
        c.op(V, lambda e: e.tensor_tensor(out=acc[:], in0=acc[:], in1=x2[:], op=ALU.mult), reads=[acc, x2], writes=[acc])
        c.op(V, lambda e: e.tensor_scalar(acc[:], acc[:], -1.0 / d, 1.0, op0=ALU.mult, op1=ALU.add), reads=[acc], writes=[acc])
    c.op(V, lambda e: e.tensor_copy(cs_c[:], acc[:]), reads=[acc], writes=[cs_c])


def emit_cdouble(c, cr, ci, t1, t2):
    V = "dve"
    c.op(V, lambda e: e.tensor_tensor(out=t1[:], in0=cr[:], in1=cr[:], op=ALU.mult), reads=[cr], writes=[t1])
    c.op(V, lambda e: e.tensor_tensor(out=t2[:], in0=ci[:], in1=ci[:], op=ALU.mult), reads=[ci], writes=[t2])
    c.op(V, lambda e: e.scalar_tensor_tensor(out=ci[:], in0=cr[:], scalar=2.0, in1=ci[:], op0=ALU.mult, op1=ALU.mult), reads=[cr, ci, t2], writes=[ci])
    c.op(V, lambda e: e.tensor_tensor(out=cr[:], in0=t1[:], in1=t2[:], op=ALU.subtract), reads=[t1, t2, ci], writes=[cr])


def build_s5(debug=False, c=None, pfx=""):
    own = c is None
    if own:
        nc = bass.Bass("TRN2", target_bir_lowering=False)
        c = Ctx(nc)
    sc_ = None if own else c.scope()
    if sc_ is not None:
        sc_.__enter__()
    U_d = c.dram(pfx + "U", [NGL, 128, NSB], F32, "ExternalInput")
    lam_d = c.dram(pfx + "lam", [128, NGL, 2], F32, "ExternalInput")
    ls_d = c.dram(pfx + "ls", [128, NGL], F32, "ExternalInput")
    B_d = c.dram(pfx + "Bm", [128, NGL, 2, 16], F32, "ExternalInput")
    C_d = c.dram(pfx + "Cm", [128, NGL, 2, 16], F32, "ExternalInput")
    D_d = c.dram(pfx + "Dm", [128, NGL], F32, "ExternalInput")
    y_d = c.dram(pfx + "y", [NGL, 128, NSB], F32, "ExternalOutput")
    V = "dve"
    G4 = NGL

    def sb(name, shape, dt=F32):
        return c.sb(name, shape, dt)

    iof = sb("iof", [128, 128])
    c.op("pool", lambda e: e.iota(iof[:], [[1, 128]], base=0, channel_multiplier=-1, allow_small_or_imprecise_dtypes=True), writes=[iof])
    ident = sb("ident", [128, 128])
    c.op(V, lambda e: e.tensor_single_scalar(ident[:], iof[:], 0.0, op=ALU.is_equal), reads=[iof], writes=[ident])
    pswap = sb("pswap", [128, 128])
    ptmp = sb("ptmp", [128, 128])
    c.op(V, lambda e: e.tensor_single_scalar(pswap[:], iof[:], 64.0, op=ALU.is_equal), reads=[iof], writes=[pswap])
    c.op(V, lambda e: e.tensor_single_scalar(ptmp[:], iof[:], -64.0, op=ALU.is_equal), reads=[iof], writes=[ptmp])
    c.op(V, lambda e: e.tensor_tensor(out=pswap[:], in0=pswap[:], in1=ptmp[:], op=ALU.add), reads=[pswap, ptmp], writes=[pswap])
    tmask = sb("tmask", [128, 8, 16])
    c.op("pool", lambda e: e.iota(tmask[:], [[16, 8], [0, 16]], base=15, channel_multiplier=-1, allow_small_or_imprecise_dtypes=True), writes=[tmask])
    c.op(V, lambda e: e.tensor_single_scalar(tmask[:], tmask[:], 0.0, op=ALU.is_ge), reads=[tmask], writes=[tmask])
    sgnh = sb("sgnh", [128, 1])
    c.op(V, lambda e: e.memset(sgnh[0:64, :], 1.0), writes=[sgnh])
    c.op(V, lambda e: e.memset(sgnh[64:128, :], -1.0), writes=[sgnh])
    mv = sb("mv", [128, G4, 9])
    c.op("pool", lambda e: e.iota(mv[:], [[0, G4], [1, 9]], base=0, channel_multiplier=0, allow_small_or_imprecise_dtypes=True), writes=[mv])

    lam = sb("lam", [128, G4, 2]); ls = sb("ls", [128, G4]); Bm = sb("Bm", [128, G4, 2, 16]); Cm = sb("Cm", [128, G4, 2, 16]); Dm = sb("Dm", [128, G4])
    c.dma("sp", lam[:], lam_d[:], writes=[lam]); c.dma("sp", ls[:], ls_d[:], writes=[ls])
    c.dma("sp", Bm[:], B_d[:], writes=[Bm]); c.dma("sp", Cm[:], C_d[:], writes=[Cm]); c.dma("sp", Dm[:], D_d[:], writes=[Dm])

    st = sb("st", [128, G4]); a = sb("a", [128, G4]); th = sb("th", [128, G4])
    c.op("act", lambda e: e.activation(out=st[:], in_=ls[:], func=AF.Exp), reads=[ls], writes=[st])
    c.op(V, lambda e: e.tensor_tensor(out=a[:], in0=lam[:, :, 0], in1=st[:], op=ALU.mult), reads=[lam, st], writes=[a])
    c.op(V, lambda e: e.tensor_tensor(out=th[:], in0=lam[:, :, 1], in1=st[:], op=ALU.mult), reads=[lam, st], writes=[th])
    kq = sb("kq", [128, G4]); ki = sb("ki", [128, G4], I32); x = sb("x", [128, G4])
    c.op(V, lambda e: e.tensor_scalar(kq[:], th[:], 1.0 / (2 * math.pi), None, op0=ALU.mult), reads=[th], writes=[kq])
    c.op(V, lambda e: e.tensor_copy(ki[:], kq[:]), reads=[kq], writes=[ki])
    c.op(V, lambda e: e.tensor_copy(kq[:], ki[:]), reads=[ki], writes=[kq])
    c.op(V, lambda e: e.scalar_tensor_tensor(out=x[:], in0=kq[:], scalar=-2 * math.pi, in1=th[:], op0=ALU.mult, op1=ALU.add), reads=[kq, th], writes=[x])
    c.op(V, lambda e: e.tensor_scalar(x[:], x[:], 0.25, None, op0=ALU.mult), reads=[x], writes=[x])
    p1r = sb("p1r", [128, G4]); p1i = sb("p1i", [128, G4]); x2 = sb("x2", [128, G4]); acc = sb("acc", [128, G4])
    t1 = sb("t1", [128, G4]); t2 = sb("t2", [128, G4])
    emit_sincos_tile(c, x, x, p1r, p1i, x2, acc, G4)
    emit_cdouble(c, p1r, p1i, t1, t2)
    emit_cdouble(c, p1r, p1i, t1, t2)
    phr = sb("phr", [128, G4, 9]); phi = sb("phi", [128, G4, 9])
    c.op(V, lambda e: e.memset(phr[:, :, 0:1], 1.0), writes=[phr])
    c.op(V, lambda e: e.memset(phi[:, :, 0:1], 0.0), writes=[phi])
    for m in range(8):
        c.op(V, lambda e: e.tensor_tensor(out=t1[:], in0=phr[:, :, m], in1=p1r[:], op=ALU.mult), reads=[phr, p1r], writes=[t1])
        c.op(V, lambda e: e.tensor_tensor(out=t2[:], in0=phi[:, :, m], in1=p1i[:], op=ALU.mult), reads=[phi, p1i], writes=[t2])
        c.op(V, lambda e: e.tensor_tensor(out=phr[:, :, m + 1], in0=t1[:], in1=t2[:], op=ALU.subtract), reads=[t1, t2], writes=[phr])
        c.op(V, lambda e: e.tensor_tensor(out=t1[:], in0=phr[:, :, m], in1=p1i[:], op=ALU.mult), reads=[phr, p1i], writes=[t1])
        c.op(V, lambda e: e.tensor_tensor(out=t2[:], in0=phi[:, :, m], in1=p1r[:], op=ALU.mult), reads=[phi, p1r], writes=[t2])
        c.op(V, lambda e: e.tensor_tensor(out=phi[:, :, m + 1], in0=t1[:], in1=t2[:], op=ALU.add), reads=[t1, t2], writes=[phi])
    am = sb("am", [128, G4, 9]); magp = sb("magp", [128, G4, 9]); magn = sb("magn", [128, G4, 9])
    c.op(V, lambda e: e.tensor_tensor(out=am[:], in0=mv[:], in1=a[:].unsqueeze(2).to_broadcast([128, G4, 9]), op=ALU.mult), reads=[mv, a], writes=[am])
    c.op("act", lambda e: e.activation(out=magp[:], in_=am[:], func=AF.Exp), reads=[am], writes=[magp])
    c.op("act", lambda e: e.activation(out=magn[:], in_=am[:], func=AF.Exp, scale=-1.0), reads=[am], writes=[magn])
    LPr = sb("LPr", [128, G4, 9]); LPi = sb("LPi", [128, G4, 9]); LNr = sb("LNr", [128, G4, 9]); LNi = sb("LNi", [128, G4, 9])
    c.op(V, lambda e: e.tensor_tensor(out=LPr[:], in0=magp[:], in1=phr[:], op=ALU.mult), reads=[magp, phr], writes=[LPr])
    c.op(V, lambda e: e.tensor_tensor(out=LPi[:], in0=magp[:], in1=phi[:], op=ALU.mult), reads=[magp, phi], writes=[LPi])
    c.op(V, lambda e: e.tensor_tensor(out=LNr[:], in0=magn[:], in1=phr[:], op=ALU.mult), reads=[magn, phr], writes=[LNr])
    c.op(V, lambda e: e.scalar_tensor_tensor(out=LNi[:], in0=magn[:], scalar=-1.0, in1=phi[:], op0=ALU.mult, op1=ALU.mult), reads=[magn, phi], writes=[LNi])
    kr = sb("kr", [128, G4]); kim = sb("kim", [128, G4]); nr = sb("nr", [128, G4]); den = sb("den", [128, G4])
    c.op(V, lambda e: e.tensor_scalar(nr[:], LPr[:, :, 1], -1.0, None, op0=ALU.add), reads=[LPr], writes=[nr])
    c.op(V, lambda e: e.tensor_tensor(out=t1[:], in0=lam[:, :, 0], in1=lam[:, :, 0], op=ALU.mult), reads=[lam], writes=[t1])
    c.op(V, lambda e: e.tensor_tensor(out=t2[:], in0=lam[:, :, 1], in1=lam[:, :, 1], op=ALU.mult), reads=[lam], writes=[t2])
    c.op(V, lambda e: e.tensor_tensor(out=den[:], in0=t1[:], in1=t2[:], op=ALU.add), reads=[t1, t2], writes=[den])
    c.op(V, lambda e: e.reciprocal(den[:], den[:]), reads=[den], writes=[den])
    c.op(V, lambda e: e.tensor_tensor(out=t1[:], in0=nr[:], in1=lam[:, :, 0], op=ALU.mult), reads=[nr, lam], writes=[t1])
    c.op(V, lambda e: e.tensor_tensor(out=t2[:], in0=LPi[:, :, 1], in1=lam[:, :, 1], op=ALU.mult), reads=[LPi, lam], writes=[t2])
    c.op(V, lambda e: e.tensor_tensor(out=kr[:], in0=t1[:], in1=t2[:], op=ALU.add), reads=[t1, t2], writes=[kr])
    c.op(V, lambda e: e.tensor_tensor(out=kr[:], in0=kr[:], in1=den[:], op=ALU.mult), reads=[kr, den], writes=[kr])
    c.op(V, lambda e: e.tensor_tensor(out=t1[:], in0=LPi[:, :, 1], in1=lam[:, :, 0], op=ALU.mult), reads=[LPi, lam, kr], writes=[t1])
    c.op(V, lambda e: e.tensor_tensor(out=t2[:], in0=nr[:], in1=lam[:, :, 1], op=ALU.mult), reads=[nr, lam, kr], writes=[t2])
    c.op(V, lambda e: e.tensor_tensor(out=kim[:], in0=t1[:], in1=t2[:], op=ALU.subtract), reads=[t1, t2], writes=[kim])
    c.op(V, lambda e: e.tensor_tensor(out=kim[:], in0=kim[:], in1=den[:], op=ALU.mult), reads=[kim, den], writes=[kim])
    Bbr = sb("Bbr", [128, G4, 16]); Bbi = sb("Bbi", [128, G4, 16]); tb = sb("tb", [128, G4, 16])
    krb = kr[:].unsqueeze(2).to_broadcast([128, G4, 16]); kib = kim[:].unsqueeze(2).to_broadcast([128, G4, 16])
    c.op(V, lambda e: e.tensor_tensor(out=Bbr[:], in0=Bm[:, :, 0, :], in1=krb, op=ALU.mult), reads=[Bm, kr], writes=[Bbr])
    c.op(V, lambda e: e.tensor_tensor(out=tb[:], in0=Bm[:, :, 1, :], in1=kib, op=ALU.mult), reads=[Bm, kim], writes=[tb])
    c.op(V, lambda e: e.tensor_tensor(out=Bbr[:], in0=Bbr[:], in1=tb[:], op=ALU.subtract), reads=[Bbr, tb], writes=[Bbr])
    c.op(V, lambda e: e.tensor_tensor(out=Bbi[:], in0=Bm[:, :, 1, :], in1=krb, op=ALU.mult), reads=[Bm, kr], writes=[Bbi])
    c.op(V, lambda e: e.tensor_tensor(out=tb[:], in0=Bm[:, :, 0, :], in1=kib, op=ALU.mult), reads=[Bm, kim, Bbr], writes=[tb])
    c.op(V, lambda e: e.tensor_tensor(out=Bbi[:], in0=Bbi[:], in1=tb[:], op=ALU.add), reads=[Bbi, tb], writes=[Bbi])
    X1 = sb("X1", [128, G4, 16]); X2 = sb("X2", [128, G4, 16]); C1 = sb("C1", [128, G4, 16]); C2 = sb("C2", [128, G4, 16])
    c.op(V, lambda e: e.tensor_copy(X1[0:64], Bbr[0:64]), reads=[Bbr], writes=[X1])
    c.op(V, lambda e: e.tensor_copy(X1[64:128], Bbi[64:128]), reads=[Bbi], writes=[X1])
    c.op(V, lambda e: e.tensor_scalar(X2[0:64], Bbi[0:64], -1.0, None, op0=ALU.mult), reads=[Bbi], writes=[X2])
    c.op(V, lambda e: e.tensor_copy(X2[64:128], Bbr[64:128]), reads=[Bbr], writes=[X2])
    c.op(V, lambda e: e.tensor_copy(C1[0:64], Cm[0:64, :, 0, :]), reads=[Cm], writes=[C1])
    c.op(V, lambda e: e.tensor_scalar(C1[64:128], Cm[64:128, :, 1, :], -1.0, None, op0=ALU.mult), reads=[Cm], writes=[C1])
    c.op(V, lambda e: e.tensor_scalar(C2[0:64], Cm[0:64, :, 1, :], -1.0, None, op0=ALU.mult), reads=[Cm], writes=[C2])
    c.op(V, lambda e: e.tensor_scalar(C2[64:128], Cm[64:128, :, 0, :], -1.0, None, op0=ALU.mult), reads=[Cm], writes=[C2])
    Z = sb("Z", [128, G4, 8, 16]); Y = sb("Y", [128, G4, 8, 16]); Wc = sb("Wc", [128, G4, 8, 16]); tz = sb("tz", [128, 16])
    for g in range(G4):
        for j in range(8):
            for (OUT, Lr_, Li_, mi, A1, A2) in ((Z, LPr, LPi, 7 - j, X1, X2), (Y, LNr, LNi, j + 1, X1, X2), (Wc, LPr, LPi, j + 1, C1, C2)):
                c.op(V, lambda e: e.tensor_scalar(tz[:], A1[:, g, :], Lr_[:, g, mi:mi + 1], None, op0=ALU.mult), reads=[A1, Lr_], writes=[tz])
                c.op(V, lambda e: e.scalar_tensor_tensor(out=OUT[:, g, j, :], in0=A2[:, g, :], scalar=Li_[:, g, mi:mi + 1], in1=tz[:],
                                                         op0=ALU.mult, op1=ALU.add), reads=[A2, Li_, tz], writes=[OUT])
    ps_rot = Rot([c.ps(f"ps{i}", [128, 512], F32) for i in range(7)])
    Toep = sb("Toep", [128, G4, 128], BF16); W1 = sb("W1", [128, G4, 128], BF16); tf = sb("tf", [128, 128])
    for g in range(G4):
        ps = ps_rot.next()
        c.op("pe", lambda e: e.matmul(ps[:, 0:128], lhsT=Y[:, g].rearrange("p j h -> p (j h)"), rhs=Wc[:, g].rearrange("p j h -> p (j h)"),
                                      start=True, stop=True), reads=[Y, Wc], writes=[ps])
        c.op(V, lambda e: e.tensor_tensor(out=tf[:], in0=ps[:, 0:128], in1=tmask[:].rearrange("p j h -> p (j h)"), op=ALU.mult),
             reads=[ps, tmask], writes=[tf])
        c.op(V, lambda e: e.scalar_tensor_tensor(out=Toep[:, g, :], in0=ident[:], scalar=Dm[:, g:g + 1], in1=tf[:], op0=ALU.mult, op1=ALU.add),
             reads=[ident, Dm, tf], writes=[Toep])
        ps = ps_rot.next()
        c.op("pe", lambda e: e.transpose(ps[:, 0:128], Z[:, g].rearrange("p j h -> p (j h)"), ident[:]), reads=[Z, ident], writes=[ps])
        c.op("act", lambda e: e.copy(out=W1[:, g, :], in_=ps[:, 0:128]), reads=[ps], writes=[W1])
    R = sb("R", [128, G4, NLEV, 128])
    qr = sb("qr", [128, G4]); qi = sb("qi", [128, G4]); mg = sb("mg", [128, G4]); s1 = sb("s1", [128, G4]); s2 = sb("s2", [128, G4])
    c.op(V, lambda e: e.tensor_copy(qr[:], phr[:, :, 8]), reads=[phr], writes=[qr])
    c.op(V, lambda e: e.tensor_copy(qi[:], phi[:, :, 8]), reads=[phi], writes=[qi])
    for k in range(NLEV):
        c.op("act", lambda e: e.activation(out=mg[:], in_=a[:], func=AF.Exp, scale=float(8 * (2 ** k))), reads=[a], writes=[mg])
        c.op(V, lambda e: e.tensor_tensor(out=s1[:], in0=mg[:], in1=qr[:], op=ALU.mult), reads=[mg, qr], writes=[s1])
        c.op(V, lambda e: e.tensor_tensor(out=s2[:], in0=mg[:], in1=qi[:], op=ALU.mult), reads=[mg, qi], writes=[s2])
        c.op(V, lambda e: e.tensor_scalar(s2[:], s2[:], sgnh[:, 0:1], None, op0=ALU.mult), reads=[s2, sgnh], writes=[s2])
        for g in range(G4):
            c.op(V, lambda e: e.tensor_scalar(R[:, g, k, :], ident[:], s1[:, g:g + 1], None, op0=ALU.mult), reads=[ident, s1], writes=[R])
            c.op(V, lambda e: e.scalar_tensor_tensor(out=R[:, g, k, :], in0=pswap[:], scalar=s2[:, g:g + 1], in1=R[:, g, k, :],
                                                     op0=ALU.mult, op1=ALU.add), reads=[pswap, s2, R], writes=[R])
        if k < NLEV - 1:
            emit_cdouble(c, qr, qi, t1, t2)

    blocks = [(0, 512), (512, 512), (1024, NSB - 1024)]
    Ub = [sb(f"Ub{g}", [128, NSB], BF16) for g in range(G4)]
    Xp = [sb(f"Xp{g}", [128, NSB + 1]) for g in range(G4)]
    for g in range(G4):
        c.dma("pool", Ub[g][:], U_d.h.ap()[g], writes=[Ub[g]])
        c.op(V, lambda e: e.memset(Xp[g][:, 0:1], 0.0), writes=[Xp[g]])
        for (c0, n) in blocks:
            ps = ps_rot.next()
            c.op("pe", lambda e: e.matmul(ps[:, :n], lhsT=W1[:, g, :], rhs=Ub[g][:, c0:c0 + n], start=True, stop=True), reads=[W1, Ub[g]], writes=[ps])
            c.op("act", lambda e: e.copy(out=Xp[g][:, 1 + c0:1 + c0 + n], in_=ps[:, :n]), reads=[ps], writes=[Xp[g]])
    for k in range(NLEV):
        d = 2 ** k
        if d >= NSB:
            break
        L = NSB - d
        for g in range(G4):
            pl = []
            cc = 0
            while cc < L:
                n = min(512, L - cc)
                ps = ps_rot.next()
                c.op("pe", lambda e: e.matmul(ps[:, :n], lhsT=R[:, g, k, :], rhs=Xp[g][:, 1 + cc:1 + cc + n], start=True, stop=True),
                     reads=[R, Xp[g]], writes=[ps])
                pl.append((ps, cc, n))
                cc += n
            for (ps, cc, n) in pl:
                c.op(V, lambda e: e.tensor_tensor(out=Xp[g][:, 1 + d + cc:1 + d + cc + n], in0=Xp[g][:, 1 + d + cc:1 + d + cc + n], in1=ps[:, :n], op=ALU.add),
                     reads=[ps, Xp[g]], writes=[Xp[g]])
    y_rot = Rot([sb(f"ysb{i}", [128, NSB]) for i in range(2)])
    for g in range(G4):
        ysb = y_rot.next()
        for (c0, n) in blocks:
            ps = ps_rot.next()
            c.op("pe", lambda e: e.matmul(ps[:, :n], lhsT=Toep[:, g, :], rhs=Ub[g][:, c0:c0 + n], start=True, stop=False), reads=[Toep, Ub[g]], writes=[ps])
            c.op("pe", lambda e: e.matmul(ps[:, :n], lhsT=Wc[:, g].rearrange("p j h -> p (j h)"), rhs=Xp[g][:, c0:c0 + n], start=False, stop=True),
                 reads=[Wc, Xp[g]], writes=[ps])
            c.op("act", lambda e: e.copy(out=ysb[:, c0:c0 + n], in_=ps[:, :n]), reads=[ps], writes=[ysb])
        c.dma("sp", y_d.h.ap()[g], ysb[:], reads=[ysb])
    c.pe_self_sync = False
    if sc_ is not None:
        sc_.__exit__(None, None, None)
        return None
    c.finish()
    c.close()
    print("s5 instructions:", c.n_inst)
    return nc


NT = 2080
def core_tok_idx(c):
    b, r = divmod(c, 4)
    return b, np.concatenate([np.arange(32 * r, 32 * r + 32), 128 + np.arange(2048 * r, 2048 * (r + 1))])

def build_H0(x, meta):
    B = x.shape[0]
    H = np.zeros((B, 8320, 1024), np.float32)
    H[:, 112:128, :] = meta[None]
    H[:, 128:, :] = x
    return H

def shard_T(H):
    out = []
    for c in range(8):
        b, idx = core_tok_idx(c)
        out.append(np.ascontiguousarray(H[b, idx, :].T))
    return out

def unshard_T(lst, C):
    H = np.zeros((2, 8320, C), lst[0].dtype)
    for c in range(8):
        b, idx = core_tok_idx(c)
        H[b, idx, :] = lst[c].T
    return H

def swap_cols():
    base = np.arange(256).reshape(8, 2, 16)[:, ::-1, :].reshape(256)
    return base

def w_in_ext(w_in_l):
    sw = swap_cols()
    q_sw = w_in_l[:, 1280:1536][:, sw]
    k_sw = w_in_l[:, 1536:1792][:, sw]
    return np.ascontiguousarray(np.concatenate([w_in_l, q_sw, k_sw], axis=1))

def gvec(g):
    return np.ascontiguousarray(g.reshape(-1, 128).T)

def s5_inputs(inp, L, u_b, r):
    gs = [4 * r + g for g in range(4)]
    U = np.stack([u_b[:, 16 * G:16 * G + 16].reshape(1040, 8, 16).transpose(1, 2, 0).reshape(128, 1040) for G in gs])
    def dup(a):
        return np.concatenate([a, a], axis=0)
    lam = np.stack([dup(np.stack([inp['s5_lam_re'][L, G], inp['s5_lam_im'][L, G]], -1)) for G in gs], 1)
    ls = np.tile(inp['s5_log_step'][L, gs][None, :], (128, 1))
    Bm = np.stack([dup(np.stack([inp['s5_b_re'][L, G], inp['s5_b_im'][L, G]], 1)) for G in gs], 1)
    Cm = np.stack([dup(np.stack([inp['s5_c_re'][L, G].T, inp['s5_c_im'][L, G].T], 1)) for G in gs], 1)
    Dm = np.stack([np.tile(inp['s5_d'][L, G], 8) for G in gs], 1)
    f = lambda a: np.ascontiguousarray(a.astype(np.float32))
    return {"U": f(U), "lam": f(lam), "ls": f(ls), "Bm": f(Bm), "Cm": f(Cm), "Dm": f(Dm)}

def s5_unpack(y, out_b, r):
    for g in range(4):
        G = 4 * r + g
        out_b[:, 16 * G:16 * G + 16] = y[g].reshape(8, 16, 1040).transpose(2, 0, 1).reshape(8320, 16)

def s5_raw_ref(inp, L, u):
    Bn, T, _ = u.shape
    out = np.zeros((Bn, T, 256))
    for G in range(16):
        lam = inp['s5_lam_re'][L, G].astype(np.float64) + 1j * inp['s5_lam_im'][L, G]
        step = np.exp(np.float64(inp['s5_log_step'][L, G]))
        lb = np.exp(lam * step)
        Bc = inp['s5_b_re'][L, G].astype(np.float64) + 1j * inp['s5_b_im'][L, G]
        Cc = inp['s5_c_re'][L, G].astype(np.float64) + 1j * inp['s5_c_im'][L, G]
        Bb = ((lb - 1) / lam)[:, None] * Bc
        d = inp['s5_d'][L, G].astype(np.float64)
        for b in range(Bn):
            ug = u[b, :, 16 * G:16 * G + 16].astype(np.float64)
            bu = ug @ Bb.T
            S = np.zeros(64, complex)
            y = np.zeros((T, 16))
            CH = 64
            pw = lb[None, :] ** np.arange(1, CH + 1)[:, None]
            ipw = lb[None, :] ** (-np.arange(1, CH + 1)[:, None])
            for t0 in range(0, T, CH):
                blk = bu[t0:t0 + CH]
                st = pw * (S[None, :] + np.cumsum(blk * ipw, axis=0))
                y[t0:t0 + CH] = (st @ Cc.T).real
                S = st[-1]
            out[b, :, 16 * G:16 * G + 16] = y + d * ug
    return out


_PROGS = {}


def _prog(key, fn):
    if key not in _PROGS:
        _PROGS[key] = fn()
    return _PROGS[key]


def _run(nc, maps):
    res = run_bass_kernel_spmd(nc, maps, core_ids=list(range(8)))
    return res.results


def build_B(L):
    nc = bass.Bass("TRN2", target_bir_lowering=False)
    c = Ctx(nc)
    build_ret(c=c, pfx="r_")
    build_hg(L, c=c, pfx="h_")
    build_s5(c=c, pfx="s_")
    c.finish()
    c.close()
    print("B instructions:", c.n_inst)
    return nc


def kernel(**inp):
    inp = {k: np.asarray(v) for k, v in inp.items()}
    f32 = lambda a: np.ascontiguousarray(a, dtype=np.float32)
    H = build_H0(inp['x'], inp['meta_tokens'])
    sw = swap_cols()
    for L in range(2):
        hs = shard_T(H)
        W = w_in_ext(inp['w_in'][L])
        g = gvec(inp['norm_mix_g'][L])
        rA = _run(_prog("A", build_A), [{"hT": hs[c], "g": g, "w": W} for c in range(8)])
        proj = unshard_T([r["projT"] for r in rA], NCOL_A)
        rq = proj[:, :, 1280:1536]; rk = proj[:, :, 1536:1792]; rv = proj[:, :, 1792:2304]; rg = proj[:, :, 2304:2816]
        rqs = proj[:, :, 2816:3072]; rks = proj[:, :, 3072:3328]
        cwL = inp['hg_conv_w'][L]
        maps = []
        for c in range(8):
            b, r = divmod(c, 4)
            hds = [2 * r, 2 * r + 1]
            m = {"r_q": f32(rq[b, :, 64 * r:64 * r + 64].T), "r_qsw": f32(rqs[b, :, 64 * r:64 * r + 64].T),
                 "r_k": f32(rk[b, :, 64 * r:64 * r + 64].T), "r_ksw": f32(rks[b, :, 64 * r:64 * r + 64].T),
                 "r_v": f32(rv[b, :, 128 * r:128 * r + 128]), "r_gate": f32(rg[b, :, 128 * r:128 * r + 128]),
                 "r_outg": f32(np.tile(inp['ret_out_g'][L][128 * r:128 * r + 128][None, :], (128, 1))),
                 "r_hpart": f32(np.repeat(np.array(hds, np.float32), 32)[:, None]),
                 "r_hsel": f32(np.tile(np.array(hds, np.float32)[None, :], (128, 1)))}
            x3 = np.stack([proj[b, :, 256 + 256 * s + 64 * r: 256 + 256 * s + 64 * r + 64].T for s in range(3)])
            convw = np.stack([cwL[:, 256 * s + 64 * r: 256 * s + 64 * r + 64].T for s in range(3)])
            m.update({"h_x3": f32(x3), "h_convw": f32(convw), "h_gate": f32(proj[b, :, 1024 + 64 * r:1024 + 64 * r + 64]),
                      "h_outg": f32(np.tile(inp['hg_out_g'][L][64 * r:64 * r + 64][None, :], (128, 1))),
                      "h_lbp": f32(inp['hg_lb_param'][:, 64 * r:64 * r + 64].T)})
            for kk, vv in s5_inputs(inp, L, proj[b, :, 0:256], r).items():
                m["s_" + kk] = vv
            maps.append(m)
        rB = _run(_prog(("B", L), lambda: build_B(L)), maps)
        yc = np.zeros((2, 8320, 512), np.float32)
        yb = np.zeros((2, 8320, 256), np.float32)
        ya = np.zeros((2, 8320, 256), np.float32)
        for c in range(8):
            b, r = divmod(c, 4)
            yc[b, :, 128 * r:128 * r + 128] = rB[c]["r_y"]
            yb[b, :, 64 * r:64 * r + 64] = rB[c]["h_y"]
            s5_unpack(rB[c]["s_y"], ya[b], r)
        yas = shard_T(ya); ybcs = shard_T(np.concatenate([yb, yc], -1))
        moe = (L % 2 == 1)
        final = (L == 1)
        if not moe:
            w1 = f32(inp['ffn_w1'][L // 2][None]); w3 = f32(inp['ffn_w3'][L // 2][None]); w2 = f32(inp['ffn_w2'][L // 2][None])
            nc = _prog(("C", 1, final), lambda: build_C(1, 2816, final))
        else:
            w1 = f32(inp['moe_w1'][L // 2]); w3 = f32(inp['moe_w3'][L // 2]); w2 = f32(inp['moe_w2'][L // 2])
            nc = _prog(("C", 8, final), lambda: build_C(8, 3584, final))
        maps = []
        for c in range(8):
            m = {"hT": hs[c], "yaT": yas[c], "ybcT": ybcs[c], "wglu": f32(inp['s5_w_glu'][L]), "s5g": gvec(inp['s5_out_g'][L]),
                 "wout": f32(inp['w_out'][L]), "gffn": gvec(inp['norm_ffn_g'][L]), "w1": w1, "w3": w3, "w2": w2}
            if moe:
                m["router"] = f32(inp['moe_router'][L // 2])
            if final:
                m["gfin"] = gvec(inp['final_norm_g'])
            maps.append(m)
        rC = _run(nc, maps)
        H = unshard_T([r["hT_out"] for r in rC], 1024)
    return np.ascontiguousarray(H[:, 128:, :], dtype=np.float32)
```

```python
import math
import contextlib
import numpy as np
import concourse.bass as bass
import concourse.mybir as mybir
from concourse.bass_utils import run_bass_kernel_spmd


F32 = mybir.dt.float32
BF16 = mybir.dt.bfloat16
I32 = mybir.dt.int32
AF = mybir.ActivationFunctionType
ALU = mybir.AluOpType
AX = mybir.AxisListType


class T:
    def __init__(self, ctx, name, handle, space):
        self.ctx = ctx
        self.name = name
        self.h = handle
        self.space = space
        self.st = {}
        self.dq = None

    def __getitem__(self, idx):
        return self.h[idx]

    def state(self, key):
        s = self.st.get(key)
        if s is None:
            s = {"w": None, "r": {}}
            self.st[key] = s
        return s


class Q:
    def __init__(self, ctx, name, step):
        self.ctx = ctx
        self.name = name
        self.sem = ctx.root.enter_context(ctx.nc.semaphore(name))
        self.count = 0
        self.step = step


class Ctx:
    def __init__(self, nc):
        self.nc = nc
        self.stack = contextlib.ExitStack()
        self.root = self.stack
        self.engs = {}
        for name, eng in (("pe", nc.tensor), ("dve", nc.vector), ("act", nc.scalar),
                          ("pool", nc.gpsimd), ("sp", nc.sync)):
            q = Q(self, "q_" + name, 1)
            self.engs[name] = (eng, q)
        self.seen = {name: {} for name in self.engs}
        self.dmaq = {}
        self.n_inst = 0
        self.uid = 0
        self.pe_self_sync = False

    def sb(self, name, shape, dtype):
        self.uid += 1
        h = self.stack.enter_context(self.nc.sbuf_tensor(f"{name}_{self.uid}", list(shape), dtype))
        return T(self, name, h, "sb")

    def ps(self, name, shape, dtype=F32):
        self.uid += 1
        h = self.stack.enter_context(self.nc.psum_tensor(f"{name}_{self.uid}", list(shape), dtype))
        return T(self, name, h, "ps")

    def dram(self, name, shape, dtype, kind):
        h = self.nc.dram_tensor(name, list(shape), dtype, kind=kind)
        return T(self, name, h, "dram")

    def dma_q(self, name):
        q = self.dmaq.get(name)
        if q is None:
            q = Q(self, "dq_" + name, 16)
            self.dmaq[name] = q
        return q

    def _need(self, engname, q, value):
        if q is None:
            return
        if engname == "pe" and q is self.engs["pe"][1] and not self.pe_self_sync:
            return
        seen = self.seen[engname]
        if q.step == 16:
            value = q.count
        if seen.get(q.name, 0) >= value:
            return
        eng = self.engs[engname][0]
        eng.wait_ge(q.sem, value)
        seen[q.name] = value

    def _deps(self, engname, reads, writes):
        for (t, key) in reads:
            s = t.state(key)
            if s["w"] is not None:
                self._need(engname, *s["w"])
        for (t, key) in writes:
            s = t.state(key)
            if s["w"] is not None:
                self._need(engname, *s["w"])
            for q, v in s["r"].values():
                self._need(engname, q, v)

    def _mark(self, q, value, reads, writes):
        for (t, key) in reads:
            s = t.state(key)
            s["r"][q.name] = (q, value)
        for (t, key) in writes:
            s = t.state(key)
            s["w"] = (q, value)
            s["r"] = {}

    @staticmethod
    def _norm(lst):
        out = []
        for x in lst:
            if isinstance(x, tuple):
                out.append(x)
            else:
                out.append((x, None))
        return out

    def op(self, engname, fn, reads=(), writes=()):
        reads = self._norm(reads)
        writes = self._norm(writes)
        eng, q = self.engs[engname]
        self._deps(engname, reads, writes)
        ins = fn(eng)
        q.count += 1
        ins.then_inc(q.sem, 1)
        self._mark(q, q.count, reads, writes)
        self.n_inst += 1
        return ins

    def dma(self, engname, out, in_, reads=(), writes=(), qt=None, **kw):
        reads = self._norm(reads)
        writes = self._norm(writes)
        eng, _ = self.engs[engname]
        self._deps(engname, reads, writes)
        if qt is None:
            cands = [t for (t, k) in writes if t.space == "sb"] + [t for (t, k) in reads if t.space == "sb"]
            qt = cands[0]
        if qt.dq is None:
            self.uid += 1
            qt.dq = Q(self, f"dq_{qt.name}_{self.uid}", 16)
            self.dmaq[qt.dq.name] = qt.dq
        dq = qt.dq
        ins = eng.dma_start(out=out, in_=in_, **kw)
        dq.count += 16
        ins.then_inc(dq.sem, 16)
        self._mark(dq, dq.count, reads, writes)
        self.n_inst += 1
        return ins

    def barrier(self):
        for name, (eng, q0) in self.engs.items():
            for dq in self.dmaq.values():
                if dq.count:
                    self._need(name, dq, dq.count)
            for other, (e2, q2) in self.engs.items():
                if other != name and q2.count:
                    self._need(name, q2, q2.count)

    @contextlib.contextmanager
    def scope(self):
        old = self.stack
        self.stack = contextlib.ExitStack()
        try:
            yield
        finally:
            self.barrier()
            self.stack.close()
            self.stack = old

    def finish(self, engname="sp"):
        eng = self.engs[engname][0]
        for dq in self.dmaq.values():
            if dq.count:
                eng.wait_ge(dq.sem, dq.count)
        for name, (e, q) in self.engs.items():
            if q.count and name != engname:
                eng.wait_ge(q.sem, q.count)

    def close(self):
        self.stack.close()


NT = 2080
D = 1024
KC = 8
EPS = 1e-6
NCOL_A = 3328


def tblocks(nt=NT, bs=512):
    if nt == 2080 and bs == 512:
        return [(416 * i, 416) for i in range(5)]
    out = []
    t = 0
    while t < nt:
        n = min(bs, nt - t)
        out.append((t, n))
        t += n
    return out


class Rot:
    def __init__(self, tiles):
        self.tiles = tiles
        self.i = 0

    def next(self):
        t = self.tiles[self.i % len(self.tiles)]
        self.i += 1
        return t


def emit_consts(c):
    k = {}
    k["ones_bf"] = c.sb("ones_bf", [128, 128], BF16)
    c.op("pool", lambda e: e.memset(k["ones_bf"][:], 1.0), writes=[k["ones_bf"]])
    return k


def emit_rmsnorm(c, k, hT, g_sb, hnT, sq_rot, rs_rot, ps_rot, d_chunks=KC, dim=D, src_key=True):
    for bi, (t0, n) in enumerate(tblocks()):
        sq = sq_rot.next()
        c.op("act", lambda e: e.activation(out=sq[:, :d_chunks, :n], in_=hT[:, :, t0:t0 + n], func=AF.Square),
             reads=[(hT, bi)], writes=[sq])
        ps = ps_rot.next()
        for kc in range(d_chunks):
            c.op("pe", lambda e: e.matmul(ps[:, :n], lhsT=k["ones_bf"][:], rhs=sq[:, kc, :n],
                                          start=(kc == 0), stop=(kc == d_chunks - 1)),
                 reads=[sq, k["ones_bf"]], writes=[ps])
        rs = rs_rot.next()
        c.op("act", lambda e: e.activation(out=rs[:, :n], in_=ps[:, :n], func=AF.Sqrt, scale=1.0 / dim, bias=k["eps"][:, 0:1]),
             reads=[ps, k["eps"]], writes=[rs])
        c.op("dve", lambda e: e.reciprocal(rs[:, :n], rs[:, :n]), reads=[rs], writes=[rs])
        for kc in range(d_chunks):
            c.op("dve", lambda e: e.scalar_tensor_tensor(out=hnT[:, kc, t0:t0 + n], in0=hT[:, kc, t0:t0 + n],
                                                         scalar=g_sb[:, kc:kc + 1], in1=rs[:, :n],
                                                         op0=ALU.mult, op1=ALU.mult),
                 reads=[(hT, bi), rs, g_sb], writes=[(hnT, bi)])


def build_A():
    nc = bass.Bass("TRN2", target_bir_lowering=False)
    c = Ctx(nc)
    hT_d = c.dram("hT", [D, NT], F32, "ExternalInput")
    g_d = c.dram("g", [128, KC], F32, "ExternalInput")
    w_d = c.dram("w", [D, NCOL_A], F32, "ExternalInput")
    out_d = c.dram("projT", [NCOL_A, NT], F32, "ExternalOutput")

    k = emit_consts(c)
    k["eps"] = c.sb("eps", [128, 1], F32)
    c.op("pool", lambda e: e.memset(k["eps"][:], EPS), writes=[k["eps"]])
    hT = c.sb("hT", [128, KC, NT], F32)
    hnT = c.sb("hnT", [128, KC, NT], BF16)
    g_sb = c.sb("g", [128, KC], F32)
    c.dma("sp", g_sb[:], g_d[:], writes=[g_sb])
    hT_v = hT_d.h.ap().rearrange("(kc kp) t -> kp kc t", kp=128)
    for bi, (t0, n) in enumerate(tblocks()):
        c.dma("sp", hT[:, :, t0:t0 + n], hT_v[:, :, t0:t0 + n], writes=[(hT, bi)])
    sq_rot = Rot([c.sb(f"sq{i}", [128, KC, 512], BF16) for i in range(2)])
    rs_rot = Rot([c.sb(f"rs{i}", [128, 512], F32) for i in range(2)])
    ps_rot = Rot([c.ps(f"ps{i}", [128, 512], F32) for i in range(6)])
    emit_rmsnorm(c, k, hT, g_sb, hnT, sq_rot, rs_rot, ps_rot)

    w_rot = Rot([c.sb(f"w{i}", [128, KC, 512], BF16) for i in range(2)])
    w_st = c.sb("w_st", [128, KC, 512], F32)
    ost_rot = Rot([c.sb(f"ost{i}", [128, NT], F32) for i in range(3)])
    w_v = w_d.h.ap().rearrange("(kc kp) n -> kp kc n", kp=128)
    ev = 0
    for c0 in range(0, NCOL_A, 512):
        ncol = min(512, NCOL_A - c0)
        w_sb = w_rot.next()
        c.dma("sp", w_st[:, :, :ncol], w_v[:, :, c0:c0 + ncol], writes=[w_st])
        c.op("pool", lambda e: e.tensor_copy(w_sb[:, :, :ncol], w_st[:, :, :ncol]), reads=[w_st], writes=[w_sb])
        for cc in range(ncol // 128):
            ost = ost_rot.next()
            for bi, (t0, n) in enumerate(tblocks()):
                ps = ps_rot.next()
                for kc in range(KC):
                    c.op("pe", lambda e: e.matmul(ps[:, :n], lhsT=w_sb[:, kc, cc * 128:(cc + 1) * 128],
                                                  rhs=hnT[:, kc, t0:t0 + n], start=(kc == 0), stop=(kc == KC - 1)),
                         reads=[w_sb, (hnT, bi)], writes=[ps])
                if ev % 2 == 0:
                    c.op("act", lambda e: e.copy(out=ost[:, t0:t0 + n], in_=ps[:, :n]), reads=[ps], writes=[ost])
                else:
                    c.op("dve", lambda e: e.tensor_copy(ost[:, t0:t0 + n], ps[:, :n]), reads=[ps], writes=[ost])
                ev += 1
            r0 = c0 + cc * 128
            c.dma("sp", out_d[r0:r0 + 128, :], ost[:], reads=[ost])
    c.finish()
    c.close()
    print("phase A instructions:", c.n_inst)
    return nc


GF = 2
GH = 2


def emit_rmsnorm2(c, k, src, g_sb, dst_fn, sq_rot, rs_rot, ps_rot, d_chunks, dim, src_keyed=True):
    for bi, (t0, n) in enumerate(tblocks()):
        sk = (src, bi) if src_keyed else src
        sq = sq_rot.next()
        c.op("act", lambda e: e.activation(out=sq[:, :d_chunks, :n], in_=src[:, :, t0:t0 + n], func=AF.Square),
             reads=[sk], writes=[sq])
        ps = ps_rot.next()
        for kc in range(d_chunks):
            c.op("pe", lambda e: e.matmul(ps[:, :n], lhsT=k["ones_bf"][:], rhs=sq[:, kc, :n],
                                          start=(kc == 0), stop=(kc == d_chunks - 1)),
                 reads=[sq, k["ones_bf"]], writes=[ps])
        rs = rs_rot.next()
        c.op("act", lambda e: e.activation(out=rs[:, :n], in_=ps[:, :n], func=AF.Sqrt, scale=1.0 / dim, bias=k["eps"][:, 0:1]),
             reads=[ps, k["eps"]], writes=[rs])
        c.op("dve", lambda e: e.reciprocal(rs[:, :n], rs[:, :n]), reads=[rs], writes=[rs])
        for kc in range(d_chunks):
            ap, wk = dst_fn(bi, t0, n, kc)
            c.op("dve", lambda e: e.scalar_tensor_tensor(out=ap, in0=src[:, kc, t0:t0 + n],
                                                         scalar=g_sb[:, kc:kc + 1], in1=rs[:, :n],
                                                         op0=ALU.mult, op1=ALU.mult),
                 reads=[sk, rs, g_sb], writes=[wk])


def build_C(n_exp, F, final, with_A=False):
    moe = n_exp > 1
    nc = bass.Bass("TRN2", target_bir_lowering=False)
    c = Ctx(nc)
    hT_d = c.dram("hT", [D, NT], F32, "ExternalInput")
    ya_d = c.dram("yaT", [256, NT], F32, "ExternalInput")
    ybc_d = c.dram("ybcT", [768, NT], F32, "ExternalInput")
    wglu_d = c.dram("wglu", [256, 256], F32, "ExternalInput")
    s5g_d = c.dram("s5g", [128, 2], F32, "ExternalInput")
    wout_d = c.dram("wout", [D, D], F32, "ExternalInput")
    gffn_d = c.dram("gffn", [128, KC], F32, "ExternalInput")
    w1_d = c.dram("w1", [n_exp, D, F], F32, "ExternalInput")
    w3_d = c.dram("w3", [n_exp, D, F], F32, "ExternalInput")
    w2_d = c.dram("w2", [n_exp, F, D], F32, "ExternalInput")
    if moe:
        rt_d = c.dram("router", [D, 8], F32, "ExternalInput")
    if final:
        gfin_d = c.dram("gfin", [128, KC], F32, "ExternalInput")
    out_d = c.dram("hT_out", [D, NT], F32, "ExternalOutput")

    k = emit_consts(c)
    k["eps"] = c.sb("eps", [128, 1], F32)
    c.op("pool", lambda e: e.memset(k["eps"][:], EPS), writes=[k["eps"]])
    TB = tblocks()

    hT = c.sb("hT", [128, KC, NT], F32)
    hT_v = hT_d.h.ap().rearrange("(kc kp) t -> kp kc t", kp=128)
    for bi, (t0, n) in enumerate(TB):
        c.dma("sp", hT[:, :, t0:t0 + n], hT_v[:, :, t0:t0 + n], writes=[(hT, bi)], qt=hT)
    gffn = c.sb("gffn", [128, KC], F32)
    c.dma("sp", gffn[:], gffn_d[:], writes=[gffn])
    s5g = c.sb("s5g", [128, 2], F32)
    c.dma("sp", s5g[:], s5g_d[:], writes=[s5g])
    if final:
        gfin = c.sb("gfin", [128, KC], F32)
        c.dma("sp", gfin[:], gfin_d[:], writes=[gfin])

    ps_all = [c.ps(f"ps{i}", [128, 512], F32) for i in range(8)]
    ps_rot = Rot(ps_all[:7])

    with c.scope():
        sq_rot = Rot([c.sb(f"sq{i}", [128, KC, 512], BF16) for i in range(2)])
        rs_rot = Rot([c.sb(f"rs{i}", [128, 512], F32) for i in range(2)])
        yaT = c.sb("yaT", [128, 2, NT], F32)
        c.dma("sp", yaT[:], ya_d.h.ap().rearrange("(kc kp) t -> kp kc t", kp=128), writes=[yaT])
        mixedT = c.sb("mixedT", [128, KC, NT], BF16)
        ybc_v = ybc_d.h.ap().rearrange("(kc kp) t -> kp kc t", kp=128)
        for bi, (t0, n) in enumerate(TB):
            c.dma("pool", mixedT[:, 2:8, t0:t0 + n], ybc_v[:, :, t0:t0 + n], writes=[(mixedT, ("bc", bi))], qt=mixedT)
        wglu = c.sb("wglu", [128, 2, 256], BF16)
        c.dma("pool", wglu[:], wglu_d.h.ap().rearrange("(kc kp) n -> kp kc n", kp=128), writes=[wglu])
        wout = c.sb("wout", [128, KC, D], BF16)
        c.dma("pool", wout[:], wout_d.h.ap().rearrange("(kc kp) n -> kp kc n", kp=128), writes=[wout])

        t1_rot = Rot([c.sb(f"t1_{i}", [128, 2, 512], F32) for i in range(1)])
        t2_rot = Rot([c.sb(f"t2_{i}", [128, 2, 512], F32) for i in range(1)])
        ygf_rot = Rot([c.sb(f"ygf{i}", [128, 2, 512], F32) for i in range(1)])
        ygb_rot = Rot([c.sb(f"ygb{i}", [128, 2, 512], BF16) for i in range(2)])
        yaf_rot = Rot([c.sb(f"yaf{i}", [128, 2, 512], F32) for i in range(1)])
        sg_rot = Rot([c.sb(f"sg{i}", [128, 512], F32) for i in range(2)])
        for bi, (t0, n) in enumerate(TB):
            x = yaT[:, :, t0:t0 + n]
            t1 = t1_rot.next(); t2 = t2_rot.next(); ygf = ygf_rot.next(); ygb = ygb_rot.next(); yaf = yaf_rot.next()
            c.op("act", lambda e: e.activation(out=t1[:, :, :n], in_=x, func=AF.Square), reads=[yaT], writes=[t1])
            c.op("dve", lambda e: e.tensor_scalar(t1[:, :, :n], t1[:, :, :n], 0.044715, 1.0, op0=ALU.mult, op1=ALU.add),
                 reads=[t1], writes=[t1])
            c.op("dve", lambda e: e.tensor_tensor(out=t1[:, :, :n], in0=t1[:, :, :n], in1=x, op=ALU.mult),
                 reads=[t1, yaT], writes=[t1])
            c.op("act", lambda e: e.activation(out=t2[:, :, :n], in_=t1[:, :, :n], func=AF.Sigmoid, scale=1.5957691216057308),
                 reads=[t1], writes=[t2])
            c.op("dve", lambda e: e.tensor_tensor(out=ygf[:, :, :n], in0=t2[:, :, :n], in1=x, op=ALU.mult),
                 reads=[t2, yaT], writes=[ygf])
            c.op("act", lambda e: e.copy(out=ygb[:, :, :n], in_=ygf[:, :, :n]), reads=[ygf], writes=[ygb])
            for mo in range(2):
                ps = ps_rot.next()
                for ch in range(2):
                    c.op("pe", lambda e: e.matmul(ps[:, :n], lhsT=wglu[:, ch, mo * 128:(mo + 1) * 128], rhs=ygb[:, ch, :n],
                                                  start=(ch == 0), stop=(ch == 1)), reads=[wglu, ygb], writes=[ps])
                sg = sg_rot.next()
                c.op("act", lambda e: e.activation(out=sg[:, :n], in_=ps[:, :n], func=AF.Sigmoid), reads=[ps], writes=[sg])
                c.op("dve", lambda e: e.tensor_tensor(out=yaf[:, mo, :n], in0=ygf[:, mo, :n], in1=sg[:, :n], op=ALU.mult),
                     reads=[ygf, sg], writes=[yaf])
            sq = sq_rot.next()
            c.op("act", lambda e: e.activation(out=sq[:, :2, :n], in_=yaf[:, :, :n], func=AF.Square), reads=[yaf], writes=[sq])
            ps = ps_rot.next()
            for ch in range(2):
                c.op("pe", lambda e: e.matmul(ps[:, :n], lhsT=k["ones_bf"][:], rhs=sq[:, ch, :n], start=(ch == 0), stop=(ch == 1)),
                     reads=[sq, k["ones_bf"]], writes=[ps])
            rs = rs_rot.next()
            c.op("act", lambda e: e.activation(out=rs[:, :n], in_=ps[:, :n], func=AF.Sqrt, scale=1.0 / 256, bias=k["eps"][:, 0:1]),
                 reads=[ps, k["eps"]], writes=[rs])
            c.op("dve", lambda e: e.reciprocal(rs[:, :n], rs[:, :n]), reads=[rs], writes=[rs])
            for ch in range(2):
                c.op("dve", lambda e: e.scalar_tensor_tensor(out=mixedT[:, ch, t0:t0 + n], in0=yaf[:, ch, :n],
                                                             scalar=s5g[:, ch:ch + 1], in1=rs[:, :n], op0=ALU.mult, op1=ALU.mult),
                     reads=[yaf, rs, s5g], writes=[(mixedT, ("a", bi))])
            for dch in range(KC):
                ps = ps_rot.next()
                for cc in range(KC):
                    c.op("pe", lambda e: e.matmul(ps[:, :n], lhsT=wout[:, cc, dch * 128:(dch + 1) * 128], rhs=mixedT[:, cc, t0:t0 + n],
                                                  start=(cc == 0), stop=(cc == KC - 1)),
                         reads=[wout, (mixedT, ("a", bi)), (mixedT, ("bc", bi))], writes=[ps])
                c.op("dve", lambda e: e.tensor_tensor(out=hT[:, dch, t0:t0 + n], in0=hT[:, dch, t0:t0 + n], in1=ps[:, :n], op=ALU.add),
                     reads=[(hT, bi), ps], writes=[(hT, bi)])

    hnT = c.sb("hnT", [128, KC, NT], BF16)
    with c.scope():
        sq_rot = Rot([c.sb(f"sq{i}", [128, KC, 512], BF16) for i in range(2)])
        rs_rot = Rot([c.sb(f"rs{i}", [128, 512], F32) for i in range(2)])
        emit_rmsnorm2(c, k, hT, gffn, lambda bi, t0, n, kc: (hnT[:, kc, t0:t0 + n], (hnT, bi)), sq_rot, rs_rot, ps_rot, KC, D)

    if moe:
        gatesT = c.sb("gatesT", [8, NT], BF16)
        with c.scope():
            ident = c.sb("identf", [128, 128], F32)
            iof = c.sb("iof", [128, 128], F32)
            c.op("pool", lambda e: e.iota(iof[:], [[1, 128]], base=0, channel_multiplier=-1, allow_small_or_imprecise_dtypes=True), writes=[iof])
            c.op("dve", lambda e: e.tensor_single_scalar(ident[:], iof[:], 0.0, op=ALU.is_equal), reads=[iof], writes=[ident])
            rt = c.sb("rt", [128, KC, 8], F32)
            c.dma("sp", rt[:], rt_d.h.ap().rearrange("(kc kp) e -> kp kc e", kp=128), writes=[rt])
            gr = c.sb("gr", [128, KC, 16], F32)
            c.op("dve", lambda e: e.memset(gr[:], 0.0), writes=[gr])
            for kc in range(KC):
                c.op("dve", lambda e: e.tensor_scalar(gr[:, kc, 0:8], rt[:, kc, :], gffn[:, kc:kc + 1], None, op0=ALU.mult),
                     reads=[rt, gffn], writes=[gr])
            onesf = c.sb("onesf", [128, 1], F32)
            c.op("dve", lambda e: e.memset(onesf[:], 1.0), writes=[onesf])
            k["gatesT"] = gatesT
            sqf_rot = Rot([c.sb(f"sqf{i}", [128, KC, 128], F32) for i in range(2)])
            sm_rot = Rot([c.sb(f"sm{i}", [128, 64], F32) for i in range(3)])
            for ti, (t0, n) in enumerate(tblocks(NT, 128)):
                bi = t0 // 512
                sqf = sqf_rot.next()
                c.op("act", lambda e: e.activation(out=sqf[:, :, :n], in_=hT[:, :, t0:t0 + n], func=AF.Square), reads=[(hT, b_) for b_ in range(5)], writes=[sqf])
                ps = ps_all[7]
                for kc in range(KC):
                    c.op("pe", lambda e: e.matmul(ps[:n, 0:8], lhsT=hT[:, kc, t0:t0 + n], rhs=gr[:, kc, 0:8], start=(kc == 0), stop=(kc == KC - 1)),
                         reads=[(hT, b_) for b_ in range(5)] + [gr], writes=[ps])
                for kc in range(KC):
                    c.op("pe", lambda e: e.matmul(ps[:n, 8:9], lhsT=sqf[:, kc, :n], rhs=onesf[:, 0:1], start=(kc == 0), stop=(kc == KC - 1)),
                         reads=[sqf, onesf], writes=[ps])
                sm = sm_rot.next()
                c.op("act", lambda e: e.activation(out=sm[:n, 0:1], in_=ps[:n, 8:9], func=AF.Sqrt, scale=1.0 / D, bias=k["eps"][:n, 0:1]),
                     reads=[ps, k["eps"]], writes=[sm])
                c.op("dve", lambda e: e.reciprocal(sm[:n, 0:1], sm[:n, 0:1]), reads=[sm], writes=[sm])
                c.op("dve", lambda e: e.tensor_scalar(sm[:n, 8:16], ps[:n, 0:8], sm[:n, 0:1], None, op0=ALU.mult), reads=[ps, sm], writes=[sm])
                c.op("dve", lambda e: e.max(out=sm[:n, 16:24], in_=sm[:n, 8:16]), reads=[sm], writes=[sm])
                c.op("dve", lambda e: e.tensor_tensor(out=sm[:n, 24:25], in0=sm[:n, 17:18], in1=sm[:n, 16:17], op=ALU.subtract), reads=[sm], writes=[sm])
                c.op("act", lambda e: e.activation(out=sm[:n, 24:25], in_=sm[:n, 24:25], func=AF.Exp), reads=[sm], writes=[sm])
                c.op("dve", lambda e: e.tensor_scalar(sm[:n, 25:26], sm[:n, 24:25], 1.0, None, op0=ALU.add), reads=[sm], writes=[sm])
                c.op("dve", lambda e: e.reciprocal(sm[:n, 25:26], sm[:n, 25:26]), reads=[sm], writes=[sm])
                c.op("dve", lambda e: e.tensor_tensor(out=sm[:n, 26:27], in0=sm[:n, 24:25], in1=sm[:n, 25:26], op=ALU.mult), reads=[sm], writes=[sm])
                c.op("dve", lambda e: e.tensor_scalar(sm[:n, 32:40], sm[:n, 8:16], sm[:n, 16:17], sm[:n, 25:26], op0=ALU.is_equal, op1=ALU.mult), reads=[sm], writes=[sm])
                c.op("dve", lambda e: e.tensor_scalar(sm[:n, 40:48], sm[:n, 8:16], sm[:n, 17:18], sm[:n, 26:27], op0=ALU.is_equal, op1=ALU.mult), reads=[sm], writes=[sm])
                c.op("dve", lambda e: e.tensor_tensor(out=sm[:n, 48:56], in0=sm[:n, 32:40], in1=sm[:n, 40:48], op=ALU.add), reads=[sm], writes=[sm])
                c.op("pe", lambda e: e.transpose(ps[0:8, 16:16 + n], sm[:n, 48:56], ident[:n, :n]), reads=[sm, ident], writes=[ps])
                c.op("act", lambda e: e.copy(out=gatesT[:, t0:t0 + n], in_=ps[0:8, 16:16 + n]), reads=[ps], writes=[gatesT])
        sel = c.sb("sel", [8, 8, 128], BF16)
        self_f = c.sb("sel_f", [8, 8, 128], F32)
        c.op("pool", lambda e: e.iota(self_f[:], [[-1, 8], [0, 128]], base=0, channel_multiplier=1, allow_small_or_imprecise_dtypes=True), writes=[self_f])
        c.op("dve", lambda e: e.tensor_single_scalar(sel[:], self_f[:], 0.0, op=ALU.is_equal), reads=[self_f], writes=[sel])

    with c.scope():
        ps_a = Rot(ps_all[0:2]); ps_b = Rot(ps_all[2:4]); ps_o = Rot(ps_all[4:7]); ps_g = Rot(ps_all[7:8])
        NWB = 3
        w1_rot = Rot([c.sb(f"w1g{i}", [128, KC, GF * 128], BF16) for i in range(NWB)])
        w3_rot = Rot([c.sb(f"w3g{i}", [128, KC, GF * 128], BF16) for i in range(NWB)])
        w2_rot = Rot([c.sb(f"w2g{i}", [128, GF, D], BF16) for i in range(NWB)])
        w1s = c.sb("w1s", [128, KC, GH * 128], F32)
        w3s = c.sb("w3s", [128, KC, GH * 128], F32)
        w2s = c.sb("w2s", [128, GF, D], F32)
        sa_rot = Rot([c.sb(f"sa{i}", [128, 512], F32) for i in range(2)])
        gT_rot = Rot([c.sb(f"gT{i}", [128, GF, 512], BF16) for i in range(3)])
        ngrp = F // (GF * 128)
        groups = [(ex, gi) for ex in range(n_exp) for gi in range(ngrp)]
        wbuf = {}

        def load_dma(gidx):
            ex, gi = groups[gidx]
            w1_v = w1_d.h.ap()[ex].rearrange("(kc kp) f -> kp kc f", kp=128)
            w3_v = w3_d.h.ap()[ex].rearrange("(kc kp) f -> kp kc f", kp=128)
            w2_v = w2_d.h.ap()[ex].rearrange("(fc fp) d -> fp fc d", fp=128)
            f0 = gi * GF * 128
            c.dma("sp", w1s[:], w1_v[:, :, f0:f0 + GF * 128], writes=[w1s])
            c.dma("sp", w3s[:], w3_v[:, :, f0:f0 + GF * 128], writes=[w3s])
            c.dma("sp", w2s[:], w2_v[:, gi * GF:(gi + 1) * GF, :], writes=[w2s])

        def load_cast(gidx):
            w1g = w1_rot.next(); w3g = w3_rot.next(); w2g = w2_rot.next()
            c.op("act", lambda e: e.copy(out=w1g[:], in_=w1s[:]), reads=[w1s], writes=[w1g])
            c.op("act", lambda e: e.copy(out=w3g[:], in_=w3s[:]), reads=[w3s], writes=[w3g])
            c.op("act", lambda e: e.copy(out=w2g[:], in_=w2s[:]), reads=[w2s], writes=[w2g])
            wbuf[gidx] = (w1g, w3g, w2g)

        def load_group(gidx):
            load_dma(gidx)
            load_cast(gidx)

        gsb_rot = Rot([c.sb(f"gsb{i}", [128, 512], BF16) for i in range(2)]) if moe else None
        sa2_rot = Rot([c.sb(f"sa2_{i}", [128, 512], F32) for i in range(2)]) if moe else None

        def stage1_fc(gidx, bi, t0, n, fc, gT, gsb):
            ex, gi = groups[gidx]
            w1g, w3g, w2g = wbuf[gidx]
            pa = ps_a.next(); pb = ps_b.next()
            for kc in range(KC):
                c.op("pe", lambda e: e.matmul(pa[:, :n], lhsT=w1g[:, kc, fc * 128:(fc + 1) * 128], rhs=hnT[:, kc, t0:t0 + n],
                                              start=(kc == 0), stop=(kc == KC - 1)), reads=[w1g, (hnT, bi)], writes=[pa])
            for kc in range(KC):
                c.op("pe", lambda e: e.matmul(pb[:, :n], lhsT=w3g[:, kc, fc * 128:(fc + 1) * 128], rhs=hnT[:, kc, t0:t0 + n],
                                              start=(kc == 0), stop=(kc == KC - 1)), reads=[w3g, (hnT, bi)], writes=[pb])
            sa = sa_rot.next()
            c.op("act", lambda e: e.activation(out=sa[:, :n], in_=pa[:, :n], func=AF.Silu), reads=[pa], writes=[sa])
            if moe:
                sa2 = sa2_rot.next()
                c.op("pool", lambda e: e.tensor_tensor(out=sa2[:, :n], in0=sa[:, :n], in1=gsb[:, :n], op=ALU.mult), reads=[sa, gsb], writes=[sa2])
                return (sa2, pb)
            return (sa, pb)

        def stage1_fc_b(n, fc, gT, st):
            sx, pb = st
            c.op("dve", lambda e: e.tensor_tensor(out=gT[:, fc, :n], in0=sx[:, :n], in1=pb[:, :n], op=ALU.mult), reads=[sx, pb], writes=[gT])

        def stage1_gate(gidx, bi, t0, n):
            ex, gi = groups[gidx]
            pg = ps_g.next()
            c.op("pe", lambda e: e.matmul(pg[:, :n], lhsT=sel[:, ex, :], rhs=k["gatesT"][:, t0:t0 + n], start=True, stop=True),
                 reads=[sel, k["gatesT"]], writes=[pg])
            gsb = gsb_rot.next()
            c.op("act", lambda e: e.copy(out=gsb[:, :n], in_=pg[:, :n]), reads=[pg], writes=[gsb])
            return gsb

        def stage2_part(gidx, bi, t0, n, gT, d0, d1):
            w1g, w3g, w2g = wbuf[gidx]
            for dch in range(d0, d1):
                po = ps_o.next()
                for fc in range(GF):
                    c.op("pe", lambda e: e.matmul(po[:, :n], lhsT=w2g[:, fc, dch * 128:(dch + 1) * 128], rhs=gT[:, fc, :n],
                                                  start=(fc == 0), stop=(fc == GF - 1)), reads=[w2g, gT], writes=[po])
                c.op("dve", lambda e: e.tensor_tensor(out=hT[:, dch, t0:t0 + n], in0=hT[:, dch, t0:t0 + n], in1=po[:, :n], op=ALU.add),
                     reads=[(hT, bi), po], writes=[(hT, bi)])

        load_group(0)
        if len(groups) > 1:
            load_group(1)
        pending = None
        DS = KC // GF
        for gidx in range(len(groups)):
            for bi, (t0, n) in enumerate(TB):
                gsb = stage1_gate(gidx, bi, t0, n) if moe else None
                gT = gT_rot.next()
                for fc in range(GF):
                    st = stage1_fc(gidx, bi, t0, n, fc, gT, gsb)
                    if pending is not None:
                        stage2_part(*pending, fc * DS, (fc + 1) * DS)
                    stage1_fc_b(n, fc, gT, st)
                pending = (gidx, bi, t0, n, gT)
                if bi == 0 and gidx + 2 < len(groups):
                    load_dma(gidx + 2)
                if bi == 3 and gidx + 2 < len(groups):
                    load_cast(gidx + 2)
        stage2_part(*pending, 0, KC)

    out_v = out_d.h.ap().rearrange("(kc kp) t -> kp kc t", kp=128)
    if final:
        sq_rot = Rot([c.sb(f"sq{i}", [128, KC, 512], BF16) for i in range(2)])
        rs_rot = Rot([c.sb(f"rs{i}", [128, 512], F32) for i in range(2)])
        fo_rot = Rot([c.sb(f"fo{i}", [128, KC, 512], F32) for i in range(2)])
        cur = {}

        def dst(bi, t0, n, kc):
            if kc == 0:
                cur["t"] = fo_rot.next()
            return cur["t"][:, kc, :n], cur["t"]
        for bi, (t0, n) in enumerate(TB):
            pass
        emit_final(c, k, hT, gfin, fo_rot, out_v, sq_rot, rs_rot, Rot(ps_all[0:6]))
    else:
        for bi, (t0, n) in enumerate(TB):
            c.dma("sp", out_v[:, :, t0:t0 + n], hT[:, :, t0:t0 + n], reads=[(hT, bi)], qt=hT)
    if with_A:
        gA_d = c.dram("gA", [128, KC], F32, "ExternalInput")
        wA_d = c.dram("wA", [D, NCOL_A], F32, "ExternalInput")
        pA_d = c.dram("projT", [NCOL_A, NT], F32, "ExternalOutput")
        gA = c.sb("gA", [128, KC], F32)
        c.dma("sp", gA[:], gA_d[:], writes=[gA])
        psA = Rot(ps_all[0:6])
        with c.scope():
            sq_rot = Rot([c.sb(f"sq{i}", [128, KC, 512], BF16) for i in range(2)])
            rs_rot = Rot([c.sb(f"rs{i}", [128, 512], F32) for i in range(2)])
            emit_rmsnorm(c, k, hT, gA, hnT, sq_rot, rs_rot, psA)
        w_rot = Rot([c.sb(f"wA{i}", [128, KC, 512], BF16) for i in range(2)])
        w_st = c.sb("wA_st", [128, KC, 512], F32)
        ost_rot = Rot([c.sb(f"ostA{i}", [128, NT], F32) for i in range(3)])
        w_v = wA_d.h.ap().rearrange("(kc kp) n -> kp kc n", kp=128)
        ev = 0
        for c0 in range(0, NCOL_A, 512):
            ncol = min(512, NCOL_A - c0)
            w_sb = w_rot.next()
            c.dma("sp", w_st[:, :, :ncol], w_v[:, :, c0:c0 + ncol], writes=[w_st])
            c.op("pool", lambda e: e.tensor_copy(w_sb[:, :, :ncol], w_st[:, :, :ncol]), reads=[w_st], writes=[w_sb])
            for cc in range(ncol // 128):
                ost = ost_rot.next()
                for bi, (t0, n) in enumerate(TB):
                    ps = psA.next()
                    for kc in range(KC):
                        c.op("pe", lambda e: e.matmul(ps[:, :n], lhsT=w_sb[:, kc, cc * 128:(cc + 1) * 128],
                                                      rhs=hnT[:, kc, t0:t0 + n], start=(kc == 0), stop=(kc == KC - 1)),
                             reads=[w_sb, (hnT, bi)], writes=[ps])
                    if ev % 2 == 0:
                        c.op("act", lambda e: e.copy(out=ost[:, t0:t0 + n], in_=ps[:, :n]), reads=[ps], writes=[ost])
                    else:
                        c.op("dve", lambda e: e.tensor_copy(ost[:, t0:t0 + n], ps[:, :n]), reads=[ps], writes=[ost])
                    ev += 1
                r0 = c0 + cc * 128
                c.dma("sp", pA_d[r0:r0 + 128, :], ost[:], reads=[ost])
    c.finish()
    c.close()
    print("phase C instructions:", c.n_inst)
    return nc


def emit_final(c, k, hT, gfin, fo_rot, out_v, sq_rot, rs_rot, ps_rot):
    for bi, (t0, n) in enumerate(tblocks()):
        sq = sq_rot.next()
        c.op("act", lambda e: e.activation(out=sq[:, :, :n], in_=hT[:, :, t0:t0 + n], func=AF.Square), reads=[(hT, bi)], writes=[sq])
        ps = ps_rot.next()
        for kc in range(KC):
            c.op("pe", lambda e: e.matmul(ps[:, :n], lhsT=k["ones_bf"][:], rhs=sq[:, kc, :n], start=(kc == 0), stop=(kc == KC - 1)),
                 reads=[sq, k["ones_bf"]], writes=[ps])
        rs = rs_rot.next()
        c.op("act", lambda e: e.activation(out=rs[:, :n], in_=ps[:, :n], func=AF.Sqrt, scale=1.0 / D, bias=k["eps"][:, 0:1]),
             reads=[ps, k["eps"]], writes=[rs])
        c.op("dve", lambda e: e.reciprocal(rs[:, :n], rs[:, :n]), reads=[rs], writes=[rs])
        fo = fo_rot.next()
        for kc in range(KC):
            c.op("dve", lambda e: e.scalar_tensor_tensor(out=fo[:, kc, :n], in0=hT[:, kc, t0:t0 + n], scalar=gfin[:, kc:kc + 1], in1=rs[:, :n],
                                                         op0=ALU.mult, op1=ALU.mult), reads=[(hT, bi), rs, gfin], writes=[fo])
        c.dma("sp", out_v[:, :, t0:t0 + n], fo[:, :, :n], reads=[fo])


NTOK = 8320
NCH = 65
EPS = 1e-6
CB = 5


def emit_pow_table(c, P, n, bT, br, bi, save_at=None):
    Gr = c.sb("Gr", [P, n], F32)
    Gi = c.sb("Gi", [P, n], F32)
    tmp = c.sb("Gtmp", [P, max(n // 2, 1)], F32)
    s = c.sb("Gs", [P, 6], F32)
    saved = c.sb("Gsaved", [P, 2], F32) if save_at else None
    V = "dve"
    c.op(V, lambda e: e.memset(Gr[:, 0:1], 1.0), writes=[Gr])
    c.op(V, lambda e: e.memset(Gi[:, 0:1], 0.0), writes=[Gi])
    c.op(V, lambda e: e.tensor_copy(s[:, 0:1], br), reads=[bT], writes=[s])
    c.op(V, lambda e: e.tensor_copy(s[:, 1:2], bi), reads=[bT], writes=[s])
    m = 1
    while m < n:
        if save_at == m:
            c.op(V, lambda e: e.tensor_copy(saved[:, 0:2], s[:, 0:2]), reads=[s], writes=[saved])
        c.op(V, lambda e: e.tensor_scalar(tmp[:, :m], Gi[:, :m], s[:, 1:2], None, op0=ALU.mult), reads=[Gi, s], writes=[tmp])
        c.op(V, lambda e: e.scalar_tensor_tensor(out=Gr[:, m:2 * m], in0=Gr[:, :m], scalar=s[:, 0:1], in1=tmp[:, :m],
                                                 op0=ALU.mult, op1=ALU.subtract), reads=[Gr, s, tmp], writes=[Gr])
        c.op(V, lambda e: e.tensor_scalar(tmp[:, :m], Gi[:, :m], s[:, 0:1], None, op0=ALU.mult), reads=[Gi, s], writes=[tmp])
        c.op(V, lambda e: e.scalar_tensor_tensor(out=Gi[:, m:2 * m], in0=Gr[:, :m], scalar=s[:, 1:2], in1=tmp[:, :m],
                                                 op0=ALU.mult, op1=ALU.add), reads=[Gr, s, tmp], writes=[Gi])
        c.op(V, lambda e: e.tensor_tensor(out=s[:, 2:3], in0=s[:, 0:1], in1=s[:, 0:1], op=ALU.mult), reads=[s], writes=[s])
        c.op(V, lambda e: e.tensor_tensor(out=s[:, 3:4], in0=s[:, 1:2], in1=s[:, 1:2], op=ALU.mult), reads=[s], writes=[s])
        c.op(V, lambda e: e.scalar_tensor_tensor(out=s[:, 1:2], in0=s[:, 0:1], scalar=2.0, in1=s[:, 1:2],
                                                 op0=ALU.mult, op1=ALU.mult), reads=[s], writes=[s])
        c.op(V, lambda e: e.tensor_tensor(out=s[:, 0:1], in0=s[:, 2:3], in1=s[:, 3:4], op=ALU.subtract), reads=[s], writes=[s])
        m *= 2
    if save_at == m:
        c.op(V, lambda e: e.tensor_copy(saved[:, 0:2], s[:, 0:2]), reads=[s], writes=[saved])
    return Gr, Gi, saved


def emit_sincos_small(c, P, wT, w, cs):
    V = "dve"
    x2 = cs[:, 2:3]
    acc = cs[:, 3:4]
    c.op(V, lambda e: e.tensor_tensor(out=x2, in0=w, in1=w, op=ALU.mult), reads=[wT], writes=[cs])
    c.op(V, lambda e: e.tensor_scalar(acc, x2, -1.0 / 110, 1.0, op0=ALU.mult, op1=ALU.add), reads=[cs], writes=[cs])
    for d in (72.0, 42.0, 20.0, 6.0):
        c.op(V, lambda e: e.tensor_tensor(out=acc, in0=acc, in1=x2, op=ALU.mult), reads=[cs], writes=[cs])
        c.op(V, lambda e: e.tensor_scalar(acc, acc, -1.0 / d, 1.0, op0=ALU.mult, op1=ALU.add), reads=[cs], writes=[cs])
    c.op(V, lambda e: e.tensor_tensor(out=cs[:, 1:2], in0=acc, in1=w, op=ALU.mult), reads=[cs, wT], writes=[cs])
    c.op(V, lambda e: e.tensor_scalar(acc, x2, -1.0 / 132, 1.0, op0=ALU.mult, op1=ALU.add), reads=[cs], writes=[cs])
    for d in (90.0, 56.0, 30.0, 12.0, 2.0):
        c.op(V, lambda e: e.tensor_tensor(out=acc, in0=acc, in1=x2, op=ALU.mult), reads=[cs], writes=[cs])
        c.op(V, lambda e: e.tensor_scalar(acc, acc, -1.0 / d, 1.0, op0=ALU.mult, op1=ALU.add), reads=[cs], writes=[cs])
    c.op(V, lambda e: e.tensor_copy(cs[:, 0:1], acc), reads=[cs], writes=[cs])


def emit_gamma(c, hT_, hidx_ap, out, P, ncol):
    LN2 = math.log(2.0)
    c.op("act", lambda e: e.activation(out=out[:, :ncol], in_=hidx_ap, func=AF.Exp, scale=-LN2, bias=c.k5[:P, 0:1]),
         reads=[c.k5, hT_], writes=[out])
    c.op("dve", lambda e: e.tensor_scalar(out[:, :ncol], out[:, :ncol], -1.0, 1.0, op0=ALU.mult, op1=ALU.add), reads=[out], writes=[out])
    c.op("act", lambda e: e.activation(out=out[:, :ncol], in_=out[:, :ncol], func=AF.Ln), reads=[out], writes=[out])


def build_ret(debug=False, c=None, pfx=""):
    own = c is None
    if own:
        nc = bass.Bass("TRN2", target_bir_lowering=False)
        c = Ctx(nc)
    sc_ = None if own else c.scope()
    if sc_ is not None:
        sc_.__enter__()
    c.pe_self_sync = True
    q_d = c.dram(pfx + "q", [64, NTOK], F32, "ExternalInput")
    qs_d = c.dram(pfx + "qsw", [64, NTOK], F32, "ExternalInput")
    k_d = c.dram(pfx + "k", [64, NTOK], F32, "ExternalInput")
    ks_d = c.dram(pfx + "ksw", [64, NTOK], F32, "ExternalInput")
    v_d = c.dram(pfx + "v", [NTOK, 128], F32, "ExternalInput")
    g_d = c.dram(pfx + "gate", [NTOK, 128], F32, "ExternalInput")
    og_d = c.dram(pfx + "outg", [128, 128], F32, "ExternalInput")
    hp_d = c.dram(pfx + "hpart", [64, 1], F32, "ExternalInput")
    hs_d = c.dram(pfx + "hsel", [128, 2], F32, "ExternalInput")
    y_d = c.dram(pfx + "y", [NTOK, 128], F32, "ExternalOutput")

    V = "dve"
    c.k5 = c.sb("k5", [128, 1], F32)
    c.op("pool", lambda e: e.memset(c.k5[:], -5.0 * math.log(2.0)), writes=[c.k5])
    epsT = c.sb("eps", [128, 1], F32)
    c.op("pool", lambda e: e.memset(epsT[:], EPS), writes=[epsT])
    identb = c.sb("identb", [128, 128], BF16)
    iof = c.sb("iof", [128, 128], F32)
    c.op("pool", lambda e: e.iota(iof[:], [[1, 128]], base=0, channel_multiplier=-1, allow_small_or_imprecise_dtypes=True), writes=[iof])
    c.op(V, lambda e: e.tensor_single_scalar(identb[:], iof[:], 0.0, op=ALU.is_equal), reads=[iof], writes=[identb])
    hp = c.sb("hp", [64, 1], F32)
    c.dma("sp", hp[:], hp_d[:], writes=[hp])
    hs = c.sb("hs", [128, 2], F32)
    c.dma("sp", hs[:], hs_d[:], writes=[hs])
    og = c.sb("og", [128, 128], F32)
    c.dma("sp", og[:], og_d[:], writes=[og])
    lgP = c.sb("lgP", [64, 2], F32)
    emit_gamma(c, hp, hp[:, 0:1], lgP, 64, 1)
    lgB = c.sb("lgB", [128, 2], F32)
    emit_gamma(c, hs, hs[:, 0:2], lgB, 128, 2)
    maskT = c.sb("maskT", [128, 2, 128], F32)
    dpos = c.sb("dpos", [128, 128], F32)
    dge = c.sb("dge", [128, 128], F32)
    c.op(V, lambda e: e.tensor_single_scalar(dpos[:], iof[:], 0.0, op=ALU.max), reads=[iof], writes=[dpos])
    c.op(V, lambda e: e.tensor_scalar(dge[:], iof[:], 0.0, 32.0 ** -0.5, op0=ALU.is_ge, op1=ALU.mult), reads=[iof], writes=[dge])
    for hl in range(2):
        c.op("act", lambda e: e.activation(out=maskT[:, hl, :], in_=dpos[:], func=AF.Exp, scale=lgB[:, hl:hl + 1]),
             reads=[dpos, lgB], writes=[maskT])
        c.op(V, lambda e: e.tensor_tensor(out=maskT[:, hl, :], in0=maskT[:, hl, :], in1=dge[:], op=ALU.mult), reads=[maskT, dge], writes=[maskT])
    io1 = c.sb("io1", [64, 128], F32)
    c.op("pool", lambda e: e.iota(io1[:], [[1, 128]], base=1, channel_multiplier=0, allow_small_or_imprecise_dtypes=True), writes=[io1])
    io2 = c.sb("io2", [64, 128], F32)
    c.op("pool", lambda e: e.iota(io2[:], [[-1, 128]], base=127, channel_multiplier=0, allow_small_or_imprecise_dtypes=True), writes=[io2])
    qdf = c.sb("qdf", [64, 128], F32)
    kdf = c.sb("kdf", [64, 128], F32)
    c.op("act", lambda e: e.activation(out=qdf[:], in_=io1[:], func=AF.Exp, scale=lgP[:, 0:1]), reads=[io1, lgP], writes=[qdf])
    c.op(V, lambda e: e.tensor_scalar(qdf[:], qdf[:], 32.0 ** -0.5, None, op0=ALU.mult), reads=[qdf], writes=[qdf])
    c.op("act", lambda e: e.activation(out=kdf[:], in_=io2[:], func=AF.Exp, scale=lgP[:, 0:1]), reads=[io2, lgP], writes=[kdf])
    sdec = c.sb("sdec", [64, 1], F32)
    c.op("act", lambda e: e.activation(out=sdec[:], in_=lgP[:, 0:1], func=AF.Exp, scale=128.0), reads=[lgP], writes=[sdec])
    fr = c.sb("fr", [64, 4], F32)
    for hb in range(2):
        c.op("pool", lambda e: e.iota(fr[32 * hb:32 * hb + 32, 0:1], [[0, 1]], base=0, channel_multiplier=1,
                                      allow_small_or_imprecise_dtypes=True), writes=[fr])
    c.op(V, lambda e: e.tensor_single_scalar(fr[:, 3:4], fr[:, 0:1], 16.0, op=ALU.is_ge), reads=[fr], writes=[fr])
    c.op(V, lambda e: e.scalar_tensor_tensor(out=fr[:, 0:1], in0=fr[:, 3:4], scalar=-16.0, in1=fr[:, 0:1], op0=ALU.mult, op1=ALU.add),
         reads=[fr], writes=[fr])
    c.op(V, lambda e: e.tensor_scalar(fr[:, 2:3], fr[:, 3:4], 2.0, -1.0, op0=ALU.mult, op1=ALU.add), reads=[fr], writes=[fr])
    c.op("act", lambda e: e.activation(out=fr[:, 1:2], in_=fr[:, 0:1], func=AF.Exp, scale=-math.log(10000.0) / 16.0), reads=[fr], writes=[fr])
    cs = c.sb("cs", [64, 4], F32)
    emit_sincos_small(c, 64, fr, fr[:, 1:2], cs)
    Gr, Gi, s128 = emit_pow_table(c, 64, 256, cs, cs[:, 0:1], cs[:, 1:2], save_at=128)
    Fr, Fi, _ = emit_pow_table(c, 64, 64, s128, s128[:, 0:1], s128[:, 1:2])
    E1r = c.sb("E1r", [64, NCH], F32)
    E1i = c.sb("E1i", [64, NCH], F32)
    c.op(V, lambda e: e.tensor_copy(E1r[:, 1:NCH], Fr[:, 0:64]), reads=[Fr], writes=[E1r])
    c.op(V, lambda e: e.tensor_copy(E1i[:, 1:NCH], Fi[:, 0:64]), reads=[Fi], writes=[E1i])
    c.op(V, lambda e: e.tensor_copy(E1r[:, 0:1], Fr[:, 1:2]), reads=[Fr], writes=[E1r])
    c.op(V, lambda e: e.tensor_scalar(E1i[:, 0:1], Fi[:, 1:2], -1.0, None, op0=ALU.mult), reads=[Fi], writes=[E1i])
    E2r = Gr
    E2i = Gi

    QR = c.sb("QR", [64, NTOK], BF16)
    KR = c.sb("KR", [64, NTOK], BF16)
    QD = c.sb("QD", [64, NTOK], BF16)
    KD = c.sb("KD", [64, NTOK], BF16)
    v_sb = c.sb("v_sb", [128, NCH, 128], BF16)
    g_sb = c.sb("g_sb", [128, NCH, 128], BF16)
    y_sb = c.sb("y_sb", [128, NCH, 128], F32)
    c.dma("pool", v_sb[:], v_d.h.ap().rearrange("(c p) f -> p c f", p=128), writes=[v_sb])
    c.dma("pool", g_sb[:], g_d.h.ap().rearrange("(c p) f -> p c f", p=128), writes=[g_sb])

    nblk = NCH // CB
    BW = CB * 128
    x_rot = Rot([c.sb(f"x{i}", [64, BW], F32) for i in range(2)])
    xs_rot = Rot([c.sb(f"xs{i}", [64, BW], F32) for i in range(2)])
    COSb = c.sb("COSb", [64, CB, 128], F32)
    SINb = c.sb("SINb", [64, CB, 128], F32)
    tb1 = c.sb("tb1", [64, CB, 128], F32)
    tb2 = c.sb("tb2", [64, CB, 128], F32)
    r1 = c.sb("r1", [64, BW], F32)
    r2 = c.sb("r2", [64, BW], F32)
    for b in range(nblk):
        c0 = b * CB
        t0 = c0 * 128
        e1r = E1r[:, c0:c0 + CB].unsqueeze(2).to_broadcast([64, CB, 128])
        e1i = E1i[:, c0:c0 + CB].unsqueeze(2).to_broadcast([64, CB, 128])
        e2r = E2r[:, 16:144].unsqueeze(1).to_broadcast([64, CB, 128])
        e2i = E2i[:, 16:144].unsqueeze(1).to_broadcast([64, CB, 128])
        P_ = "pool"
        c.op(P_, lambda e: e.tensor_tensor(out=tb1[:], in0=e1r, in1=e2r, op=ALU.mult), reads=[E1r, E2r], writes=[tb1])
        c.op(P_, lambda e: e.tensor_tensor(out=tb2[:], in0=e1i, in1=e2i, op=ALU.mult), reads=[E1i, E2i], writes=[tb2])
        c.op(P_, lambda e: e.tensor_tensor(out=COSb[:], in0=tb1[:], in1=tb2[:], op=ALU.subtract), reads=[tb1, tb2], writes=[COSb])
        c.op(P_, lambda e: e.tensor_tensor(out=tb1[:], in0=e1r, in1=e2i, op=ALU.mult), reads=[E1r, E2i], writes=[tb1])
        c.op(P_, lambda e: e.tensor_tensor(out=tb2[:], in0=e1i, in1=e2r, op=ALU.mult), reads=[E1i, E2r], writes=[tb2])
        c.op(P_, lambda e: e.tensor_tensor(out=SINb[:], in0=tb1[:], in1=tb2[:], op=ALU.add), reads=[tb1, tb2], writes=[SINb])
        cosf = COSb[:].rearrange("p c j -> p (c j)")
        sinf = SINb[:].rearrange("p c j -> p (c j)")
        for (src_d, srcs_d, OUT, DEC, fac) in ((q_d, qs_d, QR, QD, qdf), (k_d, ks_d, KR, KD, kdf)):
            x = x_rot.next(); xs = xs_rot.next()
            c.dma("sp", x[:], src_d[:, t0:t0 + BW], writes=[x])
            c.dma("sp", xs[:], srcs_d[:, t0:t0 + BW], writes=[xs])
            c.op(V, lambda e: e.tensor_tensor(out=r1[:], in0=x[:], in1=cosf, op=ALU.mult), reads=[x, COSb], writes=[r1])
            c.op(V, lambda e: e.scalar_tensor_tensor(out=r2[:], in0=xs[:], scalar=fr[:, 2:3], in1=sinf, op0=ALU.mult, op1=ALU.mult),
                 reads=[xs, fr, SINb], writes=[r2])
            c.op(V, lambda e: e.tensor_tensor(out=OUT[:, t0:t0 + BW], in0=r1[:], in1=r2[:], op=ALU.add), reads=[r1, r2], writes=[(OUT, b)])
            facb = fac[:].unsqueeze(1).to_broadcast([64, CB, 128])
            c.op(V, lambda e: e.tensor_tensor(out=DEC[:, t0:t0 + BW].rearrange("p (c j) -> p c j", j=128),
                                              in0=OUT[:, t0:t0 + BW].rearrange("p (c j) -> p c j", j=128), in1=facb, op=ALU.mult),
                 reads=[(OUT, b), fac], writes=[(DEC, b)])

    gg_all = c.sb("gg_all", [128, NCH, 128], BF16)
    c.op("act", lambda e: e.activation(out=gg_all[:], in_=g_sb[:], func=AF.Silu), reads=[g_sb], writes=[gg_all])
    c.op("pool", lambda e: e.tensor_tensor(out=gg_all[:], in0=gg_all[:], in1=og[:].unsqueeze(1).to_broadcast([128, NCH, 128]), op=ALU.mult),
         reads=[gg_all, og], writes=[gg_all])
    ps_tr = Rot([c.ps(f"ps_tr{i}", [128, 64], BF16) for i in range(2)])
    ps_s = Rot([c.ps(f"ps_s{i}", [128, 2, 128], F32) for i in range(2)])
    ps_o = Rot([c.ps(f"ps_o{i}", [128, 128], F32) for i in range(2)])
    ps_d = Rot([c.ps(f"ps_d{i}", [64, 128], F32) for i in range(2)])
    kdt_rot = Rot([c.sb(f"kdt{i}", [128, 64], BF16) for i in range(2)])
    sT_rot = Rot([c.sb(f"sT{i}", [128, 2, 128], BF16) for i in range(2)])
    S32 = c.sb("S32", [64, 128], F32)
    c.op(V, lambda e: e.memset(S32[:], 0.0), writes=[S32])
    Sb_rot = Rot([c.sb(f"Sb{i}", [64, 128], BF16) for i in range(2)])
    Sb = Sb_rot.next()
    c.op(V, lambda e: e.memset(Sb[:], 0.0), writes=[Sb])
    o_rot = Rot([c.sb(f"o{i}", [128, 2, 64], F32) for i in range(2)])
    cen_rot = Rot([c.sb(f"cen{i}", [128, 2, 64], F32) for i in range(2)])
    sq_rot = Rot([c.sb(f"sqr{i}", [128, 2, 64], F32) for i in range(2)])
    st_rot = Rot([c.sb(f"st{i}", [128, 8], F32) for i in range(2)])
    gg_rot = Rot([c.sb(f"gg{i}", [128, 128], F32) for i in range(2)])
    for ch in range(NCH):
        b = ch // CB
        t0 = ch * 128
        ptr = ps_tr.next()
        c.op("pe", lambda e: e.transpose(ptr[:, :], KD[:, t0:t0 + 128], identb[0:64, 0:64]), reads=[(KD, b), identb], writes=[ptr])
        kdt = kdt_rot.next()
        c.op("act", lambda e: e.copy(out=kdt[:], in_=ptr[:]), reads=[ptr], writes=[kdt])
        pss = ps_s.next()
        for hl in range(2):
            c.op("pe", lambda e: e.matmul(pss[:, hl, :], lhsT=KR[32 * hl:32 * hl + 32, t0:t0 + 128], rhs=QR[32 * hl:32 * hl + 32, t0:t0 + 128],
                                          start=True, stop=True), reads=[(KR, b), (QR, b)], writes=[pss])
        sT = sT_rot.next()
        c.op(V, lambda e: e.tensor_tensor(out=sT[:], in0=pss[:], in1=maskT[:], op=ALU.mult), reads=[pss, maskT], writes=[sT])
        pso = ps_o.next()
        for hl in range(2):
            c.op("pe", lambda e: e.matmul(pso[:, hl * 64:(hl + 1) * 64], lhsT=sT[:, hl, :], rhs=v_sb[:, ch, hl * 64:(hl + 1) * 64],
                                          start=True, stop=False), reads=[sT, v_sb], writes=[pso])
            c.op("pe", lambda e: e.matmul(pso[:, hl * 64:(hl + 1) * 64], lhsT=QD[32 * hl:32 * hl + 32, t0:t0 + 128],
                                          rhs=Sb[32 * hl:32 * hl + 32, hl * 64:(hl + 1) * 64], start=False, stop=True),
                 reads=[(QD, b), Sb], writes=[pso])
        psd = ps_d.next()
        c.op("pe", lambda e: e.matmul(psd[:, :], lhsT=kdt[:], rhs=v_sb[:, ch, :], start=True, stop=True), reads=[kdt, v_sb], writes=[psd])
        c.op(V, lambda e: e.scalar_tensor_tensor(out=S32[:], in0=S32[:], scalar=sdec[:, 0:1], in1=psd[:], op0=ALU.mult, op1=ALU.add),
             reads=[S32, sdec, psd], writes=[S32])
        Sb = Sb_rot.next()
        c.op("act", lambda e: e.copy(out=Sb[:], in_=S32[:]), reads=[S32], writes=[Sb])
        o = o_rot.next(); cen = cen_rot.next(); sq = sq_rot.next(); st = st_rot.next(); gg = gg_rot.next()
        c.op("act", lambda e: e.copy(out=o[:].rearrange("p h v -> p (h v)"), in_=pso[:]), reads=[pso], writes=[o])
        c.op(V, lambda e: e.tensor_reduce(out=st[:, 0:2], in_=o[:], axis=AX.X, op=ALU.add), reads=[o], writes=[st])
        c.op(V, lambda e: e.tensor_scalar(st[:, 0:2], st[:, 0:2], 1.0 / 64, None, op0=ALU.mult), reads=[st], writes=[st])
        c.op(V, lambda e: e.tensor_tensor(out=cen[:], in0=o[:], in1=st[:, 0:2].unsqueeze(2).to_broadcast([128, 2, 64]), op=ALU.subtract),
             reads=[o, st], writes=[cen])
        c.op("pool", lambda e: e.tensor_tensor(out=sq[:], in0=cen[:], in1=cen[:], op=ALU.mult), reads=[cen], writes=[sq])
        c.op(V, lambda e: e.tensor_reduce(out=st[:, 2:4], in_=sq[:], axis=AX.X, op=ALU.add), reads=[sq, st], writes=[st])
        c.op("act", lambda e: e.activation(out=st[:, 2:4], in_=st[:, 2:4], func=AF.Sqrt, scale=1.0 / 64, bias=epsT[:, 0:1]), reads=[st, epsT], writes=[st])
        c.op(V, lambda e: e.reciprocal(st[:, 2:4], st[:, 2:4]), reads=[st], writes=[st])
        c.op(V, lambda e: e.tensor_tensor(out=cen[:], in0=cen[:], in1=st[:, 2:4].unsqueeze(2).to_broadcast([128, 2, 64]), op=ALU.mult),
             reads=[cen, st], writes=[cen])
        c.op(V, lambda e: e.tensor_tensor(out=y_sb[:, ch, :], in0=cen[:].rearrange("p h v -> p (h v)"), in1=gg_all[:, ch, :], op=ALU.mult),
             reads=[cen, gg_all], writes=[(y_sb, ch)])
    c.barrier()
    if debug:
        dbg = {"lgP": lgP, "lgB": lgB, "fr": fr, "cs": cs, "E1r": E1r, "E1i": E1i, "Gr": Gr, "Gi": Gi, "maskT": maskT,
               "qdf": qdf, "kdf": kdf, "sdec": sdec, "S32": S32, "COSb": COSb, "SINb": SINb}
        for nm, t in dbg.items():
            shp = list(t.h.shape)
            dd = c.dram(pfx + "dbg_" + nm, shp, F32, "ExternalOutput")
            c.dma("sp", dd[:], t[:], reads=[t])
        for nm, t in {"QR": QR, "KR": KR, "QD": QD, "KD": KD}.items():
            tmpf = c.sb("dbgf_" + nm, [64, 1024], F32)
            c.op("dve", lambda e: e.tensor_copy(tmpf[:], t[:, 0:1024]), writes=[tmpf])
            dd = c.dram(pfx + "dbg_" + nm, [64, 1024], F32, "ExternalOutput")
            c.dma("sp", dd[:], tmpf[:], reads=[tmpf])
    c.dma("sp", y_d.h.ap().rearrange("(c p) f -> p c f", p=128), y_sb[:], reads=[y_sb], qt=y_sb)
    c.pe_self_sync = False
    if sc_ is not None:
        sc_.__exit__(None, None, None)
        return None
    c.finish()
    c.close()
    print("ret instructions:", c.n_inst)
    return nc


NTOK = 8320
NG = 65
EPS = 1e-6
GB = 13
BW = GB * 128
NBLK = NG // GB
CS = 32


def build_hg(layer, debug=False, c=None, pfx=""):
    own = c is None
    if own:
        nc = bass.Bass("TRN2", target_bir_lowering=False)
        c = Ctx(nc)
    sc_ = None if own else c.scope()
    if sc_ is not None:
        sc_.__enter__()
    x_d = c.dram(pfx + "x3", [3, 64, NTOK], F32, "ExternalInput")
    cw_d = c.dram(pfx + "convw", [3, 64, 4], F32, "ExternalInput")
    g_d = c.dram(pfx + "gate", [NTOK, 64], F32, "ExternalInput")
    og_d = c.dram(pfx + "outg", [128, 64], F32, "ExternalInput")
    lb_d = c.dram(pfx + "lbp", [64, 2], F32, "ExternalInput")
    y_d = c.dram(pfx + "y", [NTOK, 64], F32, "ExternalOutput")
    V = "dve"

    epsT = c.sb("eps", [128, 1], F32)
    c.op("pool", lambda e: e.memset(epsT[:], EPS), writes=[epsT])
    iof = c.sb("iof", [128, 128], F32)
    c.op("pool", lambda e: e.iota(iof[:], [[1, 128]], base=0, channel_multiplier=-1, allow_small_or_imprecise_dtypes=True), writes=[iof])
    identf = c.sb("identf", [128, 128], F32)
    c.op(V, lambda e: e.tensor_single_scalar(identf[:], iof[:], 0.0, op=ALU.is_equal), reads=[iof], writes=[identf])
    identb = c.sb("identb", [128, 128], BF16)
    c.op(V, lambda e: e.tensor_copy(identb[:], identf[:]), reads=[identf], writes=[identb])
    dge = c.sb("dge", [128, 128], F32)
    c.op(V, lambda e: e.tensor_single_scalar(dge[:], iof[:], 0.0, op=ALU.is_ge), reads=[iof], writes=[dge])
    bmask = c.sb("bmask", [128, 128], F32)
    c.op(V, lambda e: e.memset(bmask[:], 0.0), writes=[bmask])
    for m in range(4):
        c.op(V, lambda e: e.tensor_copy(bmask[32 * m:32 * m + 32, 32 * m:32 * m + 32], dge[32 * m:32 * m + 32, 32 * m:32 * m + 32]),
             reads=[dge], writes=[bmask])
    cmask = c.sb("cmask", [128, 4, 64], F32)
    cm2 = c.sb("cm2", [128, 4, 64], F32)
    c.op("pool", lambda e: e.iota(cmask[:], [[-32, 4], [0, 64]], base=0, channel_multiplier=1, allow_small_or_imprecise_dtypes=True), writes=[cmask])
    c.op(V, lambda e: e.tensor_single_scalar(cm2[:], cmask[:], 32.0, op=ALU.is_lt), reads=[cmask], writes=[cm2])
    c.op(V, lambda e: e.tensor_single_scalar(cmask[:], cmask[:], 0.0, op=ALU.is_ge), reads=[cmask, cm2], writes=[cmask])
    c.op(V, lambda e: e.tensor_tensor(out=cmask[:], in0=cmask[:], in1=cm2[:], op=ALU.mult), reads=[cmask, cm2], writes=[cmask])
    colmask = c.sb("colmask", [64, 4, 128], F32)
    col2 = c.sb("col2", [64, 4, 128], F32)
    c.op("pool", lambda e: e.iota(colmask[:], [[-32, 4], [1, 128]], base=0, channel_multiplier=0, allow_small_or_imprecise_dtypes=True), writes=[colmask])
    c.op(V, lambda e: e.tensor_single_scalar(col2[:], colmask[:], 32.0, op=ALU.is_lt), reads=[colmask], writes=[col2])
    c.op(V, lambda e: e.tensor_single_scalar(colmask[:], colmask[:], 0.0, op=ALU.is_ge), reads=[colmask, col2], writes=[colmask])
    c.op(V, lambda e: e.tensor_tensor(out=colmask[:], in0=colmask[:], in1=col2[:], op=ALU.mult), reads=[colmask, col2], writes=[colmask])
    rmask = c.sb("rmask", [64, BW], F32)
    c.op("pool", lambda e: e.iota(rmask[:], [[0, BW // CS], [1, CS]], base=0, channel_multiplier=0, allow_small_or_imprecise_dtypes=True), writes=[rmask])
    c.op(V, lambda e: e.tensor_single_scalar(rmask[:], rmask[:], 0.0, op=ALU.is_gt), reads=[rmask], writes=[rmask])
    cw = c.sb("cw", [64, 3, 4], F32)
    c.dma("sp", cw[:], cw_d.h.ap().rearrange("s p k -> p s k"), writes=[cw])
    og = c.sb("og", [128, 64], F32)
    c.dma("sp", og[:], og_d[:], writes=[og])
    lbp = c.sb("lbp", [64, 4], F32)
    c.dma("sp", lbp[:, 0:2], lb_d[:], writes=[lbp])
    if layer == 0:
        c.op(V, lambda e: e.memset(lbp[:, 2:3], 0.0), reads=[lbp], writes=[lbp])
    else:
        c.op(V, lambda e: e.tensor_tensor(out=lbp[:, 2:3], in0=lbp[:, 1:2], in1=lbp[:, 0:1], op=ALU.subtract), reads=[lbp], writes=[lbp])
        c.op("act", lambda e: e.activation(out=lbp[:, 2:3], in_=lbp[:, 2:3], func=AF.Sigmoid), reads=[lbp], writes=[lbp])
    c.op(V, lambda e: e.tensor_scalar(lbp[:, 3:4], lbp[:, 2:3], -1.0, 1.0, op0=ALU.mult, op1=ALU.add), reads=[lbp], writes=[lbp])

    QT = c.sb("QT", [64, NTOK], BF16)
    KT = c.sb("KT", [64, NTOK], BF16)
    KDT = c.sb("KDT", [64, NTOK], BF16)
    Vtok = c.sb("Vtok", [128, NG, 64], BF16)
    KDtok = c.sb("KDtok", [128, NG, 64], BF16)
    g_sb = c.sb("g_sb", [128, NG, 64], BF16)
    y_sb = c.sb("y_sb", [128, NG, 64], F32)
    Dec = c.sb("Dec", [64, NTOK // CS], F32)
    c.dma("pool", g_sb[:], g_d.h.ap().rearrange("(c p) f -> p c f", p=128), writes=[g_sb])

    xq = c.sb("xq", [64, BW + 3], F32); xf = c.sb("xf", [64, BW + 3], F32); xi = c.sb("xi", [64, BW + 3], F32)
    cq = c.sb("cq", [64, BW], F32); cf = c.sb("cf", [64, BW], F32); ci = c.sb("ci", [64, BW], F32)
    gg_ = c.sb("g", [64, BW], F32); gcum = c.sb("gcum", [64, BW], F32); tmp = c.sb("tmp", [64, BW], F32)
    ps_t = Rot([c.ps(f"pst{i}", [128, 64], F32) for i in range(2)])
    ps_tb = Rot([c.ps(f"pstb{i}", [128, 64], BF16) for i in range(2)])
    NCB = BW // CS
    for b in range(NBLK):
        t0 = b * BW
        for si, (xt, ct) in enumerate(((xq, cq), (xf, cf), (xi, ci))):
            if b == 0:
                c.op(V, lambda e: e.memset(xt[:, 0:3], 0.0), writes=[xt])
                c.dma("sp", xt[:, 3:BW + 3], x_d.h.ap()[si, :, 0:BW], writes=[xt])
            else:
                c.dma("sp", xt[:, :], x_d.h.ap()[si, :, t0 - 3:t0 + BW], writes=[xt])
            c.op(V, lambda e: e.tensor_scalar(ct[:], xt[:, 0:BW], cw[:, si, 0:1], None, op0=ALU.mult), reads=[xt, cw], writes=[ct])
            for kk_ in range(1, 4):
                c.op(V, lambda e: e.scalar_tensor_tensor(out=ct[:], in0=xt[:, kk_:kk_ + BW], scalar=cw[:, si, kk_:kk_ + 1], in1=ct[:],
                                                         op0=ALU.mult, op1=ALU.add), reads=[xt, cw, ct], writes=[ct])
        c.op("act", lambda e: e.activation(out=cq[:], in_=cq[:], func=AF.Silu), reads=[cq], writes=[cq])
        c.op("act", lambda e: e.activation(out=cf[:], in_=cf[:], func=AF.Sigmoid), reads=[cf], writes=[cf])
        c.op(V, lambda e: e.tensor_scalar(cf[:], cf[:], lbp[:, 3:4], lbp[:, 2:3], op0=ALU.mult, op1=ALU.add), reads=[cf, lbp], writes=[cf])
        c.op("act", lambda e: e.activation(out=gg_[:], in_=cf[:], func=AF.Ln), reads=[cf], writes=[gg_])
        c.op(V, lambda e: e.tensor_scalar(cf[:], cf[:], -1.0, 1.0, op0=ALU.mult, op1=ALU.add), reads=[cf, gg_], writes=[cf])
        c.op(V, lambda e: e.tensor_tensor_scan(out=gcum[:], data0=rmask[:], data1=gg_[:], initial=0.0, op0=ALU.mult, op1=ALU.add),
             reads=[rmask, gg_], writes=[gcum])
        c.op("act", lambda e: e.activation(out=tmp[:], in_=gcum[:], func=AF.Exp), reads=[gcum], writes=[tmp])
        c.op(V, lambda e: e.tensor_tensor(out=QT[:, t0:t0 + BW], in0=cq[:], in1=tmp[:], op=ALU.mult), reads=[cq, tmp], writes=[(QT, b)])
        c.op(V, lambda e: e.tensor_single_scalar(tmp[:], gcum[:], -80.0, op=ALU.max), reads=[gcum, (QT, b)], writes=[tmp])
        c.op("act", lambda e: e.activation(out=tmp[:], in_=tmp[:], func=AF.Exp, scale=-1.0), reads=[tmp], writes=[tmp])
        c.op(V, lambda e: e.tensor_tensor(out=KT[:, t0:t0 + BW], in0=cf[:], in1=tmp[:], op=ALU.mult), reads=[cf, tmp], writes=[(KT, b)])
        gl = gcum[:].rearrange("p (c j) -> p c j", j=CS)[:, :, CS - 1:CS]
        c.op(V, lambda e: e.tensor_tensor(out=tmp[:].rearrange("p (c j) -> p c j", j=CS), in0=gl.to_broadcast([64, NCB, CS]),
                                          in1=gcum[:].rearrange("p (c j) -> p c j", j=CS), op=ALU.subtract), reads=[gcum, (KT, b)], writes=[tmp])
        c.op("act", lambda e: e.activation(out=tmp[:], in_=tmp[:], func=AF.Exp), reads=[tmp], writes=[tmp])
        c.op(V, lambda e: e.tensor_tensor(out=KDT[:, t0:t0 + BW], in0=cf[:], in1=tmp[:], op=ALU.mult), reads=[cf, tmp], writes=[(KDT, b)])
        c.op("act", lambda e: e.activation(out=Dec[:, b * NCB:(b + 1) * NCB].unsqueeze(2), in_=gl, func=AF.Exp), reads=[gcum], writes=[(Dec, b)])
        for gi in range(GB):
            G = b * GB + gi
            pt = ps_t.next()
            c.op("pe", lambda e: e.transpose(pt[:, :], ci[:, gi * 128:(gi + 1) * 128], identf[0:64, 0:64]), reads=[ci, identf], writes=[pt])
            c.op("act", lambda e: e.copy(out=Vtok[:, G, :], in_=pt[:]), reads=[pt], writes=[(Vtok, G)])
            ptb = ps_tb.next()
            c.op("pe", lambda e: e.transpose(ptb[:, :], KDT[:, t0 + gi * 128:t0 + (gi + 1) * 128], identb[0:64, 0:64]),
                 reads=[(KDT, b), identb], writes=[ptb])
            c.op("act", lambda e: e.copy(out=KDtok[:, G, :], in_=ptb[:]), reads=[ptb], writes=[(KDtok, G)])

    gg_all = c.sb("gg_all", [128, NG, 64], F32)
    c.op("act", lambda e: e.activation(out=gg_all[:], in_=g_sb[:], func=AF.Silu), reads=[g_sb], writes=[gg_all])
    c.op("pool", lambda e: e.tensor_tensor(out=gg_all[:], in0=gg_all[:], in1=og[:].unsqueeze(1).to_broadcast([128, NG, 64]), op=ALU.mult),
         reads=[gg_all, og], writes=[gg_all])
    ps_s = Rot([c.ps(f"ps_s{i}", [128, 128], F32) for i in range(1)])
    ps_o = Rot([c.ps(f"ps_o{i}", [128, 64], F32) for i in range(2)])
    ps_d = Rot([c.ps(f"ps_d{i}", [64, 4, 64], F32) for i in range(1)])
    sT_rot = Rot([c.sb(f"sT{i}", [128, 128], BF16) for i in range(2)])
    S32 = c.sb("S32", [64, 64], F32)
    c.op(V, lambda e: e.memset(S32[:], 0.0), writes=[S32])
    Sb_rot = Rot([c.sb(f"Sb{i}", [64, 64], BF16) for i in range(6)])
    Sb = Sb_rot.next()
    c.op(V, lambda e: e.memset(Sb[:], 0.0), writes=[Sb])
    o_rot = Rot([c.sb(f"o{i}", [128, 64], F32) for i in range(2)])
    sq_rot = Rot([c.sb(f"sqr{i}", [128, 64], F32) for i in range(2)])
    st_rot = Rot([c.sb(f"st{i}", [128, 4], F32) for i in range(2)])
    gg_rot = Rot([c.sb(f"gg{i}", [128, 64], F32) for i in range(2)])
    vb_rot = Rot([c.sb(f"vb{i}", [128, 4, 64], BF16) for i in range(2)])
    qm_rot = Rot([c.sb(f"qm{i}", [64, 4, 128], BF16) for i in range(2)])
    for G in range(NG):
        b = G // GB
        t0 = G * 128
        pss = ps_s.next()
        c.op("pe", lambda e: e.matmul(pss[:, :], lhsT=KT[:, t0:t0 + 128], rhs=QT[:, t0:t0 + 128], start=True, stop=True),
             reads=[(KT, b), (QT, b)], writes=[pss])
        sT = sT_rot.next()
        c.op(V, lambda e: e.tensor_tensor(out=sT[:], in0=pss[:], in1=bmask[:], op=ALU.mult), reads=[pss, bmask], writes=[sT])
        psd = ps_d.next()
        vb = vb_rot.next()
        c.op("pool", lambda e: e.tensor_tensor(out=vb[:], in0=Vtok[:, G, :].unsqueeze(1).to_broadcast([128, 4, 64]), in1=cmask[:], op=ALU.mult),
             reads=[(Vtok, G), cmask], writes=[vb])
        c.op("pe", lambda e: e.matmul(psd[:].rearrange("p m v -> p (m v)"), lhsT=KDtok[:, G, :], rhs=vb[:].rearrange("p m v -> p (m v)"),
                                      start=True, stop=True), reads=[(KDtok, G), vb], writes=[psd])
        qm = qm_rot.next()
        c.op(V, lambda e: e.tensor_tensor(out=qm[:], in0=QT[:, t0:t0 + 128].unsqueeze(1).to_broadcast([64, 4, 128]), in1=colmask[:], op=ALU.mult),
             reads=[(QT, b), colmask], writes=[qm])
        pso = ps_o.next()
        c.op("pe", lambda e: e.matmul(pso[:, :], lhsT=sT[:], rhs=Vtok[:, G, :], start=True, stop=False), reads=[sT, (Vtok, G)], writes=[pso])
        for m in range(4):
            ch = G * 4 + m
            c.op("pe", lambda e: e.matmul(pso[:, :], lhsT=qm[:, m, :], rhs=Sb[:, :],
                                          start=False, stop=(m == 3)), reads=[qm, Sb], writes=[pso])
            c.op(V, lambda e: e.scalar_tensor_tensor(out=S32[:], in0=S32[:], scalar=Dec[:, ch:ch + 1], in1=psd[:, m, :],
                                                     op0=ALU.mult, op1=ALU.add), reads=[S32, (Dec, b), psd], writes=[S32])
            Sb = Sb_rot.next()
            c.op("act", lambda e: e.copy(out=Sb[:], in_=S32[:]), reads=[S32], writes=[Sb])
        o = o_rot.next(); sq = sq_rot.next(); st = st_rot.next(); gg = gg_rot.next()
        c.op("act", lambda e: e.copy(out=o[:], in_=pso[:]), reads=[pso], writes=[o])
        c.op("pool", lambda e: e.tensor_tensor(out=sq[:], in0=o[:], in1=o[:], op=ALU.mult), reads=[o], writes=[sq])
        c.op(V, lambda e: e.tensor_reduce(out=st[:, 0:1], in_=sq[:], axis=AX.X, op=ALU.add), reads=[sq], writes=[st])
        c.op("act", lambda e: e.activation(out=st[:, 0:1], in_=st[:, 0:1], func=AF.Sqrt, scale=1.0 / 64, bias=epsT[:, 0:1]), reads=[st, epsT], writes=[st])
        c.op(V, lambda e: e.reciprocal(st[:, 0:1], st[:, 0:1]), reads=[st], writes=[st])
        c.op(V, lambda e: e.scalar_tensor_tensor(out=y_sb[:, G, :], in0=o[:], scalar=st[:, 0:1], in1=gg_all[:, G, :], op0=ALU.mult, op1=ALU.mult),
             reads=[o, st, gg_all], writes=[(y_sb, G)])
    c.barrier()
    c.dma("sp", y_d.h.ap().rearrange("(c p) f -> p c f", p=128), y_sb[:], reads=[y_sb], qt=y_sb)
    c.pe_self_sync = False
    if sc_ is not None:
        sc_.__exit__(None, None, None)
        return None
    c.finish()
    c.close()
    print("hg instructions:", c.n_inst)
    return nc


NSB = 1040
NGL = 4
NLEV = 11


def emit_sincos_tile(c, x, xT, cs_c, cs_s, x2, acc, n):
    V = "dve"
    c.op(V, lambda e: e.tensor_tensor(out=x2[:], in0=x[:], in1=x[:], op=ALU.mult), reads=[x], writes=[x2])
    c.op(V, lambda e: e.tensor_scalar(acc[:], x2[:], -1.0 / 156, 1.0, op0=ALU.mult, op1=ALU.add), reads=[x2], writes=[acc])
    for d in (110.0, 72.0, 42.0, 20.0, 6.0):
        c.op(V, lambda e: e.tensor_tensor(out=acc[:], in0=acc[:], in1=x2[:], op=ALU.mult), reads=[acc, x2], writes=[acc])
        c.op(V, lambda e: e.tensor_scalar(acc[:], acc[:], -1.0 / d, 1.0, op0=ALU.mult, op1=ALU.add), reads=[acc], writes=[acc])
    c.op(V, lambda e: e.tensor_tensor(out=cs_s[:], in0=acc[:], in1=x[:], op=ALU.mult), reads=[acc, x], writes=[cs_s])
    c.op(V, lambda e: e.tensor_scalar(acc[:], x2[:], -1.0 / 182, 1.0, op0=ALU.mult, op1=ALU.add), reads=[x2, cs_s], writes=[acc])
    for d in (132.0, 90.0, 56.0, 30.0, 12.0, 2.0):
        c.op(V, lambda e: e.tensor_tensor(out=acc[:], in0=acc[:], in1=x2[:], op=ALU.mult), reads=[acc, x2], writes=[acc])
        c.op(V, lambda e: e.tensor_scalar(acc[:], acc[:], -1.0 / d, 1.0, op0=ALU.mult, op1=ALU.add), reads=[acc], writes=[acc])
    c.op(V, lambda e: e.tensor_copy(cs_c[:], acc[:]), reads=[acc], writes=[cs_c])


def emit_cdouble(c, cr, ci, t1, t2):
    V = "dve"
    c.op(V, lambda e: e.tensor_tensor(out=t1[:], in0=cr[:], in1=cr[:], op=ALU.mult), reads=[cr], writes=[t1])
    c.op(V, lambda e: e.tensor_tensor(out=t2[:], in0=ci[:], in1=ci[:], op=ALU.mult), reads=[ci], writes=[t2])
    c.op(V, lambda e: e.scalar_tensor_tensor(out=ci[:], in0=cr[:], scalar=2.0, in1=ci[:], op0=ALU.mult, op1=ALU.mult), reads=[cr, ci, t2], writes=[ci])
    c.op(V, lambda e: e.tensor_tensor(out=cr[:], in0=t1[:], in1=t2[:], op=ALU.subtract), reads=[t1, t2, ci], writes=[cr])


def build_s5(debug=False, c=None, pfx=""):
    own = c is None
    if own:
        nc = bass.Bass("TRN2", target_bir_lowering=False)
        c = Ctx(nc)
    sc_ = None if own else c.scope()
    if sc_ is not None:
        sc_.__enter__()
    U_d = c.dram(pfx + "U", [NGL, 128, NSB], F32, "ExternalInput")
    lam_d = c.dram(pfx + "lam", [128, NGL, 2], F32, "ExternalInput")
    ls_d = c.dram(pfx + "ls", [128, NGL], F32, "ExternalInput")
    B_d = c.dram(pfx + "Bm", [128, NGL, 2, 16], F32, "ExternalInput")
    C_d = c.dram(pfx + "Cm", [128, NGL, 2, 16], F32, "ExternalInput")
    D_d = c.dram(pfx + "Dm", [128, NGL], F32, "ExternalInput")
    y_d = c.dram(pfx + "y", [NGL, 128, NSB], F32, "ExternalOutput")
    V = "dve"
    G4 = NGL

    def sb(name, shape, dt=F32):
        return c.sb(name, shape, dt)

    iof = sb("iof", [128, 128])
    c.op("pool", lambda e: e.iota(iof[:], [[1, 128]], base=0, channel_multiplier=-1, allow_small_or_imprecise_dtypes=True), writes=[iof])
    ident = sb("ident", [128, 128])
    c.op(V, lambda e: e.tensor_single_scalar(ident[:], iof[:], 0.0, op=ALU.is_equal), reads=[iof], writes=[ident])
    pswap = sb("pswap", [128, 128])
    ptmp = sb("ptmp", [128, 128])
    c.op(V, lambda e: e.tensor_single_scalar(pswap[:], iof[:], 64.0, op=ALU.is_equal), reads=[iof], writes=[pswap])
    c.op(V, lambda e: e.tensor_single_scalar(ptmp[:], iof[:], -64.0, op=ALU.is_equal), reads=[iof], writes=[ptmp])
    c.op(V, lambda e: e.tensor_tensor(out=pswap[:], in0=pswap[:], in1=ptmp[:], op=ALU.add), reads=[pswap, ptmp], writes=[pswap])
    tmask = sb("tmask", [128, 8, 16])
    c.op("pool", lambda e: e.iota(tmask[:], [[16, 8], [0, 16]], base=15, channel_multiplier=-1, allow_small_or_imprecise_dtypes=True), writes=[tmask])
    c.op(V, lambda e: e.tensor_single_scalar(tmask[:], tmask[:], 0.0, op=ALU.is_ge), reads=[tmask], writes=[tmask])
    sgnh = sb("sgnh", [128, 1])
    c.op(V, lambda e: e.memset(sgnh[0:64, :], 1.0), writes=[sgnh])
    c.op(V, lambda e: e.memset(sgnh[64:128, :], -1.0), writes=[sgnh])
    mv = sb("mv", [128, G4, 9])
    c.op("pool", lambda e: e.iota(mv[:], [[0, G4], [1, 9]], base=0, channel_multiplier=0, allow_small_or_imprecise_dtypes=True), writes=[mv])

    lam = sb("lam", [128, G4, 2]); ls = sb("ls", [128, G4]); Bm = sb("Bm", [128, G4, 2, 16]); Cm = sb("Cm", [128, G4, 2, 16]); Dm = sb("Dm", [128, G4])
    c.dma("sp", lam[:], lam_d[:], writes=[lam]); c.dma("sp", ls[:], ls_d[:], writes=[ls])
    c.dma("sp", Bm[:], B_d[:], writes=[Bm]); c.dma("sp", Cm[:], C_d[:], writes=[Cm]); c.dma("sp", Dm[:], D_d[:], writes=[Dm])

    st = sb("st", [128, G4]); a = sb("a", [128, G4]); th = sb("th", [128, G4])
    c.op("act", lambda e: e.activation(out=st[:], in_=ls[:], func=AF.Exp), reads=[ls], writes=[st])
    c.op(V, lambda e: e.tensor_tensor(out=a[:], in0=lam[:, :, 0], in1=st[:], op=ALU.mult), reads=[lam, st], writes=[a])
    c.op(V, lambda e: e.tensor_tensor(out=th[:], in0=lam[:, :, 1], in1=st[:], op=ALU.mult), reads=[lam, st], writes=[th])
    kq = sb("kq", [128, G4]); ki = sb("ki", [128, G4], I32); x = sb("x", [128, G4])
    c.op(V, lambda e: e.tensor_scalar(kq[:], th[:], 1.0 / (2 * math.pi), None, op0=ALU.mult), reads=[th], writes=[kq])
    c.op(V, lambda e: e.tensor_copy(ki[:], kq[:]), reads=[kq], writes=[ki])
    c.op(V, lambda e: e.tensor_copy(kq[:], ki[:]), reads=[ki], writes=[kq])
    c.op(V, lambda e: e.scalar_tensor_tensor(out=x[:], in0=kq[:], scalar=-2 * math.pi, in1=th[:], op0=ALU.mult, op1=ALU.add), reads=[kq, th], writes=[x])
    c.op(V, lambda e: e.tensor_scalar(x[:], x[:], 0.25, None, op0=ALU.mult), reads=[x], writes=[x])
    p1r = sb("p1r", [128, G4]); p1i = sb("p1i", [128, G4]); x2 = sb("x2", [128, G4]); acc = sb("acc", [128, G4])
    t1 = sb("t1", [128, G4]); t2 = sb("t2", [128, G4])
    emit_sincos_tile(c, x, x, p1r, p1i, x2, acc, G4)
    emit_cdouble(c, p1r, p1i, t1, t2)
    emit_cdouble(c, p1r, p1i, t1, t2)
    phr = sb("phr", [128, G4, 9]); phi = sb("phi", [128, G4, 9])
    c.op(V, lambda e: e.memset(phr[:, :, 0:1], 1.0), writes=[phr])
    c.op(V, lambda e: e.memset(phi[:, :, 0:1], 0.0), writes=[phi])
    for m in range(8):
        c.op(V, lambda e: e.tensor_tensor(out=t1[:], in0=phr[:, :, m], in1=p1r[:], op=ALU.mult), reads=[phr, p1r], writes=[t1])
        c.op(V, lambda e: e.tensor_tensor(out=t2[:], in0=phi[:, :, m], in1=p1i[:], op=ALU.mult), reads=[phi, p1i], writes=[t2])
        c.op(V, lambda e: e.tensor_tensor(out=phr[:, :, m + 1], in0=t1[:], in1=t2[:], op=ALU.subtract), reads=[t1, t2], writes=[phr])
        c.op(V, lambda e: e.tensor_tensor(out=t1[:], in0=phr[:, :, m], in1=p1i[:], op=ALU.mult), reads=[phr, p1i], writes=[t1])
        c.op(V, lambda e: e.tensor_tensor(out=t2[:], in0=phi[:, :, m], in1=p1r[:], op=ALU.mult), reads=[phi, p1r], writes=[t2])
        c.op(V, lambda e: e.tensor_tensor(out=phi[:, :, m + 1], in0=t1[:], in1=t2[:], op=ALU.add), reads=[t1, t2], writes=[phi])
    am = sb("am", [128, G4, 9]); magp = sb("magp", [128, G4, 9]); magn = sb("magn", [128, G4, 9])
    c.op(V, lambda e: e.tensor_tensor(out=am[:], in0=mv[:], in1=a[:].unsqueeze(2).to_broadcast([128, G4, 9]), op=ALU.mult), reads=[mv, a], writes=[am])
    c.op("act", lambda e: e.activation(out=magp[:], in_=am[:], func=AF.Exp), reads=[am], writes=[magp])
    c.op("act", lambda e: e.activation(out=magn[:], in_=am[:], func=AF.Exp, scale=-1.0), reads=[am], writes=[magn])
    LPr = sb("LPr", [128, G4, 9]); LPi = sb("LPi", [128, G4, 9]); LNr = sb("LNr", [128, G4, 9]); LNi = sb("LNi", [128, G4, 9])
    c.op(V, lambda e: e.tensor_tensor(out=LPr[:], in0=magp[:], in1=phr[:], op=ALU.mult), reads=[magp, phr], writes=[LPr])
    c.op(V, lambda e: e.tensor_tensor(out=LPi[:], in0=magp[:], in1=phi[:], op=ALU.mult), reads=[magp, phi], writes=[LPi])
    c.op(V, lambda e: e.tensor_tensor(out=LNr[:], in0=magn[:], in1=phr[:], op=ALU.mult), reads=[magn, phr], writes=[LNr])
    c.op(V, lambda e: e.scalar_tensor_tensor(out=LNi[:], in0=magn[:], scalar=-1.0, in1=phi[:], op0=ALU.mult, op1=ALU.mult), reads=[magn, phi], writes=[LNi])
    kr = sb("kr", [128, G4]); kim = sb("kim", [128, G4]); nr = sb("nr", [128, G4]); den = sb("den", [128, G4])
    c.op(V, lambda e: e.tensor_scalar(nr[:], LPr[:, :, 1], -1.0, None, op0=ALU.add), reads=[LPr], writes=[nr])
    c.op(V, lambda e: e.tensor_tensor(out=t1[:], in0=lam[:, :, 0], in1=lam[:, :, 0], op=ALU.mult), reads=[lam], writes=[t1])
    c.op(V, lambda e: e.tensor_tensor(out=t2[:], in0=lam[:, :, 1], in1=lam[:, :, 1], op=ALU.mult), reads=[lam], writes=[t2])
    c.op(V, lambda e: e.tensor_tensor(out=den[:], in0=t1[:], in1=t2[:], op=ALU.add), reads=[t1, t2], writes=[den])
    c.op(V, lambda e: e.reciprocal(den[:], den[:]), reads=[den], writes=[den])
    c.op(V, lambda e: e.tensor_tensor(out=t1[:], in0=nr[:], in1=lam[:, :, 0], op=ALU.mult), reads=[nr, lam], writes=[t1])
    c.op(V, lambda e: e.tensor_tensor(out=t2[:], in0=LPi[:, :, 1], in1=lam[:, :, 1], op=ALU.mult), reads=[LPi, lam], writes=[t2])
    c.op(V, lambda e: e.tensor_tensor(out=kr[:], in0=t1[:], in1=t2[:], op=ALU.add), reads=[t1, t2], writes=[kr])
    c.op(V, lambda e: e.tensor_tensor(out=kr[:], in0=kr[:], in1=den[:], op=ALU.mult), reads=[kr, den], writes=[kr])
    c.op(V, lambda e: e.tensor_tensor(out=t1[:], in0=LPi[:, :, 1], in1=lam[:, :, 0], op=ALU.mult), reads=[LPi, lam, kr], writes=[t1])
    c.op(V, lambda e: e.tensor_tensor(out=t2[:], in0=nr[:], in1=lam[:, :, 1], op=ALU.mult), reads=[nr, lam, kr], writes=[t2])
    c.op(V, lambda e: e.tensor_tensor(out=kim[:], in0=t1[:], in1=t2[:], op=ALU.subtract), reads=[t1, t2], writes=[kim])
    c.op(V, lambda e: e.tensor_tensor(out=kim[:], in0=kim[:], in1=den[:], op=ALU.mult), reads=[kim, den], writes=[kim])
    Bbr = sb("Bbr", [128, G4, 16]); Bbi = sb("Bbi", [128, G4, 16]); tb = sb("tb", [128, G4, 16])
    krb = kr[:].unsqueeze(2).to_broadcast([128, G4, 16]); kib = kim[:].unsqueeze(2).to_broadcast([128, G4, 16])
    c.op(V, lambda e: e.tensor_tensor(out=Bbr[:], in0=Bm[:, :, 0, :], in1=krb, op=ALU.mult), reads=[Bm, kr], writes=[Bbr])
    c.op(V, lambda e: e.tensor_tensor(out=tb[:], in0=Bm[:, :, 1, :], in1=kib, op=ALU.mult), reads=[Bm, kim], writes=[tb])
    c.op(V, lambda e: e.tensor_tensor(out=Bbr[:], in0=Bbr[:], in1=tb[:], op=ALU.subtract), reads=[Bbr, tb], writes=[Bbr])
    c.op(V, lambda e: e.tensor_tensor(out=Bbi[:], in0=Bm[:, :, 1, :], in1=krb, op=ALU.mult), reads=[Bm, kr], writes=[Bbi])
    c.op(V, lambda e: e.tensor_tensor(out=tb[:], in0=Bm[:, :, 0, :], in1=kib, op=ALU.mult), reads=[Bm, kim, Bbr], writes=[tb])
    c.op(V, lambda e: e.tensor_tensor(out=Bbi[:], in0=Bbi[:], in1=tb[:], op=ALU.add), reads=[Bbi, tb], writes=[Bbi])
    X1 = sb("X1", [128, G4, 16]); X2 = sb("X2", [128, G4, 16]); C1 = sb("C1", [128, G4, 16]); C2 = sb("C2", [128, G4, 16])
    c.op(V, lambda e: e.tensor_copy(X1[0:64], Bbr[0:64]), reads=[Bbr], writes=[X1])
    c.op(V, lambda e: e.tensor_copy(X1[64:128], Bbi[64:128]), reads=[Bbi], writes=[X1])
    c.op(V, lambda e: e.tensor_scalar(X2[0:64], Bbi[0:64], -1.0, None, op0=ALU.mult), reads=[Bbi], writes=[X2])
    c.op(V, lambda e: e.tensor_copy(X2[64:128], Bbr[64:128]), reads=[Bbr], writes=[X2])
    c.op(V, lambda e: e.tensor_copy(C1[0:64], Cm[0:64, :, 0, :]), reads=[Cm], writes=[C1])
    c.op(V, lambda e: e.tensor_scalar(C1[64:128], Cm[64:128, :, 1, :], -1.0, None, op0=ALU.mult), reads=[Cm], writes=[C1])
    c.op(V, lambda e: e.tensor_scalar(C2[0:64], Cm[0:64, :, 1, :], -1.0, None, op0=ALU.mult), reads=[Cm], writes=[C2])
    c.op(V, lambda e: e.tensor_scalar(C2[64:128], Cm[64:128, :, 0, :], -1.0, None, op0=ALU.mult), reads=[Cm], writes=[C2])
    Z = sb("Z", [128, G4, 8, 16]); Y = sb("Y", [128, G4, 8, 16]); Wc = sb("Wc", [128, G4, 8, 16]); tz = sb("tz", [128, 16])
    for g in range(G4):
        for j in range(8):
            for (OUT, Lr_, Li_, mi, A1, A2) in ((Z, LPr, LPi, 7 - j, X1, X2), (Y, LNr, LNi, j + 1, X1, X2), (Wc, LPr, LPi, j + 1, C1, C2)):
                c.op(V, lambda e: e.tensor_scalar(tz[:], A1[:, g, :], Lr_[:, g, mi:mi + 1], None, op0=ALU.mult), reads=[A1, Lr_], writes=[tz])
                c.op(V, lambda e: e.scalar_tensor_tensor(out=OUT[:, g, j, :], in0=A2[:, g, :], scalar=Li_[:, g, mi:mi + 1], in1=tz[:],
                                                         op0=ALU.mult, op1=ALU.add), reads=[A2, Li_, tz], writes=[OUT])
    ps_rot = Rot([c.ps(f"ps{i}", [128, 512], F32) for i in range(7)])
    Toep = sb("Toep", [128, G4, 128], BF16); W1 = sb("W1", [128, G4, 128], BF16); tf = sb("tf", [128, 128])
    for g in range(G4):
        ps = ps_rot.next()
        c.op("pe", lambda e: e.matmul(ps[:, 0:128], lhsT=Y[:, g].rearrange("p j h -> p (j h)"), rhs=Wc[:, g].rearrange("p j h -> p (j h)"),
                                      start=True, stop=True), reads=[Y, Wc], writes=[ps])
        c.op(V, lambda e: e.tensor_tensor(out=tf[:], in0=ps[:, 0:128], in1=tmask[:].rearrange("p j h -> p (j h)"), op=ALU.mult),
             reads=[ps, tmask], writes=[tf])
        c.op(V, lambda e: e.scalar_tensor_tensor(out=Toep[:, g, :], in0=ident[:], scalar=Dm[:, g:g + 1], in1=tf[:], op0=ALU.mult, op1=ALU.add),
             reads=[ident, Dm, tf], writes=[Toep])
        ps = ps_rot.next()
        c.op("pe", lambda e: e.transpose(ps[:, 0:128], Z[:, g].rearrange("p j h -> p (j h)"), ident[:]), reads=[Z, ident], writes=[ps])
        c.op("act", lambda e: e.copy(out=W1[:, g, :], in_=ps[:, 0:128]), reads=[ps], writes=[W1])
    R = sb("R", [128, G4, NLEV, 128])
    qr = sb("qr", [128, G4]); qi = sb("qi", [128, G4]); mg = sb("mg", [128, G4]); s1 = sb("s1", [128, G4]); s2 = sb("s2", [128, G4])
    c.op(V, lambda e: e.tensor_copy(qr[:], phr[:, :, 8]), reads=[phr], writes=[qr])
    c.op(V, lambda e: e.tensor_copy(qi[:], phi[:, :, 8]), reads=[phi], writes=[qi])
    for k in range(NLEV):
        c.op("act", lambda e: e.activation(out=mg[:], in_=a[:], func=AF.Exp, scale=float(8 * (2 ** k))), reads=[a], writes=[mg])
        c.op(V, lambda e: e.tensor_tensor(out=s1[:], in0=mg[:], in1=qr[:], op=ALU.mult), reads=[mg, qr], writes=[s1])
        c.op(V, lambda e: e.tensor_tensor(out=s2[:], in0=mg[:], in1=qi[:], op=ALU.mult), reads=[mg, qi], writes=[s2])
        c.op(V, lambda e: e.tensor_scalar(s2[:], s2[:], sgnh[:, 0:1], None, op0=ALU.mult), reads=[s2, sgnh], writes=[s2])
        for g in range(G4):
            c.op(V, lambda e: e.tensor_scalar(R[:, g, k, :], ident[:], s1[:, g:g + 1], None, op0=ALU.mult), reads=[ident, s1], writes=[R])
            c.op(V, lambda e: e.scalar_tensor_tensor(out=R[:, g, k, :], in0=pswap[:], scalar=s2[:, g:g + 1], in1=R[:, g, k, :],
                                                     op0=ALU.mult, op1=ALU.add), reads=[pswap, s2, R], writes=[R])
        if k < NLEV - 1:
            emit_cdouble(c, qr, qi, t1, t2)

    blocks = [(0, 512), (512, 512), (1024, NSB - 1024)]
    Ub = [sb(f"Ub{g}", [128, NSB], BF16) for g in range(G4)]
    Xp = [sb(f"Xp{g}", [128, NSB + 1]) for g in range(G4)]
    for g in range(G4):
        c.dma("pool", Ub[g][:], U_d.h.ap()[g], writes=[Ub[g]])
        c.op(V, lambda e: e.memset(Xp[g][:, 0:1], 0.0), writes=[Xp[g]])
        for (c0, n) in blocks:
            ps = ps_rot.next()
            c.op("pe", lambda e: e.matmul(ps[:, :n], lhsT=W1[:, g, :], rhs=Ub[g][:, c0:c0 + n], start=True, stop=True), reads=[W1, Ub[g]], writes=[ps])
            c.op("act", lambda e: e.copy(out=Xp[g][:, 1 + c0:1 + c0 + n], in_=ps[:, :n]), reads=[ps], writes=[Xp[g]])
    for k in range(NLEV):
        d = 2 ** k
        if d >= NSB:
            break
        L = NSB - d
        for g in range(G4):
            pl = []
            cc = 0
            while cc < L:
                n = min(512, L - cc)
                ps = ps_rot.next()
                c.op("pe", lambda e: e.matmul(ps[:, :n], lhsT=R[:, g, k, :], rhs=Xp[g][:, 1 + cc:1 + cc + n], start=True, stop=True),
                     reads=[R, Xp[g]], writes=[ps])
                pl.append((ps, cc, n))
                cc += n
            for (ps, cc, n) in pl:
                c.op(V, lambda e: e.tensor_tensor(out=Xp[g][:, 1 + d + cc:1 + d + cc + n], in0=Xp[g][:, 1 + d + cc:1 + d + cc + n], in1=ps[:, :n], op=ALU.add),
                     reads=[ps, Xp[g]], writes=[Xp[g]])
    y_rot = Rot([sb(f"ysb{i}", [128, NSB]) for i in range(2)])
    for g in range(G4):
        ysb = y_rot.next()
        for (c0, n) in blocks:
            ps = ps_rot.next()
            c.op("pe", lambda e: e.matmul(ps[:, :n], lhsT=Toep[:, g, :], rhs=Ub[g][:, c0:c0 + n], start=True, stop=False), reads=[Toep, Ub[g]], writes=[ps])
            c.op("pe", lambda e: e.matmul(ps[:, :n], lhsT=Wc[:, g].rearrange("p j h -> p (j h)"), rhs=Xp[g][:, c0:c0 + n], start=False, stop=True),
                 reads=[Wc, Xp[g]], writes=[ps])
            c.op("act", lambda e: e.copy(out=ysb[:, c0:c0 + n], in_=ps[:, :n]), reads=[ps], writes=[ysb])
        c.dma("sp", y_d.h.ap()[g], ysb[:], reads=[ysb])
    c.pe_self_sync = False
    if sc_ is not None:
        sc_.__exit__(None, None, None)
        return None
    c.finish()
    c.close()
    print("s5 instructions:", c.n_inst)
    return nc


NT = 2080
def core_tok_idx(c):
    b, r = divmod(c, 4)
    return b, np.concatenate([np.arange(32 * r, 32 * r + 32), 128 + np.arange(2048 * r, 2048 * (r + 1))])

def build_H0(x, meta):
    B = x.shape[0]
    H = np.zeros((B, 8320, 1024), np.float32)
    H[:, 112:128, :] = meta[None]
    H[:, 128:, :] = x
    return H

def shard_T(H):
    out = []
    for c in range(8):
        b, idx = core_tok_idx(c)
        out.append(np.ascontiguousarray(H[b, idx, :].T))
    return out

def unshard_T(lst, C):
    H = np.zeros((2, 8320, C), lst[0].dtype)
    for c in range(8):
        b, idx = core_tok_idx(c)
        H[b, idx, :] = lst[c].T
    return H

def swap_cols():
    base = np.arange(256).reshape(8, 2, 16)[:, ::-1, :].reshape(256)
    return base

def w_in_ext(w_in_l):
    sw = swap_cols()
    q_sw = w_in_l[:, 1280:1536][:, sw]
    k_sw = w_in_l[:, 1536:1792][:, sw]
    return np.ascontiguousarray(np.concatenate([w_in_l, q_sw, k_sw], axis=1))

def gvec(g):
    return np.ascontiguousarray(g.reshape(-1, 128).T)

def s5_inputs(inp, L, u_b, r):
    gs = [4 * r + g for g in range(4)]
    U = np.stack([u_b[:, 16 * G:16 * G + 16].reshape(1040, 8, 16).transpose(1, 2, 0).reshape(128, 1040) for G in gs])
    def dup(a):
        return np.concatenate([a, a], axis=0)
    lam = np.stack([dup(np.stack([inp['s5_lam_re'][L, G], inp['s5_lam_im'][L, G]], -1)) for G in gs], 1)
    ls = np.tile(inp['s5_log_step'][L, gs][None, :], (128, 1))
    Bm = np.stack([dup(np.stack([inp['s5_b_re'][L, G], inp['s5_b_im'][L, G]], 1)) for G in gs], 1)
    Cm = np.stack([dup(np.stack([inp['s5_c_re'][L, G].T, inp['s5_c_im'][L, G].T], 1)) for G in gs], 1)
    Dm = np.stack([np.tile(inp['s5_d'][L, G], 8) for G in gs], 1)
    f = lambda a: np.ascontiguousarray(a.astype(np.float32))
    return {"U": f(U), "lam": f(lam), "ls": f(ls), "Bm": f(Bm), "Cm": f(Cm), "Dm": f(Dm)}

def s5_unpack(y, out_b, r):
    for g in range(4):
        G = 4 * r + g
        out_b[:, 16 * G:16 * G + 16] = y[g].reshape(8, 16, 1040).transpose(2, 0, 1).reshape(8320, 16)

def s5_raw_ref(inp, L, u):
    Bn, T, _ = u.shape
    out = np.zeros((Bn, T, 256))
    for G in range(16):
        lam = inp['s5_lam_re'][L, G].astype(np.float64) + 1j * inp['s5_lam_im'][L, G]
        step = np.exp(np.float64(inp['s5_log_step'][L, G]))
        lb = np.exp(lam * step)
        Bc = inp['s5_b_re'][L, G].astype(np.float64) + 1j * inp['s5_b_im'][L, G]
        Cc = inp['s5_c_re'][L, G].astype(np.float64) + 1j * inp['s5_c_im'][L, G]
        Bb = ((lb - 1) / lam)[:, None] * Bc
        d = inp['s5_d'][L, G].astype(np.float64)
        for b in range(Bn):
            ug = u[b, :, 16 * G:16 * G + 16].astype(np.float64)
            bu = ug @ Bb.T
            S = np.zeros(64, complex)
            y = np.zeros((T, 16))
            CH = 64
            pw = lb[None, :] ** np.arange(1, CH + 1)[:, None]
            ipw = lb[None, :] ** (-np.arange(1, CH + 1)[:, None])
            for t0 in range(0, T, CH):
                blk = bu[t0:t0 + CH]
                st = pw * (S[None, :] + np.cumsum(blk * ipw, axis=0))
                y[t0:t0 + CH] = (st @ Cc.T).real
                S = st[-1]
            out[b, :, 16 * G:16 * G + 16] = y + d * ug
    return out


_PROGS = {}


def _prog(key, fn):
    if key not in _PROGS:
        _PROGS[key] = fn()
    return _PROGS[key]


def _run(nc, maps):
    res = run_bass_kernel_spmd(nc, maps, core_ids=list(range(8)))
    return res.results


def build_B(L):
    nc = bass.Bass("TRN2", target_bir_lowering=False)
    c = Ctx(nc)
    build_ret(c=c, pfx="r_")
    build_hg(L, c=c, pfx="h_")
    build_s5(c=c, pfx="s_")
    c.finish()
    c.close()
    print("B instructions:", c.n_inst)
    return nc


def kernel(**inp):
    inp = {k: np.asarray(v) for k, v in inp.items()}
    f32 = lambda a: np.ascontiguousarray(a, dtype=np.float32)
    H = build_H0(inp['x'], inp['meta_tokens'])
    sw = swap_cols()
    proj_next = None
    for L in range(2):
        hs = shard_T(H)
        if proj_next is None:
            W = w_in_ext(inp['w_in'][L])
            g = gvec(inp['norm_mix_g'][L])
            rA = _run(_prog("A", build_A), [{"hT": hs[c], "g": g, "w": W} for c in range(8)])
            proj = unshard_T([r["projT"] for r in rA], NCOL_A)
        else:
            proj = proj_next
        rq = proj[:, :, 1280:1536]; rk = proj[:, :, 1536:1792]; rv = proj[:, :, 1792:2304]; rg = proj[:, :, 2304:2816]
        rqs = proj[:, :, 2816:3072]; rks = proj[:, :, 3072:3328]
        cwL = inp['hg_conv_w'][L]
        maps = []
        for c in range(8):
            b, r = divmod(c, 4)
            hds = [2 * r, 2 * r + 1]
            m = {"r_q": f32(rq[b, :, 64 * r:64 * r + 64].T), "r_qsw": f32(rqs[b, :, 64 * r:64 * r + 64].T),
                 "r_k": f32(rk[b, :, 64 * r:64 * r + 64].T), "r_ksw": f32(rks[b, :, 64 * r:64 * r + 64].T),
                 "r_v": f32(rv[b, :, 128 * r:128 * r + 128]), "r_gate": f32(rg[b, :, 128 * r:128 * r + 128]),
                 "r_outg": f32(np.tile(inp['ret_out_g'][L][128 * r:128 * r + 128][None, :], (128, 1))),
                 "r_hpart": f32(np.repeat(np.array(hds, np.float32), 32)[:, None]),
                 "r_hsel": f32(np.tile(np.array(hds, np.float32)[None, :], (128, 1)))}
            x3 = np.stack([proj[b, :, 256 + 256 * s + 64 * r: 256 + 256 * s + 64 * r + 64].T for s in range(3)])
            convw = np.stack([cwL[:, 256 * s + 64 * r: 256 * s + 64 * r + 64].T for s in range(3)])
            m.update({"h_x3": f32(x3), "h_convw": f32(convw), "h_gate": f32(proj[b, :, 1024 + 64 * r:1024 + 64 * r + 64]),
                      "h_outg": f32(np.tile(inp['hg_out_g'][L][64 * r:64 * r + 64][None, :], (128, 1))),
                      "h_lbp": f32(inp['hg_lb_param'][:, 64 * r:64 * r + 64].T)})
            for kk, vv in s5_inputs(inp, L, proj[b, :, 0:256], r).items():
                m["s_" + kk] = vv
            maps.append(m)
        rB = _run(_prog(("B", L), lambda: build_B(L)), maps)
        yc = np.zeros((2, 8320, 512), np.float32)
        yb = np.zeros((2, 8320, 256), np.float32)
        ya = np.zeros((2, 8320, 256), np.float32)
        for c in range(8):
            b, r = divmod(c, 4)
            yc[b, :, 128 * r:128 * r + 128] = rB[c]["r_y"]
            yb[b, :, 64 * r:64 * r + 64] = rB[c]["h_y"]
            s5_unpack(rB[c]["s_y"], ya[b], r)
        yas = shard_T(ya); ybcs = shard_T(np.concatenate([yb, yc], -1))
        moe = (L % 2 == 1)
        final = (L == 1)
        if not moe:
            w1 = f32(inp['ffn_w1'][L // 2][None]); w3 = f32(inp['ffn_w3'][L // 2][None]); w2 = f32(inp['ffn_w2'][L // 2][None])
            nc = _prog(("C", 1, final), lambda: build_C(1, 2816, final, with_A=True))
        else:
            w1 = f32(inp['moe_w1'][L // 2]); w3 = f32(inp['moe_w3'][L // 2]); w2 = f32(inp['moe_w2'][L // 2])
            nc = _prog(("C", 8, final), lambda: build_C(8, 3584, final))
        maps = []
        for c in range(8):
            m = {"hT": hs[c], "yaT": yas[c], "ybcT": ybcs[c], "wglu": f32(inp['s5_w_glu'][L]), "s5g": gvec(inp['s5_out_g'][L]),
                 "wout": f32(inp['w_out'][L]), "gffn": gvec(inp['norm_ffn_g'][L]), "w1": w1, "w3": w3, "w2": w2}
            if moe:
                m["router"] = f32(inp['moe_router'][L // 2])
            if final:
                m["gfin"] = gvec(inp['final_norm_g'])
            if not moe:
                m["gA"] = gvec(inp['norm_mix_g'][L + 1])
                m["wA"] = w_in_ext(inp['w_in'][L + 1])
            maps.append(m)
        rC = _run(nc, maps)
        H = unshard_T([r["hT_out"] for r in rC], 1024)
        proj_next = unshard_T([r["projT"] for r in rC], NCOL_A) if not moe else None
    return np.ascontiguousarray(H[:, 128:, :], dtype=np.float32)
```
